# Optimizing a Trainium2 kernel written in Bass

```python
import math
import jax, jax.numpy as jnp
from jax import lax
import numpy as np

D_MODEL = 1024
BATCH = 4
SEQ = 8192
DEPTH = 2

GRID_W = 64
ROPE_THETA = 10000.0
BLK = 128
N_BRANCH = 4
HEAD_DIM = 64
BRANCH_W = 512
LN_EPS = 1e-5
NEG_INF = -1e30

SWA_HEADS = 8
SWA_KV_HEADS = 2
SWA_WINDOW = 128
MLA_HEADS = 8
MLA_Q_RANK = 384
MLA_KV_RANK = 256
MLA_NOPE = 64
MLA_ROPE = 32
MLA_V = 64
DIFF_HEADS = 4
DIFF_QK = 64
DIFF_V = 128
NAT_HEADS = 8
NAT_KR_MAX = 8
NAT_KC = 16
PEER_HEADS = 8
PEER_NKEYS = 128
PEER_EXPERTS = PEER_NKEYS * PEER_NKEYS
PEER_DKEY = 256
PEER_TOPK = 16
PEER_TOK_BLK = 128

DN_ALPHA = (2 * DEPTH) ** 0.25
DN_BETA = (8 * DEPTH) ** -0.25

IN_WIDTHS = (
    SWA_HEADS * HEAD_DIM, SWA_KV_HEADS * HEAD_DIM, SWA_KV_HEADS * HEAD_DIM,
    MLA_Q_RANK, MLA_KV_RANK, MLA_ROPE,
    DIFF_HEADS * 2 * DIFF_QK, DIFF_HEADS * 2 * DIFF_QK, DIFF_HEADS * DIFF_V,
    NAT_HEADS * HEAD_DIM, NAT_HEADS * HEAD_DIM, NAT_HEADS * HEAD_DIM,
)
IN_SPLITS = tuple(int(v) for v in np.cumsum(IN_WIDTHS)[:-1])
D_IN = int(sum(IN_WIDTHS))

kernel_name = "hybrid_gated_parallel_mixers_peer_encoder"


def layer_norm(x, g=None, b=None):
    xf = x.astype(jnp.float32)
    mu = jnp.mean(xf, axis=-1, keepdims=True)
    var = jnp.mean(jnp.square(xf - mu), axis=-1, keepdims=True)
    y = (xf - mu) * lax.rsqrt(var + LN_EPS)
    if g is not None:
        y = y * g.astype(jnp.float32) + b.astype(jnp.float32)
    return y.astype(x.dtype)


def rms_norm(x, g):
    xf = x.astype(jnp.float32)
    y = xf * lax.rsqrt(jnp.mean(jnp.square(xf), axis=-1, keepdims=True) + LN_EPS)
    return (y * g.astype(jnp.float32)).astype(x.dtype)


def modulate(x, shift, scale):
    return layer_norm(x) * (1.0 + scale[:, None, :]) + shift[:, None, :]


def rope_tables(seq, dim):
    inv = ROPE_THETA ** (-jnp.arange(0, dim, 2, dtype=jnp.float32) / dim)
    ang = jnp.arange(seq, dtype=jnp.float32)[:, None] * inv[None, :]
    return jnp.cos(ang), jnp.sin(ang)


def apply_rope(t, cos, sin):
    t1, t2 = jnp.split(t, 2, axis=-1)
    cs = cos[None, :, None, :].astype(t.dtype)
    sn = sin[None, :, None, :].astype(t.dtype)
    return jnp.concatenate([t1 * cs - t2 * sn, t1 * sn + t2 * cs], axis=-1)


def qblock(a, i):
    return lax.dynamic_slice_in_dim(a, i * BLK, BLK, axis=1)


def unblock(out, b, s):
    return jnp.moveaxis(out, 0, 1).reshape(b, s, -1)


def swa_attention(q, k, v, sink, cos, sin):
    b, s, hq, d = q.shape
    hkv = k.shape[2]
    grp = hq // hkv
    q = apply_rope(q, cos, sin)
    k = apply_rope(k, cos, sin)
    pad = ((0, 0), (SWA_WINDOW, SWA_WINDOW), (0, 0), (0, 0))
    kp = jnp.pad(k, pad)
    vp = jnp.pad(v, pad)
    span = BLK + 2 * SWA_WINDOW
    sink_f = sink.astype(jnp.float32).reshape(hkv, grp)
    scale = d ** -0.5

    def block(i):
        s0 = i * BLK
        qb = qblock(q, i).reshape(b, BLK, hkv, grp, d)
        kb = lax.dynamic_slice_in_dim(kp, s0, span, axis=1)
        vb = lax.dynamic_slice_in_dim(vp, s0, span, axis=1)
        logits = jnp.einsum('bqkgd,bskd->bkgqs', qb, kb).astype(jnp.float32) * scale
        qpos = s0 + jnp.arange(BLK)
        kpos = s0 - SWA_WINDOW + jnp.arange(span)
        valid = ((jnp.abs(qpos[:, None] - kpos[None, :]) <= SWA_WINDOW)
                 & (kpos >= 0)[None, :] & (kpos < s)[None, :])
        logits = jnp.where(valid, logits, NEG_INF)
        sink_col = jnp.broadcast_to(sink_f[None, :, :, None, None], (b, hkv, grp, BLK, 1))
        p = jax.nn.softmax(jnp.concatenate([logits, sink_col], axis=-1), axis=-1)[..., :span]
        o = jnp.einsum('bkgqs,bskd->bqkgd', p.astype(v.dtype), vb)
        return o.reshape(b, BLK, hq * d)

    return unblock(lax.map(block, jnp.arange(s // BLK)), b, s)


def mla_attention(cq, ckv, krope, q_norm, q_up, kv_norm, kv_up, cos, sin):
    b, s, _ = cq.shape
    q = (rms_norm(cq, q_norm) @ q_up).reshape(b, s, MLA_HEADS, MLA_NOPE + MLA_ROPE)
    q_nope = q[..., :MLA_NOPE]
    q_rope = apply_rope(q[..., MLA_NOPE:], cos, sin)
    kv = (rms_norm(ckv, kv_norm) @ kv_up).reshape(b, s, MLA_HEADS, MLA_NOPE + MLA_V)
    k_nope = kv[..., :MLA_NOPE]
    v = kv[..., MLA_NOPE:]
    k_rope = apply_rope(krope[:, :, None, :], cos, sin)[:, :, 0]
    scale = (MLA_NOPE + MLA_ROPE) ** -0.5

    def block(i):
        logits = (jnp.einsum('bqhd,bshd->bhqs', qblock(q_nope, i), k_nope)
                  + jnp.einsum('bqhr,bsr->bhqs', qblock(q_rope, i), k_rope)).astype(jnp.float32) * scale
        p = jax.nn.softmax(logits, axis=-1)
        o = jnp.einsum('bhqs,bshd->bqhd', p.astype(v.dtype), v)
        return o.reshape(b, BLK, MLA_HEADS * MLA_V)

    return unblock(lax.map(block, jnp.arange(s // BLK)), b, s)


def diff_attention(q, k, v, lq1, lk1, lq2, lk2, subln, lambda_init, cos, sin):
    b, s, _ = q.shape
    q = apply_rope(q.reshape(b, s, DIFF_HEADS * 2, DIFF_QK), cos, sin).reshape(b, s, DIFF_HEADS, 2, DIFF_QK)
    k = apply_rope(k.reshape(b, s, DIFF_HEADS * 2, DIFF_QK), cos, sin).reshape(b, s, DIFF_HEADS, 2, DIFF_QK)
    v = v.reshape(b, s, DIFF_HEADS, DIFF_V)
    f32 = jnp.float32
    lam = (jnp.exp(jnp.sum(lq1.astype(f32) * lk1.astype(f32)))
           - jnp.exp(jnp.sum(lq2.astype(f32) * lk2.astype(f32))) + lambda_init)
    scale = DIFF_QK ** -0.5

    def block(i):
        logits = jnp.einsum('bqhmd,bshmd->bhmqs', qblock(q, i), k).astype(f32) * scale
        p = jax.nn.softmax(logits, axis=-1)
        a = p[:, :, 0] - lam * p[:, :, 1]
        o = jnp.einsum('bhqs,bshd->bqhd', a.astype(v.dtype), v)
        o = rms_norm(o, subln) * (1.0 - lambda_init)
        return o.reshape(b, BLK, DIFF_HEADS * DIFF_V)

    return unblock(lax.map(block, jnp.arange(s // BLK)), b, s)


def neighborhood_attention(q, k, v, rpb):
    b, s, _ = q.shape
    rows = s // GRID_W
    kr = min(NAT_KR_MAX, rows)
    grid = (b, rows, GRID_W, NAT_HEADS, HEAD_DIM)
    q = q.reshape(grid)
    k = k.reshape(grid)
    v = v.reshape(grid)
    cols = jnp.arange(GRID_W)
    col_start = jnp.clip(cols - NAT_KC // 2, 0, GRID_W - NAT_KC)
    col_idx = col_start[:, None] + jnp.arange(NAT_KC)[None, :]
    col_off = col_idx - cols[:, None] + (NAT_KC - 1)
    rpb_f = rpb.astype(jnp.float32)
    scale = HEAD_DIM ** -0.5

    def row(r):
        r0 = jnp.clip(r - kr // 2, 0, rows - kr)
        kband = lax.dynamic_slice_in_dim(k, r0, kr, axis=1)[:, :, col_idx]
        vband = lax.dynamic_slice_in_dim(v, r0, kr, axis=1)[:, :, col_idx]
        qr = lax.dynamic_index_in_dim(q, r, axis=1, keepdims=False)
        logits = jnp.einsum('bjhd,brjkhd->bhjrk', qr, kband).astype(jnp.float32) * scale
        row_off = r0 + jnp.arange(kr) - r + (NAT_KR_MAX - 1)
        bias = rpb_f[:, row_off[:, None, None], col_off[None, :, :]]
        logits = logits + jnp.transpose(bias, (0, 2, 1, 3))[None]
        p = jax.nn.softmax(logits.reshape(b, NAT_HEADS, GRID_W, kr * NAT_KC), axis=-1)
        p = p.reshape(b, NAT_HEADS, GRID_W, kr, NAT_KC)
        o = jnp.einsum('bhjrk,brjkhd->bjhd', p.astype(v.dtype), vband)
        return o.reshape(b, GRID_W, NAT_HEADS * HEAD_DIM)

    return unblock(lax.map(row, jnp.arange(rows)), b, s)


def peer_ffn(u, wq, keys, u_tab, v_tab):
    b, s, d = u.shape
    q = (u @ wq).reshape(b, s, PEER_HEADS, 2, PEER_DKEY // 2)
    scores = jnp.einsum('bshpk,hpnk->bshpn', q, keys).astype(jnp.float32)
    sub_s, sub_i = lax.top_k(scores, PEER_TOPK)
    cand_s = (sub_s[..., 0, :, None] + sub_s[..., 1, None, :]).reshape(b, s, PEER_HEADS, PEER_TOPK * PEER_TOPK)
    cand_i = (sub_i[..., 0, :, None] * PEER_NKEYS + sub_i[..., 1, None, :]).reshape(b, s, PEER_HEADS, PEER_TOPK * PEER_TOPK)
    top_s, pos = lax.top_k(cand_s, PEER_TOPK)
    eidx = jnp.take_along_axis(cand_i, pos, axis=-1)
    gates = jax.nn.softmax(top_s, axis=-1)
    nb = (b * s) // PEER_TOK_BLK
    ub = u.reshape(nb, PEER_TOK_BLK, d)
    ib = eidx.reshape(nb, PEER_TOK_BLK, PEER_HEADS, PEER_TOPK)
    gb = gates.reshape(nb, PEER_TOK_BLK, PEER_HEADS, PEER_TOPK)

    def blk(args):
        ut, it, gt = args
        ue = u_tab[it]
        h = jax.nn.gelu(jnp.einsum('td,thkd->thk', ut, ue).astype(jnp.float32), approximate=False)
        w = (gt * h).astype(ut.dtype)
        return jnp.einsum('thk,thkd->td', w, v_tab[it])

    return lax.map(blk, (ub, ib, gb)).reshape(b, s, d)


def setup_inputs(seed: int = 0) -> dict:
    key = jax.random.key(seed)
    ks = jax.random.split(key, 32)
    L, D = DEPTH, D_MODEL

    def nrm(k, shape, std):
        return std * jax.random.normal(k, shape, jnp.float32)

    def gain(k, shape):
        return 1.0 + 0.05 * jax.random.normal(k, shape, jnp.float32)

    return {
        "x": nrm(ks[0], (BATCH, SEQ, D), 1.0),
        "c": nrm(ks[1], (BATCH, D), 1.0),
        "ada_w": nrm(ks[2], (L, D, 6 * D), 0.5 * D ** -0.5),
        "ada_b": nrm(ks[3], (L, 6 * D), 0.01),
        "w_in": nrm(ks[4], (L, D, D_IN), D ** -0.5),
        "swa_sink": nrm(ks[5], (L, SWA_HEADS), 0.5),
        "mla_q_norm": gain(ks[6], (L, MLA_Q_RANK)),
        "mla_q_up": nrm(ks[7], (L, MLA_Q_RANK, MLA_HEADS * (MLA_NOPE + MLA_ROPE)), MLA_Q_RANK ** -0.5),
        "mla_kv_norm": gain(ks[8], (L, MLA_KV_RANK)),
        "mla_kv_up": nrm(ks[9], (L, MLA_KV_RANK, MLA_HEADS * (MLA_NOPE + MLA_V)), MLA_KV_RANK ** -0.5),
        "diff_lambda_q1": nrm(ks[10], (L, DIFF_QK), 0.1),
        "diff_lambda_k1": nrm(ks[11], (L, DIFF_QK), 0.1),
        "diff_lambda_q2": nrm(ks[12], (L, DIFF_QK), 0.1),
        "diff_lambda_k2": nrm(ks[13], (L, DIFF_QK), 0.1),
        "diff_subln": gain(ks[14], (L, DIFF_V)),
        "nat_rpb": nrm(ks[15], (L, NAT_HEADS, 2 * NAT_KR_MAX - 1, 2 * NAT_KC - 1), 0.1),
        "w_gate": nrm(ks[16], (L, N_BRANCH, D, D), D ** -0.5),
        "w_branch": nrm(ks[17], (L, N_BRANCH, BRANCH_W, D), DN_BETA * BRANCH_W ** -0.5),
        "w_out": nrm(ks[18], (L, D, D), DN_BETA * D ** -0.5),
        "ln1_g": gain(ks[19], (L, D)),
        "ln1_b": nrm(ks[20], (L, D), 0.02),
        "peer_wq": nrm(ks[21], (L, D, PEER_HEADS * PEER_DKEY), D ** -0.5),
        "peer_keys": nrm(ks[22], (L, PEER_HEADS, 2, PEER_NKEYS, PEER_DKEY // 2), (PEER_DKEY // 2) ** -0.5),
        "peer_u": nrm(ks[23], (L, PEER_EXPERTS, D), D ** -0.5),
        "peer_v": nrm(ks[24], (L, PEER_EXPERTS, D), DN_BETA),
        "ln2_g": gain(ks[25], (L, D)),
        "ln2_b": nrm(ks[26], (L, D), 0.02),
    }


def reference(x, c, ada_w, ada_b, w_in, swa_sink, mla_q_norm, mla_q_up, mla_kv_norm, mla_kv_up,
              diff_lambda_q1, diff_lambda_k1, diff_lambda_q2, diff_lambda_k2, diff_subln, nat_rpb,
              w_gate, w_branch, w_out, ln1_g, ln1_b, peer_wq, peer_keys, peer_u, peer_v, ln2_g, ln2_b):
    b, s, d = x.shape
    cos64, sin64 = rope_tables(s, HEAD_DIM)
    cos32, sin32 = rope_tables(s, MLA_ROPE)
    c_act = jax.nn.silu(c)
    for l in range(DEPTH):
        mod = c_act @ ada_w[l] + ada_b[l]
        sh_mix, sc_mix, g_mix, sh_ffn, sc_ffn, g_ffn = jnp.split(mod, 6, axis=-1)

        u = modulate(x, sh_mix, sc_mix)
        (qa, ka, va, cq, ckv, krope, qc, kc, vc, qd, kd, vd) = jnp.split(u @ w_in[l], IN_SPLITS, axis=-1)
        o_a = swa_attention(qa.reshape(b, s, SWA_HEADS, HEAD_DIM),
                            ka.reshape(b, s, SWA_KV_HEADS, HEAD_DIM),
                            va.reshape(b, s, SWA_KV_HEADS, HEAD_DIM), swa_sink[l], cos64, sin64)
        o_b = mla_attention(cq, ckv, krope, mla_q_norm[l], mla_q_up[l], mla_kv_norm[l], mla_kv_up[l], cos32, sin32)
        lambda_init = 0.8 - 0.6 * math.exp(-0.3 * l)
        o_c = diff_attention(qc, kc, vc, diff_lambda_q1[l], diff_lambda_k1[l], diff_lambda_q2[l],
                             diff_lambda_k2[l], diff_subln[l], lambda_init, cos64, sin64)
        o_d = neighborhood_attention(qd, kd, vd, nat_rpb[l])
        branches = (o_a, o_b, o_c, o_d)
        merged = jnp.zeros_like(x)
        for n in range(N_BRANCH):
            merged = merged + jax.nn.sigmoid(u @ w_gate[l, n]) * (branches[n] @ w_branch[l, n])
        y = merged @ w_out[l]
        x = layer_norm(DN_ALPHA * x + g_mix[:, None, :] * y, ln1_g[l], ln1_b[l])

        u = modulate(x, sh_ffn, sc_ffn)
        y = peer_ffn(u, peer_wq[l], peer_keys[l], peer_u[l], peer_v[l])
        x = layer_norm(DN_ALPHA * x + g_ffn[:, None, :] * y, ln2_g[l], ln2_b[l])
    return x
```

```python
import numpy as np
import concourse.bass as bass
import concourse.mybir as mybir
from concourse.bass_utils import run_bass_kernel_spmd
from contextlib import ExitStack

F32 = mybir.dt.float32
BF16 = mybir.dt.bfloat16
I32 = mybir.dt.int32
U32 = mybir.dt.uint32
AF = mybir.ActivationFunctionType
ALU = mybir.AluOpType
AX = mybir.AxisListType

ENGS = ["pe", "act", "dve", "pool", "sp"]
DMA_RING = 12


class Buf:
    __slots__ = ("name", "lastw", "readers", "excl", "pre")

    def __init__(self, name="", excl=False):
        self.name = name
        self.excl = excl
        self.pre = []
        self.lastw = []
        self.readers = []


class Prog:
    def __init__(self, nc, same_engine_sync=True):
        self.nc = nc
        self.es = ExitStack()
        self.stack = [self.es]
        self.q = {e: [] for e in ENGS}
        self.cnt = {e: 0 for e in ENGS}
        self.sems = {}
        self.EPOCH = 50000
        self.ring_n = {e: 0 for e in ENGS}
        self.ring_val = {}
        self.seen = {e: {} for e in ENGS}
        self.same_engine_sync = same_engine_sync
        self.n_waits = 0
        self.n_ops = 0
        self.uid = 0
        self.E = {"pe": nc.tensor, "act": nc.scalar, "dve": nc.vector, "pool": nc.gpsimd, "sp": nc.sync}

    def sb(self, name, shape, dtype):
        self.uid += 1
        return self.stack[-1].enter_context(self.nc.sbuf_tensor(f"s{self.uid}_{name}", list(shape), dtype))

    def ps(self, name, shape, dtype=F32):
        self.uid += 1
        return self.stack[-1].enter_context(self.nc.psum_tensor(f"p{self.uid}_{name}", list(shape), dtype))

    def push(self):
        self.stack.append(ExitStack())

    def pop(self):
        self.barrier()
        self.stack.pop().close()

    def barrier(self):
        evs = [self._last_ev(e) for e in ENGS if self.cnt[e] > 0]
        evs += [(k, v) for k, v in self.ring_val.items() if v > 0]
        for e in ENGS:
            for ev in evs:
                self._wait(e, ev)

    def dram(self, name, shape, dtype, kind="Internal"):
        return self.nc.dram_tensor(name, list(shape), dtype, kind=kind).ap()

    def _wait(self, eng, ev):
        key, val = ev
        if key[0] == "eng" and key[1] == eng and (eng == "pe" or not self.same_engine_sync):
            return
        if self.seen[eng].get(key, 0) >= val:
            return
        self.seen[eng][key] = val
        self.E[eng].wait_ge(self.sems[key], val)
        self.n_waits += 1

    def _deps(self, eng, reads, writes):
        for b in reads:
            for ev in b.lastw:
                self._wait(eng, ev)
            if b.excl:
                for ev in b.readers:
                    self._wait(eng, ev)
        for b in writes:
            for ev in b.lastw:
                self._wait(eng, ev)
            for ev in b.readers:
                self._wait(eng, ev)

    def _commit(self, ev, reads, writes):
        for b in writes:
            b.pre = list(b.lastw) + list(b.readers)
            b.lastw = [ev]
            b.readers = []
        for b in reads:
            b.readers = [r for r in b.readers if r[0] != ev[0]] + [ev]

    def _next_ev(self, eng):
        ep, k = divmod(self.cnt[eng], self.EPOCH)
        self.cnt[eng] += 1
        key = ("eng", eng, ep)
        if key not in self.sems:
            self.sems[key] = self.nc.alloc_semaphore(name=f"pg_{eng}_{ep}")
        return (key, k + 1)

    def _last_ev(self, eng):
        if self.cnt[eng] == 0:
            return None
        ep, k = divmod(self.cnt[eng] - 1, self.EPOCH)
        return (("eng", eng, ep), k + 1)

    def collective(self, ins_ap, outs_ap, groups, reads=(), writes=()):
        eng = "pool"
        self._deps(eng, reads, writes)
        k = self.ring_n.get("cc", 0) % 4
        self.ring_n["cc"] = self.ring_n.get("cc", 0) + 1
        key = ("ring", "cc", k)
        if key not in self.sems:
            self.sems[key] = self.nc.alloc_semaphore(name=f"pg_cc_{k}")
            self.ring_val[key] = 0
        prev = self.ring_val[key]
        if prev > 0:
            self._wait(eng, (key, prev))
        val = prev + 1
        self.ring_val[key] = val
        ev = (key, val)
        self.E[eng].collective_compute("AllGather", ALU.bypass, replica_groups=groups, ins=[ins_ap], outs=[outs_ap]).then_inc(self.sems[key])
        self.n_ops += 1
        self._commit(ev, reads, writes)
        return ev

    def op(self, eng, fn, reads=(), writes=()):
        self._deps(eng, reads, writes)
        ev = self._next_ev(eng)
        ins = fn(self.E[eng])
        ins.then_inc(self.sems[ev[0]], 1)
        self.n_ops += 1
        self._commit(ev, reads, writes)
        return ev

    def dma(self, eng, out, in_, reads=(), writes=(), append=False, **kw):
        if append:
            self._deps(eng, reads, ())
            for b in writes:
                for ev in b.pre:
                    self._wait(eng, ev)
                for ev in b.readers:
                    self._wait(eng, ev)
        else:
            self._deps(eng, reads, writes)
        k = self.ring_n[eng] % DMA_RING
        self.ring_n[eng] += 1
        key = ("ring", eng, k)
        if key not in self.sems:
            self.sems[key] = self.nc.alloc_semaphore(name=f"pg_r_{eng}_{k}")
            self.ring_val[key] = 0
        prev = self.ring_val[key]
        if prev > 0:
            self._wait(eng, (key, prev))
        val = prev + 16
        self.ring_val[key] = val
        ev = (key, val)
        self.E[eng].dma_start(out=out, in_=in_, **kw).then_inc(self.sems[key], 16)
        self.n_ops += 1
        if append:
            for b in writes:
                b.lastw = b.lastw + [ev]
            for b in reads:
                b.readers = [r for r in b.readers if r[0] != ev[0]] + [ev]
        else:
            self._commit(ev, reads, writes)
        return ev

    def finish(self):
        for key, val in list(self.ring_val.items()):
            if val > 0:
                self._wait("sp", (key, val))
        for e in ENGS:
            if e != "sp" and self.cnt[e] > 0:
                self._wait("sp", self._last_ev(e))

    def emit(self):
        self.es.close()


T = 4096
NTB = 8
NTT = 32

O_QA, O_KA, O_VA, O_CQ, O_CKV, O_KR, O_QC, O_KC, O_VC, O_QD, O_KD, O_VD = (
    0, 512, 640, 768, 1152, 1408, 1440, 1952, 2464, 2976, 3488, 4000)


class Rot:
    def __init__(self, P, name, shape, dtype, n, space="sb"):
        mk = P.sb if space == "sb" else P.ps
        self.tiles = [mk(f"{name}{i}", shape, dtype) for i in range(n)]
        self.bufs = [Buf(f"{name}{i}", excl=(space == "ps")) for i in range(n)]
        self.i = 0
        self.n = n

    def next(self):
        t, b = self.tiles[self.i % self.n], self.bufs[self.i % self.n]
        self.i += 1
        return t, b


def k1_weight_layout():
    def rot64(cols):
        cols = np.asarray(cols).reshape(-1, 64)
        return np.concatenate([cols[:, 32:], cols[:, :32]], axis=1).reshape(-1)

    units = []
    cols = []

    def add(name, kind, c):
        units.append((name, kind, sum(len(x) for x in cols), len(c)))
        cols.append(np.asarray(c))

    for name, off, n in (("swa_q", O_QA, 512), ("swa_k", O_KA, 128), ("diff_q", O_QC, 512), ("diff_k", O_KC, 512)):
        for ch in range(n // 128):
            a = np.arange(off + ch * 128, off + (ch + 1) * 128)
            add(f"{name}{ch}", "rope", np.concatenate([a, rot64(a)]))
    for name, off in (("nat_q", O_QD), ("nat_k", O_KD)):
        for ch in range(4):
            add(f"{name}{ch}", "plain", np.arange(off + ch * 128, off + (ch + 1) * 128))
    kr = np.arange(O_KR, O_KR + 32)
    krB = np.concatenate([kr[16:], kr[:16]])
    pad = np.arange(O_CQ, O_CQ + 64)
    add("mla", "mla", np.concatenate([np.arange(O_CQ, O_CQ + 384), np.arange(O_CKV, O_CKV + 256),
                                      pad, kr, pad, krB]))
    add("swa_v", "v", np.arange(O_VA, O_VA + 128))
    add("diff_v", "v", np.arange(O_VC, O_VC + 512))
    add("nat_v", "v", np.arange(O_VD, O_VD + 512))
    return units, np.concatenate(cols)


def mla_up_layout():
    qa = np.arange(768)
    qb = qa.copy().reshape(8, 96)
    qb = np.concatenate([qb[:, :64], qb[:, 80:96], qb[:, 64:80]], axis=1).reshape(-1)
    kv = np.arange(1024).reshape(8, 128)
    knope = kv[:, :64].reshape(-1)
    vcols = kv[:, 64:].reshape(-1)
    return qa, qb, knope, vcols


def rope_tables(pos0, n):
    pos = np.arange(pos0, pos0 + n, dtype=np.float32)
    inv64 = (10000.0 ** (-np.arange(0, 64, 2, dtype=np.float32) / 64)).astype(np.float32)
    ang = (pos[None, :] * inv64[:, None]).astype(np.float32)
    c, s = np.cos(ang).astype(np.float32), np.sin(ang).astype(np.float32)
    C64 = np.concatenate([c, c, c, c], 0)
    S64 = np.concatenate([-s, s, -s, s], 0)
    inv32 = (10000.0 ** (-np.arange(0, 32, 2, dtype=np.float32) / 32)).astype(np.float32)
    ang = (pos[None, :] * inv32[:, None]).astype(np.float32)
    c, s = np.cos(ang).astype(np.float32), np.sin(ang).astype(np.float32)
    CM = np.zeros((128, n), np.float32)
    SM = np.zeros((128, n), np.float32)
    CM[64:96] = np.concatenate([c, c], 0)
    SM[64:96] = np.concatenate([-s, s], 0)
    return C64, S64, CM, SM


def emit_mod_cols(P, nc, cT_d, adaw_d, adab_d, ngrp, modc, bmodc, pm_rot, wst_rot):
    cs = P.sb("mod_cs", [128, 8], F32)
    bcs = Buf("cs")
    adab = P.sb("mod_adab", [128, ngrp * 8], F32)
    badab = Buf("adab")
    P.dma("sp", cs[:], cT_d, writes=[bcs])
    P.dma("sp", adab[:], adab_d, writes=[badab])
    P.op("act", lambda e: e.activation(out=cs[:], in_=cs[:], func=AF.Silu), reads=[bcs], writes=[bcs])
    pm, bpm = pm_rot.next()
    noc = ngrp * 8
    for g in range(noc // 2):
        wst_, bw = wst_rot.next()
        wst = wst_[:, 0:2048].rearrange("p (c n) -> p c n", c=8)
        P.dma("sp" if g % 2 == 0 else "act", wst,
              adaw_d[:, g * 256:(g + 1) * 256].rearrange("(c p) n -> p c n", p=128), writes=[bw])
        for o4 in range(2):
            oc = g * 2 + o4
            for kc in range(8):
                P.op("pe", lambda e, oc=oc, kc=kc, o4=o4, wst=wst: e.matmul(
                    pm[:, oc:oc + 1], lhsT=wst[:, kc, o4 * 128:(o4 + 1) * 128], rhs=cs[:, kc:kc + 1],
                    start=(kc == 0), stop=(kc == 7)), reads=[bw, bcs], writes=[bpm])
    P.op("dve", lambda e: e.tensor_tensor(out=modc[:, 0:noc], in0=pm[:, 0:noc], in1=adab[:], op=ALU.add),
         reads=[bpm, badab], writes=[bmodc])


def emit_ln_mod(P, x_d, uT, buT_tiles, shcol, sc1col, bmod, ident, bid, epsb, beps, ptr_rot, tag=""):
    xin = Rot(P, f"ln_x{tag}", [128, 1024], F32, 2)
    xn = Rot(P, f"ln_xn{tag}", [128, 1024], BF16, 2)
    st = Rot(P, f"ln_st{tag}", [128, 2, 6], F32, 2)
    mv = Rot(P, f"ln_mv{tag}", [128, 2], F32, 2)
    rs = Rot(P, f"ln_rs{tag}", [128, 1], F32, 2)
    for t in range(NTT):
        xt, bx = xin.next()
        P.dma("sp" if t % 2 == 0 else "act", xt[:], x_d[t * 128:(t + 1) * 128, :], writes=[bx])
        s_, bs = st.next()
        for c in range(2):
            P.op("dve", lambda e, c=c, s_=s_, xt=xt: e.bn_stats(out=s_[:, c, :], in_=xt[:, c * 512:(c + 1) * 512]),
                 reads=[bx], writes=[bs])
        m_, bm = mv.next()
        P.op("dve", lambda e, m_=m_, s_=s_: e.bn_aggr(out=m_[:], in_=s_[:]), reads=[bs], writes=[bm])
        r_, br = rs.next()
        P.op("act", lambda e, r_=r_, m_=m_: e.activation(out=r_[:], in_=m_[:, 1:2], func=AF.Ln, bias=epsb[:, 0:1], scale=1.0),
             reads=[bm, beps], writes=[br])
        P.op("act", lambda e, r_=r_: e.activation(out=r_[:], in_=r_[:], func=AF.Exp, scale=-0.5), reads=[br], writes=[br])
        xn_, bxn = xn.next()
        P.op("dve", lambda e, xn_=xn_, xt=xt, m_=m_, r_=r_: e.tensor_scalar(
            out=xn_[:], in0=xt[:], scalar1=m_[:, 0:1], scalar2=r_[:, 0:1], op0=ALU.subtract, op1=ALU.mult),
            reads=[bx, bm, br], writes=[bxn])
        pt, bpt = ptr_rot.next()
        for c in range(8):
            P.op("pe", lambda e, c=c, pt=pt, xn_=xn_: e.transpose(out=pt[:, c, :], in_=xn_[:, c * 128:(c + 1) * 128], identity=ident[:]),
                 reads=[bxn, bid], writes=[bpt])
        for c in range(8):
            P.op("act", lambda e, c=c, pt=pt, t=t: e.activation(
                out=uT[:, c, t * 128:(t + 1) * 128], in_=pt[:, c, :], func=AF.Identity,
                scale=sc1col[:, c:c + 1], bias=shcol[:, c:c + 1]), reads=[bpt, bmod], writes=[buT_tiles[t]])


def k1_io():
    units, w1cols = k1_weight_layout()
    NC1 = len(w1cols)
    ins = [("x", [T, 1024]), ("cT", [128, 8]), ("adaw", [1024, 2048]), ("adab", [128, 16]), ("w1", [1024, NC1]),
           ("qupA", [384, 768]), ("qupB", [384, 768]), ("kvn", [256, 512]), ("kvv", [256, 512]), ("gq", [128, 3]), ("gkv", [128, 2]),
           ("C64", [128, T]), ("S64", [128, T]), ("CM", [128, T]), ("SM", [128, T]), ("ident", [128, 128])]
    outs = [("uT", [1024, T]), ("swa_qT", [512, T]), ("swa_kT", [128, T]), ("diff_qT", [512, T]), ("diff_kT", [512, T]),
            ("nat_qT", [512, T]), ("nat_kT", [512, T]), ("mla_qT", [8, 96, T]), ("mla_kT", [8, 96, T]),
            ("swa_v", [NTT, 128, 2 * 65]), ("diff_v", [NTT, 128, 4 * 129]), ("nat_v", [NTT, 128, 8 * 65]), ("mla_v", [NTT, 128, 8 * 65])]
    return ins, outs


def build_k1(nc, stop=None):
    P = Prog(nc)
    ins, outs = k1_io()
    D = {n: nc.dram_tensor(n, list(s), F32, kind="ExternalInput").ap() for n, s in ins}
    O = {n: nc.dram_tensor(n, list(s), BF16, kind="ExternalOutput").ap() for n, s in outs}
    emit_k1(P, nc, D, O, stop)
    P.finish()
    P.emit()
    return P


def emit_k1(P, nc, D, O, stop=None):
    units, w1cols = k1_weight_layout()
    x_d, cT_d, adaw_d, adab_d, w1_d = D["x"], D["cT"], D["adaw"], D["adab"], D["w1"]
    qupA_d, qupB_d, kvn_d, kvv_d, gq_d, gkv_d = D["qupA"], D["qupB"], D["kvn"], D["kvv"], D["gq"], D["gkv"]
    C64_d, S64_d, CM_d, SM_d, ident_d = D["C64"], D["S64"], D["CM"], D["SM"], D["ident"]
    o_uT = O["uT"]
    o = {"swa_q": O["swa_qT"], "swa_k": O["swa_kT"], "diff_q": O["diff_qT"], "diff_k": O["diff_kT"],
         "nat_q": O["nat_qT"], "nat_k": O["nat_kT"], "mla_q": O["mla_qT"], "mla_k": O["mla_kT"],
         "swa_v": O["swa_v"], "diff_v": O["diff_v"], "nat_v": O["nat_v"], "mla_v": O["mla_v"]}

    identf = P.sb("identf", [128, 128], F32); bidf = Buf()
    ident = P.sb("ident", [128, 128], BF16); bid = Buf()
    onesb = P.sb("onesb", [128, 128], BF16); bones = Buf()
    epsb = P.sb("epsb", [128, 1], F32); beps = Buf()
    P.dma("pool", identf[:], ident_d, writes=[bidf])
    P.op("dve", lambda e: e.tensor_copy(out=ident[:], in_=identf[:]), reads=[bidf], writes=[bid])
    P.op("pool", lambda e: e.memset(onesb[:], 1.0), writes=[bones])
    P.op("pool", lambda e: e.memset(epsb[:], 1e-5), writes=[beps])

    uT = P.sb("uT", [128, 8, T], BF16)
    buT = [Buf(f"uT{t}") for t in range(NTT)]
    modc = P.sb("modc", [128, 16], F32); bmodc = Buf()
    sc1 = P.sb("sc1", [128, 8], F32); bsc1 = Buf()

    pA = Rot(P, "pA", [128, 512], F32, 2, "ps")
    pB = Rot(P, "pB", [128, 512], F32, 2, "ps")
    ptr = Rot(P, "ptr", [128, 8, 128], BF16, 2, "ps")
    pmisc = Rot(P, "pmisc", [128, 512], F32, 1, "ps")

    wst = Rot(P, "wst", [128, 2048], F32, 2)
    wbf = Rot(P, "wbf", [128, 8, 256], BF16, 2)

    emit_mod_cols(P, nc, cT_d, adaw_d, adab_d, 2, modc, bmodc, pmisc, wst)
    P.op("dve", lambda e: e.tensor_scalar(out=sc1[:], in0=modc[:, 8:16], scalar1=1.0, scalar2=None, op0=ALU.add),
         reads=[bmodc], writes=[bsc1])
    bmod = Buf("mod")
    P.op("dve", lambda e: e.tensor_copy(out=modc[:, 0:8], in_=modc[:, 0:8]), reads=[bmodc, bsc1], writes=[bmod])

    if stop == "mod":
        return
    emit_ln_mod(P, x_d, uT, buT, modc, sc1, bmod, ident, bid, epsb, beps, ptr)
    if stop == "ln":
        return

    for c in range(8):
        P.dma("pool", o_uT[c * 128:(c + 1) * 128, :], uT[:, c, :], reads=buT)

    c64r = Rot(P, "c64r", [128, 512], F32, 2)
    s64r = Rot(P, "s64r", [128, 512], F32, 2)

    t1r = Rot(P, "t1r", [128, 512], F32, 2)
    t2r = Rot(P, "t2r", [128, 512], F32, 2)
    ostg = Rot(P, "ostg", [128, 512], BF16, 3)

    def load_w(col0, ncols):
        ws_, bws = wst.next()
        wb, bwb = wbf.next()
        ws = ws_[:, 0:8 * ncols].rearrange("p (c n) -> p c n", c=8)
        h = ncols // 2
        P.dma("sp", ws[:, :, 0:h], w1_d[:, col0:col0 + h].rearrange("(c p) n -> p c n", p=128), writes=[bws])
        P.dma("act", ws[:, :, h:ncols], w1_d[:, col0 + h:col0 + ncols].rearrange("(c p) n -> p c n", p=128),
              writes=[bws], append=True)
        P.op("pool", lambda e: e.tensor_copy(out=wb[:, :, 0:ncols], in_=ws), reads=[bws], writes=[bwb])
        return wb, bwb

    def mm_fm(ps, bps, wb, bwb, c0, M, tb):
        for kc in range(8):
            P.op("pe", lambda e, kc=kc: e.matmul(ps[0:M, :], lhsT=wb[:, kc, c0:c0 + M], rhs=uT[:, kc, tb * 512:(tb + 1) * 512],
                                               start=(kc == 0), stop=(kc == 7)),
                 reads=[bwb] + buT[tb * 4:(tb + 1) * 4], writes=[bps])

    dq = ["sp", "act", "pool"]
    dqi = [0]

    def nextq():
        dqi[0] += 1
        return dq[dqi[0] % 3]

    for (name, kind, col0, ncols) in units:
        if stop is not None and stop == name:
            break
        if kind == "rope":
            base = name[:-1]; ch = int(name[-1])
            wb, bwb = load_w(col0, 256)
            for tb in range(NTB):
                a, ba = pA.next(); b, bb = pB.next()
                mm_fm(a, ba, wb, bwb, 0, 128, tb)
                mm_fm(b, bb, wb, bwb, 128, 128, tb)
                t1, bt1 = t1r.next(); t2, bt2 = t2r.next()
                sl = slice(tb * 512, (tb + 1) * 512)
                C64, bc64 = c64r.next(); S64, bs64 = s64r.next()
                P.dma("sp", C64[:], C64_d[:, sl], writes=[bc64])
                P.dma("act", S64[:], S64_d[:, sl], writes=[bs64])
                P.op("dve", lambda e, t1=t1, a=a, C64=C64: e.tensor_tensor(out=t1[:], in0=a[:], in1=C64[:], op=ALU.mult),
                     reads=[ba, bc64], writes=[bt1])
                P.op("dve", lambda e, t2=t2, b=b, S64=S64: e.tensor_tensor(out=t2[:], in0=b[:], in1=S64[:], op=ALU.mult),
                     reads=[bb, bs64], writes=[bt2])
                og, bog = ostg.next()
                P.op("pool", lambda e, og=og, t1=t1, t2=t2: e.tensor_tensor(out=og[:], in0=t1[:], in1=t2[:], op=ALU.add),
                     reads=[bt1, bt2], writes=[bog])
                P.dma(nextq(), o[base][ch * 128:(ch + 1) * 128, sl], og[:], reads=[bog])
        elif kind == "plain":
            base = name[:-1]; ch = int(name[-1])
            wb, bwb = load_w(col0, 128)
            scale = 0.125 if base == "nat_q" else 1.0
            for tb in range(NTB):
                a, ba = pA.next()
                mm_fm(a, ba, wb, bwb, 0, 128, tb)
                og, bog = ostg.next()
                sl = slice(tb * 512, (tb + 1) * 512)
                P.op("act", lambda e, og=og, a=a, scale=scale: e.activation(out=og[:], in_=a[:], func=AF.Copy, scale=scale),
                     reads=[ba], writes=[bog])
                P.dma(nextq(), o[base][ch * 128:(ch + 1) * 128, sl], og[:], reads=[bog])
        elif kind == "v":
            H, dv = {"swa_v": (2, 64), "diff_v": (4, 128), "nat_v": (8, 64)}[name]
            vst = Rot(P, f"vst_{name}", [128, H, dv + 1], BF16, 2)
            for vt, vb in zip(vst.tiles, vst.bufs):
                P.op("pool", lambda e, vt=vt: e.memset(vt[:], 1.0), writes=[vb])
            nsub = max(1, ncols // 256)
            sc = ncols // nsub
            hs = H // nsub
            wbs = [load_w(col0 + i * sc, sc) for i in range(nsub)] if nsub <= 2 else None
            assert wbs is not None
            for t in range(NTT):
                vt, vb = vst.next()
                for i in range(nsub):
                    wb, bwb = wbs[i]
                    a, ba = pA.next()
                    for kc in range(8):
                        P.op("pe", lambda e, kc=kc, a=a, t=t, wb=wb: e.matmul(a[:, 0:sc], lhsT=uT[:, kc, t * 128:(t + 1) * 128],
                                                                     rhs=wb[:, kc, 0:sc], start=(kc == 0), stop=(kc == 7)),
                             reads=[bwb, buT[t]], writes=[ba])
                    P.op("act", lambda e, vt=vt, a=a, i=i: e.activation(
                        out=vt[:, i * hs:(i + 1) * hs, 0:dv], in_=a[:, 0:sc].rearrange("p (h d) -> p h d", h=hs), func=AF.Copy),
                        reads=[ba], writes=[vb])
                P.dma(nextq(), o[name][t], vt[:].rearrange("p h d -> p (h d)"), reads=[vb])
        elif kind == "mla":
            emit_mla(P, locals())


def emit_mla(P, L):
    nc = P.nc
    (w1_d, uT, buT, pA, pB, pmisc, o, onesb, bones, epsb, beps, qupA_d, qupB_d, kvn_d, kvv_d, gq_d, gkv_d,
     CM_d, SM_d, col0, nextq, ostg) = (L[k] for k in (
         "w1_d", "uT", "buT", "pA", "pB", "pmisc", "o", "onesb", "bones", "epsb", "beps", "qupA_d", "qupB_d",
         "kvn_d", "kvv_d", "gq_d", "gkv_d", "CM_d", "SM_d", "col0", "nextq", "ostg"))
    L = dict(L)
    NCM = 384 + 256 + 96 + 96
    wst = L["wst"]
    wm = P.sb("mla_w", [128, 8, NCM], BF16); bwm = Buf()
    first = True
    for i in range(4):
        ws_, bws = wst.next()
        ws = ws_[:, 0:8 * 208].rearrange("p (c n) -> p c n", c=8)
        P.dma("sp" if i % 2 == 0 else "act", ws, w1_d[:, col0 + i * 208:col0 + (i + 1) * 208].rearrange("(c p) n -> p c n", p=128), writes=[bws])
        P.op("pool", lambda e, ws=ws, i=i: e.tensor_copy(out=wm[:, :, i * 208:(i + 1) * 208], in_=ws), reads=[bws], writes=[bwm])
    qA = P.sb("mla_qA", [128, 3, 768], BF16); bqA = Buf()
    qB = P.sb("mla_qB", [128, 3, 768], BF16); bqB = Buf()
    kvn = P.sb("mla_kvn", [128, 2, 512], BF16); bkvn = Buf()
    kvv = P.sb("mla_kvv", [128, 2, 512], BF16); bkvv = Buf()
    for (src, dst, bdst, nch, ncol) in ((qupA_d, qA, bqA, 3, 768), (qupB_d, qB, bqB, 3, 768)):
        for hh in range(2):
            ws_, bws = wst.next()
            ws = ws_[:, 0:nch * 384].rearrange("p (c n) -> p c n", c=nch)
            P.dma("sp", ws, src[:, hh * 384:(hh + 1) * 384].rearrange("(c p) n -> p c n", p=128), writes=[bws])
            P.op("pool", lambda e, ws=ws, dst=dst, hh=hh: e.tensor_copy(out=dst[:, :, hh * 384:(hh + 1) * 384], in_=ws), reads=[bws], writes=[bdst])
    for (src, dst, bdst) in ((kvn_d, kvn, bkvn), (kvv_d, kvv, bkvv)):
        ws_, bws = wst.next()
        ws = ws_[:, 0:1024].rearrange("p (c n) -> p c n", c=2)
        P.dma("act", ws, src.rearrange("(c p) n -> p c n", p=128), writes=[bws])
        P.op("pool", lambda e, ws=ws, dst=dst: e.tensor_copy(out=dst[:], in_=ws), reads=[bws], writes=[bdst])
    gq = P.sb("mla_gq", [128, 3], F32); gkv = P.sb("mla_gkv", [128, 2], F32); bg = Buf()
    P.dma("pool", gq[:], gq_d, writes=[bg])
    P.dma("pool", gkv[:], gkv_d, writes=[bg], append=True)
    epsq = P.sb("mla_epsq", [128, 1], F32)
    cg = Rot(P, "mla_cg", [128, 5, 512], BF16, 1)
    sq = Rot(P, "mla_sq", [128, 5, 512], BF16, 1)
    rq = Rot(P, "mla_rq", [128, 512], F32, 1)
    rkv = Rot(P, "mla_rkv", [128, 512], F32, 1)
    rkvt = Rot(P, "mla_rkvt", [128, 4], F32, 2)
    cmt = Rot(P, "mla_cm", [128, 512], F32, 1); smt = Rot(P, "mla_sm", [128, 512], F32, 1)
    cr = Rot(P, "mla_cr", [128, 512], F32, 1); sr = Rot(P, "mla_sr", [128, 512], F32, 1)
    tA = Rot(P, "mla_tA", [128, 512], F32, 1); tB = Rot(P, "mla_tB", [128, 512], F32, 1)
    krs = Rot(P, "mla_krs", [128, 512], BF16, 2)
    vst = Rot(P, "mla_vst", [128, 8, 65], BF16, 2)
    for vt, vb in zip(vst.tiles, vst.bufs):
        P.op("pool", lambda e, vt=vt: e.memset(vt[:], 1.0), writes=[vb])

    import os
    MS = int(os.environ.get("MLASTOP", "99"))
    for tb in range(NTB):
        sl = slice(tb * 512, (tb + 1) * 512)
        ubufs = buT[tb * 4:(tb + 1) * 4]
        cg_, bcg = cg.next(); sq_, bsq = sq.next()
        if MS <= 0: continue
        for j in range(5):
            a, ba = pA.next()
            for kc in range(8):
                P.op("pe", lambda e, kc=kc, a=a, j=j: e.matmul(a[:], lhsT=wm[:, kc, j * 128:(j + 1) * 128], rhs=uT[:, kc, sl],
                                                            start=(kc == 0), stop=(kc == 7)), reads=[bwm] + ubufs, writes=[ba])
            gcol = gq[:, j:j + 1] if j < 3 else gkv[:, j - 3:j - 2]
            VAR = os.environ.get("MLAVAR", "AB")
            if "A" in VAR:
                P.op("act", lambda e, a=a, j=j, sq_=sq_: e.activation(out=sq_[:, j, :], in_=a[:], func=AF.Square), reads=[ba], writes=[bsq])
            if "B" in VAR:
              P.op("dve", lambda e, a=a, j=j, cg_=cg_, gcol=gcol: e.tensor_scalar(out=cg_[:, j, :], in0=a[:], scalar1=gcol, scalar2=None, op0=ALU.mult),
                 reads=[ba, bg], writes=[bcg])
        if MS <= 1: continue
        rq_, brq = rq.next(); rkv_, brkv = rkv.next(); rkt, brkt = rkvt.next()
        for (r_, br_, js, dim) in ((rq_, brq, (0, 1, 2), 384.0), (rkv_, brkv, (3, 4), 256.0)):
            pm, bpm = pmisc.next()
            for i, j in enumerate(js):
                P.op("pe", lambda e, pm=pm, j=j, i=i, n=len(js): e.matmul(pm[:], lhsT=onesb[:], rhs=sq_[:, j, :], start=(i == 0), stop=(i == n - 1)),
                     reads=[bones, bsq], writes=[bpm])
            P.op("act", lambda e, r_=r_, pm=pm, dim=dim: e.activation(out=r_[:], in_=pm[:], func=AF.Ln, bias=epsb[:, 0:1], scale=1.0 / dim),
                 reads=[bpm, beps], writes=[br_])
            P.op("act", lambda e, r_=r_: e.activation(out=r_[:], in_=r_[:], func=AF.Exp, scale=-0.5), reads=[br_], writes=[br_])
        if MS <= 2: continue
        pm, bpm = pmisc.next()
        for tt in range(4):
            for i, j in enumerate((3, 4)):
                P.op("pe", lambda e, pm=pm, tt=tt, j=j, i=i: e.matmul(pm[:, tt:tt + 1], lhsT=sq_[:, j, tt * 128:(tt + 1) * 128], rhs=onesb[:, 0:1],
                                                                   start=(i == 0), stop=(i == 1)), reads=[bones, bsq], writes=[bpm])
        P.op("act", lambda e, rkt=rkt, pm=pm: e.activation(out=rkt[:], in_=pm[:, 0:4], func=AF.Ln, bias=epsb[:, 0:1], scale=1.0 / 256.0),
             reads=[bpm, beps], writes=[brkt])
        P.op("act", lambda e, rkt=rkt: e.activation(out=rkt[:], in_=rkt[:], func=AF.Exp, scale=-0.5), reads=[brkt], writes=[brkt])
        if MS <= 3: continue
        cm_, bcm = cmt.next(); sm_, bsm = smt.next()
        P.dma("sp", cm_[:], CM_d[:, sl], writes=[bcm])
        P.dma("act", sm_[:], SM_d[:, sl], writes=[bsm])
        cr_, bcr = cr.next(); sr_, bsr = sr.next()
        P.op("pool", lambda e, cr_=cr_, cm_=cm_, rq_=rq_: e.tensor_tensor(out=cr_[64:96, :], in0=cm_[64:96, :], in1=rq_[64:96, :], op=ALU.mult),
             reads=[bcm, brq], writes=[bcr])
        P.op("pool", lambda e, sr_=sr_, sm_=sm_, rq_=rq_: e.tensor_tensor(out=sr_[64:96, :], in0=sm_[64:96, :], in1=rq_[64:96, :], op=ALU.mult),
             reads=[bsm, brq], writes=[bsr])
        if MS <= 4: continue
        for h in range(8):
            a, ba = pA.next(); b, bb = pB.next()
            for j in range(3):
                P.op("pe", lambda e, a=a, j=j, h=h: e.matmul(a[0:96, :], lhsT=qA[:, j, h * 96:(h + 1) * 96], rhs=cg_[:, j, :], start=(j == 0), stop=(j == 2)),
                     reads=[bqA, bcg], writes=[ba])
            for j in range(3):
                P.op("pe", lambda e, b=b, j=j, h=h: e.matmul(b[0:96, :], lhsT=qB[:, j, h * 96:(h + 1) * 96], rhs=cg_[:, j, :], start=(j == 0), stop=(j == 2)),
                     reads=[bqB, bcg], writes=[bb])
            og, bog = ostg.next()
            tA_, btA = tA.next(); tB_, btB = tB.next()
            P.op("dve", lambda e, og=og, a=a, rq_=rq_: e.tensor_tensor(out=og[0:64, :], in0=a[0:64, :], in1=rq_[0:64, :], op=ALU.mult),
                 reads=[ba, brq], writes=[bog])
            P.op("dve", lambda e, tA_=tA_, a=a, cr_=cr_: e.tensor_tensor(out=tA_[64:96, :], in0=a[64:96, :], in1=cr_[64:96, :], op=ALU.mult),
                 reads=[ba, bcr], writes=[btA])
            P.op("dve", lambda e, tB_=tB_, b=b, sr_=sr_: e.tensor_tensor(out=tB_[64:96, :], in0=b[64:96, :], in1=sr_[64:96, :], op=ALU.mult),
                 reads=[bb, bsr], writes=[btB])
            P.op("pool", lambda e, og=og, tA_=tA_, tB_=tB_: e.tensor_tensor(out=og[64:96, :], in0=tA_[64:96, :], in1=tB_[64:96, :], op=ALU.add),
                 reads=[btA, btB, bog], writes=[bog])
            P.dma(nextq(), o["mla_q"][h, :, sl], og[0:96, :], reads=[bog])
        if MS <= 5: continue
        a, ba = pA.next(); b, bb = pB.next()
        for kc in range(8):
            P.op("pe", lambda e, kc=kc, a=a: e.matmul(a[0:96, :], lhsT=wm[:, kc, 640:736], rhs=uT[:, kc, sl], start=(kc == 0), stop=(kc == 7)),
                 reads=[bwm] + ubufs, writes=[ba])
        for kc in range(8):
            P.op("pe", lambda e, kc=kc, b=b: e.matmul(b[0:96, :], lhsT=wm[:, kc, 736:832], rhs=uT[:, kc, sl], start=(kc == 0), stop=(kc == 7)),
                 reads=[bwm] + ubufs, writes=[bb])
        tA_, btA = tA.next(); tB_, btB = tB.next()
        P.op("dve", lambda e, tA_=tA_, a=a, cm_=cm_: e.tensor_tensor(out=tA_[64:96, :], in0=a[64:96, :], in1=cm_[64:96, :], op=ALU.mult),
             reads=[ba, bcm], writes=[btA])
        P.op("dve", lambda e, tB_=tB_, b=b, sm_=sm_: e.tensor_tensor(out=tB_[64:96, :], in0=b[64:96, :], in1=sm_[64:96, :], op=ALU.mult),
             reads=[bb, bsm], writes=[btB])
        kr_, bkr = krs.next()
        P.op("pool", lambda e, kr_=kr_, tA_=tA_, tB_=tB_: e.tensor_tensor(out=kr_[64:96, :], in0=tA_[64:96, :], in1=tB_[64:96, :], op=ALU.add),
             reads=[btA, btB], writes=[bkr])
        for h in range(8):
            P.dma(nextq(), o["mla_k"][h, 64:96, sl], kr_[64:96, :], reads=[bkr])
        if MS <= 6: continue
        for h in range(8):
            a, ba = pA.next()
            for j in range(2):
                P.op("pe", lambda e, a=a, j=j, h=h: e.matmul(a[0:64, :], lhsT=kvn[:, j, h * 64:(h + 1) * 64], rhs=cg_[:, 3 + j, :], start=(j == 0), stop=(j == 1)),
                     reads=[bkvn, bcg], writes=[ba])
            og, bog = ostg.next()
            P.op("dve", lambda e, og=og, a=a, rkv_=rkv_: e.tensor_tensor(out=og[0:64, :], in0=a[0:64, :], in1=rkv_[0:64, :], op=ALU.mult),
                 reads=[ba, brkv], writes=[bog])
            P.dma(nextq(), o["mla_k"][h, 0:64, sl], og[0:64, :], reads=[bog])
        if MS <= 7: continue
        for tt in range(4):
            a, ba = pA.next()
            for j in range(2):
                P.op("pe", lambda e, a=a, j=j, tt=tt: e.matmul(a[:], lhsT=cg_[:, 3 + j, tt * 128:(tt + 1) * 128], rhs=kvv[:, j, :], start=(j == 0), stop=(j == 1)),
                     reads=[bkvv, bcg], writes=[ba])
            vt, vb = vst.next()
            P.op("act", lambda e, vt=vt, a=a, rkt=rkt, tt=tt: e.activation(
                out=vt[:, :, 0:64], in_=a[:].rearrange("p (h d) -> p h d", h=8), func=AF.Copy, scale=rkt[:, tt:tt + 1]),
                reads=[ba, brkt], writes=[vb])
            P.dma(nextq(), o["mla_v"][tb * 4 + tt], vt[:].rearrange("p h d -> p (h d)"), reads=[vb])


def to_pc(v, ncol):
    return np.ascontiguousarray(np.asarray(v).reshape(ncol, 128).T)


def k1_host_inputs(inp, l, core, xcur):
    b, half = core // 2, core % 2
    units, w1cols = k1_weight_layout()
    qa, qb, knope, vcols = mla_up_layout()
    C64, S64, CM, SM = rope_tables(half * T, T)
    m = {
        "x": np.ascontiguousarray(xcur[b, half * T:(half + 1) * T]),
        "cT": to_pc(inp["c"][b], 8),
        "adaw": np.ascontiguousarray(inp["ada_w"][l][:, 0:2048]),
        "adab": to_pc(inp["ada_b"][l][0:2048], 16),
        "w1": np.ascontiguousarray(inp["w_in"][l][:, w1cols]),
        "qupA": np.ascontiguousarray(inp["mla_q_up"][l][:, qa]),
        "qupB": np.ascontiguousarray(inp["mla_q_up"][l][:, qb]),
        "kvn": np.ascontiguousarray(inp["mla_kv_up"][l][:, knope]),
        "kvv": np.ascontiguousarray(inp["mla_kv_up"][l][:, vcols]),
        "gq": to_pc(inp["mla_q_norm"][l], 3),
        "gkv": to_pc(inp["mla_kv_norm"][l], 2),
        "C64": C64, "S64": S64, "CM": CM, "SM": SM,
        "ident": np.eye(128, dtype=np.float32),
    }
    return m


NEGM = -30000.0


class AttnCtx:
    pass


def attn_block(P, C, rhs_q, bq, N, ktiles, scale, acc, bacc, nsub, v_of, pt_cols=None):
    n = len(ktiles)
    LA = 2
    pend = {}
    for i in range(n + LA):
        if i < n:
            kT_ap, kb, vkey, vb, bias_ap, bb = ktiles[i]
            st, bst = C.ST.next()
            P.op("pe", lambda e, st=st, kT_ap=kT_ap, bias_ap=bias_ap: e.matmul(
                st[:, 0:N], lhsT=kT_ap, rhs=rhs_q, start=True, stop=(bias_ap is None)),
                reads=list(kb) + list(bq), writes=[bst])
            if bias_ap is not None:
                P.op("pe", lambda e, st=st, bias_ap=bias_ap: e.matmul(
                    st[:, 0:N], lhsT=C.ident[:], rhs=bias_ap, start=False, stop=True),
                    reads=list(bb) + [C.bid], writes=[bst])
            pt, bpt = C.PT.next()
            P.op("act", lambda e, st=st, pt=pt: e.activation(out=pt[:, 0:N], in_=st[:, 0:N], func=AF.Exp, scale=scale),
                 reads=[bst], writes=[bpt])
            pend[i] = (pt, bpt, vkey, vb)
        if i >= LA:
            k = i - LA
            pt, bpt, vkey, vb = pend.pop(k)
            for j in range(nsub):
                P.op("pe", lambda e, pt=pt, j=j, vkey=vkey, k=k: e.matmul(
                    acc(j), lhsT=pt[:, j * 128:(j + 1) * 128], rhs=v_of(vkey, j), start=(k == 0), stop=(k == n - 1)),
                    reads=[bpt] + list(vb), writes=[bacc])


def emit_oT(P, C, o_tok, bo, oT_d):
    for tb in range(NTB):
        stg, bs = C.oTs.next()
        for tt in range(4):
            t = tb * 4 + tt
            ptr, bp = C.ptr.next()
            for c in range(4):
                P.op("pe", lambda e, c=c, t=t, ptr=ptr: e.transpose(out=ptr[:, c, :], in_=o_tok[:, t, c * 128:(c + 1) * 128], identity=C.ident[:]),
                     reads=[bo, C.bid], writes=[bp])
            P.op("dve", lambda e, ptr=ptr, stg=stg, tt=tt: e.tensor_copy(out=stg[:, :, tt * 128:(tt + 1) * 128], in_=ptr[:]),
                 reads=[bp], writes=[bs])
        P.dma("sp" if tb % 2 == 0 else "act", oT_d.rearrange("(c p) t -> p c t", p=128)[:, :, tb * 512:(tb + 1) * 512], stg[:], reads=[bs])


def build_k2a(nc, which=("swa", "mla", "diff", "nat")):
    P = Prog(nc)
    din = lambda n, s, d=BF16: nc.dram_tensor(n, list(s), d, kind="ExternalInput").ap()
    dout = lambda n, s, d=BF16: nc.dram_tensor(n, list(s), d, kind="ExternalOutput").ap()
    D = {}
    D["swa_qT"] = din("swa_qT", [512, T]); D["swa_kT"] = din("swa_kT", [128, 34 * 128]); D["swa_v"] = din("swa_v", [34, 128, 130])
    D["swa_masks"] = din("swa_masks", [4, 128, 512], F32); D["sinkb"] = din("sinkb", [128, 8], F32)
    D["mla_qT"] = din("mla_qT", [8, 96, T]); D["mla_kT"] = din("mla_kT", [8, 96, 2 * T]); D["mla_v"] = din("mla_v", [64, 128, 520])
    D["diff_qT"] = din("diff_qT", [512, T]); D["diff_kT"] = din("diff_kT", [512, 2 * T]); D["diff_v"] = din("diff_v", [64, 128, 516])
    D["lamb"] = din("lamb", [128, 4, 64], F32); D["subln"] = din("subln", [128, 128], F32); D["lami"] = din("lami", [128, 2], F32)
    D["nat_qT"] = din("nat_qT", [512, T]); D["nat_kT"] = din("nat_kT", [512, 36 * 128]); D["nat_v"] = din("nat_v", [36, 128, 520])
    D["nat_bias"] = din("nat_bias", [8, 128, 29 * 128], F32)
    D["ident"] = din("ident", [128, 128], F32)
    oT_d = dout("oT", [4, 512, T])
    emit_attention(P, D, oT_d, which)
    P.finish()
    P.emit()
    return P


class HostSrc:
    def __init__(self, D):
        self.D = D

    def swa_k(self, kT, kv):
        return [(kT[:, kv, :], self.D["swa_kT"][kv * 64:(kv + 1) * 64, :])]

    def swa_v(self, V):
        return [(V[:], self.D["swa_v"].rearrange("t p f -> p t f"))]

    def mla_k(self, kT, h):
        return [(kT[:, 0:T], self.D["mla_kT"][h, :, 0:T]), (kT[:, T:2 * T], self.D["mla_kT"][h, :, T:2 * T])]

    def mla_v(self, V, h):
        return [(V[:], self.D["mla_v"][:, :, h * 65:(h + 1) * 65].rearrange("t p f -> p t f"))]

    def diff_k(self, kT, row):
        return [(kT[:, 0:T], self.D["diff_kT"][row:row + 64, 0:T]), (kT[:, T:2 * T], self.D["diff_kT"][row:row + 64, T:2 * T])]

    def diff_v(self, V, h):
        return [(V[:], self.D["diff_v"][:, :, h * 129:(h + 1) * 129].rearrange("t p f -> p t f"))]

    def nat_k(self, kT, h):
        return [(kT[:], self.D["nat_kT"][h * 64:(h + 1) * 64, :])]

    def nat_v(self, V, h):
        return [(V[:], self.D["nat_v"][:, :, h * 65:(h + 1) * 65].rearrange("t p f -> p t f"))]


class GatherSrc:
    def __init__(self, O, G):
        self.O = O; self.G = G

    def swa_k(self, kT, kv):
        g = self.G["swa_kT"][0]
        r = slice(kv * 64, (kv + 1) * 64); r1 = slice(128 + kv * 64, 128 + (kv + 1) * 64)
        return [(kT[:, kv, 0:128], g[r, T - 128:T]), (kT[:, kv, 128:128 + T], self.O["swa_kT"][r, :]),
                (kT[:, kv, 128 + T:256 + T], g[r1, 0:128])]

    def swa_v(self, V):
        g = self.G["swa_v"][0]
        return [(V[:, 0, :], g[T - 128:T, :]), (V[:, 1:33, :], self.O["swa_v"].rearrange("t p f -> p t f")),
                (V[:, 33, :], g[T:T + 128, :])]

    def mla_k(self, kT, h):
        g = self.G["mla_kT"][h // 2]
        r0 = (h % 2) * 96
        return [(kT[:, 0:T], g[r0:r0 + 96, :]), (kT[:, T:2 * T], g[192 + r0:192 + r0 + 96, :])]

    def mla_v(self, V, h):
        out = []
        for k in range(4):
            g = self.G["mla_v"][k]
            for r in range(2):
                out.append((V[:, r * 32 + k * 8:r * 32 + k * 8 + 8, :],
                            g[r * 1024:(r + 1) * 1024, h * 65:(h + 1) * 65].rearrange("(t p) f -> p t f", p=128)))
        return out

    def diff_k(self, kT, row):
        g = self.G["diff_kT"][row // 128]
        r0 = row % 128
        return [(kT[:, 0:T], g[r0:r0 + 64, :]), (kT[:, T:2 * T], g[128 + r0:128 + r0 + 64, :])]

    def diff_v(self, V, h):
        out = []
        for k in range(4):
            g = self.G["diff_v"][k]
            for r in range(2):
                out.append((V[:, r * 32 + k * 8:r * 32 + k * 8 + 8, :],
                            g[r * 1024:(r + 1) * 1024, h * 129:(h + 1) * 129].rearrange("(t p) f -> p t f", p=128)))
        return out

    def nat_k(self, kT, h):
        g = self.G["nat_kT"][h // 2]
        r0 = (h % 2) * 64
        return [(kT[:, 0:256], g[r0:r0 + 64, T - 256:T]), (kT[:, 256:256 + T], self.O["nat_kT"][h * 64:(h + 1) * 64, :]),
                (kT[:, 256 + T:512 + T], g[128 + r0:128 + r0 + 64, 0:256])]

    def nat_v(self, V, h):
        gl = self.G["nat_v"][1]
        gf = self.G["nat_v"][0]
        c = slice(h * 65, (h + 1) * 65)
        return [(V[:, 0:2, :], gl[768:1024, c].rearrange("(t p) f -> p t f", p=128)),
                (V[:, 2:34, :], self.O["nat_v"][:, :, c].rearrange("t p f -> p t f")),
                (V[:, 34:36, :], gf[1024:1280, c].rearrange("(t p) f -> p t f", p=128))]


def emit_attention(P, D, oT_d, which=("swa", "mla", "diff", "nat"), SRC=None, bg=None):
    if SRC is None:
        SRC = HostSrc(D)

    def bg_step():
        if bg is not None:
            next(bg, None)
    C = AttnCtx()
    identf = P.sb("a_identf", [128, 128], F32); bidf = Buf()
    C.ident = P.sb("a_ident", [128, 128], BF16); C.bid = Buf()
    P.dma("pool", identf[:], D["ident"], writes=[bidf])
    P.op("dve", lambda e: e.tensor_copy(out=C.ident[:], in_=identf[:]), reads=[bidf], writes=[C.bid])
    epsb = P.sb("a_eps", [128, 1], F32); beps = Buf()
    P.op("pool", lambda e: e.memset(epsb[:], 1e-5), writes=[beps])
    C.ST = Rot(P, "a_ST", [128, 512], F32, 3, "ps")
    C.PT = Rot(P, "a_PT", [128, 512], BF16, 3)
    C.acc = Rot(P, "a_acc", [128, 4, 512], F32, 1, "ps")
    C.ptr = Rot(P, "a_ptr", [128, 4, 128], BF16, 1, "ps")
    C.oTs = Rot(P, "a_oTs", [128, 4, 512], BF16, 2)
    o_tok = P.sb("a_otok", [128, NTT, 512], BF16); bo = Buf("otok")
    rz = Rot(P, "a_rz", [128, 4, 1], F32, 2)

    if "swa" in which:
        P.push()
        qT = P.sb("swa_q", [64, 8, T], BF16); bq = Buf()
        for h in range(8):
            P.dma(("sp", "act", "pool")[h % 3], qT[:, h, :], D["swa_qT"][h * 64:(h + 1) * 64, :], writes=[bq], append=(h > 0))
        kT = P.sb("swa_k", [64, 2, 34 * 128], BF16); bk = Buf()
        n_ = 0
        for kv in range(2):
            for (d_, s_) in SRC.swa_k(kT, kv):
                P.dma("sp", d_, s_, writes=[bk], append=(n_ > 0)); n_ += 1
        V = P.sb("swa_vv", [128, 34, 130], BF16); bv = Buf()
        for n_, (d_, s_) in enumerate(SRC.swa_v(V)):
            P.dma("sp", d_, s_, writes=[bv], append=(n_ > 0))
        mf = P.sb("swa_mf", [128, 4, 512], F32); bmf = Buf()
        mk = P.sb("swa_mk", [128, 4, 512], BF16); bmk = Buf()
        P.dma("pool", mf[:], D["swa_masks"].rearrange("m p n -> p m n"), writes=[bmf])
        P.op("dve", lambda e: e.tensor_copy(out=mk[:], in_=mf[:]), reads=[bmf], writes=[bmk])
        es = P.sb("swa_es", [128, 8], F32); bes = Buf()
        P.dma("pool", es[:], D["sinkb"], writes=[bes])
        P.op("act", lambda e: e.activation(out=es[:], in_=es[:], func=AF.Exp), reads=[bes], writes=[bes])
        for qt in range(NTT):
            for kv in range(2):
                acc, bacc = C.acc.next()
                rhs_q = qT[:, kv * 4:(kv + 1) * 4, qt * 128:(qt + 1) * 128]
                kts = []
                for d_ in range(3):
                    ki = qt + d_
                    if d_ == 1:
                        bias = None
                    elif d_ == 0:
                        bias = mk[:, 2, :] if qt == 0 else mk[:, 0, :]
                    else:
                        bias = mk[:, 3, :] if qt == NTT - 1 else mk[:, 1, :]
                    kts.append((kT[:, kv, ki * 128:(ki + 1) * 128], [bk], ki, [bv], bias, [bmk]))
                attn_block(P, C, rhs_q, [bq], 512, kts, 0.125, lambda j, acc=acc: acc[:, j, 0:65], bacc, 4,
                           lambda ki, j, kv=kv: V[:, ki, kv * 65:(kv + 1) * 65])
                r_, br = rz.next()
                P.op("dve", lambda e, r_=r_, acc=acc, kv=kv: e.tensor_tensor(
                    out=r_[:], in0=acc[:, :, 64:65], in1=es[:, kv * 4:(kv + 1) * 4].rearrange("p (g o) -> p g o", o=1), op=ALU.add),
                    reads=[bacc, bes], writes=[br])
                P.op("dve", lambda e, r_=r_: e.reciprocal(out=r_[:], in_=r_[:]), reads=[br], writes=[br])
                P.op("dve", lambda e, r_=r_, acc=acc, kv=kv, qt=qt: e.tensor_tensor(
                    out=o_tok[:, qt, kv * 256:(kv + 1) * 256].rearrange("p (g d) -> p g d", g=4),
                    in0=acc[:, :, 0:64], in1=r_[:].to_broadcast([128, 4, 64]), op=ALU.mult),
                    reads=[bacc, br], writes=[bo])
        emit_oT(P, C, o_tok, bo, oT_d[0])
        P.pop()

    if "mla" in which:
        P.push()
        qTr = Rot(P, "mla_q", [96, T], BF16, 2)
        kTr = Rot(P, "mla_k", [96, 2 * T], BF16, 2)
        Vr = Rot(P, "mla_vv", [128, 64, 65], BF16, 2)
        sc = 96.0 ** -0.5
        for h in range(8):
            qT, bq = qTr.next(); kT, bk = kTr.next(); V, bv = Vr.next()
            P.dma("sp", qT[:], D["mla_qT"][h], writes=[bq])
            for n_, (d_, s_) in enumerate(SRC.mla_k(kT, h)):
                P.dma("sp", d_, s_, writes=[bk], append=(n_ > 0))
            for n_, (d_, s_) in enumerate(SRC.mla_v(V, h)):
                P.dma("sp", d_, s_, writes=[bv], append=(n_ > 0))
            for qb in range(NTB):
                if qb % 2 == 0:
                    bg_step()
                acc, bacc = C.acc.next()
                kts = [(kT[:, ki * 128:(ki + 1) * 128], [bk], ki, [bv], None, []) for ki in range(64)]
                attn_block(P, C, qT[:, qb * 512:(qb + 1) * 512], [bq], 512, kts, sc, lambda j, acc=acc: acc[:, j, 0:65], bacc, 4,
                           lambda ki, j, V=V: V[:, ki, :])
                r_, br = rz.next()
                P.op("dve", lambda e, r_=r_, acc=acc: e.reciprocal(out=r_[:], in_=acc[:, :, 64:65]), reads=[bacc], writes=[br])
                P.op("dve", lambda e, r_=r_, acc=acc, qb=qb, h=h: e.tensor_tensor(
                    out=o_tok[:, qb * 4:(qb + 1) * 4, h * 64:(h + 1) * 64],
                    in0=acc[:, :, 0:64], in1=r_[:].to_broadcast([128, 4, 64]), op=ALU.mult),
                    reads=[bacc, br], writes=[bo])
        emit_oT(P, C, o_tok, bo, oT_d[1])
        P.pop()

    if "diff" in which:
        P.push()
        qTr = Rot(P, "df_q", [64, T], BF16, 2)
        kTr = Rot(P, "df_k", [64, 2 * T], BF16, 2)
        Vr = Rot(P, "df_vv", [128, 64, 129], BF16, 1)
        o1 = P.sb("df_o1", [128, NTT, 128], F32); bo1 = Buf()
        lamb = P.sb("df_lamb", [128, 4, 64], F32); blamb = Buf()
        lami = P.sb("df_lami", [128, 2], F32)
        sg = P.sb("df_sg", [128, 128], F32); bsg = Buf()
        P.dma("pool", lamb[:], D["lamb"], writes=[blamb])
        P.dma("pool", lami[:], D["lami"], writes=[blamb], append=True)
        P.dma("pool", sg[:], D["subln"], writes=[bsg])
        P.op("dve", lambda e: e.tensor_scalar(out=sg[:], in0=sg[:], scalar1=lami[:, 1:2], scalar2=None, op0=ALU.mult), reads=[bsg, blamb], writes=[bsg])
        lp = P.sb("df_lp", [128, 2, 64], F32); blp = Buf()
        ls = P.sb("df_ls", [128, 2], F32); bls = Buf()
        nlam = P.sb("df_nlam", [128, 1], F32); bnl = Buf()
        P.op("dve", lambda e: e.tensor_tensor(out=lp[:, 0, :], in0=lamb[:, 0, :], in1=lamb[:, 1, :], op=ALU.mult), reads=[blamb], writes=[blp])
        P.op("dve", lambda e: e.tensor_tensor(out=lp[:, 1, :], in0=lamb[:, 2, :], in1=lamb[:, 3, :], op=ALU.mult), reads=[blamb, blp], writes=[blp])
        P.op("dve", lambda e: e.reduce_sum(out=ls[:], in_=lp[:], axis=AX.X), reads=[blp], writes=[bls])
        P.op("act", lambda e: e.activation(out=ls[:], in_=ls[:], func=AF.Exp), reads=[bls], writes=[bls])
        P.op("dve", lambda e: e.tensor_tensor(out=nlam[:], in0=ls[:, 1:2], in1=ls[:, 0:1], op=ALU.subtract), reads=[bls], writes=[bnl])
        P.op("dve", lambda e: e.tensor_tensor(out=nlam[:], in0=nlam[:], in1=lami[:, 0:1], op=ALU.subtract), reads=[bnl, blamb], writes=[bnl])
        ot = Rot(P, "df_ot", [128, 4, 128], F32, 2)
        sq = Rot(P, "df_sq", [128, 4, 128], F32, 1)
        ss = Rot(P, "df_ss", [128, 4], F32, 2)
        for h in range(4):
            V, bv = Vr.next()
            for n_, (d_, s_) in enumerate(SRC.diff_v(V, h)):
                P.dma("sp", d_, s_, writes=[bv], append=(n_ > 0))
            for m in range(2):
                row = (h * 2 + m) * 64
                qT, bq = qTr.next(); kT, bk = kTr.next()
                P.dma("sp", qT[:], D["diff_qT"][row:row + 64, :], writes=[bq])
                for n_, (d_, s_) in enumerate(SRC.diff_k(kT, row)):
                    P.dma("sp", d_, s_, writes=[bk], append=(n_ > 0))
                for qb in range(NTB):
                    acc, bacc = C.acc.next()
                    kts = [(kT[:, ki * 128:(ki + 1) * 128], [bk], ki, [bv], None, []) for ki in range(64)]
                    attn_block(P, C, qT[:, qb * 512:(qb + 1) * 512], [bq], 512, kts, 0.125, lambda j, acc=acc: acc[:, j, 0:129], bacc, 4,
                               lambda ki, j, V=V: V[:, ki, :])
                    r_, br = rz.next()
                    P.op("dve", lambda e, r_=r_, acc=acc: e.reciprocal(out=r_[:], in_=acc[:, :, 128:129]), reads=[bacc], writes=[br])
                    tl = slice(qb * 4, (qb + 1) * 4)
                    if m == 0:
                        P.op("dve", lambda e, r_=r_, acc=acc, tl=tl: e.tensor_tensor(
                            out=o1[:, tl, :], in0=acc[:, :, 0:128], in1=r_[:].to_broadcast([128, 4, 128]), op=ALU.mult),
                            reads=[bacc, br], writes=[bo1])
                    else:
                        o_, bo_ = ot.next()
                        P.op("dve", lambda e, r_=r_, acc=acc, o_=o_: e.tensor_tensor(
                            out=o_[:], in0=acc[:, :, 0:128], in1=r_[:].to_broadcast([128, 4, 128]), op=ALU.mult),
                            reads=[bacc, br], writes=[bo_])
                        P.op("dve", lambda e, o_=o_, tl=tl: e.scalar_tensor_tensor(
                            out=o_[:], in0=o_[:], scalar=nlam[:, 0:1], in1=o1[:, tl, :], op0=ALU.mult, op1=ALU.add),
                            reads=[bo_, bnl, bo1], writes=[bo_])
                        sq_, bsq = sq.next(); ss_, bss = ss.next()
                        P.op("pool", lambda e, o_=o_, sq_=sq_: e.tensor_tensor(out=sq_[:], in0=o_[:], in1=o_[:], op=ALU.mult), reads=[bo_], writes=[bsq])
                        P.op("dve", lambda e, sq_=sq_, ss_=ss_: e.reduce_sum(out=ss_[:], in_=sq_[:], axis=AX.X), reads=[bsq], writes=[bss])
                        P.op("act", lambda e, ss_=ss_: e.activation(out=ss_[:], in_=ss_[:], func=AF.Ln, bias=epsb[:, 0:1], scale=1.0 / 128.0), reads=[bss, beps], writes=[bss])
                        P.op("act", lambda e, ss_=ss_: e.activation(out=ss_[:], in_=ss_[:], func=AF.Exp, scale=-0.5), reads=[bss], writes=[bss])
                        P.op("pool", lambda e, o_=o_, ss_=ss_: e.tensor_tensor(
                            out=o_[:], in0=o_[:], in1=ss_[:].rearrange("p (g o) -> p g o", o=1).to_broadcast([128, 4, 128]), op=ALU.mult),
                            reads=[bo_, bss], writes=[bo_])
                        P.op("pool", lambda e, o_=o_, tl=tl, h=h: e.tensor_tensor(
                            out=o_tok[:, tl, h * 128:(h + 1) * 128], in0=o_[:],
                            in1=sg[:].rearrange("p (o d) -> p o d", o=1).to_broadcast([128, 4, 128]), op=ALU.mult),
                            reads=[bo_, bsg], writes=[bo])
        emit_oT(P, C, o_tok, bo, oT_d[2])
        P.pop()

    if "nat" in which:
        P.push()
        qTr = Rot(P, "nat_q", [64, T], BF16, 2)
        kTr = Rot(P, "nat_k", [64, 36 * 128], BF16, 2)
        Vr = Rot(P, "nat_vv", [128, 36, 65], BF16, 2)
        bfr = Rot(P, "nat_bf", [128, 29 * 128], F32, 1)
        bbr = Rot(P, "nat_bb", [128, 29, 128], BF16, 2)
        for h in range(8):
            qT, bq = qTr.next(); kT, bk = kTr.next(); V, bv = Vr.next()
            bf_, bbf = bfr.next(); bb_, bbb = bbr.next()
            P.dma("sp", qT[:], D["nat_qT"][h * 64:(h + 1) * 64, :], writes=[bq])
            for n_, (d_, s_) in enumerate(SRC.nat_k(kT, h)):
                P.dma("sp", d_, s_, writes=[bk], append=(n_ > 0))
            for n_, (d_, s_) in enumerate(SRC.nat_v(V, h)):
                P.dma("sp", d_, s_, writes=[bv], append=(n_ > 0))
            P.dma("sp", bf_[:], D["nat_bias"][h], writes=[bbf])
            P.op("dve", lambda e, bb_=bb_, bf_=bf_: e.tensor_copy(out=bb_[:].rearrange("p a b -> p (a b)"), in_=bf_[:]), reads=[bbf], writes=[bbb])
            for qt in range(NTT):
                if qt == 0:
                    kis = list(range(0, 6)); tbl = list(range(5, 11))
                elif qt == 1:
                    kis = list(range(1, 7)); tbl = list(range(11, 17))
                elif qt == NTT - 2:
                    kis = list(range(29, 35)); tbl = list(range(17, 23))
                elif qt == NTT - 1:
                    kis = list(range(30, 36)); tbl = list(range(23, 29))
                else:
                    kis = list(range(qt, qt + 5)); tbl = list(range(0, 5))
                acc, bacc = C.acc.next()
                kts = [(kT[:, ki * 128:(ki + 1) * 128], [bk], ki, [bv], bb_[:, ti, :], [bbb]) for ki, ti in zip(kis, tbl)]
                attn_block(P, C, qT[:, qt * 128:(qt + 1) * 128], [bq], 128, kts, 1.0, lambda j, acc=acc: acc[:, 0, 0:65], bacc, 1,
                           lambda ki, j, V=V: V[:, ki, :])
                r_, br = rz.next()
                P.op("dve", lambda e, r_=r_, acc=acc: e.reciprocal(out=r_[:, 0, :], in_=acc[:, 0, 64:65]), reads=[bacc], writes=[br])
                P.op("dve", lambda e, r_=r_, acc=acc, qt=qt, h=h: e.tensor_scalar(
                    out=o_tok[:, qt, h * 64:(h + 1) * 64], in0=acc[:, 0, 0:64], scalar1=r_[:, 0, 0:1], scalar2=None, op0=ALU.mult),
                    reads=[bacc, br], writes=[bo])
        emit_oT(P, C, o_tok, bo, oT_d[3])
        P.pop()


def nat_bias_tables(rpb, half):
    a = np.arange(128)
    out = np.full((8, 29, 128, 128), NEGM, np.float32)

    def table(tq, tk):
        if tk < 0 or tk >= 64:
            return None
        rk = 2 * tk + a // 64; ck = a % 64
        rq = 2 * tq + a // 64; cq = a % 64
        r0 = np.clip(rq - 4, 0, 120); cs = np.clip(cq - 8, 0, 48)
        valid = ((rk[:, None] >= r0[None, :]) & (rk[:, None] < r0[None, :] + 8) &
                 (ck[:, None] >= cs[None, :]) & (ck[:, None] < cs[None, :] + 16))
        ri = np.clip(rk[:, None] - rq[None, :] + 7, 0, 14)
        ci = np.clip(ck[:, None] - cq[None, :] + 15, 0, 30)
        t = rpb[:, ri, ci]
        return np.where(valid[None], t, np.float32(NEGM)).astype(np.float32)

    g0 = half * 32
    specs = [(10, off) for off in range(-2, 3)]
    specs += [(g0 + 0, off) for off in range(-2, 4)]
    specs += [(g0 + 1, off) for off in range(-2, 4)]
    specs += [(g0 + 30, off) for off in range(-3, 3)]
    specs += [(g0 + 31, off) for off in range(-3, 3)]
    for i, (tq, off) in enumerate(specs):
        t = table(tq, tq + off)
        if t is not None:
            out[:, i] = t
    return np.ascontiguousarray(out.transpose(0, 2, 1, 3).reshape(8, 128, 29 * 128))


def swa_masks(half):
    a = np.arange(128)
    L = np.where(a[None, :] <= a[:, None], 0.0, NEGM).astype(np.float32)
    R = np.where(a[:, None] <= a[None, :], 0.0, NEGM).astype(np.float32)
    allm = np.full((128, 128), NEGM, np.float32)
    first = allm if half == 0 else L
    last = allm if half == 1 else R
    return np.ascontiguousarray(np.stack([np.tile(m, (1, 4)) for m in (L, R, first, last)]))


def k2a_host_inputs(inp, l, core, k1o):
    import ml_dtypes
    b, half = core // 2, core % 2
    me, pa = k1o[core], k1o[core ^ 1]
    lo, hi = (me, pa) if half == 0 else (pa, me)
    bf = lambda a: np.ascontiguousarray(a)
    z = lambda shape: np.zeros(shape, ml_dtypes.bfloat16)
    m = {}
    m["swa_qT"] = bf(me["swa_qT"])
    left = pa["swa_kT"][:, -128:] if half == 1 else z((128, 128))
    right = pa["swa_kT"][:, :128] if half == 0 else z((128, 128))
    m["swa_kT"] = bf(np.concatenate([left, me["swa_kT"], right], 1))
    left = pa["swa_v"][-1:] if half == 1 else z((1, 128, 130))
    right = pa["swa_v"][:1] if half == 0 else z((1, 128, 130))
    m["swa_v"] = bf(np.concatenate([left, me["swa_v"], right], 0))
    m["swa_masks"] = swa_masks(half)
    m["sinkb"] = np.ascontiguousarray(np.broadcast_to(inp["swa_sink"][l][None, :], (128, 8))).astype(np.float32)
    m["mla_qT"] = bf(me["mla_qT"])
    m["mla_kT"] = bf(np.concatenate([lo["mla_kT"], hi["mla_kT"]], 2))
    m["mla_v"] = bf(np.concatenate([lo["mla_v"], hi["mla_v"]], 0))
    m["diff_qT"] = bf(me["diff_qT"])
    m["diff_kT"] = bf(np.concatenate([lo["diff_kT"], hi["diff_kT"]], 1))
    m["diff_v"] = bf(np.concatenate([lo["diff_v"], hi["diff_v"]], 0))
    lam = np.stack([inp["diff_lambda_q1"][l], inp["diff_lambda_k1"][l], inp["diff_lambda_q2"][l], inp["diff_lambda_k2"][l]])
    m["lamb"] = np.ascontiguousarray(np.broadcast_to(lam[None], (128, 4, 64))).astype(np.float32)
    m["subln"] = np.ascontiguousarray(np.broadcast_to(inp["diff_subln"][l][None], (128, 128))).astype(np.float32)
    import math
    li = 0.8 - 0.6 * math.exp(-0.3 * l)
    m["lami"] = np.ascontiguousarray(np.broadcast_to(np.array([li, 1.0 - li], np.float32)[None], (128, 2)))
    m["nat_qT"] = bf(me["nat_qT"])
    left = pa["nat_kT"][:, -256:] if half == 1 else z((512, 256))
    right = pa["nat_kT"][:, :256] if half == 0 else z((512, 256))
    m["nat_kT"] = bf(np.concatenate([left, me["nat_kT"], right], 1))
    left = pa["nat_v"][-2:] if half == 1 else z((2, 128, 520))
    right = pa["nat_v"][:2] if half == 0 else z((2, 128, 520))
    m["nat_v"] = bf(np.concatenate([left, me["nat_v"], right], 0))
    m["nat_bias"] = nat_bias_tables(inp["nat_rpb"][l], half)
    m["ident"] = np.eye(128, dtype=np.float32)
    return m


DN_ALPHA = 4.0 ** 0.25


def emit_gvec_bcast(P, cT_d, adaw_d, adabb_d, gb, bgb, pA, wst_rot, tag):
    cs = P.sb(f"{tag}_cs", [128, 8], F32); bcs = Buf()
    csr = P.sb(f"{tag}_csr", [128, 8, 128], F32); bcsr = Buf()
    ab = P.sb(f"{tag}_ab", [128, 1024], F32); bab = Buf()
    P.dma("sp", cs[:], cT_d, writes=[bcs])
    P.dma("act", ab[:], adabb_d, writes=[bab])
    P.op("act", lambda e: e.activation(out=cs[:], in_=cs[:], func=AF.Silu), reads=[bcs], writes=[bcs])
    P.op("dve", lambda e: e.tensor_copy(out=csr[:], in_=cs[:].rearrange("p (k o) -> p k o", o=1).to_broadcast([128, 8, 128])),
         reads=[bcs], writes=[bcsr])
    for hf in range(2):
        ps, bps = pA.next()
        for q4 in range(2):
            ws_, bws = wst_rot.next()
            ws = ws_[:, 0:2048].rearrange("p (c n) -> p c n", c=8)
            c0 = hf * 512 + q4 * 256
            P.dma("sp" if q4 == 0 else "act", ws, adaw_d[:, c0:c0 + 256].rearrange("(c p) n -> p c n", p=128), writes=[bws])
            for kc in range(8):
                P.op("pe", lambda e, kc=kc, ps=ps, ws=ws, q4=q4: e.matmul(ps[:, q4 * 256:(q4 + 1) * 256], lhsT=csr[:, kc, :], rhs=ws[:, kc, :],
                                                                 start=(kc == 0), stop=(kc == 7)), reads=[bcsr, bws], writes=[bps])
        P.op("dve", lambda e, ps=ps, hf=hf: e.tensor_tensor(out=gb[:, hf * 512:(hf + 1) * 512], in0=ps[:], in1=ab[:, hf * 512:(hf + 1) * 512], op=ALU.add),
             reads=[bps, bab], writes=[bgb])


def emit_resid_ln(P, C, y_ps, by, xt, bx, gb, bgb, lng, lnb, bln, epsb, beps, out_tile, bout, tag):
    t1, bt1 = C["t1"].next()
    for hf in range(2):
        P.op("dve", lambda e, hf=hf, t1=t1: e.tensor_tensor(out=t1[:, hf * 512:(hf + 1) * 512], in0=y_ps[hf][:], in1=gb[:, hf * 512:(hf + 1) * 512], op=ALU.mult),
             reads=[by[hf], bgb], writes=[bt1])
    P.op("dve", lambda e, t1=t1: e.scalar_tensor_tensor(out=t1[:], in0=xt[:], scalar=DN_ALPHA, in1=t1[:], op0=ALU.mult, op1=ALU.add),
         reads=[bx, bt1], writes=[bt1])
    s_, bs = C["st"].next(); m_, bm = C["mv"].next(); r_, br = C["rs"].next()
    for c in range(2):
        P.op("dve", lambda e, c=c, s_=s_, t1=t1: e.bn_stats(out=s_[:, c, :], in_=t1[:, c * 512:(c + 1) * 512]), reads=[bt1], writes=[bs])
    P.op("dve", lambda e, m_=m_, s_=s_: e.bn_aggr(out=m_[:], in_=s_[:]), reads=[bs], writes=[bm])
    P.op("act", lambda e, r_=r_, m_=m_: e.activation(out=r_[:], in_=m_[:, 1:2], func=AF.Ln, bias=epsb[:, 0:1], scale=1.0), reads=[bm, beps], writes=[br])
    P.op("act", lambda e, r_=r_: e.activation(out=r_[:], in_=r_[:], func=AF.Exp, scale=-0.5), reads=[br], writes=[br])
    P.op("dve", lambda e, t1=t1, m_=m_, r_=r_: e.tensor_scalar(out=t1[:], in0=t1[:], scalar1=m_[:, 0:1], scalar2=r_[:, 0:1], op0=ALU.subtract, op1=ALU.mult),
         reads=[bt1, bm, br], writes=[bt1])
    P.op("pool", lambda e, t1=t1: e.tensor_tensor(out=t1[:], in0=t1[:], in1=lng[:], op=ALU.mult), reads=[bt1, bln], writes=[bt1])
    P.op("pool", lambda e, t1=t1: e.tensor_tensor(out=out_tile[:], in0=t1[:], in1=lnb[:], op=ALU.add), reads=[bt1, bln], writes=[bout])


def build_k2b(nc):
    P = Prog(nc)
    din = lambda n, s, d=F32: nc.dram_tensor(n, list(s), d, kind="ExternalInput").ap()
    D = {}
    D["oT"] = din("oT", [4, 512, T], BF16); D["uT"] = din("uT", [1024, T], BF16); D["x"] = din("x", [T, 1024])
    D["wg"] = din("wg", [4, 1024, 1024]); D["wb"] = din("wb", [4, 512, 1024]); D["wo"] = din("wo", [1024, 1024])
    D["cT"] = din("cT", [128, 8]); D["adaw_g"] = din("adaw_g", [1024, 1024]); D["adab_g"] = din("adab_g", [128, 1024])
    D["lng"] = din("lng", [128, 1024]); D["lnb"] = din("lnb", [128, 1024])
    xo = nc.dram_tensor("xmid", [T, 1024], F32, kind="ExternalOutput").ap()
    emit_merge(P, D, xo)
    P.finish(); P.emit()
    return P


def emit_merge(P, D, xo):
    P.push()
    pA = Rot(P, "m_pA", [128, 512], F32, 3, "ps")
    pB = Rot(P, "m_pB", [128, 512], F32, 3, "ps")
    wst = Rot(P, "m_wst", [128, 2048], F32, 2)
    epsb = P.sb("m_eps", [128, 1], F32); beps = Buf()
    P.op("pool", lambda e: e.memset(epsb[:], 1e-5), writes=[beps])
    gb = P.sb("m_gb", [128, 1024], F32); bgb = Buf()
    emit_gvec_bcast(P, D["cT"], D["adaw_g"], D["adab_g"], gb, bgb, pA, wst, "m")
    mT = P.sb("m_mT", [128, 8, T], BF16)
    bmT = [[Buf() for _ in range(NTB)] for _ in range(8)]
    P.push()
    wgf = Rot(P, "m_wgf", [128, 8, 4, 128], F32, 1)
    wgb = Rot(P, "m_wgb", [128, 8, 4, 128], BF16, 2)
    wbf = Rot(P, "m_wbf", [128, 4, 4, 128], F32, 1)
    wbb = Rot(P, "m_wbb", [128, 4, 4, 128], BF16, 2)
    ub = Rot(P, "m_ub", [128, 8, 512], BF16, 2)
    ob = Rot(P, "m_ob", [128, 4, 4, 512], BF16, 2)
    gs = Rot(P, "m_gs", [128, 512], BF16, 2)
    ac = Rot(P, "m_ac", [128, 512], F32, 2)
    tm = Rot(P, "m_tm", [128, 512], F32, 2)
    for oc in range(8):
        wgf_, bwgf = wgf.next(); wgb_, bwgb = wgb.next(); wbf_, bwbf = wbf.next(); wbb_, bwbb = wbb.next()
        for n in range(4):
            P.dma(("sp", "act")[n % 2], wgf_[:, :, n, :], D["wg"][n, :, oc * 128:(oc + 1) * 128].rearrange("(c p) n -> p c n", p=128),
                  writes=[bwgf], append=(n > 0))
            P.dma(("act", "sp")[n % 2], wbf_[:, :, n, :], D["wb"][n, :, oc * 128:(oc + 1) * 128].rearrange("(c p) n -> p c n", p=128),
                  writes=[bwbf], append=(n > 0))
        P.op("pool", lambda e, a=wgb_, b=wgf_: e.tensor_copy(out=a[:], in_=b[:]), reads=[bwgf], writes=[bwgb])
        P.op("pool", lambda e, a=wbb_, b=wbf_: e.tensor_copy(out=a[:], in_=b[:]), reads=[bwbf], writes=[bwbb])
        for tb in range(NTB):
            sl = slice(tb * 512, (tb + 1) * 512)
            u_, bu = ub.next(); o_, bo_ = ob.next()
            P.dma("sp", u_[:], D["uT"].rearrange("(c p) t -> p c t", p=128)[:, :, sl], writes=[bu])
            for n in range(4):
                P.dma(("act", "pool")[n % 2], o_[:, n, :, :], D["oT"][n].rearrange("(c p) t -> p c t", p=128)[:, :, sl], writes=[bo_], append=(n > 0))
            a_, ba = ac.next()
            for n in range(4):
                pg, bpg = pA.next(); pb, bpb = pB.next()
                for kc in range(8):
                    P.op("pe", lambda e, kc=kc, n=n, pg=pg, wgb_=wgb_, u_=u_: e.matmul(pg[:], lhsT=wgb_[:, kc, n, :], rhs=u_[:, kc, :], start=(kc == 0), stop=(kc == 7)),
                         reads=[bwgb, bu], writes=[bpg])
                for kc in range(4):
                    P.op("pe", lambda e, kc=kc, n=n, pb=pb, wbb_=wbb_, o_=o_: e.matmul(pb[:], lhsT=wbb_[:, kc, n, :], rhs=o_[:, n, kc, :], start=(kc == 0), stop=(kc == 3)),
                         reads=[bwbb, bo_], writes=[bpb])
                g_, bg = gs.next()
                P.op("act", lambda e, g_=g_, pg=pg: e.activation(out=g_[:], in_=pg[:], func=AF.Sigmoid), reads=[bpg], writes=[bg])
                if n == 0:
                    P.op("dve", lambda e, a_=a_, g_=g_, pb=pb: e.tensor_tensor(out=a_[:], in0=pb[:], in1=g_[:], op=ALU.mult), reads=[bpb, bg], writes=[ba])
                else:
                    t_, bt = tm.next()
                    P.op("dve", lambda e, t_=t_, g_=g_, pb=pb: e.tensor_tensor(out=t_[:], in0=pb[:], in1=g_[:], op=ALU.mult), reads=[bpb, bg], writes=[bt])
                    if n < 3:
                        P.op("pool", lambda e, a_=a_, t_=t_: e.tensor_tensor(out=a_[:], in0=a_[:], in1=t_[:], op=ALU.add), reads=[ba, bt], writes=[ba])
                    else:
                        P.op("pool", lambda e, a_=a_, t_=t_, oc=oc, sl=sl: e.tensor_tensor(out=mT[:, oc, sl], in0=a_[:], in1=t_[:], op=ALU.add),
                             reads=[ba, bt], writes=[bmT[oc][tb]])
    P.pop()
    P.push()
    wo = P.sb("m_wo", [128, 8, 1024], BF16); bwo = Buf()
    for q8 in range(4):
        ws_, bws = wst.next()
        ws = ws_[:, 0:2048].rearrange("p (c n) -> p c n", c=8)
        P.dma(("sp", "act")[q8 % 2], ws, D["wo"][:, q8 * 256:(q8 + 1) * 256].rearrange("(c p) n -> p c n", p=128), writes=[bws])
        P.op("pool", lambda e, ws=ws, q8=q8: e.tensor_copy(out=wo[:, :, q8 * 256:(q8 + 1) * 256], in_=ws), reads=[bws], writes=[bwo], )
    lng = P.sb("m_lng", [128, 1024], F32); lnb = P.sb("m_lnb", [128, 1024], F32); bln = Buf()
    P.dma("sp", lng[:], D["lng"], writes=[bln]); P.dma("act", lnb[:], D["lnb"], writes=[bln], append=True)
    C = {"t1": Rot(P, "m_t1", [128, 1024], F32, 2), "st": Rot(P, "m_st", [128, 2, 6], F32, 2),
         "mv": Rot(P, "m_mv", [128, 2], F32, 2), "rs": Rot(P, "m_rs", [128, 1], F32, 2)}
    xin = Rot(P, "m_xin", [128, 1024], F32, 2)
    xout = Rot(P, "m_xout", [128, 1024], F32, 2)
    for t in range(NTT):
        xt, bx = xin.next()
        P.dma("sp" if t % 2 == 0 else "act", xt[:], D["x"][t * 128:(t + 1) * 128, :], writes=[bx])
        ys = []; bys = []
        for hf in range(2):
            py, bpy = pA.next()
            for kc in range(8):
                P.op("pe", lambda e, kc=kc, py=py, hf=hf, t=t: e.matmul(py[:], lhsT=mT[:, kc, t * 128:(t + 1) * 128], rhs=wo[:, kc, hf * 512:(hf + 1) * 512],
                                                                 start=(kc == 0), stop=(kc == 7)), reads=[bwo, bmT[kc][t // 4]], writes=[bpy])
            ys.append(py); bys.append(bpy)
        xo_, bxo = xout.next()
        emit_resid_ln(P, C, ys, bys, xt, bx, gb, bgb, lng, lnb, bln, epsb, beps, xo_, bxo, "m")
        P.dma("pool", xo[t * 128:(t + 1) * 128, :], xo_[:], reads=[bxo])
    P.pop()
    P.pop()


def k2b_host_inputs(inp, l, core, xcur, oT, uT):

    b, half = core // 2, core % 2
    rep = lambda v: np.ascontiguousarray(np.broadcast_to(np.asarray(v, np.float32)[None, :], (128, len(v))))
    return {
        "oT": np.ascontiguousarray(oT), "uT": np.ascontiguousarray(uT),
        "x": np.ascontiguousarray(xcur[b, half * T:(half + 1) * T]),
        "wg": np.ascontiguousarray(inp["w_gate"][l]), "wb": np.ascontiguousarray(inp["w_branch"][l]),
        "wo": np.ascontiguousarray(inp["w_out"][l]),
        "cT": to_pc(inp["c"][b], 8), "adaw_g": np.ascontiguousarray(inp["ada_w"][l][:, 2048:3072]),
        "adab_g": rep(inp["ada_b"][l][2048:3072]),
        "lng": rep(inp["ln1_g"][l]), "lnb": rep(inp["ln1_b"][l]),
    }


TBLK = 256
NBLK = T // TBLK
NCH = 128
NEG = -1.0e30
U32 = mybir.dt.uint32


def build_k3(nc, nblk=NBLK, dbg=False):
    P = Prog(nc)
    din = lambda n, s, d=F32: nc.dram_tensor(n, list(s), d, kind="ExternalInput").ap()
    D = {}
    D["xmid"] = din("xmid", [T, 1024]); D["cT"] = din("cT", [128, 8])
    D["adaw_f"] = din("adaw_f", [1024, 3072]); D["adab_f"] = din("adab_f", [128, 16]); D["adab_g"] = din("adab_g", [128, 1024])
    D["wq"] = din("wq", [1024, 2048]); D["keysT"] = din("keysT", [128, 16, 128])
    D["UT"] = din("UT", [1024, 16384]); D["V"] = din("V", [16384, 1024])
    D["lng"] = din("lng", [128, 1024]); D["lnb"] = din("lnb", [128, 1024])
    D["ident"] = din("ident", [128, 128]); D["iota"] = din("iota", [128, 128])
    xo = nc.dram_tensor("xout", [T, 1024], F32, kind="ExternalOutput").ap()
    dbg_d = nc.dram_tensor("dbg", [128, 16, 128], F32, kind="ExternalOutput").ap() if dbg else None
    emit_peer(P, nc, D, xo, nblk, dbg_d)
    P.finish(); P.emit()
    return P


def peer_scratch(nc, tag):
    Ub_d = nc.dram_tensor(f"peer_Ub{tag}", [NCH, 128, 8 * 128], BF16, kind="Internal").ap()
    Vb_d = nc.dram_tensor(f"peer_Vb{tag}", [NCH, 128, 1024], BF16, kind="Internal").ap()
    return Ub_d, Vb_d, Buf("Ub_d"), Buf("Vb_d")


def peer_cast_gen(P, UT, V, Ub_d, Vb_d, bUb, bVb, cf, cb):
    for c4 in range(NCH // 4):
        f_, bf_ = cf.next(); b_, bb_ = cb.next()
        P.dma("sp", f_[:].rearrange("p (k n) -> p k n", k=8),
              UT[:, c4 * 512:(c4 + 1) * 512].rearrange("(k p) n -> p k n", p=128), writes=[bf_])
        P.op(("dve", "pool")[c4 % 2], lambda e, f_=f_, b_=b_: e.tensor_copy(
            out=b_[:].rearrange("p (c k n) -> p c k n", c=4, k=8), in_=f_[:].rearrange("p (k c n) -> p c k n", k=8, c=4)),
            reads=[bf_], writes=[bb_])
        P.dma("pool", Ub_d[c4 * 4:(c4 + 1) * 4].rearrange("c p n -> p c n"), b_[:].rearrange("p (c n) -> p c n", c=4),
              reads=[bb_], writes=[bUb], append=True)
        f_, bf_ = cf.next(); b_, bb_ = cb.next()
        P.dma("sp", f_[:].rearrange("p (c n) -> p c n", c=4),
              V[c4 * 512:(c4 + 1) * 512, :].rearrange("(c p) n -> p c n", p=128), writes=[bf_])
        P.op(("pool", "dve")[c4 % 2], lambda e, f_=f_, b_=b_: e.tensor_copy(out=b_[:], in_=f_[:]), reads=[bf_], writes=[bb_])
        P.dma("pool", Vb_d[c4 * 4:(c4 + 1) * 4].rearrange("c p n -> p c n"), b_[:].rearrange("p (c n) -> p c n", c=4),
              reads=[bb_], writes=[bVb], append=True)
        yield


def emit_peer(P, nc, D, xo, nblk=NBLK, dbg_d=None, tag="", pre=None):
    u2_d = nc.dram_tensor(f"peer_u2{tag}", [1024, T], BF16, kind="Internal").ap()
    P.push()
    bu2d = Buf("u2_d")
    if pre is None:
        Ub_d, Vb_d, bUb, bVb = peer_scratch(nc, tag)
        P.push()
        cf = Rot(P, "pa_cf", [128, 4096], F32, 2)
        cb = Rot(P, "pa_cb", [128, 4096], BF16, 2)
        for _ in peer_cast_gen(P, D["UT"], D["V"], Ub_d, Vb_d, bUb, bVb, cf, cb):
            pass
        P.pop()
    else:
        Ub_d, Vb_d, bUb, bVb = pre

    pA = Rot(P, "p_pA", [128, 512], F32, 2, "ps")
    ident_f = P.sb("p_identf", [128, 128], F32); bidf = Buf()
    ident = P.sb("p_ident", [128, 128], BF16); bid = Buf()
    epsb = P.sb("p_eps", [128, 1], F32); beps = Buf()
    P.dma("pool", ident_f[:], D["ident"], writes=[bidf])
    P.op("dve", lambda e: e.tensor_copy(out=ident[:], in_=ident_f[:]), reads=[bidf], writes=[bid])
    P.op("pool", lambda e: e.memset(epsb[:], 1e-5), writes=[beps])
    gb = P.sb("p_gb", [128, 1024], F32); bgb = Buf()
    P.push()
    wst = Rot(P, "p_wst", [128, 2048], F32, 2)
    emit_gvec_bcast(P, D["cT"], D["adaw_f"][:, 2048:3072], D["adab_g"], gb, bgb, pA, wst, "p")
    P.push()
    modc = P.sb("p_modc", [128, 16], F32); bmodc = Buf()
    sc1 = P.sb("p_sc1", [128, 8], F32); bsc1 = Buf()
    pm = Rot(P, "p_pm", [128, 512], F32, 1, "ps")
    emit_mod_cols(P, nc, D["cT"], D["adaw_f"][:, 0:2048], D["adab_f"], 2, modc, bmodc, pm, wst)
    P.op("dve", lambda e: e.tensor_scalar(out=sc1[:], in0=modc[:, 8:16], scalar1=1.0, scalar2=None, op0=ALU.add), reads=[bmodc], writes=[bsc1])
    bmod = Buf()
    P.op("dve", lambda e: e.tensor_copy(out=modc[:, 0:8], in_=modc[:, 0:8]), reads=[bmodc, bsc1], writes=[bmod])
    u2T = P.sb("p_u2T", [128, 8, T], BF16)
    bu2 = [Buf() for _ in range(NTT)]
    ptr = Rot(P, "p_ptr", [128, 8, 128], BF16, 2, "ps")
    emit_ln_mod(P, D["xmid"], u2T, bu2, modc, sc1, bmod, ident, bid, epsb, beps, ptr, tag="p")
    for c in range(8):
        P.dma(("sp", "act")[c % 2], u2_d[c * 128:(c + 1) * 128, :], u2T[:, c, :], reads=bu2, writes=[bu2d], append=(c > 0))
    P.pop()

    wqb_d = nc.dram_tensor(f"peer_wqb{tag}", [128, 8 * 2048], BF16, kind="Internal").ap()
    bwqd = Buf("wqb_d")
    P.push()
    wtmp = P.sb("p_wqtmp", [128, 8, 2048], BF16); bwt = Buf()
    for g in range(8):
        ws_, bws = wst.next()
        ws = ws_[:, 0:2048].rearrange("p (c n) -> p c n", c=8)
        P.dma(("sp", "act")[g % 2], ws, D["wq"][:, g * 256:(g + 1) * 256].rearrange("(c p) n -> p c n", p=128), writes=[bws])
        P.op("pool", lambda e, ws=ws, g=g: e.tensor_copy(out=wtmp[:, :, g * 256:(g + 1) * 256], in_=ws), reads=[bws], writes=[bwt])
    P.dma("sp", wqb_d, wtmp[:].rearrange("p k n -> p (k n)"), reads=[bwt], writes=[bwqd])
    P.pop()
    P.pop()

    keysT = P.sb("p_keysT", [128, 16, 128], F32); bkeys = Buf()
    P.dma("sp", keysT[:], D["keysT"], writes=[bkeys])
    iota = P.sb("p_iota", [128, 128], F32); biota = Buf()
    P.dma("act", iota[:], D["iota"], writes=[biota])
    lng = P.sb("p_lng", [128, 1024], F32); lnb = P.sb("p_lnb", [128, 1024], F32); bln = Buf()
    P.dma("sp", lng[:], D["lng"], writes=[bln]); P.dma("act", lnb[:], D["lnb"], writes=[bln], append=True)

    pO = [P.ps(f"p_pO{i}", [128, 512], F32) for i in range(4)]
    bpO = [Buf(f"pO{i}", excl=True) for i in range(4)]
    pH = Rot(P, "p_pH", [128, 512], F32, 2, "ps")
    pM = pA
    NTL = TBLK // 128

    Wd = nc.dram_tensor(f"peer_W{tag}", [nblk, 8, 128, TBLK * 16], BF16, kind="Internal").ap()
    bWd = [Buf(f"Wd{b}") for b in range(nblk)]

    wq = P.sb("p_wq", [128, 8, 2048], BF16); bwq = Buf()
    P.dma("act", wq[:].rearrange("p k n -> p (k n)"), wqb_d, reads=[bwqd], writes=[bwq])
    ublk = Rot(P, "p_ublk", [128, 8, TBLK], BF16, 2)
    qT = Rot(P, "p_qT", [128, 16, TBLK], F32, 1)
    C = {"t1": Rot(P, "p_t1", [128, 1024], F32, 1), "st": Rot(P, "p_st", [128, 2, 6], F32, 2),
         "mv": Rot(P, "p_mv", [128, 2], F32, 2), "rs": Rot(P, "p_rs", [128, 1], F32, 2)}
    xin = Rot(P, "p_xin", [128, 1024], F32, 1)
    xout = Rot(P, "p_xout", [128, 1024], F32, 1)
    S = Rot(P, "p_S", [128, 16, 128], F32, 1)
    S2 = Rot(P, "p_S2", [128, 128], F32, 2)
    top = Rot(P, "p_top", [128, 16, 16], F32, 1)
    jix = Rot(P, "p_jix", [128, 8, 16], U32, 1)
    jif = Rot(P, "p_jif", [128, 128], F32, 1)
    jT = Rot(P, "p_jT", [128, 128], F32, 1)
    cand = Rot(P, "p_cand", [128, 256], F32, 2)
    ctop = Rot(P, "p_ctop", [128, 24], F32, 2)
    sm = Rot(P, "p_sm", [128, 8, 8], F32, 1)
    junk = Rot(P, "p_junk", [128, 16], F32, 2)
    e0 = Rot(P, "p_e0", [128, 8, 128], BF16, 1)
    thr2 = Rot(P, "p_thr2", [128, 8, 16], F32, 1)
    sc2 = Rot(P, "p_sc2", [128, 8, 16], F32, 1)
    sc2b = Rot(P, "p_sc2b", [128, 8, 16], BF16, 1)
    Ytm = Rot(P, "p_Ytm", [128, 128, 64], BF16, 1)
    Ysm = Rot(P, "p_Ysm", [128, 64, 128], BF16, 1)
    Xsm = Rot(P, "p_Xsm", [128, 64, 128], BF16, 1)
    Wst = Rot(P, "p_Wst", [128, 4, 128, 16], BF16, 1)
    wsl = Rot(P, "p_wsl", [128, TBLK, 16], BF16, 2)
    uch = Rot(P, "p_uch", [128, 8, 128], BF16, 4)
    vch = Rot(P, "p_vch", [128, 1024], BF16, 4)
    gl = Rot(P, "p_gl", [128, TBLK], BF16, 3)
    zt = Rot(P, "p_zt", [128, TBLK], BF16, 4)
    ublocks = {}

    def sel_block(blk):
        tsl = slice(blk * TBLK, (blk + 1) * TBLK)
        u_, bu = ublk.next()
        ublocks[blk] = (u_, bu)
        P.dma("sp", u_[:], u2_d.rearrange("(c p) t -> p c t", p=128)[:, :, tsl], reads=[bu2d], writes=[bu])
        q_, bq = qT.next()
        for hp in range(16):
            ph, bph = pH.next()
            for kc in range(8):
                P.op("pe", lambda e, kc=kc, hp=hp, ph=ph, u_=u_: e.matmul(ph[:, 0:TBLK], lhsT=wq[:, kc, hp * 128:(hp + 1) * 128], rhs=u_[:, kc, :],
                                                                  start=(kc == 0), stop=(kc == 7)), reads=[bwq, bu], writes=[bph])
            P.op("act", lambda e, hp=hp, ph=ph, q_=q_: e.activation(out=q_[:, hp, :], in_=ph[:, 0:TBLK], func=AF.Copy), reads=[bph], writes=[bq])
            if hp % 4 == 3:
                yield
        for tt in range(NTL):
            S_, bS = S.next()
            for g4 in range(4):
                pm_, bpm = pM.next()
                for i4 in range(4):
                    hp = g4 * 4 + i4
                    P.op("pe", lambda e, hp=hp, i4=i4, pm_=pm_, q_=q_, tt=tt: e.matmul(
                        pm_[:, i4 * 128:(i4 + 1) * 128], lhsT=q_[:, hp, tt * 128:(tt + 1) * 128], rhs=keysT[:, hp, :], start=True, stop=True),
                        reads=[bq, bkeys], writes=[bpm])
                P.op("act", lambda e, g4=g4, pm_=pm_, S_=S_: e.activation(out=S_[:, g4 * 4:(g4 + 1) * 4, :].rearrange("p a b -> p (a b)"), in_=pm_[:], func=AF.Copy),
                     reads=[bpm], writes=[bS])
            if dbg_d is not None and blk == 0 and tt == 0:
                P.dma("sp", dbg_d, S_[:], reads=[bS])
            yield
            top_, btop = top.next(); jix_, bjix = jix.next()
            for hp in range(16):
                s2, bs2 = S2.next()
                P.op("dve", lambda e, hp=hp, top_=top_, S_=S_: e.max(out=top_[:, hp, 0:8], in_=S_[:, hp, :]), reads=[bS], writes=[btop])
                P.op("dve", lambda e, hp=hp, top_=top_, S_=S_, s2=s2: e.match_replace(out=s2[:], in_to_replace=top_[:, hp, 0:8], in_values=S_[:, hp, :], imm_value=NEG),
                     reads=[bS, btop], writes=[bs2])
                P.op("dve", lambda e, hp=hp, top_=top_, s2=s2: e.max(out=top_[:, hp, 8:16], in_=s2[:]), reads=[bs2, btop], writes=[btop])
                if hp % 2 == 1:
                    h = hp // 2
                    P.op("dve", lambda e, hp=hp, h=h, top_=top_, S_=S_, jix_=jix_: e.max_index(out=jix_[:, h, 0:8], in_max=top_[:, hp, 0:8], in_values=S_[:, hp, :]),
                         reads=[bS, btop], writes=[bjix])
                    P.op("dve", lambda e, hp=hp, h=h, top_=top_, S_=S_, jix_=jix_: e.max_index(out=jix_[:, h, 8:16], in_max=top_[:, hp, 8:16], in_values=S_[:, hp, :]),
                         reads=[bS, btop, bjix], writes=[bjix])
                if hp % 4 == 3:
                    yield
            jif_, bjif = jif.next()
            P.op("dve", lambda e, jif_=jif_, jix_=jix_: e.tensor_copy(out=jif_[:], in_=jix_[:].rearrange("p a b -> p (a b)")), reads=[bjix], writes=[bjif])
            pm_, bpm = pM.next()
            P.op("pe", lambda e, pm_=pm_, jif_=jif_: e.transpose(out=pm_[:, 0:128], in_=jif_[:], identity=ident_f[:]), reads=[bjif, bidf], writes=[bpm])
            jT_, bjT = jT.next()
            P.op("act", lambda e, pm_=pm_, jT_=jT_: e.activation(out=jT_[:], in_=pm_[:, 0:128], func=AF.Copy), reads=[bpm], writes=[bjT])
            sm_, bsm = sm.next(); thr_, bthr = thr2.next(); sc_, bsc = sc2.next(); e0_, be0 = e0.next()
            P.op("pool", lambda e, sm_=sm_: e.memset(sm_[:], 0.0), writes=[bsm])
            for h in range(8):
                cd, bcd = cand.next(); ct, bct = ctop.next()
                P.op("dve", lambda e, h=h, cd=cd, top_=top_: e.tensor_tensor(
                    out=cd[:].rearrange("p (a b) -> p a b", a=16),
                    in0=top_[:, 2 * h, :].rearrange("p (a o) -> p a o", o=1).to_broadcast([128, 16, 16]),
                    in1=top_[:, 2 * h + 1, :].rearrange("p (o b) -> p o b", o=1).to_broadcast([128, 16, 16]), op=ALU.add),
                    reads=[btop], writes=[bcd])
                for r in range(3):
                    P.op("dve", lambda e, r=r, cd=cd, ct=ct: e.max(out=ct[:, r * 8:(r + 1) * 8], in_=cd[:]), reads=[bcd], writes=[bct])
                    if r < 2:
                        P.op("dve", lambda e, r=r, cd=cd, ct=ct: e.match_replace(out=cd[:], in_to_replace=ct[:, r * 8:(r + 1) * 8], in_values=cd[:], imm_value=NEG),
                             reads=[bct, bcd], writes=[bcd])
                P.op("dve", lambda e, h=h, ct=ct, sm_=sm_: e.tensor_tensor(out=sm_[:, h, 0:1], in0=ct[:, 15:16], in1=ct[:, 16:17], op=ALU.add), reads=[bct, bsm], writes=[bsm])
                P.op("dve", lambda e, h=h, sm_=sm_: e.tensor_scalar(out=sm_[:, h, 0:1], in0=sm_[:, h, 0:1], scalar1=0.5, scalar2=None, op0=ALU.mult), reads=[bsm], writes=[bsm])
                P.op("dve", lambda e, h=h, top_=top_, sm_=sm_: e.tensor_scalar(out=sm_[:, h, 1:2], in0=top_[:, 2 * h, 0:1], scalar1=-1.0, scalar2=None, op0=ALU.mult), reads=[btop, bsm], writes=[bsm])
                P.op("dve", lambda e, h=h, top_=top_, sm_=sm_: e.tensor_scalar(out=sm_[:, h, 2:3], in0=top_[:, 2 * h + 1, 0:1], scalar1=-1.0, scalar2=None, op0=ALU.mult), reads=[btop, bsm], writes=[bsm])
                P.op("dve", lambda e, h=h, ct=ct, sm_=sm_: e.tensor_scalar(out=sm_[:, h, 5:6], in0=ct[:, 0:1], scalar1=-1.0, scalar2=None, op0=ALU.mult), reads=[bct, bsm], writes=[bsm])
                jk, bjk = junk.next()
                P.op("act", lambda e, h=h, ct=ct, sm_=sm_, jk=jk: e.activation(out=jk[:], in_=ct[:, 0:16], func=AF.Exp, bias=sm_[:, h, 5:6], scale=1.0, accum_out=sm_[:, h, 3:4]),
                     reads=[bct, bsm], writes=[bjk, bsm])
                P.op("dve", lambda e, h=h, sm_=sm_: e.reciprocal(out=sm_[:, h, 4:5], in_=sm_[:, h, 3:4]), reads=[bsm], writes=[bsm])
                P.op("act", lambda e, h=h, S_=S_, sm_=sm_, e0_=e0_: e.activation(out=e0_[:, h, :], in_=S_[:, 2 * h, :], func=AF.Exp, bias=sm_[:, h, 1:2], scale=1.0),
                     reads=[bS, bsm], writes=[be0])
                P.op("dve", lambda e, h=h, top_=top_, sm_=sm_, thr_=thr_: e.tensor_scalar(out=thr_[:, h, :], in0=top_[:, 2 * h + 1, :], scalar1=-1.0, scalar2=sm_[:, h, 0:1], op0=ALU.mult, op1=ALU.add),
                     reads=[btop, bsm], writes=[bthr])
                P.op("act", lambda e, h=h, top_=top_, sm_=sm_, sc_=sc_: e.activation(out=sc_[:, h, :], in_=top_[:, 2 * h + 1, :], func=AF.Exp, bias=sm_[:, h, 2:3], scale=1.0),
                     reads=[btop, bsm], writes=[bsc])
                P.op("dve", lambda e, h=h, sm_=sm_, sc_=sc_: e.tensor_scalar(out=sc_[:, h, :], in0=sc_[:, h, :], scalar1=sm_[:, h, 4:5], scalar2=None, op0=ALU.mult),
                     reads=[bsc, bsm], writes=[bsc])
                if h % 2 == 1:
                    yield
            scb_, bscb = sc2b.next()
            P.op("dve", lambda e, scb_=scb_, sc_=sc_: e.tensor_copy(out=scb_[:], in_=sc_[:]), reads=[bsc], writes=[bscb])
            for ch in range(2):
                csl = slice(ch * 64, (ch + 1) * 64)
                Y_, bY = Ytm.next()
                for h in range(8):
                    yv = Y_[:, h * 16:(h + 1) * 16, :]
                    P.op("dve", lambda e, h=h, yv=yv, S_=S_, thr_=thr_, csl=csl: e.tensor_tensor(
                        out=yv, in0=S_[:, 2 * h, csl].rearrange("p (o n) -> p o n", o=1).to_broadcast([128, 16, 64]),
                        in1=thr_[:, h, :].rearrange("p (s o) -> p s o", o=1).to_broadcast([128, 16, 64]), op=ALU.is_ge),
                        reads=[bS, bthr], writes=[bY])
                    P.op("dve", lambda e, h=h, yv=yv, e0_=e0_, csl=csl: e.tensor_tensor(
                        out=yv, in0=yv, in1=e0_[:, h, csl].rearrange("p (o n) -> p o n", o=1).to_broadcast([128, 16, 64]), op=ALU.mult),
                        reads=[bY, be0], writes=[bY])
                    P.op("dve", lambda e, h=h, yv=yv, scb_=scb_: e.tensor_tensor(
                        out=yv, in0=yv, in1=scb_[:, h, :].rearrange("p (s o) -> p s o", o=1).to_broadcast([128, 16, 64]), op=ALU.mult),
                        reads=[bY, bscb], writes=[bY])
                    if h % 4 == 3:
                        yield
                Ys_, bYs = Ysm.next()
                for c4 in range(16):
                    pm_, bpm = pM.next()
                    pmb = pm_[:].bitcast(BF16)
                    for i in range(4):
                        cc = c4 * 4 + i
                        P.op("pe", lambda e, cc=cc, i=i, pmb=pmb, Y_=Y_: e.transpose(out=pmb[:, i * 128:(i + 1) * 128], in_=Y_[:, :, cc], identity=ident[:]),
                             reads=[bY, bid], writes=[bpm])
                    if c4 % 2 == 0:
                        P.op("act", lambda e, c4=c4, pmb=pmb, Ys_=Ys_: e.activation(out=Ys_[:, c4 * 4:(c4 + 1) * 4, :].rearrange("p a b -> p (a b)"), in_=pmb[:, 0:512], func=AF.Copy),
                             reads=[bpm], writes=[bYs])
                    else:
                        P.op("dve", lambda e, c4=c4, pmb=pmb, Ys_=Ys_: e.tensor_copy(out=Ys_[:, c4 * 4:(c4 + 1) * 4, :].rearrange("p a b -> p (a b)"), in_=pmb[:, 0:512]),
                             reads=[bpm], writes=[bYs])
                    if c4 % 4 == 3:
                        yield
                Ws_, bWs = Wst.next()
                for th in range(2):
                    X_, bX = Xsm.next()
                    for q2 in range(2):
                        P.op("dve", lambda e, q2=q2, th=th, X_=X_, jT_=jT_: e.tensor_tensor(
                            out=X_[:, q2 * 32:(q2 + 1) * 32, :],
                            in0=iota[:].rearrange("p (o n) -> p o n", o=1).to_broadcast([128, 32, 128]),
                            in1=jT_[:, th * 64 + q2 * 32:th * 64 + (q2 + 1) * 32].rearrange("p (s o) -> p s o", o=1).to_broadcast([128, 32, 128]), op=ALU.is_equal),
                            reads=[biota, bjT], writes=[bX])
                    for t8 in range(8):
                        pm_, bpm = pM.next()
                        for i in range(8):
                            tl = t8 * 8 + i
                            tk = th * 64 + tl
                            P.op("pe", lambda e, tk=tk, tl=tl, i=i, pm_=pm_, X_=X_, Ys_=Ys_: e.matmul(pm_[:, i * 64:(i + 1) * 64], lhsT=X_[:, tl, :], rhs=Ys_[:, :, tk], start=True, stop=True),
                                 reads=[bX, bYs], writes=[bpm])
                        tok0 = th * 64 + t8 * 8
                        src = pm_[:].rearrange("p (t g c) -> p g t c", t=8, g=4)
                        dst = Ws_[:, :, tok0:tok0 + 8, :]
                        if t8 % 2 == 0:
                            P.op("act", lambda e, src=src, dst=dst: e.activation(out=dst, in_=src, func=AF.Copy), reads=[bpm], writes=[bWs])
                        else:
                            P.op("dve", lambda e, src=src, dst=dst: e.tensor_copy(out=dst, in_=src), reads=[bpm], writes=[bWs])
                        if t8 % 4 == 3:
                            yield
                for g in range(4):
                    cg = ch * 4 + g
                    P.dma(("act", "pool")[g % 2], Wd[blk, cg, :, tt * 128 * 16:(tt + 1) * 128 * 16], Ws_[:, g, :, :].rearrange("p t c -> p (t c)"),
                          reads=[bWs], writes=[bWd[blk]], append=True)
                yield

    def drain(gen, n):
        if gen is None:
            return None
        for _ in range(n):
            try:
                next(gen)
            except StopIteration:
                return None
        return gen

    gen = sel_block(0)
    gen = drain(gen, 10 ** 9)
    for blk in range(nblk):
        u_, bu = ublocks[blk]
        nxt = sel_block(blk + 1) if blk + 1 < nblk else None
        LAG = 2
        stage = {}
        for c in range(NCH + LAG):
            if c < NCH:
                if c % 16 == 0:
                    w_, bw = wsl.next()
                    P.dma("sp", w_[:].rearrange("p t c -> p (t c)"), Wd[blk, c // 16], reads=[bWd[blk]], writes=[bw])
                uc, buc = uch.next(); vc, bvc = vch.next()
                P.dma("sp", uc[:].rearrange("p k n -> p (k n)"), Ub_d[c], reads=[bUb], writes=[buc])
                P.dma("sp", vc[:], Vb_d[c], reads=[bVb], writes=[bvc])
                ph, bph = pH.next()
                for kc in range(8):
                    P.op("pe", lambda e, kc=kc, ph=ph, uc=uc, u_=u_: e.matmul(ph[:, 0:TBLK], lhsT=uc[:, kc, :], rhs=u_[:, kc, :], start=(kc == 0), stop=(kc == 7)),
                         reads=[buc, bu], writes=[bph])
                g_, bg = gl.next()
                P.op("act", lambda e, ph=ph, g_=g_: e.activation(out=g_[:], in_=ph[:, 0:TBLK], func=AF.Gelu), reads=[bph], writes=[bg])
                z_, bz = zt.next()
                P.op("pool", lambda e, c=c, z_=z_, g_=g_, w_=w_: e.tensor_tensor(out=z_[:], in0=w_[:, :, c % 16], in1=g_[:], op=ALU.mult),
                     reads=[bw, bg], writes=[bz])
                stage[c] = (z_, bz, vc, bvc)
            if c >= LAG:
                cc = c - LAG
                z_, bz, vc, bvc = stage.pop(cc)
                for tt in range(NTL):
                    for hf in range(2):
                        k = tt * 2 + hf
                        P.op("pe", lambda e, cc=cc, tt=tt, hf=hf, k=k, z_=z_, vc=vc: e.matmul(pO[k][:], lhsT=z_[:, tt * 128:(tt + 1) * 128], rhs=vc[:, hf * 512:(hf + 1) * 512],
                                                                                  start=(cc == 0), stop=(cc == NCH - 1)), reads=[bz, bvc], writes=[bpO[k]])
            if c % 2 == 1:
                nxt = drain(nxt, 1)
        nxt = drain(nxt, 10 ** 9)
        for tt in range(NTL):
            tg = blk * NTL + tt
            xt, bx = xin.next()
            P.dma("sp", xt[:], D["xmid"][tg * 128:(tg + 1) * 128, :], writes=[bx])
            xo_, bxo = xout.next()
            emit_resid_ln(P, C, [pO[tt * 2], pO[tt * 2 + 1]], [bpO[tt * 2], bpO[tt * 2 + 1]], xt, bx, gb, bgb, lng, lnb, bln, epsb, beps, xo_, bxo, "p")
            P.dma("act", xo[tg * 128:(tg + 1) * 128, :], xo_[:], reads=[bxo])
    P.pop()


def k3_host_inputs(inp, l, core, xmid_core):
    b = core // 2
    rep = lambda v: np.ascontiguousarray(np.broadcast_to(np.asarray(v, np.float32)[None, :], (128, len(v))))
    keys = inp["peer_keys"][l].reshape(16, 128, 128)
    return {
        "xmid": np.ascontiguousarray(xmid_core),
        "cT": to_pc(inp["c"][b], 8),
        "adaw_f": np.ascontiguousarray(inp["ada_w"][l][:, 3072:6144]),
        "adab_f": to_pc(inp["ada_b"][l][3072:5120], 16),
        "adab_g": rep(inp["ada_b"][l][5120:6144]),
        "wq": np.ascontiguousarray(inp["peer_wq"][l]),
        "keysT": np.ascontiguousarray(keys.transpose(2, 0, 1)),
        "UT": np.ascontiguousarray(inp["peer_u"][l].T),
        "V": np.ascontiguousarray(inp["peer_v"][l]),
        "lng": rep(inp["ln2_g"][l]), "lnb": rep(inp["ln2_b"][l]),
        "ident": np.eye(128, dtype=np.float32),
        "iota": np.ascontiguousarray(np.broadcast_to(np.arange(128, dtype=np.float32)[None, :], (128, 128))),
    }


import math

PAIRS = [[0, 1], [2, 3], [4, 5], [6, 7]]

GATHER = [
    ("mla_kT", [768, T], 192, [0, 1, 2, 3]),
    ("mla_v", [T, 520], 1024, [0, 1, 2, 3]),
    ("diff_kT", [512, T], 128, [0, 1, 2, 3]),
    ("diff_v", [T, 516], 1024, [0, 1, 2, 3]),
    ("swa_kT", [128, T], 128, [0]),
    ("swa_v", [T, 130], 2048, [1, 0]),
    ("nat_kT", [512, T], 128, [0, 1, 2, 3]),
    ("nat_v", [T, 520], 1024, [0, 3]),
]

LAYER_IN = [("adaw", [1024, 6144]), ("adab1", [128, 16]), ("adabg", [128, 1024]), ("adabf", [128, 16]), ("adabg2", [128, 1024]),
            ("w1", None), ("qupA", [384, 768]), ("qupB", [384, 768]), ("kvn", [256, 512]), ("kvv", [256, 512]),
            ("gq", [128, 3]), ("gkv", [128, 2]), ("sinkb", [128, 8]), ("lamb", [128, 4, 64]), ("subln", [128, 128]), ("lami", [128, 2]),
            ("nat_bias", [8, 128, 29 * 128]), ("wg", [4, 1024, 1024]), ("wb", [4, 512, 1024]), ("wo", [1024, 1024]),
            ("lng1", [128, 1024]), ("lnb1", [128, 1024]), ("wq", [1024, 2048]), ("keysT", [128, 16, 128]),
            ("UT", [1024, 16384]), ("V", [16384, 1024]), ("lng2", [128, 1024]), ("lnb2", [128, 1024])]
COMMON_IN = [("x", [T, 1024]), ("cT", [128, 8]), ("C64", [128, T]), ("S64", [128, T]), ("CM", [128, T]), ("SM", [128, T]),
             ("ident", [128, 128]), ("iota", [128, 128]), ("swa_masks", [4, 128, 512])]


def build_fused(nc, nlayers=2, peer_blocks=NBLK, dbg=False):
    P = Prog(nc)
    units, w1cols = k1_weight_layout()
    NC1 = len(w1cols)
    I = {}
    for n, s in COMMON_IN:
        I[n] = nc.dram_tensor(n, list(s), F32, kind="ExternalInput").ap()
    for l in range(nlayers):
        for n, s in LAYER_IN:
            if n == "w1":
                s = [1024, NC1]
            I[f"{n}_{l}"] = nc.dram_tensor(f"{n}_{l}", list(s), F32, kind="ExternalInput").ap()
    out_d = nc.dram_tensor("out", [T, 1024], F32, kind="ExternalOutput").ap()
    x_cur = I["x"]
    for l in range(nlayers):
        L = lambda n: I[f"{n}_{l}"]
        it = lambda n, s, d=BF16: nc.dram_tensor(f"{n}_L{l}", list(s), d, kind="Internal").ap()
        O2 = {"uT": it("uT", [1024, T]), "swa_qT": it("swa_qT", [512, T]), "swa_kT": it("swa_kT", [128, T]),
              "diff_qT": it("diff_qT", [512, T]), "diff_kT": it("diff_kT", [512, T]), "nat_qT": it("nat_qT", [512, T]),
              "nat_kT": it("nat_kT", [512, T]), "mla_qT": it("mla_qT", [768, T]), "mla_kT": it("mla_kT", [768, T]),
              "swa_v": it("swa_v", [T, 130]), "diff_v": it("diff_v", [T, 516]), "nat_v": it("nat_v", [T, 520]), "mla_v": it("mla_v", [T, 520])}
        O = dict(O2)
        O["mla_qT"] = O2["mla_qT"].rearrange("(h d) t -> h d t", d=96)
        O["mla_kT"] = O2["mla_kT"].rearrange("(h d) t -> h d t", d=96)
        for n in ("swa_v", "diff_v", "nat_v", "mla_v"):
            O[n] = O2[n].rearrange("(t p) f -> t p f", p=128)
        D1 = {"x": x_cur, "cT": I["cT"], "adaw": L("adaw")[:, 0:2048], "adab": L("adab1"), "w1": L("w1"),
              "qupA": L("qupA"), "qupB": L("qupB"), "kvn": L("kvn"), "kvv": L("kvv"), "gq": L("gq"), "gkv": L("gkv"),
              "C64": I["C64"], "S64": I["S64"], "CM": I["CM"], "SM": I["SM"], "ident": I["ident"]}
        P.push()
        emit_k1(P, nc, D1, O)
        P.pop()
        G = {}
        for (name, shp, rc, chunks) in GATHER:
            G[name] = []
            if name == "swa_v":
                dst = it(f"g_{name}", [2 * T, 130])
                P.collective(O2[name], dst, PAIRS)
                G[name].append(dst)
                continue
            for k in chunks:
                dst = it(f"g_{name}{k}", [2 * rc, shp[1]])
                P.collective(O2[name][k * rc:(k + 1) * rc, :], dst, PAIRS)
                G[name].append(dst)
        P.barrier()
        oT_d = it("oT", [4, 512, T])
        D2 = {"swa_qT": O["swa_qT"], "swa_masks": I["swa_masks"], "sinkb": L("sinkb"), "mla_qT": O["mla_qT"],
              "diff_qT": O["diff_qT"], "lamb": L("lamb"), "subln": L("subln"), "lami": L("lami"),
              "nat_qT": O["nat_qT"], "nat_bias": L("nat_bias"), "ident": I["ident"]}
        P.push()
        pre = peer_scratch(nc, f"_L{l}")
        cf = Rot(P, "pa_cf", [128, 4096], F32, 2)
        cb = Rot(P, "pa_cb", [128, 4096], BF16, 2)
        bgen = peer_cast_gen(P, L("UT"), L("V"), pre[0], pre[1], pre[2], pre[3], cf, cb)
        emit_attention(P, D2, oT_d, SRC=GatherSrc(O, G), bg=bgen)
        for _ in bgen:
            pass
        P.pop()
        xmid_d = it("xmid", [T, 1024], F32)
        D3 = {"oT": oT_d, "uT": O["uT"], "x": x_cur, "wg": L("wg"), "wb": L("wb"), "wo": L("wo"), "cT": I["cT"],
              "adaw_g": L("adaw")[:, 2048:3072], "adab_g": L("adabg"), "lng": L("lng1"), "lnb": L("lnb1")}
        emit_merge(P, D3, xmid_d)
        xo = out_d if l == nlayers - 1 else it("xout", [T, 1024], F32)
        D4 = {"xmid": xmid_d, "cT": I["cT"], "adaw_f": L("adaw")[:, 3072:6144], "adab_f": L("adabf"), "adab_g": L("adabg2"),
              "wq": L("wq"), "keysT": L("keysT"), "UT": L("UT"), "V": L("V"), "lng": L("lng2"), "lnb": L("lnb2"),
              "ident": I["ident"], "iota": I["iota"]}
        emit_peer(P, nc, D4, xo, nblk=peer_blocks, tag=f"_L{l}", pre=pre)
        P.barrier()
        if dbg and l == 0:
            d1 = nc.dram_tensor("dbg_oT", [4, 512, T], BF16, kind="ExternalOutput").ap()
            d2 = nc.dram_tensor("dbg_xmid", [T, 1024], F32, kind="ExternalOutput").ap()
            d3 = nc.dram_tensor("dbg_uT", [1024, T], BF16, kind="ExternalOutput").ap()
            for n in range(4):
                P.dma("sp", d1[n], oT_d[n])
            P.dma("act", d2, xmid_d)
            P.dma("act", d3, O["uT"])
            P.barrier()
        x_cur = xo
    P.finish()
    P.emit()
    return P


def fused_host_inputs(inp, core, nlayers=2):
    b, half = core // 2, core % 2
    units, w1cols = k1_weight_layout()
    qa, qb, knope, vcols = mla_up_layout()
    C64, S64, CM, SM = rope_tables(half * T, T)
    rep = lambda v: np.ascontiguousarray(np.broadcast_to(np.asarray(v, np.float32)[None, :], (128, len(v))))
    m = {
        "x": np.ascontiguousarray(inp["x"][b, half * T:(half + 1) * T], dtype=np.float32),
        "cT": to_pc(inp["c"][b], 8), "C64": C64, "S64": S64, "CM": CM, "SM": SM,
        "ident": np.eye(128, dtype=np.float32),
        "iota": np.ascontiguousarray(np.broadcast_to(np.arange(128, dtype=np.float32)[None, :], (128, 128))),
        "swa_masks": swa_masks(half),
    }
    for l in range(nlayers):
        ab = inp["ada_b"][l]
        lam = np.stack([inp["diff_lambda_q1"][l], inp["diff_lambda_k1"][l], inp["diff_lambda_q2"][l], inp["diff_lambda_k2"][l]])
        li = 0.8 - 0.6 * math.exp(-0.3 * l)
        keys = inp["peer_keys"][l].reshape(16, 128, 128)
        d = {
            "adaw": np.ascontiguousarray(inp["ada_w"][l]), "adab1": to_pc(ab[0:2048], 16), "adabg": rep(ab[2048:3072]),
            "adabf": to_pc(ab[3072:5120], 16), "adabg2": rep(ab[5120:6144]),
            "w1": np.ascontiguousarray(inp["w_in"][l][:, w1cols]),
            "qupA": np.ascontiguousarray(inp["mla_q_up"][l][:, qa]), "qupB": np.ascontiguousarray(inp["mla_q_up"][l][:, qb]),
            "kvn": np.ascontiguousarray(inp["mla_kv_up"][l][:, knope]), "kvv": np.ascontiguousarray(inp["mla_kv_up"][l][:, vcols]),
            "gq": to_pc(inp["mla_q_norm"][l], 3), "gkv": to_pc(inp["mla_kv_norm"][l], 2),
            "sinkb": rep(inp["swa_sink"][l]),
            "lamb": np.ascontiguousarray(np.broadcast_to(lam[None], (128, 4, 64))).astype(np.float32),
            "subln": rep(inp["diff_subln"][l]),
            "lami": np.ascontiguousarray(np.broadcast_to(np.array([li, 1.0 - li], np.float32)[None], (128, 2))),
            "nat_bias": nat_bias_tables(inp["nat_rpb"][l], half),
            "wg": np.ascontiguousarray(inp["w_gate"][l]), "wb": np.ascontiguousarray(inp["w_branch"][l]), "wo": np.ascontiguousarray(inp["w_out"][l]),
            "lng1": rep(inp["ln1_g"][l]), "lnb1": rep(inp["ln1_b"][l]),
            "wq": np.ascontiguousarray(inp["peer_wq"][l]), "keysT": np.ascontiguousarray(keys.transpose(2, 0, 1)),
            "UT": np.ascontiguousarray(inp["peer_u"][l].T), "V": np.ascontiguousarray(inp["peer_v"][l]),
            "lng2": rep(inp["ln2_g"][l]), "lnb2": rep(inp["ln2_b"][l]),
        }
        for k, v in d.items():
            m[f"{k}_{l}"] = np.ascontiguousarray(v, dtype=np.float32)
    return m


NCORES = 8
_PROGS = {}


def kernel(**inputs):
    inp = {k: np.asarray(v) for k, v in inputs.items()}
    if "fused" not in _PROGS:
        nc = bass.Bass("TRN2", target_bir_lowering=False)
        build_fused(nc)
        _PROGS["fused"] = nc
    nc = _PROGS["fused"]
    maps = [fused_host_inputs(inp, c) for c in range(NCORES)]
    res = run_bass_kernel_spmd(nc, maps, core_ids=list(range(NCORES)))
    out = np.empty((4, 2 * T, 1024), np.float32)
    for c in range(NCORES):
        b, half = c // 2, c % 2
        out[b, half * T:(half + 1) * T] = np.asarray(res.results[c]["out"])
    return out
```

```python
import numpy as np
import concourse.bass as bass
import concourse.mybir as mybir
from concourse.bass_utils import run_bass_kernel_spmd
from contextlib import ExitStack

F32 = mybir.dt.float32
BF16 = mybir.dt.bfloat16
I32 = mybir.dt.int32
U32 = mybir.dt.uint32
AF = mybir.ActivationFunctionType
ALU = mybir.AluOpType
AX = mybir.AxisListType

ENGS = ["pe", "act", "dve", "pool", "sp"]
DMA_RING = 12


class Buf:
    __slots__ = ("name", "lastw", "readers", "excl", "pre")

    def __init__(self, name="", excl=False):
        self.name = name
        self.excl = excl
        self.pre = []
        self.lastw = []
        self.readers = []


class Prog:
    def __init__(self, nc, same_engine_sync=True):
        self.nc = nc
        self.es = ExitStack()
        self.stack = [self.es]
        self.q = {e: [] for e in ENGS}
        self.cnt = {e: 0 for e in ENGS}
        self.sems = {}
        self.EPOCH = 50000
        self.ring_n = {e: 0 for e in ENGS}
        self.ring_val = {}
        self.seen = {e: {} for e in ENGS}
        self.same_engine_sync = same_engine_sync
        self.n_waits = 0
        self.n_ops = 0
        self.uid = 0
        self.E = {"pe": nc.tensor, "act": nc.scalar, "dve": nc.vector, "pool": nc.gpsimd, "sp": nc.sync}

    def sb(self, name, shape, dtype):
        self.uid += 1
        return self.stack[-1].enter_context(self.nc.sbuf_tensor(f"s{self.uid}_{name}", list(shape), dtype))

    def ps(self, name, shape, dtype=F32):
        self.uid += 1
        return self.stack[-1].enter_context(self.nc.psum_tensor(f"p{self.uid}_{name}", list(shape), dtype))

    def push(self):
        self.stack.append(ExitStack())

    def pop(self):
        self.barrier()
        self.stack.pop().close()

    def barrier(self):
        evs = [self._last_ev(e) for e in ENGS if self.cnt[e] > 0]
        evs += [(k, v) for k, v in self.ring_val.items() if v > 0]
        for e in ENGS:
            for ev in evs:
                self._wait(e, ev)

    def dram(self, name, shape, dtype, kind="Internal"):
        return self.nc.dram_tensor(name, list(shape), dtype, kind=kind).ap()

    def _wait(self, eng, ev):
        key, val = ev
        if key[0] == "eng" and key[1] == eng and (eng == "pe" or not self.same_engine_sync):
            return
        if self.seen[eng].get(key, 0) >= val:
            return
        self.seen[eng][key] = val
        self.E[eng].wait_ge(self.sems[key], val)
        self.n_waits += 1

    def _deps(self, eng, reads, writes):
        for b in reads:
            for ev in b.lastw:
                self._wait(eng, ev)
            if b.excl:
                for ev in b.readers:
                    self._wait(eng, ev)
        for b in writes:
            for ev in b.lastw:
                self._wait(eng, ev)
            for ev in b.readers:
                self._wait(eng, ev)

    def _commit(self, ev, reads, writes):
        for b in writes:
            b.pre = list(b.lastw) + list(b.readers)
            b.lastw = [ev]
            b.readers = []
        for b in reads:
            b.readers = [r for r in b.readers if r[0] != ev[0]] + [ev]

    def _next_ev(self, eng):
        ep, k = divmod(self.cnt[eng], self.EPOCH)
        self.cnt[eng] += 1
        key = ("eng", eng, ep)
        if key not in self.sems:
            self.sems[key] = self.nc.alloc_semaphore(name=f"pg_{eng}_{ep}")
        return (key, k + 1)

    def _last_ev(self, eng):
        if self.cnt[eng] == 0:
            return None
        ep, k = divmod(self.cnt[eng] - 1, self.EPOCH)
        return (("eng", eng, ep), k + 1)

    def collective(self, ins_ap, outs_ap, groups, reads=(), writes=()):
        eng = "pool"
        self._deps(eng, reads, writes)
        k = self.ring_n.get("cc", 0) % 4
        self.ring_n["cc"] = self.ring_n.get("cc", 0) + 1
        key = ("ring", "cc", k)
        if key not in self.sems:
            self.sems[key] = self.nc.alloc_semaphore(name=f"pg_cc_{k}")
            self.ring_val[key] = 0
        prev = self.ring_val[key]
        if prev > 0:
            self._wait(eng, (key, prev))
        val = prev + 1
        self.ring_val[key] = val
        ev = (key, val)
        self.E[eng].collective_compute("AllGather", ALU.bypass, replica_groups=groups, ins=[ins_ap], outs=[outs_ap]).then_inc(self.sems[key])
        self.n_ops += 1
        self._commit(ev, reads, writes)
        return ev

    def op(self, eng, fn, reads=(), writes=()):
        self._deps(eng, reads, writes)
        ev = self._next_ev(eng)
        ins = fn(self.E[eng])
        ins.then_inc(self.sems[ev[0]], 1)
        self.n_ops += 1
        self._commit(ev, reads, writes)
        return ev

    def dma(self, eng, out, in_, reads=(), writes=(), append=False, **kw):
        if append:
            self._deps(eng, reads, ())
            for b in writes:
                for ev in b.pre:
                    self._wait(eng, ev)
                for ev in b.readers:
                    self._wait(eng, ev)
        else:
            self._deps(eng, reads, writes)
        k = self.ring_n[eng] % DMA_RING
        self.ring_n[eng] += 1
        key = ("ring", eng, k)
        if key not in self.sems:
            self.sems[key] = self.nc.alloc_semaphore(name=f"pg_r_{eng}_{k}")
            self.ring_val[key] = 0
        prev = self.ring_val[key]
        if prev > 0:
            self._wait(eng, (key, prev))
        val = prev + 16
        self.ring_val[key] = val
        ev = (key, val)
        self.E[eng].dma_start(out=out, in_=in_, **kw).then_inc(self.sems[key], 16)
        self.n_ops += 1
        if append:
            for b in writes:
                b.lastw = b.lastw + [ev]
            for b in reads:
                b.readers = [r for r in b.readers if r[0] != ev[0]] + [ev]
        else:
            self._commit(ev, reads, writes)
        return ev

    def finish(self):
        for key, val in list(self.ring_val.items()):
            if val > 0:
                self._wait("sp", (key, val))
        for e in ENGS:
            if e != "sp" and self.cnt[e] > 0:
                self._wait("sp", self._last_ev(e))

    def emit(self):
        self.es.close()


T = 4096
NTB = 8
NTT = 32

O_QA, O_KA, O_VA, O_CQ, O_CKV, O_KR, O_QC, O_KC, O_VC, O_QD, O_KD, O_VD = (
    0, 512, 640, 768, 1152, 1408, 1440, 1952, 2464, 2976, 3488, 4000)


class Rot:
    def __init__(self, P, name, shape, dtype, n, space="sb"):
        mk = P.sb if space == "sb" else P.ps
        self.tiles = [mk(f"{name}{i}", shape, dtype) for i in range(n)]
        self.bufs = [Buf(f"{name}{i}", excl=(space == "ps")) for i in range(n)]
        self.i = 0
        self.n = n

    def next(self):
        t, b = self.tiles[self.i % self.n], self.bufs[self.i % self.n]
        self.i += 1
        return t, b


def k1_weight_layout():
    def rot64(cols):
        cols = np.asarray(cols).reshape(-1, 64)
        return np.concatenate([cols[:, 32:], cols[:, :32]], axis=1).reshape(-1)

    units = []
    cols = []

    def add(name, kind, c):
        units.append((name, kind, sum(len(x) for x in cols), len(c)))
        cols.append(np.asarray(c))

    for name, off, n in (("swa_q", O_QA, 512), ("swa_k", O_KA, 128), ("diff_q", O_QC, 512), ("diff_k", O_KC, 512)):
        for ch in range(n // 128):
            a = np.arange(off + ch * 128, off + (ch + 1) * 128)
            add(f"{name}{ch}", "rope", np.concatenate([a, rot64(a)]))
    for name, off in (("nat_q", O_QD), ("nat_k", O_KD)):
        for ch in range(4):
            add(f"{name}{ch}", "plain", np.arange(off + ch * 128, off + (ch + 1) * 128))
    kr = np.arange(O_KR, O_KR + 32)
    krB = np.concatenate([kr[16:], kr[:16]])
    pad = np.arange(O_CQ, O_CQ + 64)
    add("mla", "mla", np.concatenate([np.arange(O_CQ, O_CQ + 384), np.arange(O_CKV, O_CKV + 256),
                                      pad, kr, pad, krB]))
    add("swa_v", "v", np.arange(O_VA, O_VA + 128))
    add("diff_v", "v", np.arange(O_VC, O_VC + 512))
    add("nat_v", "v", np.arange(O_VD, O_VD + 512))
    return units, np.concatenate(cols)


def mla_up_layout():
    qa = np.arange(768)
    qb = qa.copy().reshape(8, 96)
    qb = np.concatenate([qb[:, :64], qb[:, 80:96], qb[:, 64:80]], axis=1).reshape(-1)
    kv = np.arange(1024).reshape(8, 128)
    knope = kv[:, :64].reshape(-1)
    vcols = kv[:, 64:].reshape(-1)
    return qa, qb, knope, vcols


def rope_tables(pos0, n):
    pos = np.arange(pos0, pos0 + n, dtype=np.float32)
    inv64 = (10000.0 ** (-np.arange(0, 64, 2, dtype=np.float32) / 64)).astype(np.float32)
    ang = (pos[None, :] * inv64[:, None]).astype(np.float32)
    c, s = np.cos(ang).astype(np.float32), np.sin(ang).astype(np.float32)
    C64 = np.concatenate([c, c, c, c], 0)
    S64 = np.concatenate([-s, s, -s, s], 0)
    inv32 = (10000.0 ** (-np.arange(0, 32, 2, dtype=np.float32) / 32)).astype(np.float32)
    ang = (pos[None, :] * inv32[:, None]).astype(np.float32)
    c, s = np.cos(ang).astype(np.float32), np.sin(ang).astype(np.float32)
    CM = np.zeros((128, n), np.float32)
    SM = np.zeros((128, n), np.float32)
    CM[64:96] = np.concatenate([c, c], 0)
    SM[64:96] = np.concatenate([-s, s], 0)
    return C64, S64, CM, SM


def emit_mod_cols(P, nc, cT_d, adaw_d, adab_d, ngrp, modc, bmodc, pm_rot, wst_rot):
    cs = P.sb("mod_cs", [128, 8], F32)
    bcs = Buf("cs")
    adab = P.sb("mod_adab", [128, ngrp * 8], F32)
    badab = Buf("adab")
    P.dma("sp", cs[:], cT_d, writes=[bcs])
    P.dma("sp", adab[:], adab_d, writes=[badab])
    P.op("act", lambda e: e.activation(out=cs[:], in_=cs[:], func=AF.Silu), reads=[bcs], writes=[bcs])
    pm, bpm = pm_rot.next()
    noc = ngrp * 8
    for g in range(noc // 2):
        wst_, bw = wst_rot.next()
        wst = wst_[:, 0:2048].rearrange("p (c n) -> p c n", c=8)
        P.dma("sp" if g % 2 == 0 else "act", wst,
              adaw_d[:, g * 256:(g + 1) * 256].rearrange("(c p) n -> p c n", p=128), writes=[bw])
        for o4 in range(2):
            oc = g * 2 + o4
            for kc in range(8):
                P.op("pe", lambda e, oc=oc, kc=kc, o4=o4, wst=wst: e.matmul(
                    pm[:, oc:oc + 1], lhsT=wst[:, kc, o4 * 128:(o4 + 1) * 128], rhs=cs[:, kc:kc + 1],
                    start=(kc == 0), stop=(kc == 7)), reads=[bw, bcs], writes=[bpm])
    P.op("dve", lambda e: e.tensor_tensor(out=modc[:, 0:noc], in0=pm[:, 0:noc], in1=adab[:], op=ALU.add),
         reads=[bpm, badab], writes=[bmodc])


def emit_ln_mod(P, x_d, uT, buT_tiles, shcol, sc1col, bmod, ident, bid, epsb, beps, ptr_rot, tag=""):
    xin = Rot(P, f"ln_x{tag}", [128, 1024], F32, 2)
    xn = Rot(P, f"ln_xn{tag}", [128, 1024], BF16, 2)
    st = Rot(P, f"ln_st{tag}", [128, 2, 6], F32, 2)
    mv = Rot(P, f"ln_mv{tag}", [128, 2], F32, 2)
    rs = Rot(P, f"ln_rs{tag}", [128, 1], F32, 2)
    for t in range(NTT):
        xt, bx = xin.next()
        P.dma("sp" if t % 2 == 0 else "act", xt[:], x_d[t * 128:(t + 1) * 128, :], writes=[bx])
        s_, bs = st.next()
        for c in range(2):
            P.op("dve", lambda e, c=c, s_=s_, xt=xt: e.bn_stats(out=s_[:, c, :], in_=xt[:, c * 512:(c + 1) * 512]),
                 reads=[bx], writes=[bs])
        m_, bm = mv.next()
        P.op("dve", lambda e, m_=m_, s_=s_: e.bn_aggr(out=m_[:], in_=s_[:]), reads=[bs], writes=[bm])
        r_, br = rs.next()
        P.op("act", lambda e, r_=r_, m_=m_: e.activation(out=r_[:], in_=m_[:, 1:2], func=AF.Ln, bias=epsb[:, 0:1], scale=1.0),
             reads=[bm, beps], writes=[br])
        P.op("act", lambda e, r_=r_: e.activation(out=r_[:], in_=r_[:], func=AF.Exp, scale=-0.5), reads=[br], writes=[br])
        xn_, bxn = xn.next()
        P.op("dve", lambda e, xn_=xn_, xt=xt, m_=m_, r_=r_: e.tensor_scalar(
            out=xn_[:], in0=xt[:], scalar1=m_[:, 0:1], scalar2=r_[:, 0:1], op0=ALU.subtract, op1=ALU.mult),
            reads=[bx, bm, br], writes=[bxn])
        pt, bpt = ptr_rot.next()
        for c in range(8):
            P.op("pe", lambda e, c=c, pt=pt, xn_=xn_: e.transpose(out=pt[:, c, :], in_=xn_[:, c * 128:(c + 1) * 128], identity=ident[:]),
                 reads=[bxn, bid], writes=[bpt])
        for c in range(8):
            P.op("act", lambda e, c=c, pt=pt, t=t: e.activation(
                out=uT[:, c, t * 128:(t + 1) * 128], in_=pt[:, c, :], func=AF.Identity,
                scale=sc1col[:, c:c + 1], bias=shcol[:, c:c + 1]), reads=[bpt, bmod], writes=[buT_tiles[t]])


def k1_io():
    units, w1cols = k1_weight_layout()
    NC1 = len(w1cols)
    ins = [("x", [T, 1024]), ("cT", [128, 8]), ("adaw", [1024, 2048]), ("adab", [128, 16]), ("w1", [1024, NC1]),
           ("qupA", [384, 768]), ("qupB", [384, 768]), ("kvn", [256, 512]), ("kvv", [256, 512]), ("gq", [128, 3]), ("gkv", [128, 2]),
           ("C64", [128, T]), ("S64", [128, T]), ("CM", [128, T]), ("SM", [128, T]), ("ident", [128, 128])]
    outs = [("uT", [1024, T]), ("swa_qT", [512, T]), ("swa_kT", [128, T]), ("diff_qT", [512, T]), ("diff_kT", [512, T]),
            ("nat_qT", [512, T]), ("nat_kT", [512, T]), ("mla_qT", [8, 96, T]), ("mla_kT", [8, 96, T]),
            ("swa_v", [NTT, 128, 2 * 65]), ("diff_v", [NTT, 128, 4 * 129]), ("nat_v", [NTT, 128, 8 * 65]), ("mla_v", [NTT, 128, 8 * 65])]
    return ins, outs


def build_k1(nc, stop=None):
    P = Prog(nc)
    ins, outs = k1_io()
    D = {n: nc.dram_tensor(n, list(s), F32, kind="ExternalInput").ap() for n, s in ins}
    O = {n: nc.dram_tensor(n, list(s), BF16, kind="ExternalOutput").ap() for n, s in outs}
    emit_k1(P, nc, D, O, stop)
    P.finish()
    P.emit()
    return P


def emit_k1(P, nc, D, O, stop=None):
    units, w1cols = k1_weight_layout()
    x_d, cT_d, adaw_d, adab_d, w1_d = D["x"], D["cT"], D["adaw"], D["adab"], D["w1"]
    qupA_d, qupB_d, kvn_d, kvv_d, gq_d, gkv_d = D["qupA"], D["qupB"], D["kvn"], D["kvv"], D["gq"], D["gkv"]
    C64_d, S64_d, CM_d, SM_d, ident_d = D["C64"], D["S64"], D["CM"], D["SM"], D["ident"]
    o_uT = O["uT"]
    o = {"swa_q": O["swa_qT"], "swa_k": O["swa_kT"], "diff_q": O["diff_qT"], "diff_k": O["diff_kT"],
         "nat_q": O["nat_qT"], "nat_k": O["nat_kT"], "mla_q": O["mla_qT"], "mla_k": O["mla_kT"],
         "swa_v": O["swa_v"], "diff_v": O["diff_v"], "nat_v": O["nat_v"], "mla_v": O["mla_v"]}

    identf = P.sb("identf", [128, 128], F32); bidf = Buf()
    ident = P.sb("ident", [128, 128], BF16); bid = Buf()
    onesb = P.sb("onesb", [128, 128], BF16); bones = Buf()
    epsb = P.sb("epsb", [128, 1], F32); beps = Buf()
    P.dma("pool", identf[:], ident_d, writes=[bidf])
    P.op("dve", lambda e: e.tensor_copy(out=ident[:], in_=identf[:]), reads=[bidf], writes=[bid])
    P.op("pool", lambda e: e.memset(onesb[:], 1.0), writes=[bones])
    P.op("pool", lambda e: e.memset(epsb[:], 1e-5), writes=[beps])

    uT = P.sb("uT", [128, 8, T], BF16)
    buT = [Buf(f"uT{t}") for t in range(NTT)]
    modc = P.sb("modc", [128, 16], F32); bmodc = Buf()
    sc1 = P.sb("sc1", [128, 8], F32); bsc1 = Buf()

    pA = Rot(P, "pA", [128, 512], F32, 2, "ps")
    pB = Rot(P, "pB", [128, 512], F32, 2, "ps")
    ptr = Rot(P, "ptr", [128, 8, 128], BF16, 2, "ps")
    pmisc = Rot(P, "pmisc", [128, 512], F32, 1, "ps")

    wst = Rot(P, "wst", [128, 2048], F32, 2)
    wbf = Rot(P, "wbf", [128, 8, 256], BF16, 2)

    emit_mod_cols(P, nc, cT_d, adaw_d, adab_d, 2, modc, bmodc, pmisc, wst)
    P.op("dve", lambda e: e.tensor_scalar(out=sc1[:], in0=modc[:, 8:16], scalar1=1.0, scalar2=None, op0=ALU.add),
         reads=[bmodc], writes=[bsc1])
    bmod = Buf("mod")
    P.op("dve", lambda e: e.tensor_copy(out=modc[:, 0:8], in_=modc[:, 0:8]), reads=[bmodc, bsc1], writes=[bmod])

    if stop == "mod":
        return
    emit_ln_mod(P, x_d, uT, buT, modc, sc1, bmod, ident, bid, epsb, beps, ptr)
    if stop == "ln":
        return

    for c in range(8):
        P.dma("pool", o_uT[c * 128:(c + 1) * 128, :], uT[:, c, :], reads=buT)

    c64r = Rot(P, "c64r", [128, 512], F32, 2)
    s64r = Rot(P, "s64r", [128, 512], F32, 2)

    t1r = Rot(P, "t1r", [128, 512], F32, 2)
    t2r = Rot(P, "t2r", [128, 512], F32, 2)
    ostg = Rot(P, "ostg", [128, 512], BF16, 3)

    def load_w(col0, ncols):
        ws_, bws = wst.next()
        wb, bwb = wbf.next()
        ws = ws_[:, 0:8 * ncols].rearrange("p (c n) -> p c n", c=8)
        h = ncols // 2
        P.dma("sp", ws[:, :, 0:h], w1_d[:, col0:col0 + h].rearrange("(c p) n -> p c n", p=128), writes=[bws])
        P.dma("act", ws[:, :, h:ncols], w1_d[:, col0 + h:col0 + ncols].rearrange("(c p) n -> p c n", p=128),
              writes=[bws], append=True)
        P.op("pool", lambda e: e.tensor_copy(out=wb[:, :, 0:ncols], in_=ws), reads=[bws], writes=[bwb])
        return wb, bwb

    def mm_fm(ps, bps, wb, bwb, c0, M, tb):
        for kc in range(8):
            P.op("pe", lambda e, kc=kc: e.matmul(ps[0:M, :], lhsT=wb[:, kc, c0:c0 + M], rhs=uT[:, kc, tb * 512:(tb + 1) * 512],
                                               start=(kc == 0), stop=(kc == 7)),
                 reads=[bwb] + buT[tb * 4:(tb + 1) * 4], writes=[bps])

    dq = ["sp", "act", "pool"]
    dqi = [0]

    def nextq():
        dqi[0] += 1
        return dq[dqi[0] % 3]

    for (name, kind, col0, ncols) in units:
        if stop is not None and stop == name:
            break
        if kind == "rope":
            base = name[:-1]; ch = int(name[-1])
            wb, bwb = load_w(col0, 256)
            for tb in range(NTB):
                a, ba = pA.next(); b, bb = pB.next()
                mm_fm(a, ba, wb, bwb, 0, 128, tb)
                mm_fm(b, bb, wb, bwb, 128, 128, tb)
                t1, bt1 = t1r.next(); t2, bt2 = t2r.next()
                sl = slice(tb * 512, (tb + 1) * 512)
                C64, bc64 = c64r.next(); S64, bs64 = s64r.next()
                P.dma("sp", C64[:], C64_d[:, sl], writes=[bc64])
                P.dma("act", S64[:], S64_d[:, sl], writes=[bs64])
                P.op("dve", lambda e, t1=t1, a=a, C64=C64: e.tensor_tensor(out=t1[:], in0=a[:], in1=C64[:], op=ALU.mult),
                     reads=[ba, bc64], writes=[bt1])
                P.op("dve", lambda e, t2=t2, b=b, S64=S64: e.tensor_tensor(out=t2[:], in0=b[:], in1=S64[:], op=ALU.mult),
                     reads=[bb, bs64], writes=[bt2])
                og, bog = ostg.next()
                P.op("pool", lambda e, og=og, t1=t1, t2=t2: e.tensor_tensor(out=og[:], in0=t1[:], in1=t2[:], op=ALU.add),
                     reads=[bt1, bt2], writes=[bog])
                P.dma(nextq(), o[base][ch * 128:(ch + 1) * 128, sl], og[:], reads=[bog])
        elif kind == "plain":
            base = name[:-1]; ch = int(name[-1])
            wb, bwb = load_w(col0, 128)
            scale = 0.125 if base == "nat_q" else 1.0
            for tb in range(NTB):
                a, ba = pA.next()
                mm_fm(a, ba, wb, bwb, 0, 128, tb)
                og, bog = ostg.next()
                sl = slice(tb * 512, (tb + 1) * 512)
                P.op("act", lambda e, og=og, a=a, scale=scale: e.activation(out=og[:], in_=a[:], func=AF.Copy, scale=scale),
                     reads=[ba], writes=[bog])
                P.dma(nextq(), o[base][ch * 128:(ch + 1) * 128, sl], og[:], reads=[bog])
        elif kind == "v":
            H, dv = {"swa_v": (2, 64), "diff_v": (4, 128), "nat_v": (8, 64)}[name]
            vst = Rot(P, f"vst_{name}", [128, H, dv + 1], BF16, 2)
            for vt, vb in zip(vst.tiles, vst.bufs):
                P.op("pool", lambda e, vt=vt: e.memset(vt[:], 1.0), writes=[vb])
            nsub = max(1, ncols // 256)
            sc = ncols // nsub
            hs = H // nsub
            wbs = [load_w(col0 + i * sc, sc) for i in range(nsub)] if nsub <= 2 else None
            assert wbs is not None
            for t in range(NTT):
                vt, vb = vst.next()
                for i in range(nsub):
                    wb, bwb = wbs[i]
                    a, ba = pA.next()
                    for kc in range(8):
                        P.op("pe", lambda e, kc=kc, a=a, t=t, wb=wb: e.matmul(a[:, 0:sc], lhsT=uT[:, kc, t * 128:(t + 1) * 128],
                                                                     rhs=wb[:, kc, 0:sc], start=(kc == 0), stop=(kc == 7)),
                             reads=[bwb, buT[t]], writes=[ba])
                    P.op("act", lambda e, vt=vt, a=a, i=i: e.activation(
                        out=vt[:, i * hs:(i + 1) * hs, 0:dv], in_=a[:, 0:sc].rearrange("p (h d) -> p h d", h=hs), func=AF.Copy),
                        reads=[ba], writes=[vb])
                P.dma(nextq(), o[name][t], vt[:].rearrange("p h d -> p (h d)"), reads=[vb])
        elif kind == "mla":
            emit_mla(P, locals())


def emit_mla(P, L):
    nc = P.nc
    (w1_d, uT, buT, pA, pB, pmisc, o, onesb, bones, epsb, beps, qupA_d, qupB_d, kvn_d, kvv_d, gq_d, gkv_d,
     CM_d, SM_d, col0, nextq, ostg) = (L[k] for k in (
         "w1_d", "uT", "buT", "pA", "pB", "pmisc", "o", "onesb", "bones", "epsb", "beps", "qupA_d", "qupB_d",
         "kvn_d", "kvv_d", "gq_d", "gkv_d", "CM_d", "SM_d", "col0", "nextq", "ostg"))
    L = dict(L)
    NCM = 384 + 256 + 96 + 96
    wst = L["wst"]
    wm = P.sb("mla_w", [128, 8, NCM], BF16); bwm = Buf()
    first = True
    for i in range(4):
        ws_, bws = wst.next()
        ws = ws_[:, 0:8 * 208].rearrange("p (c n) -> p c n", c=8)
        P.dma("sp" if i % 2 == 0 else "act", ws, w1_d[:, col0 + i * 208:col0 + (i + 1) * 208].rearrange("(c p) n -> p c n", p=128), writes=[bws])
        P.op("pool", lambda e, ws=ws, i=i: e.tensor_copy(out=wm[:, :, i * 208:(i + 1) * 208], in_=ws), reads=[bws], writes=[bwm])
    qA = P.sb("mla_qA", [128, 3, 768], BF16); bqA = Buf()
    qB = P.sb("mla_qB", [128, 3, 768], BF16); bqB = Buf()
    kvn = P.sb("mla_kvn", [128, 2, 512], BF16); bkvn = Buf()
    kvv = P.sb("mla_kvv", [128, 2, 512], BF16); bkvv = Buf()
    for (src, dst, bdst, nch, ncol) in ((qupA_d, qA, bqA, 3, 768), (qupB_d, qB, bqB, 3, 768)):
        for hh in range(2):
            ws_, bws = wst.next()
            ws = ws_[:, 0:nch * 384].rearrange("p (c n) -> p c n", c=nch)
            P.dma("sp", ws, src[:, hh * 384:(hh + 1) * 384].rearrange("(c p) n -> p c n", p=128), writes=[bws])
            P.op("pool", lambda e, ws=ws, dst=dst, hh=hh: e.tensor_copy(out=dst[:, :, hh * 384:(hh + 1) * 384], in_=ws), reads=[bws], writes=[bdst])
    for (src, dst, bdst) in ((kvn_d, kvn, bkvn), (kvv_d, kvv, bkvv)):
        ws_, bws = wst.next()
        ws = ws_[:, 0:1024].rearrange("p (c n) -> p c n", c=2)
        P.dma("act", ws, src.rearrange("(c p) n -> p c n", p=128), writes=[bws])
        P.op("pool", lambda e, ws=ws, dst=dst: e.tensor_copy(out=dst[:], in_=ws), reads=[bws], writes=[bdst])
    gq = P.sb("mla_gq", [128, 3], F32); gkv = P.sb("mla_gkv", [128, 2], F32); bg = Buf()
    P.dma("pool", gq[:], gq_d, writes=[bg])
    P.dma("pool", gkv[:], gkv_d, writes=[bg], append=True)
    epsq = P.sb("mla_epsq", [128, 1], F32)
    cg = Rot(P, "mla_cg", [128, 5, 512], BF16, 1)
    sq = Rot(P, "mla_sq", [128, 5, 512], BF16, 1)
    rq = Rot(P, "mla_rq", [128, 512], F32, 1)
    rkv = Rot(P, "mla_rkv", [128, 512], F32, 1)
    rkvt = Rot(P, "mla_rkvt", [128, 4], F32, 2)
    cmt = Rot(P, "mla_cm", [128, 512], F32, 1); smt = Rot(P, "mla_sm", [128, 512], F32, 1)
    cr = Rot(P, "mla_cr", [128, 512], F32, 1); sr = Rot(P, "mla_sr", [128, 512], F32, 1)
    tA = Rot(P, "mla_tA", [128, 512], F32, 1); tB = Rot(P, "mla_tB", [128, 512], F32, 1)
    krs = Rot(P, "mla_krs", [128, 512], BF16, 2)
    vst = Rot(P, "mla_vst", [128, 8, 65], BF16, 2)
    for vt, vb in zip(vst.tiles, vst.bufs):
        P.op("pool", lambda e, vt=vt: e.memset(vt[:], 1.0), writes=[vb])

    import os
    MS = int(os.environ.get("MLASTOP", "99"))
    for tb in range(NTB):
        sl = slice(tb * 512, (tb + 1) * 512)
        ubufs = buT[tb * 4:(tb + 1) * 4]
        cg_, bcg = cg.next(); sq_, bsq = sq.next()
        if MS <= 0: continue
        for j in range(5):
            a, ba = pA.next()
            for kc in range(8):
                P.op("pe", lambda e, kc=kc, a=a, j=j: e.matmul(a[:], lhsT=wm[:, kc, j * 128:(j + 1) * 128], rhs=uT[:, kc, sl],
                                                            start=(kc == 0), stop=(kc == 7)), reads=[bwm] + ubufs, writes=[ba])
            gcol = gq[:, j:j + 1] if j < 3 else gkv[:, j - 3:j - 2]
            VAR = os.environ.get("MLAVAR", "AB")
            if "A" in VAR:
                P.op("act", lambda e, a=a, j=j, sq_=sq_: e.activation(out=sq_[:, j, :], in_=a[:], func=AF.Square), reads=[ba], writes=[bsq])
            if "B" in VAR:
              P.op("dve", lambda e, a=a, j=j, cg_=cg_, gcol=gcol: e.tensor_scalar(out=cg_[:, j, :], in0=a[:], scalar1=gcol, scalar2=None, op0=ALU.mult),
                 reads=[ba, bg], writes=[bcg])
        if MS <= 1: continue
        rq_, brq = rq.next(); rkv_, brkv = rkv.next(); rkt, brkt = rkvt.next()
        for (r_, br_, js, dim) in ((rq_, brq, (0, 1, 2), 384.0), (rkv_, brkv, (3, 4), 256.0)):
            pm, bpm = pmisc.next()
            for i, j in enumerate(js):
                P.op("pe", lambda e, pm=pm, j=j, i=i, n=len(js): e.matmul(pm[:], lhsT=onesb[:], rhs=sq_[:, j, :], start=(i == 0), stop=(i == n - 1)),
                     reads=[bones, bsq], writes=[bpm])
            P.op("act", lambda e, r_=r_, pm=pm, dim=dim: e.activation(out=r_[:], in_=pm[:], func=AF.Ln, bias=epsb[:, 0:1], scale=1.0 / dim),
                 reads=[bpm, beps], writes=[br_])
            P.op("act", lambda e, r_=r_: e.activation(out=r_[:], in_=r_[:], func=AF.Exp, scale=-0.5), reads=[br_], writes=[br_])
        if MS <= 2: continue
        pm, bpm = pmisc.next()
        for tt in range(4):
            for i, j in enumerate((3, 4)):
                P.op("pe", lambda e, pm=pm, tt=tt, j=j, i=i: e.matmul(pm[:, tt:tt + 1], lhsT=sq_[:, j, tt * 128:(tt + 1) * 128], rhs=onesb[:, 0:1],
                                                                   start=(i == 0), stop=(i == 1)), reads=[bones, bsq], writes=[bpm])
        P.op("act", lambda e, rkt=rkt, pm=pm: e.activation(out=rkt[:], in_=pm[:, 0:4], func=AF.Ln, bias=epsb[:, 0:1], scale=1.0 / 256.0),
             reads=[bpm, beps], writes=[brkt])
        P.op("act", lambda e, rkt=rkt: e.activation(out=rkt[:], in_=rkt[:], func=AF.Exp, scale=-0.5), reads=[brkt], writes=[brkt])
        if MS <= 3: continue
        cm_, bcm = cmt.next(); sm_, bsm = smt.next()
        P.dma("sp", cm_[:], CM_d[:, sl], writes=[bcm])
        P.dma("act", sm_[:], SM_d[:, sl], writes=[bsm])
        cr_, bcr = cr.next(); sr_, bsr = sr.next()
        P.op("pool", lambda e, cr_=cr_, cm_=cm_, rq_=rq_: e.tensor_tensor(out=cr_[64:96, :], in0=cm_[64:96, :], in1=rq_[64:96, :], op=ALU.mult),
             reads=[bcm, brq], writes=[bcr])
        P.op("pool", lambda e, sr_=sr_, sm_=sm_, rq_=rq_: e.tensor_tensor(out=sr_[64:96, :], in0=sm_[64:96, :], in1=rq_[64:96, :], op=ALU.mult),
             reads=[bsm, brq], writes=[bsr])
        if MS <= 4: continue
        for h in range(8):
            a, ba = pA.next(); b, bb = pB.next()
            for j in range(3):
                P.op("pe", lambda e, a=a, j=j, h=h: e.matmul(a[0:96, :], lhsT=qA[:, j, h * 96:(h + 1) * 96], rhs=cg_[:, j, :], start=(j == 0), stop=(j == 2)),
                     reads=[bqA, bcg], writes=[ba])
            for j in range(3):
                P.op("pe", lambda e, b=b, j=j, h=h: e.matmul(b[0:96, :], lhsT=qB[:, j, h * 96:(h + 1) * 96], rhs=cg_[:, j, :], start=(j == 0), stop=(j == 2)),
                     reads=[bqB, bcg], writes=[bb])
            og, bog = ostg.next()
            tA_, btA = tA.next(); tB_, btB = tB.next()
            P.op("dve", lambda e, og=og, a=a, rq_=rq_: e.tensor_tensor(out=og[0:64, :], in0=a[0:64, :], in1=rq_[0:64, :], op=ALU.mult),
                 reads=[ba, brq], writes=[bog])
            P.op("dve", lambda e, tA_=tA_, a=a, cr_=cr_: e.tensor_tensor(out=tA_[64:96, :], in0=a[64:96, :], in1=cr_[64:96, :], op=ALU.mult),
                 reads=[ba, bcr], writes=[btA])
            P.op("dve", lambda e, tB_=tB_, b=b, sr_=sr_: e.tensor_tensor(out=tB_[64:96, :], in0=b[64:96, :], in1=sr_[64:96, :], op=ALU.mult),
                 reads=[bb, bsr], writes=[btB])
            P.op("pool", lambda e, og=og, tA_=tA_, tB_=tB_: e.tensor_tensor(out=og[64:96, :], in0=tA_[64:96, :], in1=tB_[64:96, :], op=ALU.add),
                 reads=[btA, btB, bog], writes=[bog])
            P.dma(nextq(), o["mla_q"][h, :, sl], og[0:96, :], reads=[bog])
        if MS <= 5: continue
        a, ba = pA.next(); b, bb = pB.next()
        for kc in range(8):
            P.op("pe", lambda e, kc=kc, a=a: e.matmul(a[0:96, :], lhsT=wm[:, kc, 640:736], rhs=uT[:, kc, sl], start=(kc == 0), stop=(kc == 7)),
                 reads=[bwm] + ubufs, writes=[ba])
        for kc in range(8):
            P.op("pe", lambda e, kc=kc, b=b: e.matmul(b[0:96, :], lhsT=wm[:, kc, 736:832], rhs=uT[:, kc, sl], start=(kc == 0), stop=(kc == 7)),
                 reads=[bwm] + ubufs, writes=[bb])
        tA_, btA = tA.next(); tB_, btB = tB.next()
        P.op("dve", lambda e, tA_=tA_, a=a, cm_=cm_: e.tensor_tensor(out=tA_[64:96, :], in0=a[64:96, :], in1=cm_[64:96, :], op=ALU.mult),
             reads=[ba, bcm], writes=[btA])
        P.op("dve", lambda e, tB_=tB_, b=b, sm_=sm_: e.tensor_tensor(out=tB_[64:96, :], in0=b[64:96, :], in1=sm_[64:96, :], op=ALU.mult),
             reads=[bb, bsm], writes=[btB])
        kr_, bkr = krs.next()
        P.op("pool", lambda e, kr_=kr_, tA_=tA_, tB_=tB_: e.tensor_tensor(out=kr_[64:96, :], in0=tA_[64:96, :], in1=tB_[64:96, :], op=ALU.add),
             reads=[btA, btB], writes=[bkr])
        for h in range(8):
            P.dma(nextq(), o["mla_k"][h, 64:96, sl], kr_[64:96, :], reads=[bkr])
        if MS <= 6: continue
        for h in range(8):
            a, ba = pA.next()
            for j in range(2):
                P.op("pe", lambda e, a=a, j=j, h=h: e.matmul(a[0:64, :], lhsT=kvn[:, j, h * 64:(h + 1) * 64], rhs=cg_[:, 3 + j, :], start=(j == 0), stop=(j == 1)),
                     reads=[bkvn, bcg], writes=[ba])
            og, bog = ostg.next()
            P.op("dve", lambda e, og=og, a=a, rkv_=rkv_: e.tensor_tensor(out=og[0:64, :], in0=a[0:64, :], in1=rkv_[0:64, :], op=ALU.mult),
                 reads=[ba, brkv], writes=[bog])
            P.dma(nextq(), o["mla_k"][h, 0:64, sl], og[0:64, :], reads=[bog])
        if MS <= 7: continue
        for tt in range(4):
            a, ba = pA.next()
            for j in range(2):
                P.op("pe", lambda e, a=a, j=j, tt=tt: e.matmul(a[:], lhsT=cg_[:, 3 + j, tt * 128:(tt + 1) * 128], rhs=kvv[:, j, :], start=(j == 0), stop=(j == 1)),
                     reads=[bkvv, bcg], writes=[ba])
            vt, vb = vst.next()
            P.op("act", lambda e, vt=vt, a=a, rkt=rkt, tt=tt: e.activation(
                out=vt[:, :, 0:64], in_=a[:].rearrange("p (h d) -> p h d", h=8), func=AF.Copy, scale=rkt[:, tt:tt + 1]),
                reads=[ba, brkt], writes=[vb])
            P.dma(nextq(), o["mla_v"][tb * 4 + tt], vt[:].rearrange("p h d -> p (h d)"), reads=[vb])


def to_pc(v, ncol):
    return np.ascontiguousarray(np.asarray(v).reshape(ncol, 128).T)


def k1_host_inputs(inp, l, core, xcur):
    b, half = core // 2, core % 2
    units, w1cols = k1_weight_layout()
    qa, qb, knope, vcols = mla_up_layout()
    C64, S64, CM, SM = rope_tables(half * T, T)
    m = {
        "x": np.ascontiguousarray(xcur[b, half * T:(half + 1) * T]),
        "cT": to_pc(inp["c"][b], 8),
        "adaw": np.ascontiguousarray(inp["ada_w"][l][:, 0:2048]),
        "adab": to_pc(inp["ada_b"][l][0:2048], 16),
        "w1": np.ascontiguousarray(inp["w_in"][l][:, w1cols]),
        "qupA": np.ascontiguousarray(inp["mla_q_up"][l][:, qa]),
        "qupB": np.ascontiguousarray(inp["mla_q_up"][l][:, qb]),
        "kvn": np.ascontiguousarray(inp["mla_kv_up"][l][:, knope]),
        "kvv": np.ascontiguousarray(inp["mla_kv_up"][l][:, vcols]),
        "gq": to_pc(inp["mla_q_norm"][l], 3),
        "gkv": to_pc(inp["mla_kv_norm"][l], 2),
        "C64": C64, "S64": S64, "CM": CM, "SM": SM,
        "ident": np.eye(128, dtype=np.float32),
    }
    return m


NEGM = -30000.0


class AttnCtx:
    pass


def attn_block(P, C, rhs_q, bq, N, ktiles, scale, acc, bacc, nsub, v_of, pt_cols=None):
    n = len(ktiles)
    LA = 2
    pend = {}
    for i in range(n + LA):
        if i < n:
            kT_ap, kb, vkey, vb, bias_ap, bb = ktiles[i]
            st, bst = C.ST.next()
            P.op("pe", lambda e, st=st, kT_ap=kT_ap, bias_ap=bias_ap: e.matmul(
                st[:, 0:N], lhsT=kT_ap, rhs=rhs_q, start=True, stop=(bias_ap is None)),
                reads=list(kb) + list(bq), writes=[bst])
            if bias_ap is not None:
                P.op("pe", lambda e, st=st, bias_ap=bias_ap: e.matmul(
                    st[:, 0:N], lhsT=C.ident[:], rhs=bias_ap, start=False, stop=True),
                    reads=list(bb) + [C.bid], writes=[bst])
            pt, bpt = C.PT.next()
            P.op("act", lambda e, st=st, pt=pt: e.activation(out=pt[:, 0:N], in_=st[:, 0:N], func=AF.Exp, scale=scale),
                 reads=[bst], writes=[bpt])
            pend[i] = (pt, bpt, vkey, vb)
        if i >= LA:
            k = i - LA
            pt, bpt, vkey, vb = pend.pop(k)
            for j in range(nsub):
                P.op("pe", lambda e, pt=pt, j=j, vkey=vkey, k=k: e.matmul(
                    acc(j), lhsT=pt[:, j * 128:(j + 1) * 128], rhs=v_of(vkey, j), start=(k == 0), stop=(k == n - 1)),
                    reads=[bpt] + list(vb), writes=[bacc])


def emit_oT(P, C, o_tok, bo, oT_d):
    for tb in range(NTB):
        stg, bs = C.oTs.next()
        for tt in range(4):
            t = tb * 4 + tt
            ptr, bp = C.ptr.next()
            for c in range(4):
                P.op("pe", lambda e, c=c, t=t, ptr=ptr: e.transpose(out=ptr[:, c, :], in_=o_tok[:, t, c * 128:(c + 1) * 128], identity=C.ident[:]),
                     reads=[bo, C.bid], writes=[bp])
            P.op("dve", lambda e, ptr=ptr, stg=stg, tt=tt: e.tensor_copy(out=stg[:, :, tt * 128:(tt + 1) * 128], in_=ptr[:]),
                 reads=[bp], writes=[bs])
        P.dma("sp" if tb % 2 == 0 else "act", oT_d.rearrange("(c p) t -> p c t", p=128)[:, :, tb * 512:(tb + 1) * 512], stg[:], reads=[bs])


def build_k2a(nc, which=("swa", "mla", "diff", "nat")):
    P = Prog(nc)
    din = lambda n, s, d=BF16: nc.dram_tensor(n, list(s), d, kind="ExternalInput").ap()
    dout = lambda n, s, d=BF16: nc.dram_tensor(n, list(s), d, kind="ExternalOutput").ap()
    D = {}
    D["swa_qT"] = din("swa_qT", [512, T]); D["swa_kT"] = din("swa_kT", [128, 34 * 128]); D["swa_v"] = din("swa_v", [34, 128, 130])
    D["swa_masks"] = din("swa_masks", [4, 128, 512], F32); D["sinkb"] = din("sinkb", [128, 8], F32)
    D["mla_qT"] = din("mla_qT", [8, 96, T]); D["mla_kT"] = din("mla_kT", [8, 96, 2 * T]); D["mla_v"] = din("mla_v", [64, 128, 520])
    D["diff_qT"] = din("diff_qT", [512, T]); D["diff_kT"] = din("diff_kT", [512, 2 * T]); D["diff_v"] = din("diff_v", [64, 128, 516])
    D["lamb"] = din("lamb", [128, 4, 64], F32); D["subln"] = din("subln", [128, 128], F32); D["lami"] = din("lami", [128, 2], F32)
    D["nat_qT"] = din("nat_qT", [512, T]); D["nat_kT"] = din("nat_kT", [512, 36 * 128]); D["nat_v"] = din("nat_v", [36, 128, 520])
    D["nat_bias"] = din("nat_bias", [8, 128, 29 * 128], F32)
    D["ident"] = din("ident", [128, 128], F32)
    oT_d = dout("oT", [4, 512, T])
    emit_attention(P, D, oT_d, which)
    P.finish()
    P.emit()
    return P


class HostSrc:
    def __init__(self, D):
        self.D = D

    def swa_k(self, kT, kv):
        return [(kT[:, kv, :], self.D["swa_kT"][kv * 64:(kv + 1) * 64, :])]

    def swa_v(self, V):
        return [(V[:], self.D["swa_v"].rearrange("t p f -> p t f"))]

    def mla_k(self, kT, h):
        return [(kT[:, 0:T], self.D["mla_kT"][h, :, 0:T]), (kT[:, T:2 * T], self.D["mla_kT"][h, :, T:2 * T])]

    def mla_v(self, V, h):
        return [(V[:], self.D["mla_v"][:, :, h * 65:(h + 1) * 65].rearrange("t p f -> p t f"))]

    def diff_k(self, kT, row):
        return [(kT[:, 0:T], self.D["diff_kT"][row:row + 64, 0:T]), (kT[:, T:2 * T], self.D["diff_kT"][row:row + 64, T:2 * T])]

    def diff_v(self, V, h):
        return [(V[:], self.D["diff_v"][:, :, h * 129:(h + 1) * 129].rearrange("t p f -> p t f"))]

    def nat_k(self, kT, h):
        return [(kT[:], self.D["nat_kT"][h * 64:(h + 1) * 64, :])]

    def nat_v(self, V, h):
        return [(V[:], self.D["nat_v"][:, :, h * 65:(h + 1) * 65].rearrange("t p f -> p t f"))]


class GatherSrc:
    def __init__(self, O, G):
        self.O = O; self.G = G

    def swa_k(self, kT, kv):
        g = self.G["swa_kT"][0]
        r = slice(kv * 64, (kv + 1) * 64); r1 = slice(128 + kv * 64, 128 + (kv + 1) * 64)
        return [(kT[:, kv, 0:128], g[r, T - 128:T]), (kT[:, kv, 128:128 + T], self.O["swa_kT"][r, :]),
                (kT[:, kv, 128 + T:256 + T], g[r1, 0:128])]

    def swa_v(self, V):
        g = self.G["swa_v"][0]
        return [(V[:, 0, :], g[T - 128:T, :]), (V[:, 1:33, :], self.O["swa_v"].rearrange("t p f -> p t f")),
                (V[:, 33, :], g[T:T + 128, :])]

    def mla_k(self, kT, h):
        g = self.G["mla_kT"][h // 2]
        r0 = (h % 2) * 96
        return [(kT[:, 0:T], g[r0:r0 + 96, :]), (kT[:, T:2 * T], g[192 + r0:192 + r0 + 96, :])]

    def mla_v(self, V, h):
        out = []
        for k in range(4):
            g = self.G["mla_v"][k]
            for r in range(2):
                out.append((V[:, r * 32 + k * 8:r * 32 + k * 8 + 8, :],
                            g[r * 1024:(r + 1) * 1024, h * 65:(h + 1) * 65].rearrange("(t p) f -> p t f", p=128)))
        return out

    def diff_k(self, kT, row):
        g = self.G["diff_kT"][row // 128]
        r0 = row % 128
        return [(kT[:, 0:T], g[r0:r0 + 64, :]), (kT[:, T:2 * T], g[128 + r0:128 + r0 + 64, :])]

    def diff_v(self, V, h):
        out = []
        for k in range(4):
            g = self.G["diff_v"][k]
            for r in range(2):
                out.append((V[:, r * 32 + k * 8:r * 32 + k * 8 + 8, :],
                            g[r * 1024:(r + 1) * 1024, h * 129:(h + 1) * 129].rearrange("(t p) f -> p t f", p=128)))
        return out

    def nat_k(self, kT, h):
        g = self.G["nat_kT"][h // 2]
        r0 = (h % 2) * 64
        return [(kT[:, 0:256], g[r0:r0 + 64, T - 256:T]), (kT[:, 256:256 + T], self.O["nat_kT"][h * 64:(h + 1) * 64, :]),
                (kT[:, 256 + T:512 + T], g[128 + r0:128 + r0 + 64, 0:256])]

    def nat_v(self, V, h):
        gl = self.G["nat_v"][1]
        gf = self.G["nat_v"][0]
        c = slice(h * 65, (h + 1) * 65)
        return [(V[:, 0:2, :], gl[768:1024, c].rearrange("(t p) f -> p t f", p=128)),
                (V[:, 2:34, :], self.O["nat_v"][:, :, c].rearrange("t p f -> p t f")),
                (V[:, 34:36, :], gf[1024:1280, c].rearrange("(t p) f -> p t f", p=128))]


def emit_attention(P, D, oT_d, which=("swa", "mla", "diff", "nat"), SRC=None, bg=None):
    if SRC is None:
        SRC = HostSrc(D)

    def bg_step():
        if bg is not None:
            next(bg, None)
    C = AttnCtx()
    identf = P.sb("a_identf", [128, 128], F32); bidf = Buf()
    C.ident = P.sb("a_ident", [128, 128], BF16); C.bid = Buf()
    P.dma("pool", identf[:], D["ident"], writes=[bidf])
    P.op("dve", lambda e: e.tensor_copy(out=C.ident[:], in_=identf[:]), reads=[bidf], writes=[C.bid])
    epsb = P.sb("a_eps", [128, 1], F32); beps = Buf()
    P.op("pool", lambda e: e.memset(epsb[:], 1e-5), writes=[beps])
    C.ST = Rot(P, "a_ST", [128, 512], F32, 3, "ps")
    C.PT = Rot(P, "a_PT", [128, 512], BF16, 3)
    C.acc = Rot(P, "a_acc", [128, 4, 512], F32, 1, "ps")
    C.ptr = Rot(P, "a_ptr", [128, 4, 128], BF16, 1, "ps")
    C.oTs = Rot(P, "a_oTs", [128, 4, 512], BF16, 2)
    o_tok = P.sb("a_otok", [128, NTT, 512], BF16); bo = Buf("otok")
    rz = Rot(P, "a_rz", [128, 4, 1], F32, 4)

    if "swa" in which:
        P.push()
        qT = P.sb("swa_q", [64, 8, T], BF16); bq = Buf()
        for h in range(8):
            P.dma(("sp", "act", "pool")[h % 3], qT[:, h, :], D["swa_qT"][h * 64:(h + 1) * 64, :], writes=[bq], append=(h > 0))
        kT = P.sb("swa_k", [64, 2, 34 * 128], BF16); bk = Buf()
        n_ = 0
        for kv in range(2):
            for (d_, s_) in SRC.swa_k(kT, kv):
                P.dma("sp", d_, s_, writes=[bk], append=(n_ > 0)); n_ += 1
        V = P.sb("swa_vv", [128, 34, 130], BF16); bv = Buf()
        for n_, (d_, s_) in enumerate(SRC.swa_v(V)):
            P.dma("sp", d_, s_, writes=[bv], append=(n_ > 0))
        mf = P.sb("swa_mf", [128, 4, 512], F32); bmf = Buf()
        mk = P.sb("swa_mk", [128, 4, 512], BF16); bmk = Buf()
        P.dma("pool", mf[:], D["swa_masks"].rearrange("m p n -> p m n"), writes=[bmf])
        P.op("dve", lambda e: e.tensor_copy(out=mk[:], in_=mf[:]), reads=[bmf], writes=[bmk])
        es = P.sb("swa_es", [128, 8], F32); bes = Buf()
        P.dma("pool", es[:], D["sinkb"], writes=[bes])
        P.op("act", lambda e: e.activation(out=es[:], in_=es[:], func=AF.Exp), reads=[bes], writes=[bes])
        for qt in range(NTT):
            for kv in range(2):
                acc, bacc = C.acc.next()
                rhs_q = qT[:, kv * 4:(kv + 1) * 4, qt * 128:(qt + 1) * 128]
                kts = []
                for d_ in range(3):
                    ki = qt + d_
                    if d_ == 1:
                        bias = None
                    elif d_ == 0:
                        bias = mk[:, 2, :] if qt == 0 else mk[:, 0, :]
                    else:
                        bias = mk[:, 3, :] if qt == NTT - 1 else mk[:, 1, :]
                    kts.append((kT[:, kv, ki * 128:(ki + 1) * 128], [bk], ki, [bv], bias, [bmk]))
                attn_block(P, C, rhs_q, [bq], 512, kts, 0.125, lambda j, acc=acc: acc[:, j, 0:65], bacc, 4,
                           lambda ki, j, kv=kv: V[:, ki, kv * 65:(kv + 1) * 65])
                r_, br = rz.next()
                P.op("dve", lambda e, r_=r_, acc=acc, kv=kv: e.tensor_tensor(
                    out=r_[:], in0=acc[:, :, 64:65], in1=es[:, kv * 4:(kv + 1) * 4].rearrange("p (g o) -> p g o", o=1), op=ALU.add),
                    reads=[bacc, bes], writes=[br])
                P.op("dve", lambda e, r_=r_: e.reciprocal(out=r_[:], in_=r_[:]), reads=[br], writes=[br])
                P.op("dve", lambda e, r_=r_, acc=acc, kv=kv, qt=qt: e.tensor_tensor(
                    out=o_tok[:, qt, kv * 256:(kv + 1) * 256].rearrange("p (g d) -> p g d", g=4),
                    in0=acc[:, :, 0:64], in1=r_[:].to_broadcast([128, 4, 64]), op=ALU.mult),
                    reads=[bacc, br], writes=[bo])
        emit_oT(P, C, o_tok, bo, oT_d[0])
        P.pop()

    if "mla" in which:
        P.push()
        qTr = Rot(P, "mla_q", [96, T], BF16, 2)
        kTr = Rot(P, "mla_k", [96, 2 * T], BF16, 2)
        Vr = Rot(P, "mla_vv", [128, 64, 65], BF16, 2)
        sc = 96.0 ** -0.5
        for h in range(8):
            qT, bq = qTr.next(); kT, bk = kTr.next(); V, bv = Vr.next()
            P.dma("sp", qT[:], D["mla_qT"][h], writes=[bq])
            for n_, (d_, s_) in enumerate(SRC.mla_k(kT, h)):
                P.dma("sp", d_, s_, writes=[bk], append=(n_ > 0))
            for n_, (d_, s_) in enumerate(SRC.mla_v(V, h)):
                P.dma("sp", d_, s_, writes=[bv], append=(n_ > 0))
            for qb in range(NTB):
                if qb % 2 == 0:
                    bg_step()
                acc, bacc = C.acc.next()
                kts = [(kT[:, ki * 128:(ki + 1) * 128], [bk], ki, [bv], None, []) for ki in range(64)]
                attn_block(P, C, qT[:, qb * 512:(qb + 1) * 512], [bq], 512, kts, sc, lambda j, acc=acc: acc[:, j, 0:65], bacc, 4,
                           lambda ki, j, V=V: V[:, ki, :])
                r_, br = rz.next()
                P.op("dve", lambda e, r_=r_, acc=acc: e.reciprocal(out=r_[:], in_=acc[:, :, 64:65]), reads=[bacc], writes=[br])
                P.op("dve", lambda e, r_=r_, acc=acc, qb=qb, h=h: e.tensor_tensor(
                    out=o_tok[:, qb * 4:(qb + 1) * 4, h * 64:(h + 1) * 64],
                    in0=acc[:, :, 0:64], in1=r_[:].to_broadcast([128, 4, 64]), op=ALU.mult),
                    reads=[bacc, br], writes=[bo])
        emit_oT(P, C, o_tok, bo, oT_d[1])
        P.pop()

    if "diff" in which:
        P.push()
        qTr = Rot(P, "df_q", [64, T], BF16, 2)
        kTr = Rot(P, "df_k", [64, 2 * T], BF16, 2)
        Vr = Rot(P, "df_vv", [128, 64, 129], BF16, 1)
        o1 = P.sb("df_o1", [128, NTT, 128], F32); bo1 = Buf()
        lamb = P.sb("df_lamb", [128, 4, 64], F32); blamb = Buf()
        lami = P.sb("df_lami", [128, 2], F32)
        sg = P.sb("df_sg", [128, 128], F32); bsg = Buf()
        P.dma("pool", lamb[:], D["lamb"], writes=[blamb])
        P.dma("pool", lami[:], D["lami"], writes=[blamb], append=True)
        P.dma("pool", sg[:], D["subln"], writes=[bsg])
        P.op("dve", lambda e: e.tensor_scalar(out=sg[:], in0=sg[:], scalar1=lami[:, 1:2], scalar2=None, op0=ALU.mult), reads=[bsg, blamb], writes=[bsg])
        lp = P.sb("df_lp", [128, 2, 64], F32); blp = Buf()
        ls = P.sb("df_ls", [128, 2], F32); bls = Buf()
        nlam = P.sb("df_nlam", [128, 1], F32); bnl = Buf()
        P.op("dve", lambda e: e.tensor_tensor(out=lp[:, 0, :], in0=lamb[:, 0, :], in1=lamb[:, 1, :], op=ALU.mult), reads=[blamb], writes=[blp])
        P.op("dve", lambda e: e.tensor_tensor(out=lp[:, 1, :], in0=lamb[:, 2, :], in1=lamb[:, 3, :], op=ALU.mult), reads=[blamb, blp], writes=[blp])
        P.op("dve", lambda e: e.reduce_sum(out=ls[:], in_=lp[:], axis=AX.X), reads=[blp], writes=[bls])
        P.op("act", lambda e: e.activation(out=ls[:], in_=ls[:], func=AF.Exp), reads=[bls], writes=[bls])
        P.op("dve", lambda e: e.tensor_tensor(out=nlam[:], in0=ls[:, 1:2], in1=ls[:, 0:1], op=ALU.subtract), reads=[bls], writes=[bnl])
        P.op("dve", lambda e: e.tensor_tensor(out=nlam[:], in0=nlam[:], in1=lami[:, 0:1], op=ALU.subtract), reads=[bnl, blamb], writes=[bnl])
        ot = Rot(P, "df_ot", [128, 4, 128], F32, 2)
        sq = Rot(P, "df_sq", [128, 4, 128], F32, 1)
        ss = Rot(P, "df_ss", [128, 4], F32, 2)
        for h in range(4):
            V, bv = Vr.next()
            for n_, (d_, s_) in enumerate(SRC.diff_v(V, h)):
                P.dma("sp", d_, s_, writes=[bv], append=(n_ > 0))
            for m in range(2):
                row = (h * 2 + m) * 64
                qT, bq = qTr.next(); kT, bk = kTr.next()
                P.dma("sp", qT[:], D["diff_qT"][row:row + 64, :], writes=[bq])
                for n_, (d_, s_) in enumerate(SRC.diff_k(kT, row)):
                    P.dma("sp", d_, s_, writes=[bk], append=(n_ > 0))
                for qb in range(NTB):
                    acc, bacc = C.acc.next()
                    kts = [(kT[:, ki * 128:(ki + 1) * 128], [bk], ki, [bv], None, []) for ki in range(64)]
                    attn_block(P, C, qT[:, qb * 512:(qb + 1) * 512], [bq], 512, kts, 0.125, lambda j, acc=acc: acc[:, j, 0:129], bacc, 4,
                               lambda ki, j, V=V: V[:, ki, :])
                    r_, br = rz.next()
                    P.op("dve", lambda e, r_=r_, acc=acc: e.reciprocal(out=r_[:], in_=acc[:, :, 128:129]), reads=[bacc], writes=[br])
                    tl = slice(qb * 4, (qb + 1) * 4)
                    if m == 0:
                        P.op("dve", lambda e, r_=r_, acc=acc, tl=tl: e.tensor_tensor(
                            out=o1[:, tl, :], in0=acc[:, :, 0:128], in1=r_[:].to_broadcast([128, 4, 128]), op=ALU.mult),
                            reads=[bacc, br], writes=[bo1])
                    else:
                        o_, bo_ = ot.next()
                        P.op("dve", lambda e, r_=r_, acc=acc, o_=o_: e.tensor_tensor(
                            out=o_[:], in0=acc[:, :, 0:128], in1=r_[:].to_broadcast([128, 4, 128]), op=ALU.mult),
                            reads=[bacc, br], writes=[bo_])
                        P.op("dve", lambda e, o_=o_, tl=tl: e.scalar_tensor_tensor(
                            out=o_[:], in0=o_[:], scalar=nlam[:, 0:1], in1=o1[:, tl, :], op0=ALU.mult, op1=ALU.add),
                            reads=[bo_, bnl, bo1], writes=[bo_])
                        sq_, bsq = sq.next(); ss_, bss = ss.next()
                        P.op("pool", lambda e, o_=o_, sq_=sq_: e.tensor_tensor(out=sq_[:], in0=o_[:], in1=o_[:], op=ALU.mult), reads=[bo_], writes=[bsq])
                        P.op("dve", lambda e, sq_=sq_, ss_=ss_: e.reduce_sum(out=ss_[:], in_=sq_[:], axis=AX.X), reads=[bsq], writes=[bss])
                        P.op("act", lambda e, ss_=ss_: e.activation(out=ss_[:], in_=ss_[:], func=AF.Ln, bias=epsb[:, 0:1], scale=1.0 / 128.0), reads=[bss, beps], writes=[bss])
                        P.op("act", lambda e, ss_=ss_: e.activation(out=ss_[:], in_=ss_[:], func=AF.Exp, scale=-0.5), reads=[bss], writes=[bss])
                        P.op("pool", lambda e, o_=o_, ss_=ss_: e.tensor_tensor(
                            out=o_[:], in0=o_[:], in1=ss_[:].rearrange("p (g o) -> p g o", o=1).to_broadcast([128, 4, 128]), op=ALU.mult),
                            reads=[bo_, bss], writes=[bo_])
                        P.op("pool", lambda e, o_=o_, tl=tl, h=h: e.tensor_tensor(
                            out=o_tok[:, tl, h * 128:(h + 1) * 128], in0=o_[:],
                            in1=sg[:].rearrange("p (o d) -> p o d", o=1).to_broadcast([128, 4, 128]), op=ALU.mult),
                            reads=[bo_, bsg], writes=[bo])
        emit_oT(P, C, o_tok, bo, oT_d[2])
        P.pop()

    if "nat" in which:
        P.push()
        qTr = Rot(P, "nat_q", [64, T], BF16, 2)
        kTr = Rot(P, "nat_k", [64, 36 * 128], BF16, 2)
        Vr = Rot(P, "nat_vv", [128, 36, 65], BF16, 2)
        bfr = Rot(P, "nat_bf", [128, 29 * 128], F32, 1)
        bbr = Rot(P, "nat_bb", [128, 29, 128], BF16, 2)
        nat_accb = [Buf(f"nat_acc{j}", excl=True) for j in range(4)]
        for h in range(8):
            qT, bq = qTr.next(); kT, bk = kTr.next(); V, bv = Vr.next()
            bf_, bbf = bfr.next(); bb_, bbb = bbr.next()
            P.dma("sp", qT[:], D["nat_qT"][h * 64:(h + 1) * 64, :], writes=[bq])
            for n_, (d_, s_) in enumerate(SRC.nat_k(kT, h)):
                P.dma("sp", d_, s_, writes=[bk], append=(n_ > 0))
            for n_, (d_, s_) in enumerate(SRC.nat_v(V, h)):
                P.dma("sp", d_, s_, writes=[bv], append=(n_ > 0))
            P.dma("sp", bf_[:], D["nat_bias"][h], writes=[bbf])
            P.op("dve", lambda e, bb_=bb_, bf_=bf_: e.tensor_copy(out=bb_[:].rearrange("p a b -> p (a b)"), in_=bf_[:]), reads=[bbf], writes=[bbb])
            for qt in range(NTT):
                if qt == 0:
                    kis = list(range(0, 6)); tbl = list(range(5, 11))
                elif qt == 1:
                    kis = list(range(1, 7)); tbl = list(range(11, 17))
                elif qt == NTT - 2:
                    kis = list(range(29, 35)); tbl = list(range(17, 23))
                elif qt == NTT - 1:
                    kis = list(range(30, 36)); tbl = list(range(23, 29))
                else:
                    kis = list(range(qt, qt + 5)); tbl = list(range(0, 5))
                acc = C.acc.tiles[0]
                jb = qt % 4
                bacc = nat_accb[jb]
                kts = [(kT[:, ki * 128:(ki + 1) * 128], [bk], ki, [bv], bb_[:, ti, :], [bbb]) for ki, ti in zip(kis, tbl)]
                attn_block(P, C, qT[:, qt * 128:(qt + 1) * 128], [bq], 128, kts, 1.0, lambda j, acc=acc, jb=jb: acc[:, jb, 0:65], bacc, 1,
                           lambda ki, j, V=V: V[:, ki, :])
                r_, br = rz.next()
                P.op("dve", lambda e, r_=r_, acc=acc, jb=jb: e.reciprocal(out=r_[:, 0, :], in_=acc[:, jb, 64:65]), reads=[bacc], writes=[br])
                P.op("dve", lambda e, r_=r_, acc=acc, qt=qt, h=h, jb=jb: e.tensor_scalar(
                    out=o_tok[:, qt, h * 64:(h + 1) * 64], in0=acc[:, jb, 0:64], scalar1=r_[:, 0, 0:1], scalar2=None, op0=ALU.mult),
                    reads=[bacc, br], writes=[bo])
        emit_oT(P, C, o_tok, bo, oT_d[3])
        P.pop()


def nat_bias_tables(rpb, half):
    a = np.arange(128)
    out = np.full((8, 29, 128, 128), NEGM, np.float32)

    def table(tq, tk):
        if tk < 0 or tk >= 64:
            return None
        rk = 2 * tk + a // 64; ck = a % 64
        rq = 2 * tq + a // 64; cq = a % 64
        r0 = np.clip(rq - 4, 0, 120); cs = np.clip(cq - 8, 0, 48)
        valid = ((rk[:, None] >= r0[None, :]) & (rk[:, None] < r0[None, :] + 8) &
                 (ck[:, None] >= cs[None, :]) & (ck[:, None] < cs[None, :] + 16))
        ri = np.clip(rk[:, None] - rq[None, :] + 7, 0, 14)
        ci = np.clip(ck[:, None] - cq[None, :] + 15, 0, 30)
        t = rpb[:, ri, ci]
        return np.where(valid[None], t, np.float32(NEGM)).astype(np.float32)

    g0 = half * 32
    specs = [(10, off) for off in range(-2, 3)]
    specs += [(g0 + 0, off) for off in range(-2, 4)]
    specs += [(g0 + 1, off) for off in range(-2, 4)]
    specs += [(g0 + 30, off) for off in range(-3, 3)]
    specs += [(g0 + 31, off) for off in range(-3, 3)]
    for i, (tq, off) in enumerate(specs):
        t = table(tq, tq + off)
        if t is not None:
            out[:, i] = t
    return np.ascontiguousarray(out.transpose(0, 2, 1, 3).reshape(8, 128, 29 * 128))


def swa_masks(half):
    a = np.arange(128)
    L = np.where(a[None, :] <= a[:, None], 0.0, NEGM).astype(np.float32)
    R = np.where(a[:, None] <= a[None, :], 0.0, NEGM).astype(np.float32)
    allm = np.full((128, 128), NEGM, np.float32)
    first = allm if half == 0 else L
    last = allm if half == 1 else R
    return np.ascontiguousarray(np.stack([np.tile(m, (1, 4)) for m in (L, R, first, last)]))


def k2a_host_inputs(inp, l, core, k1o):
    import ml_dtypes
    b, half = core // 2, core % 2
    me, pa = k1o[core], k1o[core ^ 1]
    lo, hi = (me, pa) if half == 0 else (pa, me)
    bf = lambda a: np.ascontiguousarray(a)
    z = lambda shape: np.zeros(shape, ml_dtypes.bfloat16)
    m = {}
    m["swa_qT"] = bf(me["swa_qT"])
    left = pa["swa_kT"][:, -128:] if half == 1 else z((128, 128))
    right = pa["swa_kT"][:, :128] if half == 0 else z((128, 128))
    m["swa_kT"] = bf(np.concatenate([left, me["swa_kT"], right], 1))
    left = pa["swa_v"][-1:] if half == 1 else z((1, 128, 130))
    right = pa["swa_v"][:1] if half == 0 else z((1, 128, 130))
    m["swa_v"] = bf(np.concatenate([left, me["swa_v"], right], 0))
    m["swa_masks"] = swa_masks(half)
    m["sinkb"] = np.ascontiguousarray(np.broadcast_to(inp["swa_sink"][l][None, :], (128, 8))).astype(np.float32)
    m["mla_qT"] = bf(me["mla_qT"])
    m["mla_kT"] = bf(np.concatenate([lo["mla_kT"], hi["mla_kT"]], 2))
    m["mla_v"] = bf(np.concatenate([lo["mla_v"], hi["mla_v"]], 0))
    m["diff_qT"] = bf(me["diff_qT"])
    m["diff_kT"] = bf(np.concatenate([lo["diff_kT"], hi["diff_kT"]], 1))
    m["diff_v"] = bf(np.concatenate([lo["diff_v"], hi["diff_v"]], 0))
    lam = np.stack([inp["diff_lambda_q1"][l], inp["diff_lambda_k1"][l], inp["diff_lambda_q2"][l], inp["diff_lambda_k2"][l]])
    m["lamb"] = np.ascontiguousarray(np.broadcast_to(lam[None], (128, 4, 64))).astype(np.float32)
    m["subln"] = np.ascontiguousarray(np.broadcast_to(inp["diff_subln"][l][None], (128, 128))).astype(np.float32)
    import math
    li = 0.8 - 0.6 * math.exp(-0.3 * l)
    m["lami"] = np.ascontiguousarray(np.broadcast_to(np.array([li, 1.0 - li], np.float32)[None], (128, 2)))
    m["nat_qT"] = bf(me["nat_qT"])
    left = pa["nat_kT"][:, -256:] if half == 1 else z((512, 256))
    right = pa["nat_kT"][:, :256] if half == 0 else z((512, 256))
    m["nat_kT"] = bf(np.concatenate([left, me["nat_kT"], right], 1))
    left = pa["nat_v"][-2:] if half == 1 else z((2, 128, 520))
    right = pa["nat_v"][:2] if half == 0 else z((2, 128, 520))
    m["nat_v"] = bf(np.concatenate([left, me["nat_v"], right], 0))
    m["nat_bias"] = nat_bias_tables(inp["nat_rpb"][l], half)
    m["ident"] = np.eye(128, dtype=np.float32)
    return m


DN_ALPHA = 4.0 ** 0.25


def emit_gvec_bcast(P, cT_d, adaw_d, adabb_d, gb, bgb, pA, wst_rot, tag):
    cs = P.sb(f"{tag}_cs", [128, 8], F32); bcs = Buf()
    csr = P.sb(f"{tag}_csr", [128, 8, 128], F32); bcsr = Buf()
    ab = P.sb(f"{tag}_ab", [128, 1024], F32); bab = Buf()
    P.dma("sp", cs[:], cT_d, writes=[bcs])
    P.dma("act", ab[:], adabb_d, writes=[bab])
    P.op("act", lambda e: e.activation(out=cs[:], in_=cs[:], func=AF.Silu), reads=[bcs], writes=[bcs])
    P.op("dve", lambda e: e.tensor_copy(out=csr[:], in_=cs[:].rearrange("p (k o) -> p k o", o=1).to_broadcast([128, 8, 128])),
         reads=[bcs], writes=[bcsr])
    for hf in range(2):
        ps, bps = pA.next()
        for q4 in range(2):
            ws_, bws = wst_rot.next()
            ws = ws_[:, 0:2048].rearrange("p (c n) -> p c n", c=8)
            c0 = hf * 512 + q4 * 256
            P.dma("sp" if q4 == 0 else "act", ws, adaw_d[:, c0:c0 + 256].rearrange("(c p) n -> p c n", p=128), writes=[bws])
            for kc in range(8):
                P.op("pe", lambda e, kc=kc, ps=ps, ws=ws, q4=q4: e.matmul(ps[:, q4 * 256:(q4 + 1) * 256], lhsT=csr[:, kc, :], rhs=ws[:, kc, :],
                                                                 start=(kc == 0), stop=(kc == 7)), reads=[bcsr, bws], writes=[bps])
        P.op("dve", lambda e, ps=ps, hf=hf: e.tensor_tensor(out=gb[:, hf * 512:(hf + 1) * 512], in0=ps[:], in1=ab[:, hf * 512:(hf + 1) * 512], op=ALU.add),
             reads=[bps, bab], writes=[bgb])


def emit_resid_ln(P, C, y_ps, by, xt, bx, gb, bgb, lng, lnb, bln, epsb, beps, out_tile, bout, tag):
    t1, bt1 = C["t1"].next()
    for hf in range(2):
        P.op("dve", lambda e, hf=hf, t1=t1: e.tensor_tensor(out=t1[:, hf * 512:(hf + 1) * 512], in0=y_ps[hf][:], in1=gb[:, hf * 512:(hf + 1) * 512], op=ALU.mult),
             reads=[by[hf], bgb], writes=[bt1])
    P.op("dve", lambda e, t1=t1: e.scalar_tensor_tensor(out=t1[:], in0=xt[:], scalar=DN_ALPHA, in1=t1[:], op0=ALU.mult, op1=ALU.add),
         reads=[bx, bt1], writes=[bt1])
    s_, bs = C["st"].next(); m_, bm = C["mv"].next(); r_, br = C["rs"].next()
    for c in range(2):
        P.op("dve", lambda e, c=c, s_=s_, t1=t1: e.bn_stats(out=s_[:, c, :], in_=t1[:, c * 512:(c + 1) * 512]), reads=[bt1], writes=[bs])
    P.op("dve", lambda e, m_=m_, s_=s_: e.bn_aggr(out=m_[:], in_=s_[:]), reads=[bs], writes=[bm])
    P.op("act", lambda e, r_=r_, m_=m_: e.activation(out=r_[:], in_=m_[:, 1:2], func=AF.Ln, bias=epsb[:, 0:1], scale=1.0), reads=[bm, beps], writes=[br])
    P.op("act", lambda e, r_=r_: e.activation(out=r_[:], in_=r_[:], func=AF.Exp, scale=-0.5), reads=[br], writes=[br])
    P.op("dve", lambda e, t1=t1, m_=m_, r_=r_: e.tensor_scalar(out=t1[:], in0=t1[:], scalar1=m_[:, 0:1], scalar2=r_[:, 0:1], op0=ALU.subtract, op1=ALU.mult),
         reads=[bt1, bm, br], writes=[bt1])
    P.op("pool", lambda e, t1=t1: e.tensor_tensor(out=t1[:], in0=t1[:], in1=lng[:], op=ALU.mult), reads=[bt1, bln], writes=[bt1])
    P.op("pool", lambda e, t1=t1: e.tensor_tensor(out=out_tile[:], in0=t1[:], in1=lnb[:], op=ALU.add), reads=[bt1, bln], writes=[bout])


def build_k2b(nc):
    P = Prog(nc)
    din = lambda n, s, d=F32: nc.dram_tensor(n, list(s), d, kind="ExternalInput").ap()
    D = {}
    D["oT"] = din("oT", [4, 512, T], BF16); D["uT"] = din("uT", [1024, T], BF16); D["x"] = din("x", [T, 1024])
    D["wg"] = din("wg", [4, 1024, 1024]); D["wb"] = din("wb", [4, 512, 1024]); D["wo"] = din("wo", [1024, 1024])
    D["cT"] = din("cT", [128, 8]); D["adaw_g"] = din("adaw_g", [1024, 1024]); D["adab_g"] = din("adab_g", [128, 1024])
    D["lng"] = din("lng", [128, 1024]); D["lnb"] = din("lnb", [128, 1024])
    xo = nc.dram_tensor("xmid", [T, 1024], F32, kind="ExternalOutput").ap()
    emit_merge(P, D, xo)
    P.finish(); P.emit()
    return P


def emit_merge(P, D, xo):
    P.push()
    pA = Rot(P, "m_pA", [128, 512], F32, 3, "ps")
    pB = Rot(P, "m_pB", [128, 512], F32, 3, "ps")
    wst = Rot(P, "m_wst", [128, 2048], F32, 2)
    epsb = P.sb("m_eps", [128, 1], F32); beps = Buf()
    P.op("pool", lambda e: e.memset(epsb[:], 1e-5), writes=[beps])
    gb = P.sb("m_gb", [128, 1024], F32); bgb = Buf()
    emit_gvec_bcast(P, D["cT"], D["adaw_g"], D["adab_g"], gb, bgb, pA, wst, "m")
    mT = P.sb("m_mT", [128, 8, T], BF16)
    bmT = [[Buf() for _ in range(NTB)] for _ in range(8)]
    P.push()
    wgf = Rot(P, "m_wgf", [128, 8, 4, 128], F32, 1)
    wgb = Rot(P, "m_wgb", [128, 8, 4, 128], BF16, 2)
    wbf = Rot(P, "m_wbf", [128, 4, 4, 128], F32, 1)
    wbb = Rot(P, "m_wbb", [128, 4, 4, 128], BF16, 2)
    ub = Rot(P, "m_ub", [128, 8, 512], BF16, 2)
    ob = Rot(P, "m_ob", [128, 4, 4, 512], BF16, 2)
    gs = Rot(P, "m_gs", [128, 512], BF16, 2)
    ac = Rot(P, "m_ac", [128, 512], F32, 2)
    tm = Rot(P, "m_tm", [128, 512], F32, 2)
    for oc in range(8):
        wgf_, bwgf = wgf.next(); wgb_, bwgb = wgb.next(); wbf_, bwbf = wbf.next(); wbb_, bwbb = wbb.next()
        for n in range(4):
            P.dma(("sp", "act")[n % 2], wgf_[:, :, n, :], D["wg"][n, :, oc * 128:(oc + 1) * 128].rearrange("(c p) n -> p c n", p=128),
                  writes=[bwgf], append=(n > 0))
            P.dma(("act", "sp")[n % 2], wbf_[:, :, n, :], D["wb"][n, :, oc * 128:(oc + 1) * 128].rearrange("(c p) n -> p c n", p=128),
                  writes=[bwbf], append=(n > 0))
        P.op("pool", lambda e, a=wgb_, b=wgf_: e.tensor_copy(out=a[:], in_=b[:]), reads=[bwgf], writes=[bwgb])
        P.op("pool", lambda e, a=wbb_, b=wbf_: e.tensor_copy(out=a[:], in_=b[:]), reads=[bwbf], writes=[bwbb])
        for tb in range(NTB):
            sl = slice(tb * 512, (tb + 1) * 512)
            u_, bu = ub.next(); o_, bo_ = ob.next()
            P.dma("sp", u_[:], D["uT"].rearrange("(c p) t -> p c t", p=128)[:, :, sl], writes=[bu])
            for n in range(4):
                P.dma(("act", "pool")[n % 2], o_[:, n, :, :], D["oT"][n].rearrange("(c p) t -> p c t", p=128)[:, :, sl], writes=[bo_], append=(n > 0))
            a_, ba = ac.next()
            for n in range(4):
                pg, bpg = pA.next(); pb, bpb = pB.next()
                for kc in range(8):
                    P.op("pe", lambda e, kc=kc, n=n, pg=pg, wgb_=wgb_, u_=u_: e.matmul(pg[:], lhsT=wgb_[:, kc, n, :], rhs=u_[:, kc, :], start=(kc == 0), stop=(kc == 7)),
                         reads=[bwgb, bu], writes=[bpg])
                for kc in range(4):
                    P.op("pe", lambda e, kc=kc, n=n, pb=pb, wbb_=wbb_, o_=o_: e.matmul(pb[:], lhsT=wbb_[:, kc, n, :], rhs=o_[:, n, kc, :], start=(kc == 0), stop=(kc == 3)),
                         reads=[bwbb, bo_], writes=[bpb])
                g_, bg = gs.next()
                P.op("act", lambda e, g_=g_, pg=pg: e.activation(out=g_[:], in_=pg[:], func=AF.Sigmoid), reads=[bpg], writes=[bg])
                if n == 0:
                    P.op("dve", lambda e, a_=a_, g_=g_, pb=pb: e.tensor_tensor(out=a_[:], in0=pb[:], in1=g_[:], op=ALU.mult), reads=[bpb, bg], writes=[ba])
                else:
                    t_, bt = tm.next()
                    P.op("dve", lambda e, t_=t_, g_=g_, pb=pb: e.tensor_tensor(out=t_[:], in0=pb[:], in1=g_[:], op=ALU.mult), reads=[bpb, bg], writes=[bt])
                    if n < 3:
                        P.op("pool", lambda e, a_=a_, t_=t_: e.tensor_tensor(out=a_[:], in0=a_[:], in1=t_[:], op=ALU.add), reads=[ba, bt], writes=[ba])
                    else:
                        P.op("pool", lambda e, a_=a_, t_=t_, oc=oc, sl=sl: e.tensor_tensor(out=mT[:, oc, sl], in0=a_[:], in1=t_[:], op=ALU.add),
                             reads=[ba, bt], writes=[bmT[oc][tb]])
    P.pop()
    P.push()
    wo = P.sb("m_wo", [128, 8, 1024], BF16); bwo = Buf()
    for q8 in range(4):
        ws_, bws = wst.next()
        ws = ws_[:, 0:2048].rearrange("p (c n) -> p c n", c=8)
        P.dma(("sp", "act")[q8 % 2], ws, D["wo"][:, q8 * 256:(q8 + 1) * 256].rearrange("(c p) n -> p c n", p=128), writes=[bws])
        P.op("pool", lambda e, ws=ws, q8=q8: e.tensor_copy(out=wo[:, :, q8 * 256:(q8 + 1) * 256], in_=ws), reads=[bws], writes=[bwo], )
    lng = P.sb("m_lng", [128, 1024], F32); lnb = P.sb("m_lnb", [128, 1024], F32); bln = Buf()
    P.dma("sp", lng[:], D["lng"], writes=[bln]); P.dma("act", lnb[:], D["lnb"], writes=[bln], append=True)
    C = {"t1": Rot(P, "m_t1", [128, 1024], F32, 2), "st": Rot(P, "m_st", [128, 2, 6], F32, 2),
         "mv": Rot(P, "m_mv", [128, 2], F32, 2), "rs": Rot(P, "m_rs", [128, 1], F32, 2)}
    xin = Rot(P, "m_xin", [128, 1024], F32, 2)
    xout = Rot(P, "m_xout", [128, 1024], F32, 2)
    for t in range(NTT):
        xt, bx = xin.next()
        P.dma("sp" if t % 2 == 0 else "act", xt[:], D["x"][t * 128:(t + 1) * 128, :], writes=[bx])
        ys = []; bys = []
        for hf in range(2):
            py, bpy = pA.next()
            for kc in range(8):
                P.op("pe", lambda e, kc=kc, py=py, hf=hf, t=t: e.matmul(py[:], lhsT=mT[:, kc, t * 128:(t + 1) * 128], rhs=wo[:, kc, hf * 512:(hf + 1) * 512],
                                                                 start=(kc == 0), stop=(kc == 7)), reads=[bwo, bmT[kc][t // 4]], writes=[bpy])
            ys.append(py); bys.append(bpy)
        xo_, bxo = xout.next()
        emit_resid_ln(P, C, ys, bys, xt, bx, gb, bgb, lng, lnb, bln, epsb, beps, xo_, bxo, "m")
        P.dma("pool", xo[t * 128:(t + 1) * 128, :], xo_[:], reads=[bxo])
    P.pop()
    P.pop()


def k2b_host_inputs(inp, l, core, xcur, oT, uT):

    b, half = core // 2, core % 2
    rep = lambda v: np.ascontiguousarray(np.broadcast_to(np.asarray(v, np.float32)[None, :], (128, len(v))))
    return {
        "oT": np.ascontiguousarray(oT), "uT": np.ascontiguousarray(uT),
        "x": np.ascontiguousarray(xcur[b, half * T:(half + 1) * T]),
        "wg": np.ascontiguousarray(inp["w_gate"][l]), "wb": np.ascontiguousarray(inp["w_branch"][l]),
        "wo": np.ascontiguousarray(inp["w_out"][l]),
        "cT": to_pc(inp["c"][b], 8), "adaw_g": np.ascontiguousarray(inp["ada_w"][l][:, 2048:3072]),
        "adab_g": rep(inp["ada_b"][l][2048:3072]),
        "lng": rep(inp["ln1_g"][l]), "lnb": rep(inp["ln1_b"][l]),
    }


TBLK = 256
NBLK = T // TBLK
NCH = 128
NEG = -1.0e30
U32 = mybir.dt.uint32


def build_k3(nc, nblk=NBLK, dbg=False):
    P = Prog(nc)
    din = lambda n, s, d=F32: nc.dram_tensor(n, list(s), d, kind="ExternalInput").ap()
    D = {}
    D["xmid"] = din("xmid", [T, 1024]); D["cT"] = din("cT", [128, 8])
    D["adaw_f"] = din("adaw_f", [1024, 3072]); D["adab_f"] = din("adab_f", [128, 16]); D["adab_g"] = din("adab_g", [128, 1024])
    D["wq"] = din("wq", [1024, 2048]); D["keysT"] = din("keysT", [128, 16, 128])
    D["UT"] = din("UT", [1024, 16384]); D["V"] = din("V", [16384, 1024])
    D["lng"] = din("lng", [128, 1024]); D["lnb"] = din("lnb", [128, 1024])
    D["ident"] = din("ident", [128, 128]); D["iota"] = din("iota", [128, 128])
    xo = nc.dram_tensor("xout", [T, 1024], F32, kind="ExternalOutput").ap()
    dbg_d = nc.dram_tensor("dbg", [128, 16, 128], F32, kind="ExternalOutput").ap() if dbg else None
    emit_peer(P, nc, D, xo, nblk, dbg_d)
    P.finish(); P.emit()
    return P


def peer_scratch(nc, tag):
    Ub_d = nc.dram_tensor(f"peer_Ub{tag}", [NCH, 128, 8 * 128], BF16, kind="Internal").ap()
    Vb_d = nc.dram_tensor(f"peer_Vb{tag}", [NCH, 128, 1024], BF16, kind="Internal").ap()
    return Ub_d, Vb_d, Buf("Ub_d"), Buf("Vb_d")


def peer_cast_gen(P, UT, V, Ub_d, Vb_d, bUb, bVb, cf, cb):
    for c4 in range(NCH // 4):
        f_, bf_ = cf.next(); b_, bb_ = cb.next()
        P.dma("sp", f_[:].rearrange("p (k n) -> p k n", k=8),
              UT[:, c4 * 512:(c4 + 1) * 512].rearrange("(k p) n -> p k n", p=128), writes=[bf_])
        P.op(("dve", "pool")[c4 % 2], lambda e, f_=f_, b_=b_: e.tensor_copy(
            out=b_[:].rearrange("p (c k n) -> p c k n", c=4, k=8), in_=f_[:].rearrange("p (k c n) -> p c k n", k=8, c=4)),
            reads=[bf_], writes=[bb_])
        P.dma("pool", Ub_d[c4 * 4:(c4 + 1) * 4].rearrange("c p n -> p c n"), b_[:].rearrange("p (c n) -> p c n", c=4),
              reads=[bb_], writes=[bUb], append=True)
        f_, bf_ = cf.next(); b_, bb_ = cb.next()
        P.dma("sp", f_[:].rearrange("p (c n) -> p c n", c=4),
              V[c4 * 512:(c4 + 1) * 512, :].rearrange("(c p) n -> p c n", p=128), writes=[bf_])
        P.op(("pool", "dve")[c4 % 2], lambda e, f_=f_, b_=b_: e.tensor_copy(out=b_[:], in_=f_[:]), reads=[bf_], writes=[bb_])
        P.dma("pool", Vb_d[c4 * 4:(c4 + 1) * 4].rearrange("c p n -> p c n"), b_[:].rearrange("p (c n) -> p c n", c=4),
              reads=[bb_], writes=[bVb], append=True)
        yield


def emit_peer(P, nc, D, xo, nblk=NBLK, dbg_d=None, tag="", pre=None):
    u2_d = nc.dram_tensor(f"peer_u2{tag}", [1024, T], BF16, kind="Internal").ap()
    P.push()
    bu2d = Buf("u2_d")
    if pre is None:
        Ub_d, Vb_d, bUb, bVb = peer_scratch(nc, tag)
        P.push()
        cf = Rot(P, "pa_cf", [128, 4096], F32, 2)
        cb = Rot(P, "pa_cb", [128, 4096], BF16, 2)
        for _ in peer_cast_gen(P, D["UT"], D["V"], Ub_d, Vb_d, bUb, bVb, cf, cb):
            pass
        P.pop()
    else:
        Ub_d, Vb_d, bUb, bVb = pre

    pA = Rot(P, "p_pA", [128, 512], F32, 2, "ps")
    ident_f = P.sb("p_identf", [128, 128], F32); bidf = Buf()
    ident = P.sb("p_ident", [128, 128], BF16); bid = Buf()
    epsb = P.sb("p_eps", [128, 1], F32); beps = Buf()
    P.dma("pool", ident_f[:], D["ident"], writes=[bidf])
    P.op("dve", lambda e: e.tensor_copy(out=ident[:], in_=ident_f[:]), reads=[bidf], writes=[bid])
    P.op("pool", lambda e: e.memset(epsb[:], 1e-5), writes=[beps])
    gb = P.sb("p_gb", [128, 1024], F32); bgb = Buf()
    P.push()
    wst = Rot(P, "p_wst", [128, 2048], F32, 2)
    emit_gvec_bcast(P, D["cT"], D["adaw_f"][:, 2048:3072], D["adab_g"], gb, bgb, pA, wst, "p")
    P.push()
    modc = P.sb("p_modc", [128, 16], F32); bmodc = Buf()
    sc1 = P.sb("p_sc1", [128, 8], F32); bsc1 = Buf()
    pm = Rot(P, "p_pm", [128, 512], F32, 1, "ps")
    emit_mod_cols(P, nc, D["cT"], D["adaw_f"][:, 0:2048], D["adab_f"], 2, modc, bmodc, pm, wst)
    P.op("dve", lambda e: e.tensor_scalar(out=sc1[:], in0=modc[:, 8:16], scalar1=1.0, scalar2=None, op0=ALU.add), reads=[bmodc], writes=[bsc1])
    bmod = Buf()
    P.op("dve", lambda e: e.tensor_copy(out=modc[:, 0:8], in_=modc[:, 0:8]), reads=[bmodc, bsc1], writes=[bmod])
    u2T = P.sb("p_u2T", [128, 8, T], BF16)
    bu2 = [Buf() for _ in range(NTT)]
    ptr = Rot(P, "p_ptr", [128, 8, 128], BF16, 2, "ps")
    emit_ln_mod(P, D["xmid"], u2T, bu2, modc, sc1, bmod, ident, bid, epsb, beps, ptr, tag="p")
    for c in range(8):
        P.dma(("sp", "act")[c % 2], u2_d[c * 128:(c + 1) * 128, :], u2T[:, c, :], reads=bu2, writes=[bu2d], append=(c > 0))
    P.pop()

    wqb_d = nc.dram_tensor(f"peer_wqb{tag}", [128, 8 * 2048], BF16, kind="Internal").ap()
    bwqd = Buf("wqb_d")
    P.push()
    wtmp = P.sb("p_wqtmp", [128, 8, 2048], BF16); bwt = Buf()
    for g in range(8):
        ws_, bws = wst.next()
        ws = ws_[:, 0:2048].rearrange("p (c n) -> p c n", c=8)
        P.dma(("sp", "act")[g % 2], ws, D["wq"][:, g * 256:(g + 1) * 256].rearrange("(c p) n -> p c n", p=128), writes=[bws])
        P.op("pool", lambda e, ws=ws, g=g: e.tensor_copy(out=wtmp[:, :, g * 256:(g + 1) * 256], in_=ws), reads=[bws], writes=[bwt])
    P.dma("sp", wqb_d, wtmp[:].rearrange("p k n -> p (k n)"), reads=[bwt], writes=[bwqd])
    P.pop()
    P.pop()

    keysT = P.sb("p_keysT", [128, 16, 128], F32); bkeys = Buf()
    P.dma("sp", keysT[:], D["keysT"], writes=[bkeys])
    iota = P.sb("p_iota", [128, 128], F32); biota = Buf()
    P.dma("act", iota[:], D["iota"], writes=[biota])
    lng = P.sb("p_lng", [128, 1024], F32); lnb = P.sb("p_lnb", [128, 1024], F32); bln = Buf()
    P.dma("sp", lng[:], D["lng"], writes=[bln]); P.dma("act", lnb[:], D["lnb"], writes=[bln], append=True)

    pO = [P.ps(f"p_pO{i}", [128, 512], F32) for i in range(4)]
    bpO = [Buf(f"pO{i}", excl=True) for i in range(4)]
    pH = Rot(P, "p_pH", [128, 512], F32, 2, "ps")
    pM = pA
    NTL = TBLK // 128

    Wd = nc.dram_tensor(f"peer_W{tag}", [nblk, 8, 128, TBLK * 16], BF16, kind="Internal").ap()
    bWd = [Buf(f"Wd{b}") for b in range(nblk)]

    wq = P.sb("p_wq", [128, 8, 2048], BF16); bwq = Buf()
    P.dma("act", wq[:].rearrange("p k n -> p (k n)"), wqb_d, reads=[bwqd], writes=[bwq])
    ublk = Rot(P, "p_ublk", [128, 8, TBLK], BF16, 2)
    qT = Rot(P, "p_qT", [128, 16, TBLK], F32, 1)
    C = {"t1": Rot(P, "p_t1", [128, 1024], F32, 2), "st": Rot(P, "p_st", [128, 2, 6], F32, 2),
         "mv": Rot(P, "p_mv", [128, 2], F32, 2), "rs": Rot(P, "p_rs", [128, 1], F32, 2)}
    xin = Rot(P, "p_xin", [128, 1024], F32, 1)
    xout = Rot(P, "p_xout", [128, 1024], F32, 1)
    S = Rot(P, "p_S", [128, 16, 128], F32, 1)
    S2 = Rot(P, "p_S2", [128, 128], F32, 2)
    top = Rot(P, "p_top", [128, 16, 16], F32, 1)
    jix = Rot(P, "p_jix", [128, 8, 16], U32, 1)
    jif = Rot(P, "p_jif", [128, 128], F32, 1)
    jT = Rot(P, "p_jT", [128, 128], F32, 1)
    cand = Rot(P, "p_cand", [128, 256], F32, 2)
    ctop = Rot(P, "p_ctop", [128, 24], F32, 2)
    sm = Rot(P, "p_sm", [128, 8, 8], F32, 1)
    junk = Rot(P, "p_junk", [128, 16], F32, 2)
    e0 = Rot(P, "p_e0", [128, 8, 128], BF16, 1)
    thr2 = Rot(P, "p_thr2", [128, 8, 16], F32, 1)
    sc2 = Rot(P, "p_sc2", [128, 8, 16], F32, 1)
    sc2b = Rot(P, "p_sc2b", [128, 8, 16], BF16, 1)
    Ytm = Rot(P, "p_Ytm", [128, 128, 64], BF16, 1)
    Ysm = Rot(P, "p_Ysm", [128, 64, 128], BF16, 1)
    Xsm = Rot(P, "p_Xsm", [128, 64, 128], BF16, 1)
    Wst = Rot(P, "p_Wst", [128, 4, 128, 16], BF16, 1)
    wsl = Rot(P, "p_wsl", [128, TBLK, 16], BF16, 2)
    uch = Rot(P, "p_uch", [128, 8, 128], BF16, 3)
    vch = Rot(P, "p_vch", [128, 1024], BF16, 4)
    gl = Rot(P, "p_gl", [128, TBLK], BF16, 2)
    zt = Rot(P, "p_zt", [128, TBLK], BF16, 3)
    ublocks = {}

    def sel_block(blk):
        tsl = slice(blk * TBLK, (blk + 1) * TBLK)
        u_, bu = ublk.next()
        ublocks[blk] = (u_, bu)
        P.dma("sp", u_[:], u2_d.rearrange("(c p) t -> p c t", p=128)[:, :, tsl], reads=[bu2d], writes=[bu])
        q_, bq = qT.next()
        for hp in range(16):
            ph, bph = pH.next()
            for kc in range(8):
                P.op("pe", lambda e, kc=kc, hp=hp, ph=ph, u_=u_: e.matmul(ph[:, 0:TBLK], lhsT=wq[:, kc, hp * 128:(hp + 1) * 128], rhs=u_[:, kc, :],
                                                                  start=(kc == 0), stop=(kc == 7)), reads=[bwq, bu], writes=[bph])
            P.op("act", lambda e, hp=hp, ph=ph, q_=q_: e.activation(out=q_[:, hp, :], in_=ph[:, 0:TBLK], func=AF.Copy), reads=[bph], writes=[bq])
            if hp % 4 == 3:
                yield
        for tt in range(NTL):
            S_, bS = S.next()
            for g4 in range(4):
                pm_, bpm = pM.next()
                for i4 in range(4):
                    hp = g4 * 4 + i4
                    P.op("pe", lambda e, hp=hp, i4=i4, pm_=pm_, q_=q_, tt=tt: e.matmul(
                        pm_[:, i4 * 128:(i4 + 1) * 128], lhsT=q_[:, hp, tt * 128:(tt + 1) * 128], rhs=keysT[:, hp, :], start=True, stop=True),
                        reads=[bq, bkeys], writes=[bpm])
                P.op("act", lambda e, g4=g4, pm_=pm_, S_=S_: e.activation(out=S_[:, g4 * 4:(g4 + 1) * 4, :].rearrange("p a b -> p (a b)"), in_=pm_[:], func=AF.Copy),
                     reads=[bpm], writes=[bS])
            if dbg_d is not None and blk == 0 and tt == 0:
                P.dma("sp", dbg_d, S_[:], reads=[bS])
            yield
            top_, btop = top.next(); jix_, bjix = jix.next()
            for hp in range(16):
                s2, bs2 = S2.next()
                P.op("dve", lambda e, hp=hp, top_=top_, S_=S_: e.max(out=top_[:, hp, 0:8], in_=S_[:, hp, :]), reads=[bS], writes=[btop])
                P.op("dve", lambda e, hp=hp, top_=top_, S_=S_, s2=s2: e.match_replace(out=s2[:], in_to_replace=top_[:, hp, 0:8], in_values=S_[:, hp, :], imm_value=NEG),
                     reads=[bS, btop], writes=[bs2])
                P.op("dve", lambda e, hp=hp, top_=top_, s2=s2: e.max(out=top_[:, hp, 8:16], in_=s2[:]), reads=[bs2, btop], writes=[btop])
                if hp % 2 == 1:
                    h = hp // 2
                    P.op("dve", lambda e, hp=hp, h=h, top_=top_, S_=S_, jix_=jix_: e.max_index(out=jix_[:, h, 0:8], in_max=top_[:, hp, 0:8], in_values=S_[:, hp, :]),
                         reads=[bS, btop], writes=[bjix])
                    P.op("dve", lambda e, hp=hp, h=h, top_=top_, S_=S_, jix_=jix_: e.max_index(out=jix_[:, h, 8:16], in_max=top_[:, hp, 8:16], in_values=S_[:, hp, :]),
                         reads=[bS, btop, bjix], writes=[bjix])
                if hp % 4 == 3:
                    yield
            jif_, bjif = jif.next()
            P.op("dve", lambda e, jif_=jif_, jix_=jix_: e.tensor_copy(out=jif_[:], in_=jix_[:].rearrange("p a b -> p (a b)")), reads=[bjix], writes=[bjif])
            pm_, bpm = pM.next()
            P.op("pe", lambda e, pm_=pm_, jif_=jif_: e.transpose(out=pm_[:, 0:128], in_=jif_[:], identity=ident_f[:]), reads=[bjif, bidf], writes=[bpm])
            jT_, bjT = jT.next()
            P.op("act", lambda e, pm_=pm_, jT_=jT_: e.activation(out=jT_[:], in_=pm_[:, 0:128], func=AF.Copy), reads=[bpm], writes=[bjT])
            sm_, bsm = sm.next(); thr_, bthr = thr2.next(); sc_, bsc = sc2.next(); e0_, be0 = e0.next()
            P.op("pool", lambda e, sm_=sm_: e.memset(sm_[:], 0.0), writes=[bsm])
            for h in range(8):
                cd, bcd = cand.next(); ct, bct = ctop.next()
                P.op("dve", lambda e, h=h, cd=cd, top_=top_: e.tensor_tensor(
                    out=cd[:].rearrange("p (a b) -> p a b", a=16),
                    in0=top_[:, 2 * h, :].rearrange("p (a o) -> p a o", o=1).to_broadcast([128, 16, 16]),
                    in1=top_[:, 2 * h + 1, :].rearrange("p (o b) -> p o b", o=1).to_broadcast([128, 16, 16]), op=ALU.add),
                    reads=[btop], writes=[bcd])
                for r in range(3):
                    P.op("dve", lambda e, r=r, cd=cd, ct=ct: e.max(out=ct[:, r * 8:(r + 1) * 8], in_=cd[:]), reads=[bcd], writes=[bct])
                    if r < 2:
                        P.op("dve", lambda e, r=r, cd=cd, ct=ct: e.match_replace(out=cd[:], in_to_replace=ct[:, r * 8:(r + 1) * 8], in_values=cd[:], imm_value=NEG),
                             reads=[bct, bcd], writes=[bcd])
                P.op("dve", lambda e, h=h, ct=ct, sm_=sm_: e.tensor_tensor(out=sm_[:, h, 0:1], in0=ct[:, 15:16], in1=ct[:, 16:17], op=ALU.add), reads=[bct, bsm], writes=[bsm])
                P.op("dve", lambda e, h=h, sm_=sm_: e.tensor_scalar(out=sm_[:, h, 0:1], in0=sm_[:, h, 0:1], scalar1=0.5, scalar2=None, op0=ALU.mult), reads=[bsm], writes=[bsm])
                P.op("dve", lambda e, h=h, top_=top_, sm_=sm_: e.tensor_scalar(out=sm_[:, h, 1:2], in0=top_[:, 2 * h, 0:1], scalar1=-1.0, scalar2=None, op0=ALU.mult), reads=[btop, bsm], writes=[bsm])
                P.op("dve", lambda e, h=h, top_=top_, sm_=sm_: e.tensor_scalar(out=sm_[:, h, 2:3], in0=top_[:, 2 * h + 1, 0:1], scalar1=-1.0, scalar2=None, op0=ALU.mult), reads=[btop, bsm], writes=[bsm])
                P.op("dve", lambda e, h=h, ct=ct, sm_=sm_: e.tensor_scalar(out=sm_[:, h, 5:6], in0=ct[:, 0:1], scalar1=-1.0, scalar2=None, op0=ALU.mult), reads=[bct, bsm], writes=[bsm])
                jk, bjk = junk.next()
                P.op("act", lambda e, h=h, ct=ct, sm_=sm_, jk=jk: e.activation(out=jk[:], in_=ct[:, 0:16], func=AF.Exp, bias=sm_[:, h, 5:6], scale=1.0, accum_out=sm_[:, h, 3:4]),
                     reads=[bct, bsm], writes=[bjk, bsm])
                P.op("dve", lambda e, h=h, sm_=sm_: e.reciprocal(out=sm_[:, h, 4:5], in_=sm_[:, h, 3:4]), reads=[bsm], writes=[bsm])
                P.op("act", lambda e, h=h, S_=S_, sm_=sm_, e0_=e0_: e.activation(out=e0_[:, h, :], in_=S_[:, 2 * h, :], func=AF.Exp, bias=sm_[:, h, 1:2], scale=1.0),
                     reads=[bS, bsm], writes=[be0])
                P.op("dve", lambda e, h=h, top_=top_, sm_=sm_, thr_=thr_: e.tensor_scalar(out=thr_[:, h, :], in0=top_[:, 2 * h + 1, :], scalar1=-1.0, scalar2=sm_[:, h, 0:1], op0=ALU.mult, op1=ALU.add),
                     reads=[btop, bsm], writes=[bthr])
                P.op("act", lambda e, h=h, top_=top_, sm_=sm_, sc_=sc_: e.activation(out=sc_[:, h, :], in_=top_[:, 2 * h + 1, :], func=AF.Exp, bias=sm_[:, h, 2:3], scale=1.0),
                     reads=[btop, bsm], writes=[bsc])
                P.op("dve", lambda e, h=h, sm_=sm_, sc_=sc_: e.tensor_scalar(out=sc_[:, h, :], in0=sc_[:, h, :], scalar1=sm_[:, h, 4:5], scalar2=None, op0=ALU.mult),
                     reads=[bsc, bsm], writes=[bsc])
                if h % 2 == 1:
                    yield
            scb_, bscb = sc2b.next()
            P.op("dve", lambda e, scb_=scb_, sc_=sc_: e.tensor_copy(out=scb_[:], in_=sc_[:]), reads=[bsc], writes=[bscb])
            for ch in range(2):
                csl = slice(ch * 64, (ch + 1) * 64)
                Y_, bY = Ytm.next()
                for h in range(8):
                    yv = Y_[:, h * 16:(h + 1) * 16, :]
                    P.op("dve", lambda e, h=h, yv=yv, S_=S_, thr_=thr_, csl=csl: e.tensor_tensor(
                        out=yv, in0=S_[:, 2 * h, csl].rearrange("p (o n) -> p o n", o=1).to_broadcast([128, 16, 64]),
                        in1=thr_[:, h, :].rearrange("p (s o) -> p s o", o=1).to_broadcast([128, 16, 64]), op=ALU.is_ge),
                        reads=[bS, bthr], writes=[bY])
                    P.op("dve", lambda e, h=h, yv=yv, e0_=e0_, csl=csl: e.tensor_tensor(
                        out=yv, in0=yv, in1=e0_[:, h, csl].rearrange("p (o n) -> p o n", o=1).to_broadcast([128, 16, 64]), op=ALU.mult),
                        reads=[bY, be0], writes=[bY])
                    P.op("dve", lambda e, h=h, yv=yv, scb_=scb_: e.tensor_tensor(
                        out=yv, in0=yv, in1=scb_[:, h, :].rearrange("p (s o) -> p s o", o=1).to_broadcast([128, 16, 64]), op=ALU.mult),
                        reads=[bY, bscb], writes=[bY])
                    if h % 4 == 3:
                        yield
                Ys_, bYs = Ysm.next()
                for c4 in range(16):
                    pm_, bpm = pM.next()
                    pmb = pm_[:].bitcast(BF16)
                    for i in range(4):
                        cc = c4 * 4 + i
                        P.op("pe", lambda e, cc=cc, i=i, pmb=pmb, Y_=Y_: e.transpose(out=pmb[:, i * 128:(i + 1) * 128], in_=Y_[:, :, cc], identity=ident[:]),
                             reads=[bY, bid], writes=[bpm])
                    if c4 % 2 == 0:
                        P.op("act", lambda e, c4=c4, pmb=pmb, Ys_=Ys_: e.activation(out=Ys_[:, c4 * 4:(c4 + 1) * 4, :].rearrange("p a b -> p (a b)"), in_=pmb[:, 0:512], func=AF.Copy),
                             reads=[bpm], writes=[bYs])
                    else:
                        P.op("dve", lambda e, c4=c4, pmb=pmb, Ys_=Ys_: e.tensor_copy(out=Ys_[:, c4 * 4:(c4 + 1) * 4, :].rearrange("p a b -> p (a b)"), in_=pmb[:, 0:512]),
                             reads=[bpm], writes=[bYs])
                    if c4 % 4 == 3:
                        yield
                Ws_, bWs = Wst.next()
                for th in range(2):
                    X_, bX = Xsm.next()
                    for q2 in range(2):
                        P.op("dve", lambda e, q2=q2, th=th, X_=X_, jT_=jT_: e.tensor_tensor(
                            out=X_[:, q2 * 32:(q2 + 1) * 32, :],
                            in0=iota[:].rearrange("p (o n) -> p o n", o=1).to_broadcast([128, 32, 128]),
                            in1=jT_[:, th * 64 + q2 * 32:th * 64 + (q2 + 1) * 32].rearrange("p (s o) -> p s o", o=1).to_broadcast([128, 32, 128]), op=ALU.is_equal),
                            reads=[biota, bjT], writes=[bX])
                    for t8 in range(8):
                        pm_, bpm = pM.next()
                        for i in range(8):
                            tl = t8 * 8 + i
                            tk = th * 64 + tl
                            P.op("pe", lambda e, tk=tk, tl=tl, i=i, pm_=pm_, X_=X_, Ys_=Ys_: e.matmul(pm_[:, i * 64:(i + 1) * 64], lhsT=X_[:, tl, :], rhs=Ys_[:, :, tk], start=True, stop=True),
                                 reads=[bX, bYs], writes=[bpm])
                        tok0 = th * 64 + t8 * 8
                        src = pm_[:].rearrange("p (t g c) -> p g t c", t=8, g=4)
                        dst = Ws_[:, :, tok0:tok0 + 8, :]
                        if t8 % 2 == 0:
                            P.op("act", lambda e, src=src, dst=dst: e.activation(out=dst, in_=src, func=AF.Copy), reads=[bpm], writes=[bWs])
                        else:
                            P.op("dve", lambda e, src=src, dst=dst: e.tensor_copy(out=dst, in_=src), reads=[bpm], writes=[bWs])
                        if t8 % 4 == 3:
                            yield
                for g in range(4):
                    cg = ch * 4 + g
                    P.dma(("act", "pool")[g % 2], Wd[blk, cg, :, tt * 128 * 16:(tt + 1) * 128 * 16], Ws_[:, g, :, :].rearrange("p t c -> p (t c)"),
                          reads=[bWs], writes=[bWd[blk]], append=True)
                yield

    def drain(gen, n):
        if gen is None:
            return None
        for _ in range(n):
            try:
                next(gen)
            except StopIteration:
                return None
        return gen

    gen = sel_block(0)
    gen = drain(gen, 10 ** 9)
    for blk in range(nblk):
        u_, bu = ublocks[blk]
        nxt = sel_block(blk + 1) if blk + 1 < nblk else None
        LAG = 2
        stage = {}
        for c in range(NCH + LAG):
            if c < NCH:
                if c % 16 == 0:
                    w_, bw = wsl.next()
                    P.dma("sp", w_[:].rearrange("p t c -> p (t c)"), Wd[blk, c // 16], reads=[bWd[blk]], writes=[bw])
                uc, buc = uch.next(); vc, bvc = vch.next()
                P.dma("sp", uc[:].rearrange("p k n -> p (k n)"), Ub_d[c], reads=[bUb], writes=[buc])
                P.dma("sp", vc[:], Vb_d[c], reads=[bVb], writes=[bvc])
                ph, bph = pH.next()
                for kc in range(8):
                    P.op("pe", lambda e, kc=kc, ph=ph, uc=uc, u_=u_: e.matmul(ph[:, 0:TBLK], lhsT=uc[:, kc, :], rhs=u_[:, kc, :], start=(kc == 0), stop=(kc == 7)),
                         reads=[buc, bu], writes=[bph])
                g_, bg = gl.next()
                P.op("act", lambda e, ph=ph, g_=g_: e.activation(out=g_[:], in_=ph[:, 0:TBLK], func=AF.Gelu), reads=[bph], writes=[bg])
                z_, bz = zt.next()
                P.op("pool", lambda e, c=c, z_=z_, g_=g_, w_=w_: e.tensor_tensor(out=z_[:], in0=w_[:, :, c % 16], in1=g_[:], op=ALU.mult),
                     reads=[bw, bg], writes=[bz])
                stage[c] = (z_, bz, vc, bvc)
            if c >= LAG:
                cc = c - LAG
                z_, bz, vc, bvc = stage.pop(cc)
                for tt in range(NTL):
                    for hf in range(2):
                        k = tt * 2 + hf
                        P.op("pe", lambda e, cc=cc, tt=tt, hf=hf, k=k, z_=z_, vc=vc: e.matmul(pO[k][:], lhsT=z_[:, tt * 128:(tt + 1) * 128], rhs=vc[:, hf * 512:(hf + 1) * 512],
                                                                                  start=(cc == 0), stop=(cc == NCH - 1)), reads=[bz, bvc], writes=[bpO[k]])
            if c % 2 == 1:
                nxt = drain(nxt, 1)
        nxt = drain(nxt, 10 ** 9)
        for tt in range(NTL):
            tg = blk * NTL + tt
            xt, bx = xin.next()
            P.dma("sp", xt[:], D["xmid"][tg * 128:(tg + 1) * 128, :], writes=[bx])
            xo_, bxo = xout.next()
            emit_resid_ln(P, C, [pO[tt * 2], pO[tt * 2 + 1]], [bpO[tt * 2], bpO[tt * 2 + 1]], xt, bx, gb, bgb, lng, lnb, bln, epsb, beps, xo_, bxo, "p")
            P.dma("act", xo[tg * 128:(tg + 1) * 128, :], xo_[:], reads=[bxo])
    P.pop()


def k3_host_inputs(inp, l, core, xmid_core):
    b = core // 2
    rep = lambda v: np.ascontiguousarray(np.broadcast_to(np.asarray(v, np.float32)[None, :], (128, len(v))))
    keys = inp["peer_keys"][l].reshape(16, 128, 128)
    return {
        "xmid": np.ascontiguousarray(xmid_core),
        "cT": to_pc(inp["c"][b], 8),
        "adaw_f": np.ascontiguousarray(inp["ada_w"][l][:, 3072:6144]),
        "adab_f": to_pc(inp["ada_b"][l][3072:5120], 16),
        "adab_g": rep(inp["ada_b"][l][5120:6144]),
        "wq": np.ascontiguousarray(inp["peer_wq"][l]),
        "keysT": np.ascontiguousarray(keys.transpose(2, 0, 1)),
        "UT": np.ascontiguousarray(inp["peer_u"][l].T),
        "V": np.ascontiguousarray(inp["peer_v"][l]),
        "lng": rep(inp["ln2_g"][l]), "lnb": rep(inp["ln2_b"][l]),
        "ident": np.eye(128, dtype=np.float32),
        "iota": np.ascontiguousarray(np.broadcast_to(np.arange(128, dtype=np.float32)[None, :], (128, 128))),
    }


import math

PAIRS = [[0, 1], [2, 3], [4, 5], [6, 7]]

GATHER = [
    ("mla_kT", [768, T], 192, [0, 1, 2, 3]),
    ("mla_v", [T, 520], 1024, [0, 1, 2, 3]),
    ("diff_kT", [512, T], 128, [0, 1, 2, 3]),
    ("diff_v", [T, 516], 1024, [0, 1, 2, 3]),
    ("swa_kT", [128, T], 128, [0]),
    ("swa_v", [T, 130], 2048, [1, 0]),
    ("nat_kT", [512, T], 128, [0, 1, 2, 3]),
    ("nat_v", [T, 520], 1024, [0, 3]),
]

LAYER_IN = [("adaw", [1024, 6144]), ("adab1", [128, 16]), ("adabg", [128, 1024]), ("adabf", [128, 16]), ("adabg2", [128, 1024]),
            ("w1", None), ("qupA", [384, 768]), ("qupB", [384, 768]), ("kvn", [256, 512]), ("kvv", [256, 512]),
            ("gq", [128, 3]), ("gkv", [128, 2]), ("sinkb", [128, 8]), ("lamb", [128, 4, 64]), ("subln", [128, 128]), ("lami", [128, 2]),
            ("nat_bias", [8, 128, 29 * 128]), ("wg", [4, 1024, 1024]), ("wb", [4, 512, 1024]), ("wo", [1024, 1024]),
            ("lng1", [128, 1024]), ("lnb1", [128, 1024]), ("wq", [1024, 2048]), ("keysT", [128, 16, 128]),
            ("UT", [1024, 16384]), ("V", [16384, 1024]), ("lng2", [128, 1024]), ("lnb2", [128, 1024])]
COMMON_IN = [("x", [T, 1024]), ("cT", [128, 8]), ("C64", [128, T]), ("S64", [128, T]), ("CM", [128, T]), ("SM", [128, T]),
             ("ident", [128, 128]), ("iota", [128, 128]), ("swa_masks", [4, 128, 512])]


def build_fused(nc, nlayers=2, peer_blocks=NBLK, dbg=False):
    P = Prog(nc)
    units, w1cols = k1_weight_layout()
    NC1 = len(w1cols)
    I = {}
    for n, s in COMMON_IN:
        I[n] = nc.dram_tensor(n, list(s), F32, kind="ExternalInput").ap()
    for l in range(nlayers):
        for n, s in LAYER_IN:
            if n == "w1":
                s = [1024, NC1]
            I[f"{n}_{l}"] = nc.dram_tensor(f"{n}_{l}", list(s), F32, kind="ExternalInput").ap()
    out_d = nc.dram_tensor("out", [T, 1024], F32, kind="ExternalOutput").ap()
    x_cur = I["x"]
    for l in range(nlayers):
        L = lambda n: I[f"{n}_{l}"]
        it = lambda n, s, d=BF16: nc.dram_tensor(f"{n}_L{l}", list(s), d, kind="Internal").ap()
        O2 = {"uT": it("uT", [1024, T]), "swa_qT": it("swa_qT", [512, T]), "swa_kT": it("swa_kT", [128, T]),
              "diff_qT": it("diff_qT", [512, T]), "diff_kT": it("diff_kT", [512, T]), "nat_qT": it("nat_qT", [512, T]),
              "nat_kT": it("nat_kT", [512, T]), "mla_qT": it("mla_qT", [768, T]), "mla_kT": it("mla_kT", [768, T]),
              "swa_v": it("swa_v", [T, 130]), "diff_v": it("diff_v", [T, 516]), "nat_v": it("nat_v", [T, 520]), "mla_v": it("mla_v", [T, 520])}
        O = dict(O2)
        O["mla_qT"] = O2["mla_qT"].rearrange("(h d) t -> h d t", d=96)
        O["mla_kT"] = O2["mla_kT"].rearrange("(h d) t -> h d t", d=96)
        for n in ("swa_v", "diff_v", "nat_v", "mla_v"):
            O[n] = O2[n].rearrange("(t p) f -> t p f", p=128)
        D1 = {"x": x_cur, "cT": I["cT"], "adaw": L("adaw")[:, 0:2048], "adab": L("adab1"), "w1": L("w1"),
              "qupA": L("qupA"), "qupB": L("qupB"), "kvn": L("kvn"), "kvv": L("kvv"), "gq": L("gq"), "gkv": L("gkv"),
              "C64": I["C64"], "S64": I["S64"], "CM": I["CM"], "SM": I["SM"], "ident": I["ident"]}
        P.push()
        emit_k1(P, nc, D1, O)
        P.pop()
        G = {}
        for (name, shp, rc, chunks) in GATHER:
            G[name] = []
            if name == "swa_v":
                dst = it(f"g_{name}", [2 * T, 130])
                P.collective(O2[name], dst, PAIRS)
                G[name].append(dst)
                continue
            for k in chunks:
                dst = it(f"g_{name}{k}", [2 * rc, shp[1]])
                P.collective(O2[name][k * rc:(k + 1) * rc, :], dst, PAIRS)
                G[name].append(dst)
        P.barrier()
        oT_d = it("oT", [4, 512, T])
        D2 = {"swa_qT": O["swa_qT"], "swa_masks": I["swa_masks"], "sinkb": L("sinkb"), "mla_qT": O["mla_qT"],
              "diff_qT": O["diff_qT"], "lamb": L("lamb"), "subln": L("subln"), "lami": L("lami"),
              "nat_qT": O["nat_qT"], "nat_bias": L("nat_bias"), "ident": I["ident"]}
        P.push()
        pre = peer_scratch(nc, f"_L{l}")
        cf = Rot(P, "pa_cf", [128, 4096], F32, 2)
        cb = Rot(P, "pa_cb", [128, 4096], BF16, 2)
        bgen = peer_cast_gen(P, L("UT"), L("V"), pre[0], pre[1], pre[2], pre[3], cf, cb)
        emit_attention(P, D2, oT_d, SRC=GatherSrc(O, G), bg=bgen)
        for _ in bgen:
            pass
        P.pop()
        xmid_d = it("xmid", [T, 1024], F32)
        D3 = {"oT": oT_d, "uT": O["uT"], "x": x_cur, "wg": L("wg"), "wb": L("wb"), "wo": L("wo"), "cT": I["cT"],
              "adaw_g": L("adaw")[:, 2048:3072], "adab_g": L("adabg"), "lng": L("lng1"), "lnb": L("lnb1")}
        emit_merge(P, D3, xmid_d)
        xo = out_d if l == nlayers - 1 else it("xout", [T, 1024], F32)
        D4 = {"xmid": xmid_d, "cT": I["cT"], "adaw_f": L("adaw")[:, 3072:6144], "adab_f": L("adabf"), "adab_g": L("adabg2"),
              "wq": L("wq"), "keysT": L("keysT"), "UT": L("UT"), "V": L("V"), "lng": L("lng2"), "lnb": L("lnb2"),
              "ident": I["ident"], "iota": I["iota"]}
        emit_peer(P, nc, D4, xo, nblk=peer_blocks, tag=f"_L{l}", pre=pre)
        P.barrier()
        if dbg and l == 0:
            d1 = nc.dram_tensor("dbg_oT", [4, 512, T], BF16, kind="ExternalOutput").ap()
            d2 = nc.dram_tensor("dbg_xmid", [T, 1024], F32, kind="ExternalOutput").ap()
            d3 = nc.dram_tensor("dbg_uT", [1024, T], BF16, kind="ExternalOutput").ap()
            for n in range(4):
                P.dma("sp", d1[n], oT_d[n])
            P.dma("act", d2, xmid_d)
            P.dma("act", d3, O["uT"])
            P.barrier()
        x_cur = xo
    P.finish()
    P.emit()
    return P


def fused_host_inputs(inp, core, nlayers=2):
    b, half = core // 2, core % 2
    units, w1cols = k1_weight_layout()
    qa, qb, knope, vcols = mla_up_layout()
    C64, S64, CM, SM = rope_tables(half * T, T)
    rep = lambda v: np.ascontiguousarray(np.broadcast_to(np.asarray(v, np.float32)[None, :], (128, len(v))))
    m = {
        "x": np.ascontiguousarray(inp["x"][b, half * T:(half + 1) * T], dtype=np.float32),
        "cT": to_pc(inp["c"][b], 8), "C64": C64, "S64": S64, "CM": CM, "SM": SM,
        "ident": np.eye(128, dtype=np.float32),
        "iota": np.ascontiguousarray(np.broadcast_to(np.arange(128, dtype=np.float32)[None, :], (128, 128))),
        "swa_masks": swa_masks(half),
    }
    for l in range(nlayers):
        ab = inp["ada_b"][l]
        lam = np.stack([inp["diff_lambda_q1"][l], inp["diff_lambda_k1"][l], inp["diff_lambda_q2"][l], inp["diff_lambda_k2"][l]])
        li = 0.8 - 0.6 * math.exp(-0.3 * l)
        keys = inp["peer_keys"][l].reshape(16, 128, 128)
        d = {
            "adaw": np.ascontiguousarray(inp["ada_w"][l]), "adab1": to_pc(ab[0:2048], 16), "adabg": rep(ab[2048:3072]),
            "adabf": to_pc(ab[3072:5120], 16), "adabg2": rep(ab[5120:6144]),
            "w1": np.ascontiguousarray(inp["w_in"][l][:, w1cols]),
            "qupA": np.ascontiguousarray(inp["mla_q_up"][l][:, qa]), "qupB": np.ascontiguousarray(inp["mla_q_up"][l][:, qb]),
            "kvn": np.ascontiguousarray(inp["mla_kv_up"][l][:, knope]), "kvv": np.ascontiguousarray(inp["mla_kv_up"][l][:, vcols]),
            "gq": to_pc(inp["mla_q_norm"][l], 3), "gkv": to_pc(inp["mla_kv_norm"][l], 2),
            "sinkb": rep(inp["swa_sink"][l]),
            "lamb": np.ascontiguousarray(np.broadcast_to(lam[None], (128, 4, 64))).astype(np.float32),
            "subln": rep(inp["diff_subln"][l]),
            "lami": np.ascontiguousarray(np.broadcast_to(np.array([li, 1.0 - li], np.float32)[None], (128, 2))),
            "nat_bias": nat_bias_tables(inp["nat_rpb"][l], half),
            "wg": np.ascontiguousarray(inp["w_gate"][l]), "wb": np.ascontiguousarray(inp["w_branch"][l]), "wo": np.ascontiguousarray(inp["w_out"][l]),
            "lng1": rep(inp["ln1_g"][l]), "lnb1": rep(inp["ln1_b"][l]),
            "wq": np.ascontiguousarray(inp["peer_wq"][l]), "keysT": np.ascontiguousarray(keys.transpose(2, 0, 1)),
            "UT": np.ascontiguousarray(inp["peer_u"][l].T), "V": np.ascontiguousarray(inp["peer_v"][l]),
            "lng2": rep(inp["ln2_g"][l]), "lnb2": rep(inp["ln2_b"][l]),
        }
        for k, v in d.items():
            m[f"{k}_{l}"] = np.ascontiguousarray(v, dtype=np.float32)
    return m


NCORES = 8
_PROGS = {}


def kernel(**inputs):
    inp = {k: np.asarray(v) for k, v in inputs.items()}
    if "fused" not in _PROGS:
        nc = bass.Bass("TRN2", target_bir_lowering=False)
        build_fused(nc)
        _PROGS["fused"] = nc
    nc = _PROGS["fused"]
    maps = [fused_host_inputs(inp, c) for c in range(NCORES)]
    res = run_bass_kernel_spmd(nc, maps, core_ids=list(range(NCORES)))
    out = np.empty((4, 2 * T, 1024), np.float32)
    for c in range(NCORES):
        b, half = c // 2, c % 2
        out[b, half * T:(half + 1) * T] = np.asarray(res.results[c]["out"])
    return out
```

```python
import numpy as np
import concourse.bass as bass
import concourse.mybir as mybir
from concourse.bass_utils import run_bass_kernel_spmd
from contextlib import ExitStack

F32 = mybir.dt.float32
BF16 = mybir.dt.bfloat16
I32 = mybir.dt.int32
U32 = mybir.dt.uint32
AF = mybir.ActivationFunctionType
ALU = mybir.AluOpType
AX = mybir.AxisListType

ENGS = ["pe", "act", "dve", "pool", "sp"]
DMA_RING = 12


class Buf:
    __slots__ = ("name", "lastw", "readers", "excl", "pre")

    def __init__(self, name="", excl=False):
        self.name = name
        self.excl = excl
        self.pre = []
        self.lastw = []
        self.readers = []


class Prog:
    def __init__(self, nc, same_engine_sync=True):
        self.nc = nc
        self.es = ExitStack()
        self.stack = [self.es]
        self.q = {e: [] for e in ENGS}
        self.cnt = {e: 0 for e in ENGS}
        self.sems = {}
        self.EPOCH = 50000
        self.ring_n = {e: 0 for e in ENGS}
        self.ring_val = {}
        self.seen = {e: {} for e in ENGS}
        self.same_engine_sync = same_engine_sync
        self.n_waits = 0
        self.n_ops = 0
        self.uid = 0
        self.E = {"pe": nc.tensor, "act": nc.scalar, "dve": nc.vector, "pool": nc.gpsimd, "sp": nc.sync}

    def sb(self, name, shape, dtype):
        self.uid += 1
        return self.stack[-1].enter_context(self.nc.sbuf_tensor(f"s{self.uid}_{name}", list(shape), dtype))

    def ps(self, name, shape, dtype=F32):
        self.uid += 1
        return self.stack[-1].enter_context(self.nc.psum_tensor(f"p{self.uid}_{name}", list(shape), dtype))

    def push(self):
        self.stack.append(ExitStack())

    def pop(self):
        self.barrier()
        self.stack.pop().close()

    def barrier(self):
        evs = [self._last_ev(e) for e in ENGS if self.cnt[e] > 0]
        evs += [(k, v) for k, v in self.ring_val.items() if v > 0]
        for e in ENGS:
            for ev in evs:
                self._wait(e, ev)

    def dram(self, name, shape, dtype, kind="Internal"):
        return self.nc.dram_tensor(name, list(shape), dtype, kind=kind).ap()

    def _wait(self, eng, ev):
        key, val = ev
        if key[0] == "eng" and key[1] == eng and (eng == "pe" or not self.same_engine_sync):
            return
        if self.seen[eng].get(key, 0) >= val:
            return
        self.seen[eng][key] = val
        self.E[eng].wait_ge(self.sems[key], val)
        self.n_waits += 1

    def _deps(self, eng, reads, writes):
        for b in reads:
            for ev in b.lastw:
                self._wait(eng, ev)
            if b.excl:
                for ev in b.readers:
                    self._wait(eng, ev)
        for b in writes:
            for ev in b.lastw:
                self._wait(eng, ev)
            for ev in b.readers:
                self._wait(eng, ev)

    def _commit(self, ev, reads, writes):
        for b in writes:
            b.pre = list(b.lastw) + list(b.readers)
            b.lastw = [ev]
            b.readers = []
        for b in reads:
            b.readers = [r for r in b.readers if r[0] != ev[0]] + [ev]

    def _next_ev(self, eng):
        ep, k = divmod(self.cnt[eng], self.EPOCH)
        self.cnt[eng] += 1
        key = ("eng", eng, ep)
        if key not in self.sems:
            self.sems[key] = self.nc.alloc_semaphore(name=f"pg_{eng}_{ep}")
        return (key, k + 1)

    def _last_ev(self, eng):
        if self.cnt[eng] == 0:
            return None
        ep, k = divmod(self.cnt[eng] - 1, self.EPOCH)
        return (("eng", eng, ep), k + 1)

    def collective(self, ins_ap, outs_ap, groups, reads=(), writes=()):
        eng = "pool"
        self._deps(eng, reads, writes)
        k = self.ring_n.get("cc", 0) % 4
        self.ring_n["cc"] = self.ring_n.get("cc", 0) + 1
        key = ("ring", "cc", k)
        if key not in self.sems:
            self.sems[key] = self.nc.alloc_semaphore(name=f"pg_cc_{k}")
            self.ring_val[key] = 0
        prev = self.ring_val[key]
        if prev > 0:
            self._wait(eng, (key, prev))
        val = prev + 1
        self.ring_val[key] = val
        ev = (key, val)
        self.E[eng].collective_compute("AllGather", ALU.bypass, replica_groups=groups, ins=[ins_ap], outs=[outs_ap]).then_inc(self.sems[key])
        self.n_ops += 1
        self._commit(ev, reads, writes)
        return ev

    def op(self, eng, fn, reads=(), writes=()):
        self._deps(eng, reads, writes)
        ev = self._next_ev(eng)
        ins = fn(self.E[eng])
        ins.then_inc(self.sems[ev[0]], 1)
        self.n_ops += 1
        self._commit(ev, reads, writes)
        return ev

    def dma(self, eng, out, in_, reads=(), writes=(), append=False, **kw):
        if append:
            self._deps(eng, reads, ())
            for b in writes:
                for ev in b.pre:
                    self._wait(eng, ev)
                for ev in b.readers:
                    self._wait(eng, ev)
        else:
            self._deps(eng, reads, writes)
        k = self.ring_n[eng] % DMA_RING
        self.ring_n[eng] += 1
        key = ("ring", eng, k)
        if key not in self.sems:
            self.sems[key] = self.nc.alloc_semaphore(name=f"pg_r_{eng}_{k}")
            self.ring_val[key] = 0
        prev = self.ring_val[key]
        if prev > 0:
            self._wait(eng, (key, prev))
        val = prev + 16
        self.ring_val[key] = val
        ev = (key, val)
        self.E[eng].dma_start(out=out, in_=in_, **kw).then_inc(self.sems[key], 16)
        self.n_ops += 1
        if append:
            for b in writes:
                b.lastw = b.lastw + [ev]
            for b in reads:
                b.readers = [r for r in b.readers if r[0] != ev[0]] + [ev]
        else:
            self._commit(ev, reads, writes)
        return ev

    def finish(self):
        for key, val in list(self.ring_val.items()):
            if val > 0:
                self._wait("sp", (key, val))
        for e in ENGS:
            if e != "sp" and self.cnt[e] > 0:
                self._wait("sp", self._last_ev(e))

    def emit(self):
        self.es.close()


T = 4096
NTB = 8
NTT = 32

O_QA, O_KA, O_VA, O_CQ, O_CKV, O_KR, O_QC, O_KC, O_VC, O_QD, O_KD, O_VD = (
    0, 512, 640, 768, 1152, 1408, 1440, 1952, 2464, 2976, 3488, 4000)


class Rot:
    def __init__(self, P, name, shape, dtype, n, space="sb"):
        mk = P.sb if space == "sb" else P.ps
        self.tiles = [mk(f"{name}{i}", shape, dtype) for i in range(n)]
        self.bufs = [Buf(f"{name}{i}", excl=(space == "ps")) for i in range(n)]
        self.i = 0
        self.n = n

    def next(self):
        t, b = self.tiles[self.i % self.n], self.bufs[self.i % self.n]
        self.i += 1
        return t, b


def k1_weight_layout():
    def rot64(cols):
        cols = np.asarray(cols).reshape(-1, 64)
        return np.concatenate([cols[:, 32:], cols[:, :32]], axis=1).reshape(-1)

    units = []
    cols = []

    def add(name, kind, c):
        units.append((name, kind, sum(len(x) for x in cols), len(c)))
        cols.append(np.asarray(c))

    for name, off, n in (("swa_q", O_QA, 512), ("swa_k", O_KA, 128), ("diff_q", O_QC, 512), ("diff_k", O_KC, 512)):
        for ch in range(n // 128):
            a = np.arange(off + ch * 128, off + (ch + 1) * 128)
            add(f"{name}{ch}", "rope", np.concatenate([a, rot64(a)]))
    for name, off in (("nat_q", O_QD), ("nat_k", O_KD)):
        for ch in range(4):
            add(f"{name}{ch}", "plain", np.arange(off + ch * 128, off + (ch + 1) * 128))
    kr = np.arange(O_KR, O_KR + 32)
    krB = np.concatenate([kr[16:], kr[:16]])
    pad = np.arange(O_CQ, O_CQ + 64)
    add("mla", "mla", np.concatenate([np.arange(O_CQ, O_CQ + 384), np.arange(O_CKV, O_CKV + 256),
                                      pad, kr, pad, krB]))
    add("swa_v", "v", np.arange(O_VA, O_VA + 128))
    add("diff_v", "v", np.arange(O_VC, O_VC + 512))
    add("nat_v", "v", np.arange(O_VD, O_VD + 512))
    return units, np.concatenate(cols)


def mla_up_layout():
    qa = np.arange(768)
    qb = qa.copy().reshape(8, 96)
    qb = np.concatenate([qb[:, :64], qb[:, 80:96], qb[:, 64:80]], axis=1).reshape(-1)
    kv = np.arange(1024).reshape(8, 128)
    knope = kv[:, :64].reshape(-1)
    vcols = kv[:, 64:].reshape(-1)
    return qa, qb, knope, vcols


def rope_tables(pos0, n):
    pos = np.arange(pos0, pos0 + n, dtype=np.float32)
    inv64 = (10000.0 ** (-np.arange(0, 64, 2, dtype=np.float32) / 64)).astype(np.float32)
    ang = (pos[None, :] * inv64[:, None]).astype(np.float32)
    c, s = np.cos(ang).astype(np.float32), np.sin(ang).astype(np.float32)
    C64 = np.concatenate([c, c, c, c], 0)
    S64 = np.concatenate([-s, s, -s, s], 0)
    inv32 = (10000.0 ** (-np.arange(0, 32, 2, dtype=np.float32) / 32)).astype(np.float32)
    ang = (pos[None, :] * inv32[:, None]).astype(np.float32)
    c, s = np.cos(ang).astype(np.float32), np.sin(ang).astype(np.float32)
    CM = np.zeros((128, n), np.float32)
    SM = np.zeros((128, n), np.float32)
    CM[64:96] = np.concatenate([c, c], 0)
    SM[64:96] = np.concatenate([-s, s], 0)
    return C64, S64, CM, SM


def emit_mod_cols(P, nc, cT_d, adaw_d, adab_d, ngrp, modc, bmodc, pm_rot, wst_rot):
    cs = P.sb("mod_cs", [128, 8], F32)
    bcs = Buf("cs")
    adab = P.sb("mod_adab", [128, ngrp * 8], F32)
    badab = Buf("adab")
    P.dma("sp", cs[:], cT_d, writes=[bcs])
    P.dma("sp", adab[:], adab_d, writes=[badab])
    P.op("act", lambda e: e.activation(out=cs[:], in_=cs[:], func=AF.Silu), reads=[bcs], writes=[bcs])
    pm, bpm = pm_rot.next()
    noc = ngrp * 8
    for g in range(noc // 2):
        wst_, bw = wst_rot.next()
        wst = wst_[:, 0:2048].rearrange("p (c n) -> p c n", c=8)
        P.dma("sp" if g % 2 == 0 else "act", wst,
              adaw_d[:, g * 256:(g + 1) * 256].rearrange("(c p) n -> p c n", p=128), writes=[bw])
        for o4 in range(2):
            oc = g * 2 + o4
            for kc in range(8):
                P.op("pe", lambda e, oc=oc, kc=kc, o4=o4, wst=wst: e.matmul(
                    pm[:, oc:oc + 1], lhsT=wst[:, kc, o4 * 128:(o4 + 1) * 128], rhs=cs[:, kc:kc + 1],
                    start=(kc == 0), stop=(kc == 7)), reads=[bw, bcs], writes=[bpm])
    P.op("dve", lambda e: e.tensor_tensor(out=modc[:, 0:noc], in0=pm[:, 0:noc], in1=adab[:], op=ALU.add),
         reads=[bpm, badab], writes=[bmodc])


def emit_ln_mod(P, x_d, uT, buT_tiles, shcol, sc1col, bmod, ident, bid, epsb, beps, ptr_rot, tag=""):
    xin = Rot(P, f"ln_x{tag}", [128, 1024], F32, 2)
    xn = Rot(P, f"ln_xn{tag}", [128, 1024], BF16, 2)
    st = Rot(P, f"ln_st{tag}", [128, 2, 6], F32, 2)
    mv = Rot(P, f"ln_mv{tag}", [128, 2], F32, 2)
    rs = Rot(P, f"ln_rs{tag}", [128, 1], F32, 2)
    for t in range(NTT):
        xt, bx = xin.next()
        P.dma("sp", xt[:], x_d[t * 128:(t + 1) * 128, :], writes=[bx])
        s_, bs = st.next()
        for c in range(2):
            P.op("dve", lambda e, c=c, s_=s_, xt=xt: e.bn_stats(out=s_[:, c, :], in_=xt[:, c * 512:(c + 1) * 512]),
                 reads=[bx], writes=[bs])
        m_, bm = mv.next()
        P.op("dve", lambda e, m_=m_, s_=s_: e.bn_aggr(out=m_[:], in_=s_[:]), reads=[bs], writes=[bm])
        r_, br = rs.next()
        P.op("act", lambda e, r_=r_, m_=m_: e.activation(out=r_[:], in_=m_[:, 1:2], func=AF.Ln, bias=epsb[:, 0:1], scale=1.0),
             reads=[bm, beps], writes=[br])
        P.op("act", lambda e, r_=r_: e.activation(out=r_[:], in_=r_[:], func=AF.Exp, scale=-0.5), reads=[br], writes=[br])
        xn_, bxn = xn.next()
        P.op("dve", lambda e, xn_=xn_, xt=xt, m_=m_, r_=r_: e.tensor_scalar(
            out=xn_[:], in0=xt[:], scalar1=m_[:, 0:1], scalar2=r_[:, 0:1], op0=ALU.subtract, op1=ALU.mult),
            reads=[bx, bm, br], writes=[bxn])
        pt, bpt = ptr_rot.next()
        for c in range(8):
            P.op("pe", lambda e, c=c, pt=pt, xn_=xn_: e.transpose(out=pt[:, c, :], in_=xn_[:, c * 128:(c + 1) * 128], identity=ident[:]),
                 reads=[bxn, bid], writes=[bpt])
        for c in range(8):
            P.op("act", lambda e, c=c, pt=pt, t=t: e.activation(
                out=uT[:, c, t * 128:(t + 1) * 128], in_=pt[:, c, :], func=AF.Identity,
                scale=sc1col[:, c:c + 1], bias=shcol[:, c:c + 1]), reads=[bpt, bmod], writes=[buT_tiles[t]])


def k1_io():
    units, w1cols = k1_weight_layout()
    NC1 = len(w1cols)
    ins = [("x", [T, 1024]), ("cT", [128, 8]), ("adaw", [1024, 2048]), ("adab", [128, 16]), ("w1", [1024, NC1]),
           ("qupA", [384, 768]), ("qupB", [384, 768]), ("kvn", [256, 512]), ("kvv", [256, 512]), ("gq", [128, 3]), ("gkv", [128, 2]),
           ("C64", [128, T]), ("S64", [128, T]), ("CM", [128, T]), ("SM", [128, T]), ("ident", [128, 128])]
    outs = [("uT", [1024, T]), ("swa_qT", [512, T]), ("swa_kT", [128, T]), ("diff_qT", [512, T]), ("diff_kT", [512, T]),
            ("nat_qT", [512, T]), ("nat_kT", [512, T]), ("mla_qT", [8, 96, T]), ("mla_kT", [8, 96, T]),
            ("swa_v", [NTT, 128, 2 * 65]), ("diff_v", [NTT, 128, 4 * 129]), ("nat_v", [NTT, 128, 8 * 65]), ("mla_v", [NTT, 128, 8 * 65])]
    return ins, outs


def build_k1(nc, stop=None):
    P = Prog(nc)
    ins, outs = k1_io()
    D = {n: nc.dram_tensor(n, list(s), F32, kind="ExternalInput").ap() for n, s in ins}
    O = {n: nc.dram_tensor(n, list(s), BF16, kind="ExternalOutput").ap() for n, s in outs}
    emit_k1(P, nc, D, O, stop)
    P.finish()
    P.emit()
    return P


def emit_k1(P, nc, D, O, stop=None):
    units, w1cols = k1_weight_layout()
    x_d, cT_d, adaw_d, adab_d, w1_d = D["x"], D["cT"], D["adaw"], D["adab"], D["w1"]
    qupA_d, qupB_d, kvn_d, kvv_d, gq_d, gkv_d = D["qupA"], D["qupB"], D["kvn"], D["kvv"], D["gq"], D["gkv"]
    C64_d, S64_d, CM_d, SM_d, ident_d = D["C64"], D["S64"], D["CM"], D["SM"], D["ident"]
    o_uT = O["uT"]
    o = {"swa_q": O["swa_qT"], "swa_k": O["swa_kT"], "diff_q": O["diff_qT"], "diff_k": O["diff_kT"],
         "nat_q": O["nat_qT"], "nat_k": O["nat_kT"], "mla_q": O["mla_qT"], "mla_k": O["mla_kT"],
         "swa_v": O["swa_v"], "diff_v": O["diff_v"], "nat_v": O["nat_v"], "mla_v": O["mla_v"]}

    identf = P.sb("identf", [128, 128], F32); bidf = Buf()
    ident = P.sb("ident", [128, 128], BF16); bid = Buf()
    onesb = P.sb("onesb", [128, 128], BF16); bones = Buf()
    epsb = P.sb("epsb", [128, 1], F32); beps = Buf()
    P.dma("pool", identf[:], ident_d, writes=[bidf])
    P.op("dve", lambda e: e.tensor_copy(out=ident[:], in_=identf[:]), reads=[bidf], writes=[bid])
    P.op("pool", lambda e: e.memset(onesb[:], 1.0), writes=[bones])
    P.op("pool", lambda e: e.memset(epsb[:], 1e-5), writes=[beps])

    uT = P.sb("uT", [128, 8, T], BF16)
    buT = [Buf(f"uT{t}") for t in range(NTT)]
    modc = P.sb("modc", [128, 16], F32); bmodc = Buf()
    sc1 = P.sb("sc1", [128, 8], F32); bsc1 = Buf()

    pA = Rot(P, "pA", [128, 512], F32, 2, "ps")
    pB = Rot(P, "pB", [128, 512], F32, 2, "ps")
    ptr = Rot(P, "ptr", [128, 8, 128], BF16, 2, "ps")
    pmisc = Rot(P, "pmisc", [128, 512], F32, 1, "ps")

    wst = Rot(P, "wst", [128, 2048], F32, 2)
    wbf = Rot(P, "wbf", [128, 8, 256], BF16, 2)

    emit_mod_cols(P, nc, cT_d, adaw_d, adab_d, 2, modc, bmodc, pmisc, wst)
    P.op("dve", lambda e: e.tensor_scalar(out=sc1[:], in0=modc[:, 8:16], scalar1=1.0, scalar2=None, op0=ALU.add),
         reads=[bmodc], writes=[bsc1])
    bmod = Buf("mod")
    P.op("dve", lambda e: e.tensor_copy(out=modc[:, 0:8], in_=modc[:, 0:8]), reads=[bmodc, bsc1], writes=[bmod])

    if stop == "mod":
        return
    emit_ln_mod(P, x_d, uT, buT, modc, sc1, bmod, ident, bid, epsb, beps, ptr)
    if stop == "ln":
        return

    for c in range(8):
        P.dma("pool", o_uT[c * 128:(c + 1) * 128, :], uT[:, c, :], reads=buT)

    c64r = Rot(P, "c64r", [128, 512], F32, 2)
    s64r = Rot(P, "s64r", [128, 512], F32, 2)

    t1r = Rot(P, "t1r", [128, 512], F32, 2)
    t2r = Rot(P, "t2r", [128, 512], F32, 2)
    ostg = Rot(P, "ostg", [128, 512], BF16, 3)

    def load_w(col0, ncols):
        ws_, bws = wst.next()
        wb, bwb = wbf.next()
        ws = ws_[:, 0:8 * ncols].rearrange("p (c n) -> p c n", c=8)
        h = ncols // 2
        P.dma("sp", ws[:, :, 0:h], w1_d[:, col0:col0 + h].rearrange("(c p) n -> p c n", p=128), writes=[bws])
        P.dma("sp", ws[:, :, h:ncols], w1_d[:, col0 + h:col0 + ncols].rearrange("(c p) n -> p c n", p=128),
              writes=[bws], append=True)
        P.op("pool", lambda e: e.tensor_copy(out=wb[:, :, 0:ncols], in_=ws), reads=[bws], writes=[bwb])
        return wb, bwb

    def mm_fm(ps, bps, wb, bwb, c0, M, tb):
        for kc in range(8):
            P.op("pe", lambda e, kc=kc: e.matmul(ps[0:M, :], lhsT=wb[:, kc, c0:c0 + M], rhs=uT[:, kc, tb * 512:(tb + 1) * 512],
                                               start=(kc == 0), stop=(kc == 7)),
                 reads=[bwb] + buT[tb * 4:(tb + 1) * 4], writes=[bps])

    dq = ["act", "pool"]
    dqi = [0]

    def nextq():
        dqi[0] += 1
        return dq[dqi[0] % 2]

    for (name, kind, col0, ncols) in units:
        if stop is not None and stop == name:
            break
        if kind == "rope":
            base = name[:-1]; ch = int(name[-1])
            wb, bwb = load_w(col0, 256)
            for tb in range(NTB):
                a, ba = pA.next(); b, bb = pB.next()
                mm_fm(a, ba, wb, bwb, 0, 128, tb)
                mm_fm(b, bb, wb, bwb, 128, 128, tb)
                t1, bt1 = t1r.next(); t2, bt2 = t2r.next()
                sl = slice(tb * 512, (tb + 1) * 512)
                C64, bc64 = c64r.next(); S64, bs64 = s64r.next()
                P.dma("sp", C64[:], C64_d[:, sl], writes=[bc64])
                P.dma("sp", S64[:], S64_d[:, sl], writes=[bs64])
                P.op("dve", lambda e, t1=t1, a=a, C64=C64: e.tensor_tensor(out=t1[:], in0=a[:], in1=C64[:], op=ALU.mult),
                     reads=[ba, bc64], writes=[bt1])
                P.op("dve", lambda e, t2=t2, b=b, S64=S64: e.tensor_tensor(out=t2[:], in0=b[:], in1=S64[:], op=ALU.mult),
                     reads=[bb, bs64], writes=[bt2])
                og, bog = ostg.next()
                P.op("pool", lambda e, og=og, t1=t1, t2=t2: e.tensor_tensor(out=og[:], in0=t1[:], in1=t2[:], op=ALU.add),
                     reads=[bt1, bt2], writes=[bog])
                P.dma(nextq(), o[base][ch * 128:(ch + 1) * 128, sl], og[:], reads=[bog])
        elif kind == "plain":
            base = name[:-1]; ch = int(name[-1])
            wb, bwb = load_w(col0, 128)
            scale = 0.125 if base == "nat_q" else 1.0
            for tb in range(NTB):
                a, ba = pA.next()
                mm_fm(a, ba, wb, bwb, 0, 128, tb)
                og, bog = ostg.next()
                sl = slice(tb * 512, (tb + 1) * 512)
                P.op("act", lambda e, og=og, a=a, scale=scale: e.activation(out=og[:], in_=a[:], func=AF.Copy, scale=scale),
                     reads=[ba], writes=[bog])
                P.dma(nextq(), o[base][ch * 128:(ch + 1) * 128, sl], og[:], reads=[bog])
        elif kind == "v":
            H, dv = {"swa_v": (2, 64), "diff_v": (4, 128), "nat_v": (8, 64)}[name]
            vst = Rot(P, f"vst_{name}", [128, H, dv + 1], BF16, 2)
            for vt, vb in zip(vst.tiles, vst.bufs):
                P.op("pool", lambda e, vt=vt: e.memset(vt[:], 1.0), writes=[vb])
            nsub = max(1, ncols // 256)
            sc = ncols // nsub
            hs = H // nsub
            wbs = [load_w(col0 + i * sc, sc) for i in range(nsub)] if nsub <= 2 else None
            assert wbs is not None
            for t in range(NTT):
                vt, vb = vst.next()
                for i in range(nsub):
                    wb, bwb = wbs[i]
                    a, ba = pA.next()
                    for kc in range(8):
                        P.op("pe", lambda e, kc=kc, a=a, t=t, wb=wb: e.matmul(a[:, 0:sc], lhsT=uT[:, kc, t * 128:(t + 1) * 128],
                                                                     rhs=wb[:, kc, 0:sc], start=(kc == 0), stop=(kc == 7)),
                             reads=[bwb, buT[t]], writes=[ba])
                    P.op("act", lambda e, vt=vt, a=a, i=i: e.activation(
                        out=vt[:, i * hs:(i + 1) * hs, 0:dv], in_=a[:, 0:sc].rearrange("p (h d) -> p h d", h=hs), func=AF.Copy),
                        reads=[ba], writes=[vb])
                P.dma(nextq(), o[name][t], vt[:].rearrange("p h d -> p (h d)"), reads=[vb])
        elif kind == "mla":
            emit_mla(P, locals())


def emit_mla(P, L):
    nc = P.nc
    (w1_d, uT, buT, pA, pB, pmisc, o, onesb, bones, epsb, beps, qupA_d, qupB_d, kvn_d, kvv_d, gq_d, gkv_d,
     CM_d, SM_d, col0, nextq, ostg) = (L[k] for k in (
         "w1_d", "uT", "buT", "pA", "pB", "pmisc", "o", "onesb", "bones", "epsb", "beps", "qupA_d", "qupB_d",
         "kvn_d", "kvv_d", "gq_d", "gkv_d", "CM_d", "SM_d", "col0", "nextq", "ostg"))
    L = dict(L)
    NCM = 384 + 256 + 96 + 96
    wst = L["wst"]
    wm = P.sb("mla_w", [128, 8, NCM], BF16); bwm = Buf()
    first = True
    for i in range(4):
        ws_, bws = wst.next()
        ws = ws_[:, 0:8 * 208].rearrange("p (c n) -> p c n", c=8)
        P.dma("sp" if i % 2 == 0 else "act", ws, w1_d[:, col0 + i * 208:col0 + (i + 1) * 208].rearrange("(c p) n -> p c n", p=128), writes=[bws])
        P.op("pool", lambda e, ws=ws, i=i: e.tensor_copy(out=wm[:, :, i * 208:(i + 1) * 208], in_=ws), reads=[bws], writes=[bwm])
    qA = P.sb("mla_qA", [128, 3, 768], BF16); bqA = Buf()
    qB = P.sb("mla_qB", [128, 3, 768], BF16); bqB = Buf()
    kvn = P.sb("mla_kvn", [128, 2, 512], BF16); bkvn = Buf()
    kvv = P.sb("mla_kvv", [128, 2, 512], BF16); bkvv = Buf()
    for (src, dst, bdst, nch, ncol) in ((qupA_d, qA, bqA, 3, 768), (qupB_d, qB, bqB, 3, 768)):
        for hh in range(2):
            ws_, bws = wst.next()
            ws = ws_[:, 0:nch * 384].rearrange("p (c n) -> p c n", c=nch)
            P.dma("sp", ws, src[:, hh * 384:(hh + 1) * 384].rearrange("(c p) n -> p c n", p=128), writes=[bws])
            P.op("pool", lambda e, ws=ws, dst=dst, hh=hh: e.tensor_copy(out=dst[:, :, hh * 384:(hh + 1) * 384], in_=ws), reads=[bws], writes=[bdst])
    for (src, dst, bdst) in ((kvn_d, kvn, bkvn), (kvv_d, kvv, bkvv)):
        ws_, bws = wst.next()
        ws = ws_[:, 0:1024].rearrange("p (c n) -> p c n", c=2)
        P.dma("act", ws, src.rearrange("(c p) n -> p c n", p=128), writes=[bws])
        P.op("pool", lambda e, ws=ws, dst=dst: e.tensor_copy(out=dst[:], in_=ws), reads=[bws], writes=[bdst])
    gq = P.sb("mla_gq", [128, 3], F32); gkv = P.sb("mla_gkv", [128, 2], F32); bg = Buf()
    P.dma("pool", gq[:], gq_d, writes=[bg])
    P.dma("pool", gkv[:], gkv_d, writes=[bg], append=True)
    epsq = P.sb("mla_epsq", [128, 1], F32)
    cg = Rot(P, "mla_cg", [128, 5, 512], BF16, 1)
    sq = Rot(P, "mla_sq", [128, 5, 512], BF16, 1)
    rq = Rot(P, "mla_rq", [128, 512], F32, 1)
    rkv = Rot(P, "mla_rkv", [128, 512], F32, 1)
    rkvt = Rot(P, "mla_rkvt", [128, 4], F32, 2)
    cmt = Rot(P, "mla_cm", [128, 512], F32, 1); smt = Rot(P, "mla_sm", [128, 512], F32, 1)
    cr = Rot(P, "mla_cr", [128, 512], F32, 1); sr = Rot(P, "mla_sr", [128, 512], F32, 1)
    tA = Rot(P, "mla_tA", [128, 512], F32, 1); tB = Rot(P, "mla_tB", [128, 512], F32, 1)
    krs = Rot(P, "mla_krs", [128, 512], BF16, 2)
    vst = Rot(P, "mla_vst", [128, 8, 65], BF16, 2)
    for vt, vb in zip(vst.tiles, vst.bufs):
        P.op("pool", lambda e, vt=vt: e.memset(vt[:], 1.0), writes=[vb])

    import os
    MS = int(os.environ.get("MLASTOP", "99"))
    for tb in range(NTB):
        sl = slice(tb * 512, (tb + 1) * 512)
        ubufs = buT[tb * 4:(tb + 1) * 4]
        cg_, bcg = cg.next(); sq_, bsq = sq.next()
        if MS <= 0: continue
        for j in range(5):
            a, ba = pA.next()
            for kc in range(8):
                P.op("pe", lambda e, kc=kc, a=a, j=j: e.matmul(a[:], lhsT=wm[:, kc, j * 128:(j + 1) * 128], rhs=uT[:, kc, sl],
                                                            start=(kc == 0), stop=(kc == 7)), reads=[bwm] + ubufs, writes=[ba])
            gcol = gq[:, j:j + 1] if j < 3 else gkv[:, j - 3:j - 2]
            VAR = os.environ.get("MLAVAR", "AB")
            if "A" in VAR:
                P.op("act", lambda e, a=a, j=j, sq_=sq_: e.activation(out=sq_[:, j, :], in_=a[:], func=AF.Square), reads=[ba], writes=[bsq])
            if "B" in VAR:
              P.op("dve", lambda e, a=a, j=j, cg_=cg_, gcol=gcol: e.tensor_scalar(out=cg_[:, j, :], in0=a[:], scalar1=gcol, scalar2=None, op0=ALU.mult),
                 reads=[ba, bg], writes=[bcg])
        if MS <= 1: continue
        rq_, brq = rq.next(); rkv_, brkv = rkv.next(); rkt, brkt = rkvt.next()
        for (r_, br_, js, dim) in ((rq_, brq, (0, 1, 2), 384.0), (rkv_, brkv, (3, 4), 256.0)):
            pm, bpm = pmisc.next()
            for i, j in enumerate(js):
                P.op("pe", lambda e, pm=pm, j=j, i=i, n=len(js): e.matmul(pm[:], lhsT=onesb[:], rhs=sq_[:, j, :], start=(i == 0), stop=(i == n - 1)),
                     reads=[bones, bsq], writes=[bpm])
            P.op("act", lambda e, r_=r_, pm=pm, dim=dim: e.activation(out=r_[:], in_=pm[:], func=AF.Ln, bias=epsb[:, 0:1], scale=1.0 / dim),
                 reads=[bpm, beps], writes=[br_])
            P.op("act", lambda e, r_=r_: e.activation(out=r_[:], in_=r_[:], func=AF.Exp, scale=-0.5), reads=[br_], writes=[br_])
        if MS <= 2: continue
        pm, bpm = pmisc.next()
        for tt in range(4):
            for i, j in enumerate((3, 4)):
                P.op("pe", lambda e, pm=pm, tt=tt, j=j, i=i: e.matmul(pm[:, tt:tt + 1], lhsT=sq_[:, j, tt * 128:(tt + 1) * 128], rhs=onesb[:, 0:1],
                                                                   start=(i == 0), stop=(i == 1)), reads=[bones, bsq], writes=[bpm])
        P.op("act", lambda e, rkt=rkt, pm=pm: e.activation(out=rkt[:], in_=pm[:, 0:4], func=AF.Ln, bias=epsb[:, 0:1], scale=1.0 / 256.0),
             reads=[bpm, beps], writes=[brkt])
        P.op("act", lambda e, rkt=rkt: e.activation(out=rkt[:], in_=rkt[:], func=AF.Exp, scale=-0.5), reads=[brkt], writes=[brkt])
        if MS <= 3: continue
        cm_, bcm = cmt.next(); sm_, bsm = smt.next()
        P.dma("sp", cm_[:], CM_d[:, sl], writes=[bcm])
        P.dma("sp", sm_[:], SM_d[:, sl], writes=[bsm])
        cr_, bcr = cr.next(); sr_, bsr = sr.next()
        P.op("pool", lambda e, cr_=cr_, cm_=cm_, rq_=rq_: e.tensor_tensor(out=cr_[64:96, :], in0=cm_[64:96, :], in1=rq_[64:96, :], op=ALU.mult),
             reads=[bcm, brq], writes=[bcr])
        P.op("pool", lambda e, sr_=sr_, sm_=sm_, rq_=rq_: e.tensor_tensor(out=sr_[64:96, :], in0=sm_[64:96, :], in1=rq_[64:96, :], op=ALU.mult),
             reads=[bsm, brq], writes=[bsr])
        if MS <= 4: continue
        for h in range(8):
            a, ba = pA.next(); b, bb = pB.next()
            for j in range(3):
                P.op("pe", lambda e, a=a, j=j, h=h: e.matmul(a[0:96, :], lhsT=qA[:, j, h * 96:(h + 1) * 96], rhs=cg_[:, j, :], start=(j == 0), stop=(j == 2)),
                     reads=[bqA, bcg], writes=[ba])
            for j in range(3):
                P.op("pe", lambda e, b=b, j=j, h=h: e.matmul(b[0:96, :], lhsT=qB[:, j, h * 96:(h + 1) * 96], rhs=cg_[:, j, :], start=(j == 0), stop=(j == 2)),
                     reads=[bqB, bcg], writes=[bb])
            og, bog = ostg.next()
            tA_, btA = tA.next(); tB_, btB = tB.next()
            P.op("dve", lambda e, og=og, a=a, rq_=rq_: e.tensor_tensor(out=og[0:64, :], in0=a[0:64, :], in1=rq_[0:64, :], op=ALU.mult),
                 reads=[ba, brq], writes=[bog])
            P.op("dve", lambda e, tA_=tA_, a=a, cr_=cr_: e.tensor_tensor(out=tA_[64:96, :], in0=a[64:96, :], in1=cr_[64:96, :], op=ALU.mult),
                 reads=[ba, bcr], writes=[btA])
            P.op("dve", lambda e, tB_=tB_, b=b, sr_=sr_: e.tensor_tensor(out=tB_[64:96, :], in0=b[64:96, :], in1=sr_[64:96, :], op=ALU.mult),
                 reads=[bb, bsr], writes=[btB])
            P.op("pool", lambda e, og=og, tA_=tA_, tB_=tB_: e.tensor_tensor(out=og[64:96, :], in0=tA_[64:96, :], in1=tB_[64:96, :], op=ALU.add),
                 reads=[btA, btB, bog], writes=[bog])
            P.dma(nextq(), o["mla_q"][h, :, sl], og[0:96, :], reads=[bog])
        if MS <= 5: continue
        a, ba = pA.next(); b, bb = pB.next()
        for kc in range(8):
            P.op("pe", lambda e, kc=kc, a=a: e.matmul(a[0:96, :], lhsT=wm[:, kc, 640:736], rhs=uT[:, kc, sl], start=(kc == 0), stop=(kc == 7)),
                 reads=[bwm] + ubufs, writes=[ba])
        for kc in range(8):
            P.op("pe", lambda e, kc=kc, b=b: e.matmul(b[0:96, :], lhsT=wm[:, kc, 736:832], rhs=uT[:, kc, sl], start=(kc == 0), stop=(kc == 7)),
                 reads=[bwm] + ubufs, writes=[bb])
        tA_, btA = tA.next(); tB_, btB = tB.next()
        P.op("dve", lambda e, tA_=tA_, a=a, cm_=cm_: e.tensor_tensor(out=tA_[64:96, :], in0=a[64:96, :], in1=cm_[64:96, :], op=ALU.mult),
             reads=[ba, bcm], writes=[btA])
        P.op("dve", lambda e, tB_=tB_, b=b, sm_=sm_: e.tensor_tensor(out=tB_[64:96, :], in0=b[64:96, :], in1=sm_[64:96, :], op=ALU.mult),
             reads=[bb, bsm], writes=[btB])
        kr_, bkr = krs.next()
        P.op("pool", lambda e, kr_=kr_, tA_=tA_, tB_=tB_: e.tensor_tensor(out=kr_[64:96, :], in0=tA_[64:96, :], in1=tB_[64:96, :], op=ALU.add),
             reads=[btA, btB], writes=[bkr])
        for h in range(8):
            P.dma(nextq(), o["mla_k"][h, 64:96, sl], kr_[64:96, :], reads=[bkr])
        if MS <= 6: continue
        for h in range(8):
            a, ba = pA.next()
            for j in range(2):
                P.op("pe", lambda e, a=a, j=j, h=h: e.matmul(a[0:64, :], lhsT=kvn[:, j, h * 64:(h + 1) * 64], rhs=cg_[:, 3 + j, :], start=(j == 0), stop=(j == 1)),
                     reads=[bkvn, bcg], writes=[ba])
            og, bog = ostg.next()
            P.op("dve", lambda e, og=og, a=a, rkv_=rkv_: e.tensor_tensor(out=og[0:64, :], in0=a[0:64, :], in1=rkv_[0:64, :], op=ALU.mult),
                 reads=[ba, brkv], writes=[bog])
            P.dma(nextq(), o["mla_k"][h, 0:64, sl], og[0:64, :], reads=[bog])
        if MS <= 7: continue
        for tt in range(4):
            a, ba = pA.next()
            for j in range(2):
                P.op("pe", lambda e, a=a, j=j, tt=tt: e.matmul(a[:], lhsT=cg_[:, 3 + j, tt * 128:(tt + 1) * 128], rhs=kvv[:, j, :], start=(j == 0), stop=(j == 1)),
                     reads=[bkvv, bcg], writes=[ba])
            vt, vb = vst.next()
            P.op("act", lambda e, vt=vt, a=a, rkt=rkt, tt=tt: e.activation(
                out=vt[:, :, 0:64], in_=a[:].rearrange("p (h d) -> p h d", h=8), func=AF.Copy, scale=rkt[:, tt:tt + 1]),
                reads=[ba, brkt], writes=[vb])
            P.dma(nextq(), o["mla_v"][tb * 4 + tt], vt[:].rearrange("p h d -> p (h d)"), reads=[vb])


def to_pc(v, ncol):
    return np.ascontiguousarray(np.asarray(v).reshape(ncol, 128).T)


def k1_host_inputs(inp, l, core, xcur):
    b, half = core // 2, core % 2
    units, w1cols = k1_weight_layout()
    qa, qb, knope, vcols = mla_up_layout()
    C64, S64, CM, SM = rope_tables(half * T, T)
    m = {
        "x": np.ascontiguousarray(xcur[b, half * T:(half + 1) * T]),
        "cT": to_pc(inp["c"][b], 8),
        "adaw": np.ascontiguousarray(inp["ada_w"][l][:, 0:2048]),
        "adab": to_pc(inp["ada_b"][l][0:2048], 16),
        "w1": np.ascontiguousarray(inp["w_in"][l][:, w1cols]),
        "qupA": np.ascontiguousarray(inp["mla_q_up"][l][:, qa]),
        "qupB": np.ascontiguousarray(inp["mla_q_up"][l][:, qb]),
        "kvn": np.ascontiguousarray(inp["mla_kv_up"][l][:, knope]),
        "kvv": np.ascontiguousarray(inp["mla_kv_up"][l][:, vcols]),
        "gq": to_pc(inp["mla_q_norm"][l], 3),
        "gkv": to_pc(inp["mla_kv_norm"][l], 2),
        "C64": C64, "S64": S64, "CM": CM, "SM": SM,
        "ident": np.eye(128, dtype=np.float32),
    }
    return m


NEGM = -30000.0


class AttnCtx:
    pass


def attn_block(P, C, rhs_q, bq, N, ktiles, scale, acc, bacc, nsub, v_of, pt_cols=None):
    n = len(ktiles)
    LA = 2
    pend = {}
    for i in range(n + LA):
        if i < n:
            kT_ap, kb, vkey, vb, bias_ap, bb = ktiles[i]
            st, bst = C.ST.next()
            P.op("pe", lambda e, st=st, kT_ap=kT_ap, bias_ap=bias_ap: e.matmul(
                st[:, 0:N], lhsT=kT_ap, rhs=rhs_q, start=True, stop=(bias_ap is None)),
                reads=list(kb) + list(bq), writes=[bst])
            if bias_ap is not None:
                P.op("pe", lambda e, st=st, bias_ap=bias_ap: e.matmul(
                    st[:, 0:N], lhsT=C.ident[:], rhs=bias_ap, start=False, stop=True),
                    reads=list(bb) + [C.bid], writes=[bst])
            pt, bpt = C.PT.next()
            P.op("act", lambda e, st=st, pt=pt: e.activation(out=pt[:, 0:N], in_=st[:, 0:N], func=AF.Exp, scale=scale),
                 reads=[bst], writes=[bpt])
            pend[i] = (pt, bpt, vkey, vb)
        if i >= LA:
            k = i - LA
            pt, bpt, vkey, vb = pend.pop(k)
            for j in range(nsub):
                P.op("pe", lambda e, pt=pt, j=j, vkey=vkey, k=k: e.matmul(
                    acc(j), lhsT=pt[:, j * 128:(j + 1) * 128], rhs=v_of(vkey, j), start=(k == 0), stop=(k == n - 1)),
                    reads=[bpt] + list(vb), writes=[bacc])


def emit_oT(P, C, o_tok, bo, oT_d):
    for tb in range(NTB):
        stg, bs = C.oTs.next()
        for tt in range(4):
            t = tb * 4 + tt
            ptr, bp = C.ptr.next()
            for c in range(4):
                P.op("pe", lambda e, c=c, t=t, ptr=ptr: e.transpose(out=ptr[:, c, :], in_=o_tok[:, t, c * 128:(c + 1) * 128], identity=C.ident[:]),
                     reads=[bo, C.bid], writes=[bp])
            P.op("dve", lambda e, ptr=ptr, stg=stg, tt=tt: e.tensor_copy(out=stg[:, :, tt * 128:(tt + 1) * 128], in_=ptr[:]),
                 reads=[bp], writes=[bs])
        P.dma("sp" if tb % 2 == 0 else "act", oT_d.rearrange("(c p) t -> p c t", p=128)[:, :, tb * 512:(tb + 1) * 512], stg[:], reads=[bs])


def build_k2a(nc, which=("swa", "mla", "diff", "nat")):
    P = Prog(nc)
    din = lambda n, s, d=BF16: nc.dram_tensor(n, list(s), d, kind="ExternalInput").ap()
    dout = lambda n, s, d=BF16: nc.dram_tensor(n, list(s), d, kind="ExternalOutput").ap()
    D = {}
    D["swa_qT"] = din("swa_qT", [512, T]); D["swa_kT"] = din("swa_kT", [128, 34 * 128]); D["swa_v"] = din("swa_v", [34, 128, 130])
    D["swa_masks"] = din("swa_masks", [4, 128, 512], F32); D["sinkb"] = din("sinkb", [128, 8], F32)
    D["mla_qT"] = din("mla_qT", [8, 96, T]); D["mla_kT"] = din("mla_kT", [8, 96, 2 * T]); D["mla_v"] = din("mla_v", [64, 128, 520])
    D["diff_qT"] = din("diff_qT", [512, T]); D["diff_kT"] = din("diff_kT", [512, 2 * T]); D["diff_v"] = din("diff_v", [64, 128, 516])
    D["lamb"] = din("lamb", [128, 4, 64], F32); D["subln"] = din("subln", [128, 128], F32); D["lami"] = din("lami", [128, 2], F32)
    D["nat_qT"] = din("nat_qT", [512, T]); D["nat_kT"] = din("nat_kT", [512, 36 * 128]); D["nat_v"] = din("nat_v", [36, 128, 520])
    D["nat_bias"] = din("nat_bias", [8, 128, 29 * 128], F32)
    D["ident"] = din("ident", [128, 128], F32)
    oT_d = dout("oT", [4, 512, T])
    emit_attention(P, D, oT_d, which)
    P.finish()
    P.emit()
    return P


class HostSrc:
    def __init__(self, D):
        self.D = D

    def swa_k(self, kT, kv):
        return [(kT[:, kv, :], self.D["swa_kT"][kv * 64:(kv + 1) * 64, :])]

    def swa_v(self, V):
        return [(V[:], self.D["swa_v"].rearrange("t p f -> p t f"))]

    def mla_k(self, kT, h):
        return [(kT[:, 0:T], self.D["mla_kT"][h, :, 0:T]), (kT[:, T:2 * T], self.D["mla_kT"][h, :, T:2 * T])]

    def mla_v(self, V, h):
        return [(V[:], self.D["mla_v"][:, :, h * 65:(h + 1) * 65].rearrange("t p f -> p t f"))]

    def diff_k(self, kT, row):
        return [(kT[:, 0:T], self.D["diff_kT"][row:row + 64, 0:T]), (kT[:, T:2 * T], self.D["diff_kT"][row:row + 64, T:2 * T])]

    def diff_v(self, V, h):
        return [(V[:], self.D["diff_v"][:, :, h * 129:(h + 1) * 129].rearrange("t p f -> p t f"))]

    def nat_k(self, kT, h):
        return [(kT[:], self.D["nat_kT"][h * 64:(h + 1) * 64, :])]

    def nat_v(self, V, h):
        return [(V[:], self.D["nat_v"][:, :, h * 65:(h + 1) * 65].rearrange("t p f -> p t f"))]


class GatherSrc:
    def __init__(self, O, G):
        self.O = O; self.G = G

    def swa_k(self, kT, kv):
        g = self.G["swa_kT"][0]
        r = slice(kv * 64, (kv + 1) * 64); r1 = slice(128 + kv * 64, 128 + (kv + 1) * 64)
        return [(kT[:, kv, 0:128], g[r, T - 128:T]), (kT[:, kv, 128:128 + T], self.O["swa_kT"][r, :]),
                (kT[:, kv, 128 + T:256 + T], g[r1, 0:128])]

    def swa_v(self, V):
        g = self.G["swa_v"][0]
        return [(V[:, 0, :], g[T - 128:T, :]), (V[:, 1:33, :], self.O["swa_v"].rearrange("t p f -> p t f")),
                (V[:, 33, :], g[T:T + 128, :])]

    def mla_k(self, kT, h):
        g = self.G["mla_kT"][h // 2]
        r0 = (h % 2) * 96
        return [(kT[:, 0:T], g[r0:r0 + 96, :]), (kT[:, T:2 * T], g[192 + r0:192 + r0 + 96, :])]

    def mla_v(self, V, h):
        out = []
        for k in range(4):
            g = self.G["mla_v"][k]
            for r in range(2):
                out.append((V[:, r * 32 + k * 8:r * 32 + k * 8 + 8, :],
                            g[r * 1024:(r + 1) * 1024, h * 65:(h + 1) * 65].rearrange("(t p) f -> p t f", p=128)))
        return out

    def diff_k(self, kT, row):
        g = self.G["diff_kT"][row // 128]
        r0 = row % 128
        return [(kT[:, 0:T], g[r0:r0 + 64, :]), (kT[:, T:2 * T], g[128 + r0:128 + r0 + 64, :])]

    def diff_v(self, V, h):
        out = []
        for k in range(4):
            g = self.G["diff_v"][k]
            for r in range(2):
                out.append((V[:, r * 32 + k * 8:r * 32 + k * 8 + 8, :],
                            g[r * 1024:(r + 1) * 1024, h * 129:(h + 1) * 129].rearrange("(t p) f -> p t f", p=128)))
        return out

    def nat_k(self, kT, h):
        g = self.G["nat_kT"][h // 2]
        r0 = (h % 2) * 64
        return [(kT[:, 0:256], g[r0:r0 + 64, T - 256:T]), (kT[:, 256:256 + T], self.O["nat_kT"][h * 64:(h + 1) * 64, :]),
                (kT[:, 256 + T:512 + T], g[128 + r0:128 + r0 + 64, 0:256])]

    def nat_v(self, V, h):
        gl = self.G["nat_v"][1]
        gf = self.G["nat_v"][0]
        c = slice(h * 65, (h + 1) * 65)
        return [(V[:, 0:2, :], gl[768:1024, c].rearrange("(t p) f -> p t f", p=128)),
                (V[:, 2:34, :], self.O["nat_v"][:, :, c].rearrange("t p f -> p t f")),
                (V[:, 34:36, :], gf[1024:1280, c].rearrange("(t p) f -> p t f", p=128))]


def emit_attention(P, D, oT_d, which=("swa", "mla", "diff", "nat"), SRC=None, bg=None):
    if SRC is None:
        SRC = HostSrc(D)

    def bg_step():
        if bg is not None:
            next(bg, None)
    C = AttnCtx()
    identf = P.sb("a_identf", [128, 128], F32); bidf = Buf()
    C.ident = P.sb("a_ident", [128, 128], BF16); C.bid = Buf()
    P.dma("pool", identf[:], D["ident"], writes=[bidf])
    P.op("dve", lambda e: e.tensor_copy(out=C.ident[:], in_=identf[:]), reads=[bidf], writes=[C.bid])
    epsb = P.sb("a_eps", [128, 1], F32); beps = Buf()
    P.op("pool", lambda e: e.memset(epsb[:], 1e-5), writes=[beps])
    C.ST = Rot(P, "a_ST", [128, 512], F32, 3, "ps")
    C.PT = Rot(P, "a_PT", [128, 512], BF16, 3)
    C.acc = Rot(P, "a_acc", [128, 4, 512], F32, 1, "ps")
    C.ptr = Rot(P, "a_ptr", [128, 4, 128], BF16, 1, "ps")
    C.oTs = Rot(P, "a_oTs", [128, 4, 512], BF16, 2)
    o_tok = P.sb("a_otok", [128, NTT, 512], BF16); bo = Buf("otok")
    rz = Rot(P, "a_rz", [128, 4, 1], F32, 4)

    if "swa" in which:
        P.push()
        qT = P.sb("swa_q", [64, 8, T], BF16); bq = Buf()
        for h in range(8):
            P.dma(("sp", "act", "pool")[h % 3], qT[:, h, :], D["swa_qT"][h * 64:(h + 1) * 64, :], writes=[bq], append=(h > 0))
        kT = P.sb("swa_k", [64, 2, 34 * 128], BF16); bk = Buf()
        n_ = 0
        for kv in range(2):
            for (d_, s_) in SRC.swa_k(kT, kv):
                P.dma("sp", d_, s_, writes=[bk], append=(n_ > 0)); n_ += 1
        V = P.sb("swa_vv", [128, 34, 130], BF16); bv = Buf()
        for n_, (d_, s_) in enumerate(SRC.swa_v(V)):
            P.dma("sp", d_, s_, writes=[bv], append=(n_ > 0))
        mf = P.sb("swa_mf", [128, 4, 512], F32); bmf = Buf()
        mk = P.sb("swa_mk", [128, 4, 512], BF16); bmk = Buf()
        P.dma("pool", mf[:], D["swa_masks"].rearrange("m p n -> p m n"), writes=[bmf])
        P.op("dve", lambda e: e.tensor_copy(out=mk[:], in_=mf[:]), reads=[bmf], writes=[bmk])
        es = P.sb("swa_es", [128, 8], F32); bes = Buf()
        P.dma("pool", es[:], D["sinkb"], writes=[bes])
        P.op("act", lambda e: e.activation(out=es[:], in_=es[:], func=AF.Exp), reads=[bes], writes=[bes])
        for qt in range(NTT):
            for kv in range(2):
                acc, bacc = C.acc.next()
                rhs_q = qT[:, kv * 4:(kv + 1) * 4, qt * 128:(qt + 1) * 128]
                kts = []
                for d_ in range(3):
                    ki = qt + d_
                    if d_ == 1:
                        bias = None
                    elif d_ == 0:
                        bias = mk[:, 2, :] if qt == 0 else mk[:, 0, :]
                    else:
                        bias = mk[:, 3, :] if qt == NTT - 1 else mk[:, 1, :]
                    kts.append((kT[:, kv, ki * 128:(ki + 1) * 128], [bk], ki, [bv], bias, [bmk]))
                attn_block(P, C, rhs_q, [bq], 512, kts, 0.125, lambda j, acc=acc: acc[:, j, 0:65], bacc, 4,
                           lambda ki, j, kv=kv: V[:, ki, kv * 65:(kv + 1) * 65])
                r_, br = rz.next()
                P.op("dve", lambda e, r_=r_, acc=acc, kv=kv: e.tensor_tensor(
                    out=r_[:], in0=acc[:, :, 64:65], in1=es[:, kv * 4:(kv + 1) * 4].rearrange("p (g o) -> p g o", o=1), op=ALU.add),
                    reads=[bacc, bes], writes=[br])
                P.op("dve", lambda e, r_=r_: e.reciprocal(out=r_[:], in_=r_[:]), reads=[br], writes=[br])
                P.op("dve", lambda e, r_=r_, acc=acc, kv=kv, qt=qt: e.tensor_tensor(
                    out=o_tok[:, qt, kv * 256:(kv + 1) * 256].rearrange("p (g d) -> p g d", g=4),
                    in0=acc[:, :, 0:64], in1=r_[:].to_broadcast([128, 4, 64]), op=ALU.mult),
                    reads=[bacc, br], writes=[bo])
        emit_oT(P, C, o_tok, bo, oT_d[0])
        P.pop()

    if "mla" in which:
        P.push()
        qTr = Rot(P, "mla_q", [96, T], BF16, 2)
        kTr = Rot(P, "mla_k", [96, 2 * T], BF16, 2)
        Vr = Rot(P, "mla_vv", [128, 64, 65], BF16, 2)
        sc = 96.0 ** -0.5
        for h in range(8):
            qT, bq = qTr.next(); kT, bk = kTr.next(); V, bv = Vr.next()
            P.dma("sp", qT[:], D["mla_qT"][h], writes=[bq])
            for n_, (d_, s_) in enumerate(SRC.mla_k(kT, h)):
                P.dma("sp", d_, s_, writes=[bk], append=(n_ > 0))
            for n_, (d_, s_) in enumerate(SRC.mla_v(V, h)):
                P.dma("sp", d_, s_, writes=[bv], append=(n_ > 0))
            for qb in range(NTB):
                if qb % 2 == 0:
                    bg_step()
                acc, bacc = C.acc.next()
                kts = [(kT[:, ki * 128:(ki + 1) * 128], [bk], ki, [bv], None, []) for ki in range(64)]
                attn_block(P, C, qT[:, qb * 512:(qb + 1) * 512], [bq], 512, kts, sc, lambda j, acc=acc: acc[:, j, 0:65], bacc, 4,
                           lambda ki, j, V=V: V[:, ki, :])
                r_, br = rz.next()
                P.op("dve", lambda e, r_=r_, acc=acc: e.reciprocal(out=r_[:], in_=acc[:, :, 64:65]), reads=[bacc], writes=[br])
                P.op("dve", lambda e, r_=r_, acc=acc, qb=qb, h=h: e.tensor_tensor(
                    out=o_tok[:, qb * 4:(qb + 1) * 4, h * 64:(h + 1) * 64],
                    in0=acc[:, :, 0:64], in1=r_[:].to_broadcast([128, 4, 64]), op=ALU.mult),
                    reads=[bacc, br], writes=[bo])
        emit_oT(P, C, o_tok, bo, oT_d[1])
        P.pop()

    if "diff" in which:
        P.push()
        qTr = Rot(P, "df_q", [64, T], BF16, 2)
        kTr = Rot(P, "df_k", [64, 2 * T], BF16, 2)
        Vr = Rot(P, "df_vv", [128, 64, 129], BF16, 1)
        o1 = P.sb("df_o1", [128, NTT, 128], F32); bo1 = Buf()
        lamb = P.sb("df_lamb", [128, 4, 64], F32); blamb = Buf()
        lami = P.sb("df_lami", [128, 2], F32)
        sg = P.sb("df_sg", [128, 128], F32); bsg = Buf()
        P.dma("pool", lamb[:], D["lamb"], writes=[blamb])
        P.dma("pool", lami[:], D["lami"], writes=[blamb], append=True)
        P.dma("pool", sg[:], D["subln"], writes=[bsg])
        P.op("dve", lambda e: e.tensor_scalar(out=sg[:], in0=sg[:], scalar1=lami[:, 1:2], scalar2=None, op0=ALU.mult), reads=[bsg, blamb], writes=[bsg])
        lp = P.sb("df_lp", [128, 2, 64], F32); blp = Buf()
        ls = P.sb("df_ls", [128, 2], F32); bls = Buf()
        nlam = P.sb("df_nlam", [128, 1], F32); bnl = Buf()
        P.op("dve", lambda e: e.tensor_tensor(out=lp[:, 0, :], in0=lamb[:, 0, :], in1=lamb[:, 1, :], op=ALU.mult), reads=[blamb], writes=[blp])
        P.op("dve", lambda e: e.tensor_tensor(out=lp[:, 1, :], in0=lamb[:, 2, :], in1=lamb[:, 3, :], op=ALU.mult), reads=[blamb, blp], writes=[blp])
        P.op("dve", lambda e: e.reduce_sum(out=ls[:], in_=lp[:], axis=AX.X), reads=[blp], writes=[bls])
        P.op("act", lambda e: e.activation(out=ls[:], in_=ls[:], func=AF.Exp), reads=[bls], writes=[bls])
        P.op("dve", lambda e: e.tensor_tensor(out=nlam[:], in0=ls[:, 1:2], in1=ls[:, 0:1], op=ALU.subtract), reads=[bls], writes=[bnl])
        P.op("dve", lambda e: e.tensor_tensor(out=nlam[:], in0=nlam[:], in1=lami[:, 0:1], op=ALU.subtract), reads=[bnl, blamb], writes=[bnl])
        ot = Rot(P, "df_ot", [128, 4, 128], F32, 2)
        sq = Rot(P, "df_sq", [128, 4, 128], F32, 1)
        ss = Rot(P, "df_ss", [128, 4], F32, 2)
        for h in range(4):
            V, bv = Vr.next()
            for n_, (d_, s_) in enumerate(SRC.diff_v(V, h)):
                P.dma("sp", d_, s_, writes=[bv], append=(n_ > 0))
            for m in range(2):
                row = (h * 2 + m) * 64
                qT, bq = qTr.next(); kT, bk = kTr.next()
                P.dma("sp", qT[:], D["diff_qT"][row:row + 64, :], writes=[bq])
                for n_, (d_, s_) in enumerate(SRC.diff_k(kT, row)):
                    P.dma("sp", d_, s_, writes=[bk], append=(n_ > 0))
                for qb in range(NTB):
                    acc, bacc = C.acc.next()
                    kts = [(kT[:, ki * 128:(ki + 1) * 128], [bk], ki, [bv], None, []) for ki in range(64)]
                    attn_block(P, C, qT[:, qb * 512:(qb + 1) * 512], [bq], 512, kts, 0.125, lambda j, acc=acc: acc[:, j, 0:129], bacc, 4,
                               lambda ki, j, V=V: V[:, ki, :])
                    r_, br = rz.next()
                    P.op("dve", lambda e, r_=r_, acc=acc: e.reciprocal(out=r_[:], in_=acc[:, :, 128:129]), reads=[bacc], writes=[br])
                    tl = slice(qb * 4, (qb + 1) * 4)
                    if m == 0:
                        P.op("dve", lambda e, r_=r_, acc=acc, tl=tl: e.tensor_tensor(
                            out=o1[:, tl, :], in0=acc[:, :, 0:128], in1=r_[:].to_broadcast([128, 4, 128]), op=ALU.mult),
                            reads=[bacc, br], writes=[bo1])
                    else:
                        o_, bo_ = ot.next()
                        P.op("dve", lambda e, r_=r_, acc=acc, o_=o_: e.tensor_tensor(
                            out=o_[:], in0=acc[:, :, 0:128], in1=r_[:].to_broadcast([128, 4, 128]), op=ALU.mult),
                            reads=[bacc, br], writes=[bo_])
                        P.op("dve", lambda e, o_=o_, tl=tl: e.scalar_tensor_tensor(
                            out=o_[:], in0=o_[:], scalar=nlam[:, 0:1], in1=o1[:, tl, :], op0=ALU.mult, op1=ALU.add),
                            reads=[bo_, bnl, bo1], writes=[bo_])
                        sq_, bsq = sq.next(); ss_, bss = ss.next()
                        P.op("pool", lambda e, o_=o_, sq_=sq_: e.tensor_tensor(out=sq_[:], in0=o_[:], in1=o_[:], op=ALU.mult), reads=[bo_], writes=[bsq])
                        P.op("dve", lambda e, sq_=sq_, ss_=ss_: e.reduce_sum(out=ss_[:], in_=sq_[:], axis=AX.X), reads=[bsq], writes=[bss])
                        P.op("act", lambda e, ss_=ss_: e.activation(out=ss_[:], in_=ss_[:], func=AF.Ln, bias=epsb[:, 0:1], scale=1.0 / 128.0), reads=[bss, beps], writes=[bss])
                        P.op("act", lambda e, ss_=ss_: e.activation(out=ss_[:], in_=ss_[:], func=AF.Exp, scale=-0.5), reads=[bss], writes=[bss])
                        P.op("pool", lambda e, o_=o_, ss_=ss_: e.tensor_tensor(
                            out=o_[:], in0=o_[:], in1=ss_[:].rearrange("p (g o) -> p g o", o=1).to_broadcast([128, 4, 128]), op=ALU.mult),
                            reads=[bo_, bss], writes=[bo_])
                        P.op("pool", lambda e, o_=o_, tl=tl, h=h: e.tensor_tensor(
                            out=o_tok[:, tl, h * 128:(h + 1) * 128], in0=o_[:],
                            in1=sg[:].rearrange("p (o d) -> p o d", o=1).to_broadcast([128, 4, 128]), op=ALU.mult),
                            reads=[bo_, bsg], writes=[bo])
        emit_oT(P, C, o_tok, bo, oT_d[2])
        P.pop()

    if "nat" in which:
        P.push()
        qTr = Rot(P, "nat_q", [64, T], BF16, 2)
        kTr = Rot(P, "nat_k", [64, 36 * 128], BF16, 2)
        Vr = Rot(P, "nat_vv", [128, 36, 65], BF16, 2)
        bfr = Rot(P, "nat_bf", [128, 29 * 128], F32, 1)
        bbr = Rot(P, "nat_bb", [128, 29, 128], BF16, 2)
        nat_accb = [Buf(f"nat_acc{j}", excl=True) for j in range(4)]
        for h in range(8):
            qT, bq = qTr.next(); kT, bk = kTr.next(); V, bv = Vr.next()
            bf_, bbf = bfr.next(); bb_, bbb = bbr.next()
            P.dma("sp", qT[:], D["nat_qT"][h * 64:(h + 1) * 64, :], writes=[bq])
            for n_, (d_, s_) in enumerate(SRC.nat_k(kT, h)):
                P.dma("sp", d_, s_, writes=[bk], append=(n_ > 0))
            for n_, (d_, s_) in enumerate(SRC.nat_v(V, h)):
                P.dma("sp", d_, s_, writes=[bv], append=(n_ > 0))
            P.dma("sp", bf_[:], D["nat_bias"][h], writes=[bbf])
            P.op("dve", lambda e, bb_=bb_, bf_=bf_: e.tensor_copy(out=bb_[:].rearrange("p a b -> p (a b)"), in_=bf_[:]), reads=[bbf], writes=[bbb])
            for qt in range(NTT):
                if qt == 0:
                    kis = list(range(0, 6)); tbl = list(range(5, 11))
                elif qt == 1:
                    kis = list(range(1, 7)); tbl = list(range(11, 17))
                elif qt == NTT - 2:
                    kis = list(range(29, 35)); tbl = list(range(17, 23))
                elif qt == NTT - 1:
                    kis = list(range(30, 36)); tbl = list(range(23, 29))
                else:
                    kis = list(range(qt, qt + 5)); tbl = list(range(0, 5))
                acc = C.acc.tiles[0]
                jb = qt % 4
                bacc = nat_accb[jb]
                kts = [(kT[:, ki * 128:(ki + 1) * 128], [bk], ki, [bv], bb_[:, ti, :], [bbb]) for ki, ti in zip(kis, tbl)]
                attn_block(P, C, qT[:, qt * 128:(qt + 1) * 128], [bq], 128, kts, 1.0, lambda j, acc=acc, jb=jb: acc[:, jb, 0:65], bacc, 1,
                           lambda ki, j, V=V: V[:, ki, :])
                r_, br = rz.next()
                P.op("dve", lambda e, r_=r_, acc=acc, jb=jb: e.reciprocal(out=r_[:, 0, :], in_=acc[:, jb, 64:65]), reads=[bacc], writes=[br])
                P.op("dve", lambda e, r_=r_, acc=acc, qt=qt, h=h, jb=jb: e.tensor_scalar(
                    out=o_tok[:, qt, h * 64:(h + 1) * 64], in0=acc[:, jb, 0:64], scalar1=r_[:, 0, 0:1], scalar2=None, op0=ALU.mult),
                    reads=[bacc, br], writes=[bo])
        emit_oT(P, C, o_tok, bo, oT_d[3])
        P.pop()


def nat_bias_tables(rpb, half):
    a = np.arange(128)
    out = np.full((8, 29, 128, 128), NEGM, np.float32)

    def table(tq, tk):
        if tk < 0 or tk >= 64:
            return None
        rk = 2 * tk + a // 64; ck = a % 64
        rq = 2 * tq + a // 64; cq = a % 64
        r0 = np.clip(rq - 4, 0, 120); cs = np.clip(cq - 8, 0, 48)
        valid = ((rk[:, None] >= r0[None, :]) & (rk[:, None] < r0[None, :] + 8) &
                 (ck[:, None] >= cs[None, :]) & (ck[:, None] < cs[None, :] + 16))
        ri = np.clip(rk[:, None] - rq[None, :] + 7, 0, 14)
        ci = np.clip(ck[:, None] - cq[None, :] + 15, 0, 30)
        t = rpb[:, ri, ci]
        return np.where(valid[None], t, np.float32(NEGM)).astype(np.float32)

    g0 = half * 32
    specs = [(10, off) for off in range(-2, 3)]
    specs += [(g0 + 0, off) for off in range(-2, 4)]
    specs += [(g0 + 1, off) for off in range(-2, 4)]
    specs += [(g0 + 30, off) for off in range(-3, 3)]
    specs += [(g0 + 31, off) for off in range(-3, 3)]
    for i, (tq, off) in enumerate(specs):
        t = table(tq, tq + off)
        if t is not None:
            out[:, i] = t
    return np.ascontiguousarray(out.transpose(0, 2, 1, 3).reshape(8, 128, 29 * 128))


def swa_masks(half):
    a = np.arange(128)
    L = np.where(a[None, :] <= a[:, None], 0.0, NEGM).astype(np.float32)
    R = np.where(a[:, None] <= a[None, :], 0.0, NEGM).astype(np.float32)
    allm = np.full((128, 128), NEGM, np.float32)
    first = allm if half == 0 else L
    last = allm if half == 1 else R
    return np.ascontiguousarray(np.stack([np.tile(m, (1, 4)) for m in (L, R, first, last)]))


def k2a_host_inputs(inp, l, core, k1o):
    import ml_dtypes
    b, half = core // 2, core % 2
    me, pa = k1o[core], k1o[core ^ 1]
    lo, hi = (me, pa) if half == 0 else (pa, me)
    bf = lambda a: np.ascontiguousarray(a)
    z = lambda shape: np.zeros(shape, ml_dtypes.bfloat16)
    m = {}
    m["swa_qT"] = bf(me["swa_qT"])
    left = pa["swa_kT"][:, -128:] if half == 1 else z((128, 128))
    right = pa["swa_kT"][:, :128] if half == 0 else z((128, 128))
    m["swa_kT"] = bf(np.concatenate([left, me["swa_kT"], right], 1))
    left = pa["swa_v"][-1:] if half == 1 else z((1, 128, 130))
    right = pa["swa_v"][:1] if half == 0 else z((1, 128, 130))
    m["swa_v"] = bf(np.concatenate([left, me["swa_v"], right], 0))
    m["swa_masks"] = swa_masks(half)
    m["sinkb"] = np.ascontiguousarray(np.broadcast_to(inp["swa_sink"][l][None, :], (128, 8))).astype(np.float32)
    m["mla_qT"] = bf(me["mla_qT"])
    m["mla_kT"] = bf(np.concatenate([lo["mla_kT"], hi["mla_kT"]], 2))
    m["mla_v"] = bf(np.concatenate([lo["mla_v"], hi["mla_v"]], 0))
    m["diff_qT"] = bf(me["diff_qT"])
    m["diff_kT"] = bf(np.concatenate([lo["diff_kT"], hi["diff_kT"]], 1))
    m["diff_v"] = bf(np.concatenate([lo["diff_v"], hi["diff_v"]], 0))
    lam = np.stack([inp["diff_lambda_q1"][l], inp["diff_lambda_k1"][l], inp["diff_lambda_q2"][l], inp["diff_lambda_k2"][l]])
    m["lamb"] = np.ascontiguousarray(np.broadcast_to(lam[None], (128, 4, 64))).astype(np.float32)
    m["subln"] = np.ascontiguousarray(np.broadcast_to(inp["diff_subln"][l][None], (128, 128))).astype(np.float32)
    import math
    li = 0.8 - 0.6 * math.exp(-0.3 * l)
    m["lami"] = np.ascontiguousarray(np.broadcast_to(np.array([li, 1.0 - li], np.float32)[None], (128, 2)))
    m["nat_qT"] = bf(me["nat_qT"])
    left = pa["nat_kT"][:, -256:] if half == 1 else z((512, 256))
    right = pa["nat_kT"][:, :256] if half == 0 else z((512, 256))
    m["nat_kT"] = bf(np.concatenate([left, me["nat_kT"], right], 1))
    left = pa["nat_v"][-2:] if half == 1 else z((2, 128, 520))
    right = pa["nat_v"][:2] if half == 0 else z((2, 128, 520))
    m["nat_v"] = bf(np.concatenate([left, me["nat_v"], right], 0))
    m["nat_bias"] = nat_bias_tables(inp["nat_rpb"][l], half)
    m["ident"] = np.eye(128, dtype=np.float32)
    return m


DN_ALPHA = 4.0 ** 0.25


def emit_gvec_bcast(P, cT_d, adaw_d, adabb_d, gb, bgb, pA, wst_rot, tag):
    cs = P.sb(f"{tag}_cs", [128, 8], F32); bcs = Buf()
    csr = P.sb(f"{tag}_csr", [128, 8, 128], F32); bcsr = Buf()
    ab = P.sb(f"{tag}_ab", [128, 1024], F32); bab = Buf()
    P.dma("sp", cs[:], cT_d, writes=[bcs])
    P.dma("act", ab[:], adabb_d, writes=[bab])
    P.op("act", lambda e: e.activation(out=cs[:], in_=cs[:], func=AF.Silu), reads=[bcs], writes=[bcs])
    P.op("dve", lambda e: e.tensor_copy(out=csr[:], in_=cs[:].rearrange("p (k o) -> p k o", o=1).to_broadcast([128, 8, 128])),
         reads=[bcs], writes=[bcsr])
    for hf in range(2):
        ps, bps = pA.next()
        for q4 in range(2):
            ws_, bws = wst_rot.next()
            ws = ws_[:, 0:2048].rearrange("p (c n) -> p c n", c=8)
            c0 = hf * 512 + q4 * 256
            P.dma("sp" if q4 == 0 else "act", ws, adaw_d[:, c0:c0 + 256].rearrange("(c p) n -> p c n", p=128), writes=[bws])
            for kc in range(8):
                P.op("pe", lambda e, kc=kc, ps=ps, ws=ws, q4=q4: e.matmul(ps[:, q4 * 256:(q4 + 1) * 256], lhsT=csr[:, kc, :], rhs=ws[:, kc, :],
                                                                 start=(kc == 0), stop=(kc == 7)), reads=[bcsr, bws], writes=[bps])
        P.op("dve", lambda e, ps=ps, hf=hf: e.tensor_tensor(out=gb[:, hf * 512:(hf + 1) * 512], in0=ps[:], in1=ab[:, hf * 512:(hf + 1) * 512], op=ALU.add),
             reads=[bps, bab], writes=[bgb])


def emit_resid_ln(P, C, y_ps, by, xt, bx, gb, bgb, lng, lnb, bln, epsb, beps, out_tile, bout, tag):
    t1, bt1 = C["t1"].next()
    for hf in range(2):
        P.op("dve", lambda e, hf=hf, t1=t1: e.tensor_tensor(out=t1[:, hf * 512:(hf + 1) * 512], in0=y_ps[hf][:], in1=gb[:, hf * 512:(hf + 1) * 512], op=ALU.mult),
             reads=[by[hf], bgb], writes=[bt1])
    P.op("dve", lambda e, t1=t1: e.scalar_tensor_tensor(out=t1[:], in0=xt[:], scalar=DN_ALPHA, in1=t1[:], op0=ALU.mult, op1=ALU.add),
         reads=[bx, bt1], writes=[bt1])
    s_, bs = C["st"].next(); m_, bm = C["mv"].next(); r_, br = C["rs"].next()
    for c in range(2):
        P.op("dve", lambda e, c=c, s_=s_, t1=t1: e.bn_stats(out=s_[:, c, :], in_=t1[:, c * 512:(c + 1) * 512]), reads=[bt1], writes=[bs])
    P.op("dve", lambda e, m_=m_, s_=s_: e.bn_aggr(out=m_[:], in_=s_[:]), reads=[bs], writes=[bm])
    P.op("act", lambda e, r_=r_, m_=m_: e.activation(out=r_[:], in_=m_[:, 1:2], func=AF.Ln, bias=epsb[:, 0:1], scale=1.0), reads=[bm, beps], writes=[br])
    P.op("act", lambda e, r_=r_: e.activation(out=r_[:], in_=r_[:], func=AF.Exp, scale=-0.5), reads=[br], writes=[br])
    P.op("dve", lambda e, t1=t1, m_=m_, r_=r_: e.tensor_scalar(out=t1[:], in0=t1[:], scalar1=m_[:, 0:1], scalar2=r_[:, 0:1], op0=ALU.subtract, op1=ALU.mult),
         reads=[bt1, bm, br], writes=[bt1])
    P.op("pool", lambda e, t1=t1: e.tensor_tensor(out=t1[:], in0=t1[:], in1=lng[:], op=ALU.mult), reads=[bt1, bln], writes=[bt1])
    P.op("pool", lambda e, t1=t1: e.tensor_tensor(out=out_tile[:], in0=t1[:], in1=lnb[:], op=ALU.add), reads=[bt1, bln], writes=[bout])


def build_k2b(nc):
    P = Prog(nc)
    din = lambda n, s, d=F32: nc.dram_tensor(n, list(s), d, kind="ExternalInput").ap()
    D = {}
    D["oT"] = din("oT", [4, 512, T], BF16); D["uT"] = din("uT", [1024, T], BF16); D["x"] = din("x", [T, 1024])
    D["wg"] = din("wg", [4, 1024, 1024]); D["wb"] = din("wb", [4, 512, 1024]); D["wo"] = din("wo", [1024, 1024])
    D["cT"] = din("cT", [128, 8]); D["adaw_g"] = din("adaw_g", [1024, 1024]); D["adab_g"] = din("adab_g", [128, 1024])
    D["lng"] = din("lng", [128, 1024]); D["lnb"] = din("lnb", [128, 1024])
    xo = nc.dram_tensor("xmid", [T, 1024], F32, kind="ExternalOutput").ap()
    emit_merge(P, D, xo)
    P.finish(); P.emit()
    return P


def emit_merge(P, D, xo):
    P.push()
    pA = Rot(P, "m_pA", [128, 512], F32, 3, "ps")
    pB = Rot(P, "m_pB", [128, 512], F32, 3, "ps")
    wst = Rot(P, "m_wst", [128, 2048], F32, 2)
    epsb = P.sb("m_eps", [128, 1], F32); beps = Buf()
    P.op("pool", lambda e: e.memset(epsb[:], 1e-5), writes=[beps])
    gb = P.sb("m_gb", [128, 1024], F32); bgb = Buf()
    emit_gvec_bcast(P, D["cT"], D["adaw_g"], D["adab_g"], gb, bgb, pA, wst, "m")
    mT = P.sb("m_mT", [128, 8, T], BF16)
    bmT = [[Buf() for _ in range(NTB)] for _ in range(8)]
    P.push()
    wgf = Rot(P, "m_wgf", [128, 8, 4, 128], F32, 1)
    wgb = Rot(P, "m_wgb", [128, 8, 4, 128], BF16, 2)
    wbf = Rot(P, "m_wbf", [128, 4, 4, 128], F32, 1)
    wbb = Rot(P, "m_wbb", [128, 4, 4, 128], BF16, 2)
    ub = Rot(P, "m_ub", [128, 8, 512], BF16, 2)
    ob = Rot(P, "m_ob", [128, 4, 4, 512], BF16, 2)
    gs = Rot(P, "m_gs", [128, 512], BF16, 2)
    ac = Rot(P, "m_ac", [128, 512], F32, 2)
    tm = Rot(P, "m_tm", [128, 512], F32, 2)
    for oc in range(8):
        wgf_, bwgf = wgf.next(); wgb_, bwgb = wgb.next(); wbf_, bwbf = wbf.next(); wbb_, bwbb = wbb.next()
        for n in range(4):
            P.dma("sp", wgf_[:, :, n, :], D["wg"][n, :, oc * 128:(oc + 1) * 128].rearrange("(c p) n -> p c n", p=128),
                  writes=[bwgf], append=(n > 0))
            P.dma("sp", wbf_[:, :, n, :], D["wb"][n, :, oc * 128:(oc + 1) * 128].rearrange("(c p) n -> p c n", p=128),
                  writes=[bwbf], append=(n > 0))
        P.op("pool", lambda e, a=wgb_, b=wgf_: e.tensor_copy(out=a[:], in_=b[:]), reads=[bwgf], writes=[bwgb])
        P.op("pool", lambda e, a=wbb_, b=wbf_: e.tensor_copy(out=a[:], in_=b[:]), reads=[bwbf], writes=[bwbb])
        for tb in range(NTB):
            sl = slice(tb * 512, (tb + 1) * 512)
            u_, bu = ub.next(); o_, bo_ = ob.next()
            P.dma("sp", u_[:], D["uT"].rearrange("(c p) t -> p c t", p=128)[:, :, sl], writes=[bu])
            for n in range(4):
                P.dma("sp", o_[:, n, :, :], D["oT"][n].rearrange("(c p) t -> p c t", p=128)[:, :, sl], writes=[bo_], append=(n > 0))
            a_, ba = ac.next()
            for n in range(4):
                pg, bpg = pA.next(); pb, bpb = pB.next()
                for kc in range(8):
                    P.op("pe", lambda e, kc=kc, n=n, pg=pg, wgb_=wgb_, u_=u_: e.matmul(pg[:], lhsT=wgb_[:, kc, n, :], rhs=u_[:, kc, :], start=(kc == 0), stop=(kc == 7)),
                         reads=[bwgb, bu], writes=[bpg])
                for kc in range(4):
                    P.op("pe", lambda e, kc=kc, n=n, pb=pb, wbb_=wbb_, o_=o_: e.matmul(pb[:], lhsT=wbb_[:, kc, n, :], rhs=o_[:, n, kc, :], start=(kc == 0), stop=(kc == 3)),
                         reads=[bwbb, bo_], writes=[bpb])
                g_, bg = gs.next()
                P.op("act", lambda e, g_=g_, pg=pg: e.activation(out=g_[:], in_=pg[:], func=AF.Sigmoid), reads=[bpg], writes=[bg])
                if n == 0:
                    P.op("dve", lambda e, a_=a_, g_=g_, pb=pb: e.tensor_tensor(out=a_[:], in0=pb[:], in1=g_[:], op=ALU.mult), reads=[bpb, bg], writes=[ba])
                else:
                    t_, bt = tm.next()
                    P.op("dve", lambda e, t_=t_, g_=g_, pb=pb: e.tensor_tensor(out=t_[:], in0=pb[:], in1=g_[:], op=ALU.mult), reads=[bpb, bg], writes=[bt])
                    if n < 3:
                        P.op("pool", lambda e, a_=a_, t_=t_: e.tensor_tensor(out=a_[:], in0=a_[:], in1=t_[:], op=ALU.add), reads=[ba, bt], writes=[ba])
                    else:
                        P.op("pool", lambda e, a_=a_, t_=t_, oc=oc, sl=sl: e.tensor_tensor(out=mT[:, oc, sl], in0=a_[:], in1=t_[:], op=ALU.add),
                             reads=[ba, bt], writes=[bmT[oc][tb]])
    P.pop()
    P.push()
    wo = P.sb("m_wo", [128, 8, 1024], BF16); bwo = Buf()
    for q8 in range(4):
        ws_, bws = wst.next()
        ws = ws_[:, 0:2048].rearrange("p (c n) -> p c n", c=8)
        P.dma(("sp", "act")[q8 % 2], ws, D["wo"][:, q8 * 256:(q8 + 1) * 256].rearrange("(c p) n -> p c n", p=128), writes=[bws])
        P.op("pool", lambda e, ws=ws, q8=q8: e.tensor_copy(out=wo[:, :, q8 * 256:(q8 + 1) * 256], in_=ws), reads=[bws], writes=[bwo], )
    lng = P.sb("m_lng", [128, 1024], F32); lnb = P.sb("m_lnb", [128, 1024], F32); bln = Buf()
    P.dma("sp", lng[:], D["lng"], writes=[bln]); P.dma("act", lnb[:], D["lnb"], writes=[bln], append=True)
    C = {"t1": Rot(P, "m_t1", [128, 1024], F32, 2), "st": Rot(P, "m_st", [128, 2, 6], F32, 2),
         "mv": Rot(P, "m_mv", [128, 2], F32, 2), "rs": Rot(P, "m_rs", [128, 1], F32, 2)}
    xin = Rot(P, "m_xin", [128, 1024], F32, 2)
    xout = Rot(P, "m_xout", [128, 1024], F32, 2)
    for t in range(NTT):
        xt, bx = xin.next()
        P.dma("sp", xt[:], D["x"][t * 128:(t + 1) * 128, :], writes=[bx])
        ys = []; bys = []
        for hf in range(2):
            py, bpy = pA.next()
            for kc in range(8):
                P.op("pe", lambda e, kc=kc, py=py, hf=hf, t=t: e.matmul(py[:], lhsT=mT[:, kc, t * 128:(t + 1) * 128], rhs=wo[:, kc, hf * 512:(hf + 1) * 512],
                                                                 start=(kc == 0), stop=(kc == 7)), reads=[bwo, bmT[kc][t // 4]], writes=[bpy])
            ys.append(py); bys.append(bpy)
        xo_, bxo = xout.next()
        emit_resid_ln(P, C, ys, bys, xt, bx, gb, bgb, lng, lnb, bln, epsb, beps, xo_, bxo, "m")
        P.dma("pool", xo[t * 128:(t + 1) * 128, :], xo_[:], reads=[bxo])
    P.pop()
    P.pop()


def k2b_host_inputs(inp, l, core, xcur, oT, uT):

    b, half = core // 2, core % 2
    rep = lambda v: np.ascontiguousarray(np.broadcast_to(np.asarray(v, np.float32)[None, :], (128, len(v))))
    return {
        "oT": np.ascontiguousarray(oT), "uT": np.ascontiguousarray(uT),
        "x": np.ascontiguousarray(xcur[b, half * T:(half + 1) * T]),
        "wg": np.ascontiguousarray(inp["w_gate"][l]), "wb": np.ascontiguousarray(inp["w_branch"][l]),
        "wo": np.ascontiguousarray(inp["w_out"][l]),
        "cT": to_pc(inp["c"][b], 8), "adaw_g": np.ascontiguousarray(inp["ada_w"][l][:, 2048:3072]),
        "adab_g": rep(inp["ada_b"][l][2048:3072]),
        "lng": rep(inp["ln1_g"][l]), "lnb": rep(inp["ln1_b"][l]),
    }


TBLK = 256
NBLK = T // TBLK
NCH = 128
NEG = -1.0e30
U32 = mybir.dt.uint32


def build_k3(nc, nblk=NBLK, dbg=False):
    P = Prog(nc)
    din = lambda n, s, d=F32: nc.dram_tensor(n, list(s), d, kind="ExternalInput").ap()
    D = {}
    D["xmid"] = din("xmid", [T, 1024]); D["cT"] = din("cT", [128, 8])
    D["adaw_f"] = din("adaw_f", [1024, 3072]); D["adab_f"] = din("adab_f", [128, 16]); D["adab_g"] = din("adab_g", [128, 1024])
    D["wq"] = din("wq", [1024, 2048]); D["keysT"] = din("keysT", [128, 16, 128])
    D["UT"] = din("UT", [1024, 16384]); D["V"] = din("V", [16384, 1024])
    D["lng"] = din("lng", [128, 1024]); D["lnb"] = din("lnb", [128, 1024])
    D["ident"] = din("ident", [128, 128]); D["iota"] = din("iota", [128, 128])
    xo = nc.dram_tensor("xout", [T, 1024], F32, kind="ExternalOutput").ap()
    dbg_d = nc.dram_tensor("dbg", [128, 16, 128], F32, kind="ExternalOutput").ap() if dbg else None
    emit_peer(P, nc, D, xo, nblk, dbg_d)
    P.finish(); P.emit()
    return P


def peer_scratch(nc, tag):
    Ub_d = nc.dram_tensor(f"peer_Ub{tag}", [NCH, 128, 8 * 128], BF16, kind="Internal").ap()
    Vb_d = nc.dram_tensor(f"peer_Vb{tag}", [NCH, 128, 1024], BF16, kind="Internal").ap()
    return Ub_d, Vb_d, Buf("Ub_d"), Buf("Vb_d")


def peer_cast_gen(P, UT, V, Ub_d, Vb_d, bUb, bVb, cf, cb):
    for c4 in range(NCH // 4):
        f_, bf_ = cf.next(); b_, bb_ = cb.next()
        P.dma("sp", f_[:].rearrange("p (k n) -> p k n", k=8),
              UT[:, c4 * 512:(c4 + 1) * 512].rearrange("(k p) n -> p k n", p=128), writes=[bf_])
        P.op(("dve", "pool")[c4 % 2], lambda e, f_=f_, b_=b_: e.tensor_copy(
            out=b_[:].rearrange("p (c k n) -> p c k n", c=4, k=8), in_=f_[:].rearrange("p (k c n) -> p c k n", k=8, c=4)),
            reads=[bf_], writes=[bb_])
        P.dma("pool", Ub_d[c4 * 4:(c4 + 1) * 4].rearrange("c p n -> p c n"), b_[:].rearrange("p (c n) -> p c n", c=4),
              reads=[bb_], writes=[bUb], append=True)
        f_, bf_ = cf.next(); b_, bb_ = cb.next()
        P.dma("sp", f_[:].rearrange("p (c n) -> p c n", c=4),
              V[c4 * 512:(c4 + 1) * 512, :].rearrange("(c p) n -> p c n", p=128), writes=[bf_])
        P.op(("pool", "dve")[c4 % 2], lambda e, f_=f_, b_=b_: e.tensor_copy(out=b_[:], in_=f_[:]), reads=[bf_], writes=[bb_])
        P.dma("pool", Vb_d[c4 * 4:(c4 + 1) * 4].rearrange("c p n -> p c n"), b_[:].rearrange("p (c n) -> p c n", c=4),
              reads=[bb_], writes=[bVb], append=True)
        yield


def emit_peer(P, nc, D, xo, nblk=NBLK, dbg_d=None, tag="", pre=None):
    u2_d = nc.dram_tensor(f"peer_u2{tag}", [1024, T], BF16, kind="Internal").ap()
    P.push()
    bu2d = Buf("u2_d")
    if pre is None:
        Ub_d, Vb_d, bUb, bVb = peer_scratch(nc, tag)
        P.push()
        cf = Rot(P, "pa_cf", [128, 4096], F32, 2)
        cb = Rot(P, "pa_cb", [128, 4096], BF16, 2)
        for _ in peer_cast_gen(P, D["UT"], D["V"], Ub_d, Vb_d, bUb, bVb, cf, cb):
            pass
        P.pop()
    else:
        Ub_d, Vb_d, bUb, bVb = pre

    pA = Rot(P, "p_pA", [128, 512], F32, 2, "ps")
    ident_f = P.sb("p_identf", [128, 128], F32); bidf = Buf()
    ident = P.sb("p_ident", [128, 128], BF16); bid = Buf()
    epsb = P.sb("p_eps", [128, 1], F32); beps = Buf()
    P.dma("pool", ident_f[:], D["ident"], writes=[bidf])
    P.op("dve", lambda e: e.tensor_copy(out=ident[:], in_=ident_f[:]), reads=[bidf], writes=[bid])
    P.op("pool", lambda e: e.memset(epsb[:], 1e-5), writes=[beps])
    gb = P.sb("p_gb", [128, 1024], F32); bgb = Buf()
    P.push()
    wst = Rot(P, "p_wst", [128, 2048], F32, 2)
    emit_gvec_bcast(P, D["cT"], D["adaw_f"][:, 2048:3072], D["adab_g"], gb, bgb, pA, wst, "p")
    P.push()
    modc = P.sb("p_modc", [128, 16], F32); bmodc = Buf()
    sc1 = P.sb("p_sc1", [128, 8], F32); bsc1 = Buf()
    pm = Rot(P, "p_pm", [128, 512], F32, 1, "ps")
    emit_mod_cols(P, nc, D["cT"], D["adaw_f"][:, 0:2048], D["adab_f"], 2, modc, bmodc, pm, wst)
    P.op("dve", lambda e: e.tensor_scalar(out=sc1[:], in0=modc[:, 8:16], scalar1=1.0, scalar2=None, op0=ALU.add), reads=[bmodc], writes=[bsc1])
    bmod = Buf()
    P.op("dve", lambda e: e.tensor_copy(out=modc[:, 0:8], in_=modc[:, 0:8]), reads=[bmodc, bsc1], writes=[bmod])
    u2T = P.sb("p_u2T", [128, 8, T], BF16)
    bu2 = [Buf() for _ in range(NTT)]
    ptr = Rot(P, "p_ptr", [128, 8, 128], BF16, 2, "ps")
    emit_ln_mod(P, D["xmid"], u2T, bu2, modc, sc1, bmod, ident, bid, epsb, beps, ptr, tag="p")
    for c in range(8):
        P.dma(("sp", "act")[c % 2], u2_d[c * 128:(c + 1) * 128, :], u2T[:, c, :], reads=bu2, writes=[bu2d], append=(c > 0))
    P.pop()

    wqb_d = nc.dram_tensor(f"peer_wqb{tag}", [128, 8 * 2048], BF16, kind="Internal").ap()
    bwqd = Buf("wqb_d")
    P.push()
    wtmp = P.sb("p_wqtmp", [128, 8, 2048], BF16); bwt = Buf()
    for g in range(8):
        ws_, bws = wst.next()
        ws = ws_[:, 0:2048].rearrange("p (c n) -> p c n", c=8)
        P.dma(("sp", "act")[g % 2], ws, D["wq"][:, g * 256:(g + 1) * 256].rearrange("(c p) n -> p c n", p=128), writes=[bws])
        P.op("pool", lambda e, ws=ws, g=g: e.tensor_copy(out=wtmp[:, :, g * 256:(g + 1) * 256], in_=ws), reads=[bws], writes=[bwt])
    P.dma("sp", wqb_d, wtmp[:].rearrange("p k n -> p (k n)"), reads=[bwt], writes=[bwqd])
    P.pop()
    P.pop()

    keysT = P.sb("p_keysT", [128, 16, 128], F32); bkeys = Buf()
    P.dma("sp", keysT[:], D["keysT"], writes=[bkeys])
    iota = P.sb("p_iota", [128, 128], F32); biota = Buf()
    P.dma("act", iota[:], D["iota"], writes=[biota])
    lng = P.sb("p_lng", [128, 1024], F32); lnb = P.sb("p_lnb", [128, 1024], F32); bln = Buf()
    P.dma("sp", lng[:], D["lng"], writes=[bln]); P.dma("act", lnb[:], D["lnb"], writes=[bln], append=True)

    pO = [P.ps(f"p_pO{i}", [128, 512], F32) for i in range(4)]
    bpO = [Buf(f"pO{i}", excl=True) for i in range(4)]
    pH = Rot(P, "p_pH", [128, 512], F32, 2, "ps")
    pM = pA
    NTL = TBLK // 128

    Wd = nc.dram_tensor(f"peer_W{tag}", [nblk, 8, 128, TBLK * 16], BF16, kind="Internal").ap()
    bWd = [Buf(f"Wd{b}") for b in range(nblk)]

    wq = P.sb("p_wq", [128, 8, 2048], BF16); bwq = Buf()
    P.dma("act", wq[:].rearrange("p k n -> p (k n)"), wqb_d, reads=[bwqd], writes=[bwq])
    ublk = Rot(P, "p_ublk", [128, 8, TBLK], BF16, 2)
    qT = Rot(P, "p_qT", [128, 16, TBLK], F32, 1)
    C = {"t1": Rot(P, "p_t1", [128, 1024], F32, 2), "st": Rot(P, "p_st", [128, 2, 6], F32, 2),
         "mv": Rot(P, "p_mv", [128, 2], F32, 2), "rs": Rot(P, "p_rs", [128, 1], F32, 2)}
    xin = Rot(P, "p_xin", [128, 1024], F32, 1)
    xout = Rot(P, "p_xout", [128, 1024], F32, 1)
    S = Rot(P, "p_S", [128, 16, 128], F32, 1)
    S2 = Rot(P, "p_S2", [128, 128], F32, 2)
    top = Rot(P, "p_top", [128, 16, 16], F32, 1)
    jix = Rot(P, "p_jix", [128, 8, 16], U32, 1)
    jif = Rot(P, "p_jif", [128, 128], F32, 1)
    jT = Rot(P, "p_jT", [128, 128], F32, 1)
    cand = Rot(P, "p_cand", [128, 256], F32, 2)
    ctop = Rot(P, "p_ctop", [128, 24], F32, 2)
    sm = Rot(P, "p_sm", [128, 8, 8], F32, 1)
    junk = Rot(P, "p_junk", [128, 16], F32, 2)
    e0 = Rot(P, "p_e0", [128, 8, 128], BF16, 1)
    thr2 = Rot(P, "p_thr2", [128, 8, 16], F32, 1)
    sc2 = Rot(P, "p_sc2", [128, 8, 16], F32, 1)
    sc2b = Rot(P, "p_sc2b", [128, 8, 16], BF16, 1)
    Ytm = Rot(P, "p_Ytm", [128, 128, 64], BF16, 1)
    Ysm = Rot(P, "p_Ysm", [128, 64, 128], BF16, 1)
    Xsm = Rot(P, "p_Xsm", [128, 64, 128], BF16, 1)
    Wst = Rot(P, "p_Wst", [128, 4, 128, 16], BF16, 1)
    wsl = Rot(P, "p_wsl", [128, TBLK, 16], BF16, 2)
    uch = Rot(P, "p_uch", [128, 8, 128], BF16, 3)
    vch = Rot(P, "p_vch", [128, 1024], BF16, 4)
    gl = Rot(P, "p_gl", [128, TBLK], BF16, 2)
    zt = Rot(P, "p_zt", [128, TBLK], BF16, 3)
    ublocks = {}

    def sel_block(blk):
        tsl = slice(blk * TBLK, (blk + 1) * TBLK)
        u_, bu = ublk.next()
        ublocks[blk] = (u_, bu)
        P.dma("sp", u_[:], u2_d.rearrange("(c p) t -> p c t", p=128)[:, :, tsl], reads=[bu2d], writes=[bu])
        q_, bq = qT.next()
        for hp in range(16):
            ph, bph = pH.next()
            for kc in range(8):
                P.op("pe", lambda e, kc=kc, hp=hp, ph=ph, u_=u_: e.matmul(ph[:, 0:TBLK], lhsT=wq[:, kc, hp * 128:(hp + 1) * 128], rhs=u_[:, kc, :],
                                                                  start=(kc == 0), stop=(kc == 7)), reads=[bwq, bu], writes=[bph])
            P.op("act", lambda e, hp=hp, ph=ph, q_=q_: e.activation(out=q_[:, hp, :], in_=ph[:, 0:TBLK], func=AF.Copy), reads=[bph], writes=[bq])
            if hp % 4 == 3:
                yield
        for tt in range(NTL):
            S_, bS = S.next()
            for g4 in range(4):
                pm_, bpm = pM.next()
                for i4 in range(4):
                    hp = g4 * 4 + i4
                    P.op("pe", lambda e, hp=hp, i4=i4, pm_=pm_, q_=q_, tt=tt: e.matmul(
                        pm_[:, i4 * 128:(i4 + 1) * 128], lhsT=q_[:, hp, tt * 128:(tt + 1) * 128], rhs=keysT[:, hp, :], start=True, stop=True),
                        reads=[bq, bkeys], writes=[bpm])
                P.op("act", lambda e, g4=g4, pm_=pm_, S_=S_: e.activation(out=S_[:, g4 * 4:(g4 + 1) * 4, :].rearrange("p a b -> p (a b)"), in_=pm_[:], func=AF.Copy),
                     reads=[bpm], writes=[bS])
            if dbg_d is not None and blk == 0 and tt == 0:
                P.dma("sp", dbg_d, S_[:], reads=[bS])
            yield
            top_, btop = top.next(); jix_, bjix = jix.next()
            for hp in range(16):
                s2, bs2 = S2.next()
                P.op("dve", lambda e, hp=hp, top_=top_, S_=S_: e.max(out=top_[:, hp, 0:8], in_=S_[:, hp, :]), reads=[bS], writes=[btop])
                P.op("dve", lambda e, hp=hp, top_=top_, S_=S_, s2=s2: e.match_replace(out=s2[:], in_to_replace=top_[:, hp, 0:8], in_values=S_[:, hp, :], imm_value=NEG),
                     reads=[bS, btop], writes=[bs2])
                P.op("dve", lambda e, hp=hp, top_=top_, s2=s2: e.max(out=top_[:, hp, 8:16], in_=s2[:]), reads=[bs2, btop], writes=[btop])
                if hp % 2 == 1:
                    h = hp // 2
                    P.op("dve", lambda e, hp=hp, h=h, top_=top_, S_=S_, jix_=jix_: e.max_index(out=jix_[:, h, 0:8], in_max=top_[:, hp, 0:8], in_values=S_[:, hp, :]),
                         reads=[bS, btop], writes=[bjix])
                    P.op("dve", lambda e, hp=hp, h=h, top_=top_, S_=S_, jix_=jix_: e.max_index(out=jix_[:, h, 8:16], in_max=top_[:, hp, 8:16], in_values=S_[:, hp, :]),
                         reads=[bS, btop, bjix], writes=[bjix])
                if hp % 4 == 3:
                    yield
            jif_, bjif = jif.next()
            P.op("dve", lambda e, jif_=jif_, jix_=jix_: e.tensor_copy(out=jif_[:], in_=jix_[:].rearrange("p a b -> p (a b)")), reads=[bjix], writes=[bjif])
            pm_, bpm = pM.next()
            P.op("pe", lambda e, pm_=pm_, jif_=jif_: e.transpose(out=pm_[:, 0:128], in_=jif_[:], identity=ident_f[:]), reads=[bjif, bidf], writes=[bpm])
            jT_, bjT = jT.next()
            P.op("act", lambda e, pm_=pm_, jT_=jT_: e.activation(out=jT_[:], in_=pm_[:, 0:128], func=AF.Copy), reads=[bpm], writes=[bjT])
            sm_, bsm = sm.next(); thr_, bthr = thr2.next(); sc_, bsc = sc2.next(); e0_, be0 = e0.next()
            P.op("pool", lambda e, sm_=sm_: e.memset(sm_[:], 0.0), writes=[bsm])
            for h in range(8):
                cd, bcd = cand.next(); ct, bct = ctop.next()
                P.op("dve", lambda e, h=h, cd=cd, top_=top_: e.tensor_tensor(
                    out=cd[:].rearrange("p (a b) -> p a b", a=16),
                    in0=top_[:, 2 * h, :].rearrange("p (a o) -> p a o", o=1).to_broadcast([128, 16, 16]),
                    in1=top_[:, 2 * h + 1, :].rearrange("p (o b) -> p o b", o=1).to_broadcast([128, 16, 16]), op=ALU.add),
                    reads=[btop], writes=[bcd])
                for r in range(3):
                    P.op("dve", lambda e, r=r, cd=cd, ct=ct: e.max(out=ct[:, r * 8:(r + 1) * 8], in_=cd[:]), reads=[bcd], writes=[bct])
                    if r < 2:
                        P.op("dve", lambda e, r=r, cd=cd, ct=ct: e.match_replace(out=cd[:], in_to_replace=ct[:, r * 8:(r + 1) * 8], in_values=cd[:], imm_value=NEG),
                             reads=[bct, bcd], writes=[bcd])
                P.op("dve", lambda e, h=h, ct=ct, sm_=sm_: e.tensor_tensor(out=sm_[:, h, 0:1], in0=ct[:, 15:16], in1=ct[:, 16:17], op=ALU.add), reads=[bct, bsm], writes=[bsm])
                P.op("dve", lambda e, h=h, sm_=sm_: e.tensor_scalar(out=sm_[:, h, 0:1], in0=sm_[:, h, 0:1], scalar1=0.5, scalar2=None, op0=ALU.mult), reads=[bsm], writes=[bsm])
                P.op("dve", lambda e, h=h, top_=top_, sm_=sm_: e.tensor_scalar(out=sm_[:, h, 1:2], in0=top_[:, 2 * h, 0:1], scalar1=-1.0, scalar2=None, op0=ALU.mult), reads=[btop, bsm], writes=[bsm])
                P.op("dve", lambda e, h=h, top_=top_, sm_=sm_: e.tensor_scalar(out=sm_[:, h, 2:3], in0=top_[:, 2 * h + 1, 0:1], scalar1=-1.0, scalar2=None, op0=ALU.mult), reads=[btop, bsm], writes=[bsm])
                P.op("dve", lambda e, h=h, ct=ct, sm_=sm_: e.tensor_scalar(out=sm_[:, h, 5:6], in0=ct[:, 0:1], scalar1=-1.0, scalar2=None, op0=ALU.mult), reads=[bct, bsm], writes=[bsm])
                jk, bjk = junk.next()
                P.op("act", lambda e, h=h, ct=ct, sm_=sm_, jk=jk: e.activation(out=jk[:], in_=ct[:, 0:16], func=AF.Exp, bias=sm_[:, h, 5:6], scale=1.0, accum_out=sm_[:, h, 3:4]),
                     reads=[bct, bsm], writes=[bjk, bsm])
                P.op("dve", lambda e, h=h, sm_=sm_: e.reciprocal(out=sm_[:, h, 4:5], in_=sm_[:, h, 3:4]), reads=[bsm], writes=[bsm])
                P.op("act", lambda e, h=h, S_=S_, sm_=sm_, e0_=e0_: e.activation(out=e0_[:, h, :], in_=S_[:, 2 * h, :], func=AF.Exp, bias=sm_[:, h, 1:2], scale=1.0),
                     reads=[bS, bsm], writes=[be0])
                P.op("dve", lambda e, h=h, top_=top_, sm_=sm_, thr_=thr_: e.tensor_scalar(out=thr_[:, h, :], in0=top_[:, 2 * h + 1, :], scalar1=-1.0, scalar2=sm_[:, h, 0:1], op0=ALU.mult, op1=ALU.add),
                     reads=[btop, bsm], writes=[bthr])
                P.op("act", lambda e, h=h, top_=top_, sm_=sm_, sc_=sc_: e.activation(out=sc_[:, h, :], in_=top_[:, 2 * h + 1, :], func=AF.Exp, bias=sm_[:, h, 2:3], scale=1.0),
                     reads=[btop, bsm], writes=[bsc])
                P.op("dve", lambda e, h=h, sm_=sm_, sc_=sc_: e.tensor_scalar(out=sc_[:, h, :], in0=sc_[:, h, :], scalar1=sm_[:, h, 4:5], scalar2=None, op0=ALU.mult),
                     reads=[bsc, bsm], writes=[bsc])
                if h % 2 == 1:
                    yield
            scb_, bscb = sc2b.next()
            P.op("dve", lambda e, scb_=scb_, sc_=sc_: e.tensor_copy(out=scb_[:], in_=sc_[:]), reads=[bsc], writes=[bscb])
            for ch in range(2):
                csl = slice(ch * 64, (ch + 1) * 64)
                Y_, bY = Ytm.next()
                for h in range(8):
                    yv = Y_[:, h * 16:(h + 1) * 16, :]
                    P.op("dve", lambda e, h=h, yv=yv, S_=S_, thr_=thr_, csl=csl: e.tensor_tensor(
                        out=yv, in0=S_[:, 2 * h, csl].rearrange("p (o n) -> p o n", o=1).to_broadcast([128, 16, 64]),
                        in1=thr_[:, h, :].rearrange("p (s o) -> p s o", o=1).to_broadcast([128, 16, 64]), op=ALU.is_ge),
                        reads=[bS, bthr], writes=[bY])
                    P.op("dve", lambda e, h=h, yv=yv, e0_=e0_, csl=csl: e.tensor_tensor(
                        out=yv, in0=yv, in1=e0_[:, h, csl].rearrange("p (o n) -> p o n", o=1).to_broadcast([128, 16, 64]), op=ALU.mult),
                        reads=[bY, be0], writes=[bY])
                    P.op("dve", lambda e, h=h, yv=yv, scb_=scb_: e.tensor_tensor(
                        out=yv, in0=yv, in1=scb_[:, h, :].rearrange("p (s o) -> p s o", o=1).to_broadcast([128, 16, 64]), op=ALU.mult),
                        reads=[bY, bscb], writes=[bY])
                    if h % 4 == 3:
                        yield
                Ys_, bYs = Ysm.next()
                for c4 in range(16):
                    pm_, bpm = pM.next()
                    pmb = pm_[:].bitcast(BF16)
                    for i in range(4):
                        cc = c4 * 4 + i
                        P.op("pe", lambda e, cc=cc, i=i, pmb=pmb, Y_=Y_: e.transpose(out=pmb[:, i * 128:(i + 1) * 128], in_=Y_[:, :, cc], identity=ident[:]),
                             reads=[bY, bid], writes=[bpm])
                    if c4 % 2 == 0:
                        P.op("act", lambda e, c4=c4, pmb=pmb, Ys_=Ys_: e.activation(out=Ys_[:, c4 * 4:(c4 + 1) * 4, :].rearrange("p a b -> p (a b)"), in_=pmb[:, 0:512], func=AF.Copy),
                             reads=[bpm], writes=[bYs])
                    else:
                        P.op("dve", lambda e, c4=c4, pmb=pmb, Ys_=Ys_: e.tensor_copy(out=Ys_[:, c4 * 4:(c4 + 1) * 4, :].rearrange("p a b -> p (a b)"), in_=pmb[:, 0:512]),
                             reads=[bpm], writes=[bYs])
                    if c4 % 4 == 3:
                        yield
                Ws_, bWs = Wst.next()
                for th in range(2):
                    X_, bX = Xsm.next()
                    for q2 in range(2):
                        P.op("dve", lambda e, q2=q2, th=th, X_=X_, jT_=jT_: e.tensor_tensor(
                            out=X_[:, q2 * 32:(q2 + 1) * 32, :],
                            in0=iota[:].rearrange("p (o n) -> p o n", o=1).to_broadcast([128, 32, 128]),
                            in1=jT_[:, th * 64 + q2 * 32:th * 64 + (q2 + 1) * 32].rearrange("p (s o) -> p s o", o=1).to_broadcast([128, 32, 128]), op=ALU.is_equal),
                            reads=[biota, bjT], writes=[bX])
                    for t8 in range(8):
                        pm_, bpm = pM.next()
                        for i in range(8):
                            tl = t8 * 8 + i
                            tk = th * 64 + tl
                            P.op("pe", lambda e, tk=tk, tl=tl, i=i, pm_=pm_, X_=X_, Ys_=Ys_: e.matmul(pm_[:, i * 64:(i + 1) * 64], lhsT=X_[:, tl, :], rhs=Ys_[:, :, tk], start=True, stop=True),
                                 reads=[bX, bYs], writes=[bpm])
                        tok0 = th * 64 + t8 * 8
                        src = pm_[:].rearrange("p (t g c) -> p g t c", t=8, g=4)
                        dst = Ws_[:, :, tok0:tok0 + 8, :]
                        if t8 % 2 == 0:
                            P.op("act", lambda e, src=src, dst=dst: e.activation(out=dst, in_=src, func=AF.Copy), reads=[bpm], writes=[bWs])
                        else:
                            P.op("dve", lambda e, src=src, dst=dst: e.tensor_copy(out=dst, in_=src), reads=[bpm], writes=[bWs])
                        if t8 % 4 == 3:
                            yield
                for g in range(4):
                    cg = ch * 4 + g
                    P.dma(("act", "pool")[g % 2], Wd[blk, cg, :, tt * 128 * 16:(tt + 1) * 128 * 16], Ws_[:, g, :, :].rearrange("p t c -> p (t c)"),
                          reads=[bWs], writes=[bWd[blk]], append=True)
                yield

    def drain(gen, n):
        if gen is None:
            return None
        for _ in range(n):
            try:
                next(gen)
            except StopIteration:
                return None
        return gen

    gen = sel_block(0)
    gen = drain(gen, 10 ** 9)
    for blk in range(nblk):
        u_, bu = ublocks[blk]
        nxt = sel_block(blk + 1) if blk + 1 < nblk else None
        LAG = 2
        stage = {}
        for c in range(NCH + LAG):
            if c < NCH:
                if c % 16 == 0:
                    w_, bw = wsl.next()
                    P.dma("sp", w_[:].rearrange("p t c -> p (t c)"), Wd[blk, c // 16], reads=[bWd[blk]], writes=[bw])
                uc, buc = uch.next(); vc, bvc = vch.next()
                P.dma("sp", uc[:].rearrange("p k n -> p (k n)"), Ub_d[c], reads=[bUb], writes=[buc])
                P.dma("sp", vc[:], Vb_d[c], reads=[bVb], writes=[bvc])
                ph, bph = pH.next()
                for kc in range(8):
                    P.op("pe", lambda e, kc=kc, ph=ph, uc=uc, u_=u_: e.matmul(ph[:, 0:TBLK], lhsT=uc[:, kc, :], rhs=u_[:, kc, :], start=(kc == 0), stop=(kc == 7)),
                         reads=[buc, bu], writes=[bph])
                g_, bg = gl.next()
                P.op("act", lambda e, ph=ph, g_=g_: e.activation(out=g_[:], in_=ph[:, 0:TBLK], func=AF.Gelu), reads=[bph], writes=[bg])
                z_, bz = zt.next()
                P.op("pool", lambda e, c=c, z_=z_, g_=g_, w_=w_: e.tensor_tensor(out=z_[:], in0=w_[:, :, c % 16], in1=g_[:], op=ALU.mult),
                     reads=[bw, bg], writes=[bz])
                stage[c] = (z_, bz, vc, bvc)
            if c >= LAG:
                cc = c - LAG
                z_, bz, vc, bvc = stage.pop(cc)
                for tt in range(NTL):
                    for hf in range(2):
                        k = tt * 2 + hf
                        P.op("pe", lambda e, cc=cc, tt=tt, hf=hf, k=k, z_=z_, vc=vc: e.matmul(pO[k][:], lhsT=z_[:, tt * 128:(tt + 1) * 128], rhs=vc[:, hf * 512:(hf + 1) * 512],
                                                                                  start=(cc == 0), stop=(cc == NCH - 1)), reads=[bz, bvc], writes=[bpO[k]])
            if c % 2 == 1:
                nxt = drain(nxt, 1)
        nxt = drain(nxt, 10 ** 9)
        for tt in range(NTL):
            tg = blk * NTL + tt
            xt, bx = xin.next()
            P.dma("sp", xt[:], D["xmid"][tg * 128:(tg + 1) * 128, :], writes=[bx])
            xo_, bxo = xout.next()
            emit_resid_ln(P, C, [pO[tt * 2], pO[tt * 2 + 1]], [bpO[tt * 2], bpO[tt * 2 + 1]], xt, bx, gb, bgb, lng, lnb, bln, epsb, beps, xo_, bxo, "p")
            P.dma("act", xo[tg * 128:(tg + 1) * 128, :], xo_[:], reads=[bxo])
    P.pop()


def k3_host_inputs(inp, l, core, xmid_core):
    b = core // 2
    rep = lambda v: np.ascontiguousarray(np.broadcast_to(np.asarray(v, np.float32)[None, :], (128, len(v))))
    keys = inp["peer_keys"][l].reshape(16, 128, 128)
    return {
        "xmid": np.ascontiguousarray(xmid_core),
        "cT": to_pc(inp["c"][b], 8),
        "adaw_f": np.ascontiguousarray(inp["ada_w"][l][:, 3072:6144]),
        "adab_f": to_pc(inp["ada_b"][l][3072:5120], 16),
        "adab_g": rep(inp["ada_b"][l][5120:6144]),
        "wq": np.ascontiguousarray(inp["peer_wq"][l]),
        "keysT": np.ascontiguousarray(keys.transpose(2, 0, 1)),
        "UT": np.ascontiguousarray(inp["peer_u"][l].T),
        "V": np.ascontiguousarray(inp["peer_v"][l]),
        "lng": rep(inp["ln2_g"][l]), "lnb": rep(inp["ln2_b"][l]),
        "ident": np.eye(128, dtype=np.float32),
        "iota": np.ascontiguousarray(np.broadcast_to(np.arange(128, dtype=np.float32)[None, :], (128, 128))),
    }


import math

PAIRS = [[0, 1], [2, 3], [4, 5], [6, 7]]

GATHER = [
    ("mla_kT", [768, T], 192, [0, 1, 2, 3]),
    ("mla_v", [T, 520], 1024, [0, 1, 2, 3]),
    ("diff_kT", [512, T], 128, [0, 1, 2, 3]),
    ("diff_v", [T, 516], 1024, [0, 1, 2, 3]),
    ("swa_kT", [128, T], 128, [0]),
    ("swa_v", [T, 130], 2048, [1, 0]),
    ("nat_kT", [512, T], 128, [0, 1, 2, 3]),
    ("nat_v", [T, 520], 1024, [0, 3]),
]

LAYER_IN = [("adaw", [1024, 6144]), ("adab1", [128, 16]), ("adabg", [128, 1024]), ("adabf", [128, 16]), ("adabg2", [128, 1024]),
            ("w1", None), ("qupA", [384, 768]), ("qupB", [384, 768]), ("kvn", [256, 512]), ("kvv", [256, 512]),
            ("gq", [128, 3]), ("gkv", [128, 2]), ("sinkb", [128, 8]), ("lamb", [128, 4, 64]), ("subln", [128, 128]), ("lami", [128, 2]),
            ("nat_bias", [8, 128, 29 * 128]), ("wg", [4, 1024, 1024]), ("wb", [4, 512, 1024]), ("wo", [1024, 1024]),
            ("lng1", [128, 1024]), ("lnb1", [128, 1024]), ("wq", [1024, 2048]), ("keysT", [128, 16, 128]),
            ("UT", [1024, 16384]), ("V", [16384, 1024]), ("lng2", [128, 1024]), ("lnb2", [128, 1024])]
COMMON_IN = [("x", [T, 1024]), ("cT", [128, 8]), ("C64", [128, T]), ("S64", [128, T]), ("CM", [128, T]), ("SM", [128, T]),
             ("ident", [128, 128]), ("iota", [128, 128]), ("swa_masks", [4, 128, 512])]


def build_fused(nc, nlayers=2, peer_blocks=NBLK, dbg=False):
    P = Prog(nc)
    units, w1cols = k1_weight_layout()
    NC1 = len(w1cols)
    I = {}
    for n, s in COMMON_IN:
        I[n] = nc.dram_tensor(n, list(s), F32, kind="ExternalInput").ap()
    for l in range(nlayers):
        for n, s in LAYER_IN:
            if n == "w1":
                s = [1024, NC1]
            I[f"{n}_{l}"] = nc.dram_tensor(f"{n}_{l}", list(s), F32, kind="ExternalInput").ap()
    out_d = nc.dram_tensor("out", [T, 1024], F32, kind="ExternalOutput").ap()
    x_cur = I["x"]
    for l in range(nlayers):
        L = lambda n: I[f"{n}_{l}"]
        it = lambda n, s, d=BF16: nc.dram_tensor(f"{n}_L{l}", list(s), d, kind="Internal").ap()
        O2 = {"uT": it("uT", [1024, T]), "swa_qT": it("swa_qT", [512, T]), "swa_kT": it("swa_kT", [128, T]),
              "diff_qT": it("diff_qT", [512, T]), "diff_kT": it("diff_kT", [512, T]), "nat_qT": it("nat_qT", [512, T]),
              "nat_kT": it("nat_kT", [512, T]), "mla_qT": it("mla_qT", [768, T]), "mla_kT": it("mla_kT", [768, T]),
              "swa_v": it("swa_v", [T, 130]), "diff_v": it("diff_v", [T, 516]), "nat_v": it("nat_v", [T, 520]), "mla_v": it("mla_v", [T, 520])}
        O = dict(O2)
        O["mla_qT"] = O2["mla_qT"].rearrange("(h d) t -> h d t", d=96)
        O["mla_kT"] = O2["mla_kT"].rearrange("(h d) t -> h d t", d=96)
        for n in ("swa_v", "diff_v", "nat_v", "mla_v"):
            O[n] = O2[n].rearrange("(t p) f -> t p f", p=128)
        D1 = {"x": x_cur, "cT": I["cT"], "adaw": L("adaw")[:, 0:2048], "adab": L("adab1"), "w1": L("w1"),
              "qupA": L("qupA"), "qupB": L("qupB"), "kvn": L("kvn"), "kvv": L("kvv"), "gq": L("gq"), "gkv": L("gkv"),
              "C64": I["C64"], "S64": I["S64"], "CM": I["CM"], "SM": I["SM"], "ident": I["ident"]}
        P.push()
        emit_k1(P, nc, D1, O)
        P.pop()
        G = {}
        for (name, shp, rc, chunks) in GATHER:
            G[name] = []
            if name == "swa_v":
                dst = it(f"g_{name}", [2 * T, 130])
                P.collective(O2[name], dst, PAIRS)
                G[name].append(dst)
                continue
            for k in chunks:
                dst = it(f"g_{name}{k}", [2 * rc, shp[1]])
                P.collective(O2[name][k * rc:(k + 1) * rc, :], dst, PAIRS)
                G[name].append(dst)
        P.barrier()
        oT_d = it("oT", [4, 512, T])
        D2 = {"swa_qT": O["swa_qT"], "swa_masks": I["swa_masks"], "sinkb": L("sinkb"), "mla_qT": O["mla_qT"],
              "diff_qT": O["diff_qT"], "lamb": L("lamb"), "subln": L("subln"), "lami": L("lami"),
              "nat_qT": O["nat_qT"], "nat_bias": L("nat_bias"), "ident": I["ident"]}
        P.push()
        pre = peer_scratch(nc, f"_L{l}")
        cf = Rot(P, "pa_cf", [128, 4096], F32, 2)
        cb = Rot(P, "pa_cb", [128, 4096], BF16, 2)
        bgen = peer_cast_gen(P, L("UT"), L("V"), pre[0], pre[1], pre[2], pre[3], cf, cb)
        emit_attention(P, D2, oT_d, SRC=GatherSrc(O, G), bg=bgen)
        for _ in bgen:
            pass
        P.pop()
        xmid_d = it("xmid", [T, 1024], F32)
        D3 = {"oT": oT_d, "uT": O["uT"], "x": x_cur, "wg": L("wg"), "wb": L("wb"), "wo": L("wo"), "cT": I["cT"],
              "adaw_g": L("adaw")[:, 2048:3072], "adab_g": L("adabg"), "lng": L("lng1"), "lnb": L("lnb1")}
        emit_merge(P, D3, xmid_d)
        xo = out_d if l == nlayers - 1 else it("xout", [T, 1024], F32)
        D4 = {"xmid": xmid_d, "cT": I["cT"], "adaw_f": L("adaw")[:, 3072:6144], "adab_f": L("adabf"), "adab_g": L("adabg2"),
              "wq": L("wq"), "keysT": L("keysT"), "UT": L("UT"), "V": L("V"), "lng": L("lng2"), "lnb": L("lnb2"),
              "ident": I["ident"], "iota": I["iota"]}
        emit_peer(P, nc, D4, xo, nblk=peer_blocks, tag=f"_L{l}", pre=pre)
        P.barrier()
        if dbg and l == 0:
            d1 = nc.dram_tensor("dbg_oT", [4, 512, T], BF16, kind="ExternalOutput").ap()
            d2 = nc.dram_tensor("dbg_xmid", [T, 1024], F32, kind="ExternalOutput").ap()
            d3 = nc.dram_tensor("dbg_uT", [1024, T], BF16, kind="ExternalOutput").ap()
            for n in range(4):
                P.dma("sp", d1[n], oT_d[n])
            P.dma("act", d2, xmid_d)
            P.dma("act", d3, O["uT"])
            P.barrier()
        x_cur = xo
    P.finish()
    P.emit()
    return P


def fused_host_inputs(inp, core, nlayers=2):
    b, half = core // 2, core % 2
    units, w1cols = k1_weight_layout()
    qa, qb, knope, vcols = mla_up_layout()
    C64, S64, CM, SM = rope_tables(half * T, T)
    rep = lambda v: np.ascontiguousarray(np.broadcast_to(np.asarray(v, np.float32)[None, :], (128, len(v))))
    m = {
        "x": np.ascontiguousarray(inp["x"][b, half * T:(half + 1) * T], dtype=np.float32),
        "cT": to_pc(inp["c"][b], 8), "C64": C64, "S64": S64, "CM": CM, "SM": SM,
        "ident": np.eye(128, dtype=np.float32),
        "iota": np.ascontiguousarray(np.broadcast_to(np.arange(128, dtype=np.float32)[None, :], (128, 128))),
        "swa_masks": swa_masks(half),
    }
    for l in range(nlayers):
        ab = inp["ada_b"][l]
        lam = np.stack([inp["diff_lambda_q1"][l], inp["diff_lambda_k1"][l], inp["diff_lambda_q2"][l], inp["diff_lambda_k2"][l]])
        li = 0.8 - 0.6 * math.exp(-0.3 * l)
        keys = inp["peer_keys"][l].reshape(16, 128, 128)
        d = {
            "adaw": np.ascontiguousarray(inp["ada_w"][l]), "adab1": to_pc(ab[0:2048], 16), "adabg": rep(ab[2048:3072]),
            "adabf": to_pc(ab[3072:5120], 16), "adabg2": rep(ab[5120:6144]),
            "w1": np.ascontiguousarray(inp["w_in"][l][:, w1cols]),
            "qupA": np.ascontiguousarray(inp["mla_q_up"][l][:, qa]), "qupB": np.ascontiguousarray(inp["mla_q_up"][l][:, qb]),
            "kvn": np.ascontiguousarray(inp["mla_kv_up"][l][:, knope]), "kvv": np.ascontiguousarray(inp["mla_kv_up"][l][:, vcols]),
            "gq": to_pc(inp["mla_q_norm"][l], 3), "gkv": to_pc(inp["mla_kv_norm"][l], 2),
            "sinkb": rep(inp["swa_sink"][l]),
            "lamb": np.ascontiguousarray(np.broadcast_to(lam[None], (128, 4, 64))).astype(np.float32),
            "subln": rep(inp["diff_subln"][l]),
            "lami": np.ascontiguousarray(np.broadcast_to(np.array([li, 1.0 - li], np.float32)[None], (128, 2))),
            "nat_bias": nat_bias_tables(inp["nat_rpb"][l], half),
            "wg": np.ascontiguousarray(inp["w_gate"][l]), "wb": np.ascontiguousarray(inp["w_branch"][l]), "wo": np.ascontiguousarray(inp["w_out"][l]),
            "lng1": rep(inp["ln1_g"][l]), "lnb1": rep(inp["ln1_b"][l]),
            "wq": np.ascontiguousarray(inp["peer_wq"][l]), "keysT": np.ascontiguousarray(keys.transpose(2, 0, 1)),
            "UT": np.ascontiguousarray(inp["peer_u"][l].T), "V": np.ascontiguousarray(inp["peer_v"][l]),
            "lng2": rep(inp["ln2_g"][l]), "lnb2": rep(inp["ln2_b"][l]),
        }
        for k, v in d.items():
            m[f"{k}_{l}"] = np.ascontiguousarray(v, dtype=np.float32)
    return m


NCORES = 8
_PROGS = {}


def kernel(**inputs):
    inp = {k: np.asarray(v) for k, v in inputs.items()}
    if "fused" not in _PROGS:
        nc = bass.Bass("TRN2", target_bir_lowering=False)
        build_fused(nc)
        _PROGS["fused"] = nc
    nc = _PROGS["fused"]
    maps = [fused_host_inputs(inp, c) for c in range(NCORES)]
    res = run_bass_kernel_spmd(nc, maps, core_ids=list(range(NCORES)))
    out = np.empty((4, 2 * T, 1024), np.float32)
    for c in range(NCORES):
        b, half = c // 2, c % 2
        out[b, half * T:(half + 1) * T] = np.asarray(res.results[c]["out"])
    return out
```

```python
import numpy as np
import concourse.bass as bass
import concourse.mybir as mybir
from concourse.bass_utils import run_bass_kernel_spmd
from contextlib import ExitStack

F32 = mybir.dt.float32
BF16 = mybir.dt.bfloat16
I32 = mybir.dt.int32
U32 = mybir.dt.uint32
AF = mybir.ActivationFunctionType
ALU = mybir.AluOpType
AX = mybir.AxisListType

ENGS = ["pe", "act", "dve", "pool", "sp"]
DMA_RING = 12


class Buf:
    __slots__ = ("name", "lastw", "readers", "excl", "pre")

    def __init__(self, name="", excl=False):
        self.name = name
        self.excl = excl
        self.pre = []
        self.lastw = []
        self.readers = []


class Prog:
    def __init__(self, nc, same_engine_sync=True):
        self.nc = nc
        self.es = ExitStack()
        self.stack = [self.es]
        self.q = {e: [] for e in ENGS}
        self.cnt = {e: 0 for e in ENGS}
        self.sems = {}
        self.EPOCH = 50000
        self.ring_n = {e: 0 for e in ENGS}
        self.ring_val = {}
        self.seen = {e: {} for e in ENGS}
        self.same_engine_sync = same_engine_sync
        self.n_waits = 0
        self.n_ops = 0
        self.uid = 0
        self.E = {"pe": nc.tensor, "act": nc.scalar, "dve": nc.vector, "pool": nc.gpsimd, "sp": nc.sync}

    def sb(self, name, shape, dtype):
        self.uid += 1
        return self.stack[-1].enter_context(self.nc.sbuf_tensor(f"s{self.uid}_{name}", list(shape), dtype))

    def ps(self, name, shape, dtype=F32):
        self.uid += 1
        return self.stack[-1].enter_context(self.nc.psum_tensor(f"p{self.uid}_{name}", list(shape), dtype))

    def push(self):
        self.stack.append(ExitStack())

    def pop(self):
        self.barrier()
        self.stack.pop().close()

    def barrier(self):
        evs = [self._last_ev(e) for e in ENGS if self.cnt[e] > 0]
        evs += [(k, v) for k, v in self.ring_val.items() if v > 0]
        for e in ENGS:
            for ev in evs:
                self._wait(e, ev)

    def dram(self, name, shape, dtype, kind="Internal"):
        return self.nc.dram_tensor(name, list(shape), dtype, kind=kind).ap()

    def _wait(self, eng, ev):
        key, val = ev
        if key[0] == "eng" and key[1] == eng and (eng == "pe" or not self.same_engine_sync):
            return
        if self.seen[eng].get(key, 0) >= val:
            return
        self.seen[eng][key] = val
        self.E[eng].wait_ge(self.sems[key], val)
        self.n_waits += 1

    def _deps(self, eng, reads, writes):
        for b in reads:
            for ev in b.lastw:
                self._wait(eng, ev)
            if b.excl:
                for ev in b.readers:
                    self._wait(eng, ev)
        for b in writes:
            for ev in b.lastw:
                self._wait(eng, ev)
            for ev in b.readers:
                self._wait(eng, ev)

    def _commit(self, ev, reads, writes):
        for b in writes:
            b.pre = list(b.lastw) + list(b.readers)
            b.lastw = [ev]
            b.readers = []
        for b in reads:
            b.readers = [r for r in b.readers if r[0] != ev[0]] + [ev]

    def _next_ev(self, eng):
        ep, k = divmod(self.cnt[eng], self.EPOCH)
        self.cnt[eng] += 1
        key = ("eng", eng, ep)
        if key not in self.sems:
            self.sems[key] = self.nc.alloc_semaphore(name=f"pg_{eng}_{ep}")
        return (key, k + 1)

    def _last_ev(self, eng):
        if self.cnt[eng] == 0:
            return None
        ep, k = divmod(self.cnt[eng] - 1, self.EPOCH)
        return (("eng", eng, ep), k + 1)

    def collective(self, ins_ap, outs_ap, groups, reads=(), writes=()):
        eng = "pool"
        self._deps(eng, reads, writes)
        k = self.ring_n.get("cc", 0) % 4
        self.ring_n["cc"] = self.ring_n.get("cc", 0) + 1
        key = ("ring", "cc", k)
        if key not in self.sems:
            self.sems[key] = self.nc.alloc_semaphore(name=f"pg_cc_{k}")
            self.ring_val[key] = 0
        prev = self.ring_val[key]
        if prev > 0:
            self._wait(eng, (key, prev))
        val = prev + 1
        self.ring_val[key] = val
        ev = (key, val)
        self.E[eng].collective_compute("AllGather", ALU.bypass, replica_groups=groups, ins=[ins_ap], outs=[outs_ap]).then_inc(self.sems[key])
        self.n_ops += 1
        self._commit(ev, reads, writes)
        return ev

    def op(self, eng, fn, reads=(), writes=()):
        self._deps(eng, reads, writes)
        ev = self._next_ev(eng)
        ins = fn(self.E[eng])
        ins.then_inc(self.sems[ev[0]], 1)
        self.n_ops += 1
        self._commit(ev, reads, writes)
        return ev

    def dma(self, eng, out, in_, reads=(), writes=(), append=False, **kw):
        if append:
            self._deps(eng, reads, ())
            for b in writes:
                for ev in b.pre:
                    self._wait(eng, ev)
                for ev in b.readers:
                    self._wait(eng, ev)
        else:
            self._deps(eng, reads, writes)
        k = self.ring_n[eng] % DMA_RING
        self.ring_n[eng] += 1
        key = ("ring", eng, k)
        if key not in self.sems:
            self.sems[key] = self.nc.alloc_semaphore(name=f"pg_r_{eng}_{k}")
            self.ring_val[key] = 0
        prev = self.ring_val[key]
        if prev > 0:
            self._wait(eng, (key, prev))
        val = prev + 16
        self.ring_val[key] = val
        ev = (key, val)
        self.E[eng].dma_start(out=out, in_=in_, **kw).then_inc(self.sems[key], 16)
        self.n_ops += 1
        if append:
            for b in writes:
                b.lastw = b.lastw + [ev]
            for b in reads:
                b.readers = [r for r in b.readers if r[0] != ev[0]] + [ev]
        else:
            self._commit(ev, reads, writes)
        return ev

    def finish(self):
        for key, val in list(self.ring_val.items()):
            if val > 0:
                self._wait("sp", (key, val))
        for e in ENGS:
            if e != "sp" and self.cnt[e] > 0:
                self._wait("sp", self._last_ev(e))

    def emit(self):
        self.es.close()


T = 4096
NTB = 8
NTT = 32

O_QA, O_KA, O_VA, O_CQ, O_CKV, O_KR, O_QC, O_KC, O_VC, O_QD, O_KD, O_VD = (
    0, 512, 640, 768, 1152, 1408, 1440, 1952, 2464, 2976, 3488, 4000)


class Rot:
    def __init__(self, P, name, shape, dtype, n, space="sb"):
        mk = P.sb if space == "sb" else P.ps
        self.tiles = [mk(f"{name}{i}", shape, dtype) for i in range(n)]
        self.bufs = [Buf(f"{name}{i}", excl=(space == "ps")) for i in range(n)]
        self.i = 0
        self.n = n

    def next(self):
        t, b = self.tiles[self.i % self.n], self.bufs[self.i % self.n]
        self.i += 1
        return t, b


def k1_weight_layout():
    def rot64(cols):
        cols = np.asarray(cols).reshape(-1, 64)
        return np.concatenate([cols[:, 32:], cols[:, :32]], axis=1).reshape(-1)

    units = []
    cols = []

    def add(name, kind, c):
        units.append((name, kind, sum(len(x) for x in cols), len(c)))
        cols.append(np.asarray(c))

    for name, off, n in (("swa_q", O_QA, 512), ("swa_k", O_KA, 128), ("diff_q", O_QC, 512), ("diff_k", O_KC, 512)):
        for ch in range(n // 128):
            a = np.arange(off + ch * 128, off + (ch + 1) * 128)
            add(f"{name}{ch}", "rope", np.concatenate([a, rot64(a)]))
    for name, off in (("nat_q", O_QD), ("nat_k", O_KD)):
        for ch in range(4):
            add(f"{name}{ch}", "plain", np.arange(off + ch * 128, off + (ch + 1) * 128))
    kr = np.arange(O_KR, O_KR + 32)
    krB = np.concatenate([kr[16:], kr[:16]])
    pad = np.arange(O_CQ, O_CQ + 64)
    add("mla", "mla", np.concatenate([np.arange(O_CQ, O_CQ + 384), np.arange(O_CKV, O_CKV + 256),
                                      pad, kr, pad, krB]))
    add("swa_v", "v", np.arange(O_VA, O_VA + 128))
    add("diff_v", "v", np.arange(O_VC, O_VC + 512))
    add("nat_v", "v", np.arange(O_VD, O_VD + 512))
    return units, np.concatenate(cols)


def mla_up_layout():
    qa = np.arange(768)
    qb = qa.copy().reshape(8, 96)
    qb = np.concatenate([qb[:, :64], qb[:, 80:96], qb[:, 64:80]], axis=1).reshape(-1)
    kv = np.arange(1024).reshape(8, 128)
    knope = kv[:, :64].reshape(-1)
    vcols = kv[:, 64:].reshape(-1)
    return qa, qb, knope, vcols


def rope_tables(pos0, n):
    pos = np.arange(pos0, pos0 + n, dtype=np.float32)
    inv64 = (10000.0 ** (-np.arange(0, 64, 2, dtype=np.float32) / 64)).astype(np.float32)
    ang = (pos[None, :] * inv64[:, None]).astype(np.float32)
    c, s = np.cos(ang).astype(np.float32), np.sin(ang).astype(np.float32)
    C64 = np.concatenate([c, c, c, c], 0)
    S64 = np.concatenate([-s, s, -s, s], 0)
    inv32 = (10000.0 ** (-np.arange(0, 32, 2, dtype=np.float32) / 32)).astype(np.float32)
    ang = (pos[None, :] * inv32[:, None]).astype(np.float32)
    c, s = np.cos(ang).astype(np.float32), np.sin(ang).astype(np.float32)
    CM = np.zeros((128, n), np.float32)
    SM = np.zeros((128, n), np.float32)
    CM[64:96] = np.concatenate([c, c], 0)
    SM[64:96] = np.concatenate([-s, s], 0)
    return C64, S64, CM, SM


def emit_mod_cols(P, nc, cT_d, adaw_d, adab_d, ngrp, modc, bmodc, pm_rot, wst_rot):
    cs = P.sb("mod_cs", [128, 8], F32)
    bcs = Buf("cs")
    adab = P.sb("mod_adab", [128, ngrp * 8], F32)
    badab = Buf("adab")
    P.dma("sp", cs[:], cT_d, writes=[bcs])
    P.dma("sp", adab[:], adab_d, writes=[badab])
    P.op("act", lambda e: e.activation(out=cs[:], in_=cs[:], func=AF.Silu), reads=[bcs], writes=[bcs])
    pm, bpm = pm_rot.next()
    noc = ngrp * 8
    for g in range(noc // 2):
        wst_, bw = wst_rot.next()
        wst = wst_[:, 0:2048].rearrange("p (c n) -> p c n", c=8)
        P.dma("sp" if g % 2 == 0 else "act", wst,
              adaw_d[:, g * 256:(g + 1) * 256].rearrange("(c p) n -> p c n", p=128), writes=[bw])
        for o4 in range(2):
            oc = g * 2 + o4
            for kc in range(8):
                P.op("pe", lambda e, oc=oc, kc=kc, o4=o4, wst=wst: e.matmul(
                    pm[:, oc:oc + 1], lhsT=wst[:, kc, o4 * 128:(o4 + 1) * 128], rhs=cs[:, kc:kc + 1],
                    start=(kc == 0), stop=(kc == 7)), reads=[bw, bcs], writes=[bpm])
    P.op("dve", lambda e: e.tensor_tensor(out=modc[:, 0:noc], in0=pm[:, 0:noc], in1=adab[:], op=ALU.add),
         reads=[bpm, badab], writes=[bmodc])


def emit_ln_mod(P, x_d, uT, buT_tiles, shcol, sc1col, bmod, ident, bid, epsb, beps, ptr_rot, tag=""):
    xin = Rot(P, f"ln_x{tag}", [128, 1024], F32, 2)
    xn = Rot(P, f"ln_xn{tag}", [128, 1024], BF16, 2)
    st = Rot(P, f"ln_st{tag}", [128, 2, 6], F32, 2)
    mv = Rot(P, f"ln_mv{tag}", [128, 2], F32, 2)
    rs = Rot(P, f"ln_rs{tag}", [128, 1], F32, 2)
    for t in range(NTT):
        xt, bx = xin.next()
        P.dma("sp", xt[:], x_d[t * 128:(t + 1) * 128, :], writes=[bx])
        s_, bs = st.next()
        for c in range(2):
            P.op("dve", lambda e, c=c, s_=s_, xt=xt: e.bn_stats(out=s_[:, c, :], in_=xt[:, c * 512:(c + 1) * 512]),
                 reads=[bx], writes=[bs])
        m_, bm = mv.next()
        P.op("dve", lambda e, m_=m_, s_=s_: e.bn_aggr(out=m_[:], in_=s_[:]), reads=[bs], writes=[bm])
        r_, br = rs.next()
        P.op("act", lambda e, r_=r_, m_=m_: e.activation(out=r_[:], in_=m_[:, 1:2], func=AF.Ln, bias=epsb[:, 0:1], scale=1.0),
             reads=[bm, beps], writes=[br])
        P.op("act", lambda e, r_=r_: e.activation(out=r_[:], in_=r_[:], func=AF.Exp, scale=-0.5), reads=[br], writes=[br])
        xn_, bxn = xn.next()
        P.op("dve", lambda e, xn_=xn_, xt=xt, m_=m_, r_=r_: e.tensor_scalar(
            out=xn_[:], in0=xt[:], scalar1=m_[:, 0:1], scalar2=r_[:, 0:1], op0=ALU.subtract, op1=ALU.mult),
            reads=[bx, bm, br], writes=[bxn])
        pt, bpt = ptr_rot.next()
        for c in range(8):
            P.op("pe", lambda e, c=c, pt=pt, xn_=xn_: e.transpose(out=pt[:, c, :], in_=xn_[:, c * 128:(c + 1) * 128], identity=ident[:]),
                 reads=[bxn, bid], writes=[bpt])
        for c in range(8):
            P.op("act", lambda e, c=c, pt=pt, t=t: e.activation(
                out=uT[:, c, t * 128:(t + 1) * 128], in_=pt[:, c, :], func=AF.Identity,
                scale=sc1col[:, c:c + 1], bias=shcol[:, c:c + 1]), reads=[bpt, bmod], writes=[buT_tiles[t]])


def k1_io():
    units, w1cols = k1_weight_layout()
    NC1 = len(w1cols)
    ins = [("x", [T, 1024]), ("cT", [128, 8]), ("adaw", [1024, 2048]), ("adab", [128, 16]), ("w1", [1024, NC1]),
           ("qupA", [384, 768]), ("qupB", [384, 768]), ("kvn", [256, 512]), ("kvv", [256, 512]), ("gq", [128, 3]), ("gkv", [128, 2]),
           ("C64", [128, T]), ("S64", [128, T]), ("CM", [128, T]), ("SM", [128, T]), ("ident", [128, 128])]
    outs = [("uT", [1024, T]), ("swa_qT", [512, T]), ("swa_kT", [128, T]), ("diff_qT", [512, T]), ("diff_kT", [512, T]),
            ("nat_qT", [512, T]), ("nat_kT", [512, T]), ("mla_qT", [8, 96, T]), ("mla_kT", [8, 96, T]),
            ("swa_v", [NTT, 128, 2 * 65]), ("diff_v", [NTT, 128, 4 * 129]), ("nat_v", [NTT, 128, 8 * 65]), ("mla_v", [NTT, 128, 8 * 65])]
    return ins, outs


def build_k1(nc, stop=None):
    P = Prog(nc)
    ins, outs = k1_io()
    D = {n: nc.dram_tensor(n, list(s), F32, kind="ExternalInput").ap() for n, s in ins}
    O = {n: nc.dram_tensor(n, list(s), BF16, kind="ExternalOutput").ap() for n, s in outs}
    emit_k1(P, nc, D, O, stop)
    P.finish()
    P.emit()
    return P


def emit_k1(P, nc, D, O, stop=None):
    units, w1cols = k1_weight_layout()
    x_d, cT_d, adaw_d, adab_d, w1_d = D["x"], D["cT"], D["adaw"], D["adab"], D["w1"]
    qupA_d, qupB_d, kvn_d, kvv_d, gq_d, gkv_d = D["qupA"], D["qupB"], D["kvn"], D["kvv"], D["gq"], D["gkv"]
    C64_d, S64_d, CM_d, SM_d, ident_d = D["C64"], D["S64"], D["CM"], D["SM"], D["ident"]
    o_uT = O["uT"]
    o = {"swa_q": O["swa_qT"], "swa_k": O["swa_kT"], "diff_q": O["diff_qT"], "diff_k": O["diff_kT"],
         "nat_q": O["nat_qT"], "nat_k": O["nat_kT"], "mla_q": O["mla_qT"], "mla_k": O["mla_kT"],
         "swa_v": O["swa_v"], "diff_v": O["diff_v"], "nat_v": O["nat_v"], "mla_v": O["mla_v"]}

    identf = P.sb("identf", [128, 128], F32); bidf = Buf()
    ident = P.sb("ident", [128, 128], BF16); bid = Buf()
    onesb = P.sb("onesb", [128, 128], BF16); bones = Buf()
    epsb = P.sb("epsb", [128, 1], F32); beps = Buf()
    P.dma("pool", identf[:], ident_d, writes=[bidf])
    P.op("dve", lambda e: e.tensor_copy(out=ident[:], in_=identf[:]), reads=[bidf], writes=[bid])
    P.op("pool", lambda e: e.memset(onesb[:], 1.0), writes=[bones])
    P.op("pool", lambda e: e.memset(epsb[:], 1e-5), writes=[beps])

    uT = P.sb("uT", [128, 8, T], BF16)
    buT = [Buf(f"uT{t}") for t in range(NTT)]
    modc = P.sb("modc", [128, 16], F32); bmodc = Buf()
    sc1 = P.sb("sc1", [128, 8], F32); bsc1 = Buf()

    pA = Rot(P, "pA", [128, 512], F32, 2, "ps")
    pB = Rot(P, "pB", [128, 512], F32, 2, "ps")
    ptr = Rot(P, "ptr", [128, 8, 128], BF16, 2, "ps")
    pmisc = Rot(P, "pmisc", [128, 512], F32, 1, "ps")

    wst = Rot(P, "wst", [128, 2048], F32, 2)
    wbf = Rot(P, "wbf", [128, 8, 256], BF16, 2)

    emit_mod_cols(P, nc, cT_d, adaw_d, adab_d, 2, modc, bmodc, pmisc, wst)
    P.op("dve", lambda e: e.tensor_scalar(out=sc1[:], in0=modc[:, 8:16], scalar1=1.0, scalar2=None, op0=ALU.add),
         reads=[bmodc], writes=[bsc1])
    bmod = Buf("mod")
    P.op("dve", lambda e: e.tensor_copy(out=modc[:, 0:8], in_=modc[:, 0:8]), reads=[bmodc, bsc1], writes=[bmod])

    if stop == "mod":
        return
    emit_ln_mod(P, x_d, uT, buT, modc, sc1, bmod, ident, bid, epsb, beps, ptr)
    if stop == "ln":
        return

    for c in range(8):
        P.dma("pool", o_uT[c * 128:(c + 1) * 128, :], uT[:, c, :], reads=buT)

    c64r = Rot(P, "c64r", [128, 512], F32, 2)
    s64r = Rot(P, "s64r", [128, 512], F32, 2)

    t1r = Rot(P, "t1r", [128, 512], F32, 2)
    t2r = Rot(P, "t2r", [128, 512], F32, 2)
    ostg = Rot(P, "ostg", [128, 512], BF16, 3)

    def load_w(col0, ncols):
        ws_, bws = wst.next()
        wb, bwb = wbf.next()
        ws = ws_[:, 0:8 * ncols].rearrange("p (c n) -> p c n", c=8)
        h = ncols // 2
        P.dma("sp", ws[:, :, 0:h], w1_d[:, col0:col0 + h].rearrange("(c p) n -> p c n", p=128), writes=[bws])
        P.dma("sp", ws[:, :, h:ncols], w1_d[:, col0 + h:col0 + ncols].rearrange("(c p) n -> p c n", p=128),
              writes=[bws], append=True)
        P.op("pool", lambda e: e.tensor_copy(out=wb[:, :, 0:ncols], in_=ws), reads=[bws], writes=[bwb])
        return wb, bwb

    def mm_fm(ps, bps, wb, bwb, c0, M, tb):
        for kc in range(8):
            P.op("pe", lambda e, kc=kc: e.matmul(ps[0:M, :], lhsT=wb[:, kc, c0:c0 + M], rhs=uT[:, kc, tb * 512:(tb + 1) * 512],
                                               start=(kc == 0), stop=(kc == 7)),
                 reads=[bwb] + buT[tb * 4:(tb + 1) * 4], writes=[bps])

    dq = ["act", "pool"]
    dqi = [0]

    def nextq():
        dqi[0] += 1
        return dq[dqi[0] % 2]

    for (name, kind, col0, ncols) in units:
        if stop is not None and stop == name:
            break
        if kind == "rope":
            base = name[:-1]; ch = int(name[-1])
            wb, bwb = load_w(col0, 256)
            for tb in range(NTB):
                a, ba = pA.next(); b, bb = pB.next()
                mm_fm(a, ba, wb, bwb, 0, 128, tb)
                mm_fm(b, bb, wb, bwb, 128, 128, tb)
                t1, bt1 = t1r.next(); t2, bt2 = t2r.next()
                sl = slice(tb * 512, (tb + 1) * 512)
                C64, bc64 = c64r.next(); S64, bs64 = s64r.next()
                P.dma("sp", C64[:], C64_d[:, sl], writes=[bc64])
                P.dma("sp", S64[:], S64_d[:, sl], writes=[bs64])
                P.op("dve", lambda e, t1=t1, a=a, C64=C64: e.tensor_tensor(out=t1[:], in0=a[:], in1=C64[:], op=ALU.mult),
                     reads=[ba, bc64], writes=[bt1])
                P.op("dve", lambda e, t2=t2, b=b, S64=S64: e.tensor_tensor(out=t2[:], in0=b[:], in1=S64[:], op=ALU.mult),
                     reads=[bb, bs64], writes=[bt2])
                og, bog = ostg.next()
                P.op("pool", lambda e, og=og, t1=t1, t2=t2: e.tensor_tensor(out=og[:], in0=t1[:], in1=t2[:], op=ALU.add),
                     reads=[bt1, bt2], writes=[bog])
                P.dma("pool", o[base][ch * 128:(ch + 1) * 128, sl], og[:], reads=[bog])
        elif kind == "plain":
            base = name[:-1]; ch = int(name[-1])
            wb, bwb = load_w(col0, 128)
            scale = 0.125 if base == "nat_q" else 1.0
            for tb in range(NTB):
                a, ba = pA.next()
                mm_fm(a, ba, wb, bwb, 0, 128, tb)
                og, bog = ostg.next()
                sl = slice(tb * 512, (tb + 1) * 512)
                P.op("act", lambda e, og=og, a=a, scale=scale: e.activation(out=og[:], in_=a[:], func=AF.Copy, scale=scale),
                     reads=[ba], writes=[bog])
                P.dma("act", o[base][ch * 128:(ch + 1) * 128, sl], og[:], reads=[bog])
        elif kind == "v":
            H, dv = {"swa_v": (2, 64), "diff_v": (4, 128), "nat_v": (8, 64)}[name]
            vst = Rot(P, f"vst_{name}", [128, H, dv + 1], BF16, 2)
            for vt, vb in zip(vst.tiles, vst.bufs):
                P.op("pool", lambda e, vt=vt: e.memset(vt[:], 1.0), writes=[vb])
            nsub = max(1, ncols // 256)
            sc = ncols // nsub
            hs = H // nsub
            wbs = [load_w(col0 + i * sc, sc) for i in range(nsub)] if nsub <= 2 else None
            assert wbs is not None
            for t in range(NTT):
                vt, vb = vst.next()
                for i in range(nsub):
                    wb, bwb = wbs[i]
                    a, ba = pA.next()
                    for kc in range(8):
                        P.op("pe", lambda e, kc=kc, a=a, t=t, wb=wb: e.matmul(a[:, 0:sc], lhsT=uT[:, kc, t * 128:(t + 1) * 128],
                                                                     rhs=wb[:, kc, 0:sc], start=(kc == 0), stop=(kc == 7)),
                             reads=[bwb, buT[t]], writes=[ba])
                    P.op("act", lambda e, vt=vt, a=a, i=i: e.activation(
                        out=vt[:, i * hs:(i + 1) * hs, 0:dv], in_=a[:, 0:sc].rearrange("p (h d) -> p h d", h=hs), func=AF.Copy),
                        reads=[ba], writes=[vb])
                P.dma("act", o[name][t], vt[:].rearrange("p h d -> p (h d)"), reads=[vb])
        elif kind == "mla":
            emit_mla(P, locals())


def emit_mla(P, L):
    nc = P.nc
    (w1_d, uT, buT, pA, pB, pmisc, o, onesb, bones, epsb, beps, qupA_d, qupB_d, kvn_d, kvv_d, gq_d, gkv_d,
     CM_d, SM_d, col0, nextq, ostg) = (L[k] for k in (
         "w1_d", "uT", "buT", "pA", "pB", "pmisc", "o", "onesb", "bones", "epsb", "beps", "qupA_d", "qupB_d",
         "kvn_d", "kvv_d", "gq_d", "gkv_d", "CM_d", "SM_d", "col0", "nextq", "ostg"))
    L = dict(L)
    NCM = 384 + 256 + 96 + 96
    wst = L["wst"]
    wm = P.sb("mla_w", [128, 8, NCM], BF16); bwm = Buf()
    first = True
    for i in range(4):
        ws_, bws = wst.next()
        ws = ws_[:, 0:8 * 208].rearrange("p (c n) -> p c n", c=8)
        P.dma("sp" if i % 2 == 0 else "act", ws, w1_d[:, col0 + i * 208:col0 + (i + 1) * 208].rearrange("(c p) n -> p c n", p=128), writes=[bws])
        P.op("pool", lambda e, ws=ws, i=i: e.tensor_copy(out=wm[:, :, i * 208:(i + 1) * 208], in_=ws), reads=[bws], writes=[bwm])
    qA = P.sb("mla_qA", [128, 3, 768], BF16); bqA = Buf()
    qB = P.sb("mla_qB", [128, 3, 768], BF16); bqB = Buf()
    kvn = P.sb("mla_kvn", [128, 2, 512], BF16); bkvn = Buf()
    kvv = P.sb("mla_kvv", [128, 2, 512], BF16); bkvv = Buf()
    for (src, dst, bdst, nch, ncol) in ((qupA_d, qA, bqA, 3, 768), (qupB_d, qB, bqB, 3, 768)):
        for hh in range(2):
            ws_, bws = wst.next()
            ws = ws_[:, 0:nch * 384].rearrange("p (c n) -> p c n", c=nch)
            P.dma("sp", ws, src[:, hh * 384:(hh + 1) * 384].rearrange("(c p) n -> p c n", p=128), writes=[bws])
            P.op("pool", lambda e, ws=ws, dst=dst, hh=hh: e.tensor_copy(out=dst[:, :, hh * 384:(hh + 1) * 384], in_=ws), reads=[bws], writes=[bdst])
    for (src, dst, bdst) in ((kvn_d, kvn, bkvn), (kvv_d, kvv, bkvv)):
        ws_, bws = wst.next()
        ws = ws_[:, 0:1024].rearrange("p (c n) -> p c n", c=2)
        P.dma("act", ws, src.rearrange("(c p) n -> p c n", p=128), writes=[bws])
        P.op("pool", lambda e, ws=ws, dst=dst: e.tensor_copy(out=dst[:], in_=ws), reads=[bws], writes=[bdst])
    gq = P.sb("mla_gq", [128, 3], F32); gkv = P.sb("mla_gkv", [128, 2], F32); bg = Buf()
    P.dma("pool", gq[:], gq_d, writes=[bg])
    P.dma("pool", gkv[:], gkv_d, writes=[bg], append=True)
    epsq = P.sb("mla_epsq", [128, 1], F32)
    cg = Rot(P, "mla_cg", [128, 5, 512], BF16, 1)
    sq = Rot(P, "mla_sq", [128, 5, 512], BF16, 1)
    rq = Rot(P, "mla_rq", [128, 512], F32, 1)
    rkv = Rot(P, "mla_rkv", [128, 512], F32, 1)
    rkvt = Rot(P, "mla_rkvt", [128, 4], F32, 2)
    cmt = Rot(P, "mla_cm", [128, 512], F32, 1); smt = Rot(P, "mla_sm", [128, 512], F32, 1)
    cr = Rot(P, "mla_cr", [128, 512], F32, 1); sr = Rot(P, "mla_sr", [128, 512], F32, 1)
    tA = Rot(P, "mla_tA", [128, 512], F32, 1); tB = Rot(P, "mla_tB", [128, 512], F32, 1)
    krs = Rot(P, "mla_krs", [128, 512], BF16, 2)
    vst = Rot(P, "mla_vst", [128, 8, 65], BF16, 2)
    for vt, vb in zip(vst.tiles, vst.bufs):
        P.op("pool", lambda e, vt=vt: e.memset(vt[:], 1.0), writes=[vb])

    import os
    MS = int(os.environ.get("MLASTOP", "99"))
    for tb in range(NTB):
        sl = slice(tb * 512, (tb + 1) * 512)
        ubufs = buT[tb * 4:(tb + 1) * 4]
        cg_, bcg = cg.next(); sq_, bsq = sq.next()
        if MS <= 0: continue
        for j in range(5):
            a, ba = pA.next()
            for kc in range(8):
                P.op("pe", lambda e, kc=kc, a=a, j=j: e.matmul(a[:], lhsT=wm[:, kc, j * 128:(j + 1) * 128], rhs=uT[:, kc, sl],
                                                            start=(kc == 0), stop=(kc == 7)), reads=[bwm] + ubufs, writes=[ba])
            gcol = gq[:, j:j + 1] if j < 3 else gkv[:, j - 3:j - 2]
            VAR = os.environ.get("MLAVAR", "AB")
            if "A" in VAR:
                P.op("act", lambda e, a=a, j=j, sq_=sq_: e.activation(out=sq_[:, j, :], in_=a[:], func=AF.Square), reads=[ba], writes=[bsq])
            if "B" in VAR:
              P.op("dve", lambda e, a=a, j=j, cg_=cg_, gcol=gcol: e.tensor_scalar(out=cg_[:, j, :], in0=a[:], scalar1=gcol, scalar2=None, op0=ALU.mult),
                 reads=[ba, bg], writes=[bcg])
        if MS <= 1: continue
        rq_, brq = rq.next(); rkv_, brkv = rkv.next(); rkt, brkt = rkvt.next()
        for (r_, br_, js, dim) in ((rq_, brq, (0, 1, 2), 384.0), (rkv_, brkv, (3, 4), 256.0)):
            pm, bpm = pmisc.next()
            for i, j in enumerate(js):
                P.op("pe", lambda e, pm=pm, j=j, i=i, n=len(js): e.matmul(pm[:], lhsT=onesb[:], rhs=sq_[:, j, :], start=(i == 0), stop=(i == n - 1)),
                     reads=[bones, bsq], writes=[bpm])
            P.op("act", lambda e, r_=r_, pm=pm, dim=dim: e.activation(out=r_[:], in_=pm[:], func=AF.Ln, bias=epsb[:, 0:1], scale=1.0 / dim),
                 reads=[bpm, beps], writes=[br_])
            P.op("act", lambda e, r_=r_: e.activation(out=r_[:], in_=r_[:], func=AF.Exp, scale=-0.5), reads=[br_], writes=[br_])
        if MS <= 2: continue
        pm, bpm = pmisc.next()
        for tt in range(4):
            for i, j in enumerate((3, 4)):
                P.op("pe", lambda e, pm=pm, tt=tt, j=j, i=i: e.matmul(pm[:, tt:tt + 1], lhsT=sq_[:, j, tt * 128:(tt + 1) * 128], rhs=onesb[:, 0:1],
                                                                   start=(i == 0), stop=(i == 1)), reads=[bones, bsq], writes=[bpm])
        P.op("act", lambda e, rkt=rkt, pm=pm: e.activation(out=rkt[:], in_=pm[:, 0:4], func=AF.Ln, bias=epsb[:, 0:1], scale=1.0 / 256.0),
             reads=[bpm, beps], writes=[brkt])
        P.op("act", lambda e, rkt=rkt: e.activation(out=rkt[:], in_=rkt[:], func=AF.Exp, scale=-0.5), reads=[brkt], writes=[brkt])
        if MS <= 3: continue
        cm_, bcm = cmt.next(); sm_, bsm = smt.next()
        P.dma("sp", cm_[:], CM_d[:, sl], writes=[bcm])
        P.dma("sp", sm_[:], SM_d[:, sl], writes=[bsm])
        cr_, bcr = cr.next(); sr_, bsr = sr.next()
        P.op("pool", lambda e, cr_=cr_, cm_=cm_, rq_=rq_: e.tensor_tensor(out=cr_[64:96, :], in0=cm_[64:96, :], in1=rq_[64:96, :], op=ALU.mult),
             reads=[bcm, brq], writes=[bcr])
        P.op("pool", lambda e, sr_=sr_, sm_=sm_, rq_=rq_: e.tensor_tensor(out=sr_[64:96, :], in0=sm_[64:96, :], in1=rq_[64:96, :], op=ALU.mult),
             reads=[bsm, brq], writes=[bsr])
        if MS <= 4: continue
        for h in range(8):
            a, ba = pA.next(); b, bb = pB.next()
            for j in range(3):
                P.op("pe", lambda e, a=a, j=j, h=h: e.matmul(a[0:96, :], lhsT=qA[:, j, h * 96:(h + 1) * 96], rhs=cg_[:, j, :], start=(j == 0), stop=(j == 2)),
                     reads=[bqA, bcg], writes=[ba])
            for j in range(3):
                P.op("pe", lambda e, b=b, j=j, h=h: e.matmul(b[0:96, :], lhsT=qB[:, j, h * 96:(h + 1) * 96], rhs=cg_[:, j, :], start=(j == 0), stop=(j == 2)),
                     reads=[bqB, bcg], writes=[bb])
            og, bog = ostg.next()
            tA_, btA = tA.next(); tB_, btB = tB.next()
            P.op("dve", lambda e, og=og, a=a, rq_=rq_: e.tensor_tensor(out=og[0:64, :], in0=a[0:64, :], in1=rq_[0:64, :], op=ALU.mult),
                 reads=[ba, brq], writes=[bog])
            P.op("dve", lambda e, tA_=tA_, a=a, cr_=cr_: e.tensor_tensor(out=tA_[64:96, :], in0=a[64:96, :], in1=cr_[64:96, :], op=ALU.mult),
                 reads=[ba, bcr], writes=[btA])
            P.op("dve", lambda e, tB_=tB_, b=b, sr_=sr_: e.tensor_tensor(out=tB_[64:96, :], in0=b[64:96, :], in1=sr_[64:96, :], op=ALU.mult),
                 reads=[bb, bsr], writes=[btB])
            P.op("pool", lambda e, og=og, tA_=tA_, tB_=tB_: e.tensor_tensor(out=og[64:96, :], in0=tA_[64:96, :], in1=tB_[64:96, :], op=ALU.add),
                 reads=[btA, btB, bog], writes=[bog])
            P.dma("pool", o["mla_q"][h, :, sl], og[0:96, :], reads=[bog])
        if MS <= 5: continue
        a, ba = pA.next(); b, bb = pB.next()
        for kc in range(8):
            P.op("pe", lambda e, kc=kc, a=a: e.matmul(a[0:96, :], lhsT=wm[:, kc, 640:736], rhs=uT[:, kc, sl], start=(kc == 0), stop=(kc == 7)),
                 reads=[bwm] + ubufs, writes=[ba])
        for kc in range(8):
            P.op("pe", lambda e, kc=kc, b=b: e.matmul(b[0:96, :], lhsT=wm[:, kc, 736:832], rhs=uT[:, kc, sl], start=(kc == 0), stop=(kc == 7)),
                 reads=[bwm] + ubufs, writes=[bb])
        tA_, btA = tA.next(); tB_, btB = tB.next()
        P.op("dve", lambda e, tA_=tA_, a=a, cm_=cm_: e.tensor_tensor(out=tA_[64:96, :], in0=a[64:96, :], in1=cm_[64:96, :], op=ALU.mult),
             reads=[ba, bcm], writes=[btA])
        P.op("dve", lambda e, tB_=tB_, b=b, sm_=sm_: e.tensor_tensor(out=tB_[64:96, :], in0=b[64:96, :], in1=sm_[64:96, :], op=ALU.mult),
             reads=[bb, bsm], writes=[btB])
        kr_, bkr = krs.next()
        P.op("pool", lambda e, kr_=kr_, tA_=tA_, tB_=tB_: e.tensor_tensor(out=kr_[64:96, :], in0=tA_[64:96, :], in1=tB_[64:96, :], op=ALU.add),
             reads=[btA, btB], writes=[bkr])
        for h in range(8):
            P.dma("pool", o["mla_k"][h, 64:96, sl], kr_[64:96, :], reads=[bkr])
        if MS <= 6: continue
        for h in range(8):
            a, ba = pA.next()
            for j in range(2):
                P.op("pe", lambda e, a=a, j=j, h=h: e.matmul(a[0:64, :], lhsT=kvn[:, j, h * 64:(h + 1) * 64], rhs=cg_[:, 3 + j, :], start=(j == 0), stop=(j == 1)),
                     reads=[bkvn, bcg], writes=[ba])
            og, bog = ostg.next()
            P.op("dve", lambda e, og=og, a=a, rkv_=rkv_: e.tensor_tensor(out=og[0:64, :], in0=a[0:64, :], in1=rkv_[0:64, :], op=ALU.mult),
                 reads=[ba, brkv], writes=[bog])
            P.dma(nextq(), o["mla_k"][h, 0:64, sl], og[0:64, :], reads=[bog])
        if MS <= 7: continue
        for tt in range(4):
            a, ba = pA.next()
            for j in range(2):
                P.op("pe", lambda e, a=a, j=j, tt=tt: e.matmul(a[:], lhsT=cg_[:, 3 + j, tt * 128:(tt + 1) * 128], rhs=kvv[:, j, :], start=(j == 0), stop=(j == 1)),
                     reads=[bkvv, bcg], writes=[ba])
            vt, vb = vst.next()
            P.op("act", lambda e, vt=vt, a=a, rkt=rkt, tt=tt: e.activation(
                out=vt[:, :, 0:64], in_=a[:].rearrange("p (h d) -> p h d", h=8), func=AF.Copy, scale=rkt[:, tt:tt + 1]),
                reads=[ba, brkt], writes=[vb])
            P.dma("act", o["mla_v"][tb * 4 + tt], vt[:].rearrange("p h d -> p (h d)"), reads=[vb])


def to_pc(v, ncol):
    return np.ascontiguousarray(np.asarray(v).reshape(ncol, 128).T)


def k1_host_inputs(inp, l, core, xcur):
    b, half = core // 2, core % 2
    units, w1cols = k1_weight_layout()
    qa, qb, knope, vcols = mla_up_layout()
    C64, S64, CM, SM = rope_tables(half * T, T)
    m = {
        "x": np.ascontiguousarray(xcur[b, half * T:(half + 1) * T]),
        "cT": to_pc(inp["c"][b], 8),
        "adaw": np.ascontiguousarray(inp["ada_w"][l][:, 0:2048]),
        "adab": to_pc(inp["ada_b"][l][0:2048], 16),
        "w1": np.ascontiguousarray(inp["w_in"][l][:, w1cols]),
        "qupA": np.ascontiguousarray(inp["mla_q_up"][l][:, qa]),
        "qupB": np.ascontiguousarray(inp["mla_q_up"][l][:, qb]),
        "kvn": np.ascontiguousarray(inp["mla_kv_up"][l][:, knope]),
        "kvv": np.ascontiguousarray(inp["mla_kv_up"][l][:, vcols]),
        "gq": to_pc(inp["mla_q_norm"][l], 3),
        "gkv": to_pc(inp["mla_kv_norm"][l], 2),
        "C64": C64, "S64": S64, "CM": CM, "SM": SM,
        "ident": np.eye(128, dtype=np.float32),
    }
    return m


NEGM = -30000.0


class AttnCtx:
    pass


def attn_block(P, C, rhs_q, bq, N, ktiles, scale, acc, bacc, nsub, v_of, pt_cols=None):
    n = len(ktiles)
    LA = 2
    pend = {}
    for i in range(n + LA):
        if i < n:
            kT_ap, kb, vkey, vb, bias_ap, bb = ktiles[i]
            st, bst = C.ST.next()
            P.op("pe", lambda e, st=st, kT_ap=kT_ap, bias_ap=bias_ap: e.matmul(
                st[:, 0:N], lhsT=kT_ap, rhs=rhs_q, start=True, stop=(bias_ap is None)),
                reads=list(kb) + list(bq), writes=[bst])
            if bias_ap is not None:
                P.op("pe", lambda e, st=st, bias_ap=bias_ap: e.matmul(
                    st[:, 0:N], lhsT=C.ident[:], rhs=bias_ap, start=False, stop=True),
                    reads=list(bb) + [C.bid], writes=[bst])
            pt, bpt = C.PT.next()
            P.op("act", lambda e, st=st, pt=pt: e.activation(out=pt[:, 0:N], in_=st[:, 0:N], func=AF.Exp, scale=scale),
                 reads=[bst], writes=[bpt])
            pend[i] = (pt, bpt, vkey, vb)
        if i >= LA:
            k = i - LA
            pt, bpt, vkey, vb = pend.pop(k)
            for j in range(nsub):
                P.op("pe", lambda e, pt=pt, j=j, vkey=vkey, k=k: e.matmul(
                    acc(j), lhsT=pt[:, j * 128:(j + 1) * 128], rhs=v_of(vkey, j), start=(k == 0), stop=(k == n - 1)),
                    reads=[bpt] + list(vb), writes=[bacc])


def emit_oT(P, C, o_tok, bo, oT_d):
    for tb in range(NTB):
        stg, bs = C.oTs.next()
        for tt in range(4):
            t = tb * 4 + tt
            ptr, bp = C.ptr.next()
            for c in range(4):
                P.op("pe", lambda e, c=c, t=t, ptr=ptr: e.transpose(out=ptr[:, c, :], in_=o_tok[:, t, c * 128:(c + 1) * 128], identity=C.ident[:]),
                     reads=[bo, C.bid], writes=[bp])
            P.op("dve", lambda e, ptr=ptr, stg=stg, tt=tt: e.tensor_copy(out=stg[:, :, tt * 128:(tt + 1) * 128], in_=ptr[:]),
                 reads=[bp], writes=[bs])
        P.dma("sp" if tb % 2 == 0 else "act", oT_d.rearrange("(c p) t -> p c t", p=128)[:, :, tb * 512:(tb + 1) * 512], stg[:], reads=[bs])


def build_k2a(nc, which=("swa", "mla", "diff", "nat")):
    P = Prog(nc)
    din = lambda n, s, d=BF16: nc.dram_tensor(n, list(s), d, kind="ExternalInput").ap()
    dout = lambda n, s, d=BF16: nc.dram_tensor(n, list(s), d, kind="ExternalOutput").ap()
    D = {}
    D["swa_qT"] = din("swa_qT", [512, T]); D["swa_kT"] = din("swa_kT", [128, 34 * 128]); D["swa_v"] = din("swa_v", [34, 128, 130])
    D["swa_masks"] = din("swa_masks", [4, 128, 512], F32); D["sinkb"] = din("sinkb", [128, 8], F32)
    D["mla_qT"] = din("mla_qT", [8, 96, T]); D["mla_kT"] = din("mla_kT", [8, 96, 2 * T]); D["mla_v"] = din("mla_v", [64, 128, 520])
    D["diff_qT"] = din("diff_qT", [512, T]); D["diff_kT"] = din("diff_kT", [512, 2 * T]); D["diff_v"] = din("diff_v", [64, 128, 516])
    D["lamb"] = din("lamb", [128, 4, 64], F32); D["subln"] = din("subln", [128, 128], F32); D["lami"] = din("lami", [128, 2], F32)
    D["nat_qT"] = din("nat_qT", [512, T]); D["nat_kT"] = din("nat_kT", [512, 36 * 128]); D["nat_v"] = din("nat_v", [36, 128, 520])
    D["nat_bias"] = din("nat_bias", [8, 128, 29 * 128], F32)
    D["ident"] = din("ident", [128, 128], F32)
    oT_d = dout("oT", [4, 512, T])
    emit_attention(P, D, oT_d, which)
    P.finish()
    P.emit()
    return P


class HostSrc:
    def __init__(self, D):
        self.D = D

    def swa_k(self, kT, kv):
        return [(kT[:, kv, :], self.D["swa_kT"][kv * 64:(kv + 1) * 64, :])]

    def swa_v(self, V):
        return [(V[:], self.D["swa_v"].rearrange("t p f -> p t f"))]

    def mla_k(self, kT, h):
        return [(kT[:, 0:T], self.D["mla_kT"][h, :, 0:T]), (kT[:, T:2 * T], self.D["mla_kT"][h, :, T:2 * T])]

    def mla_v(self, V, h):
        return [(V[:], self.D["mla_v"][:, :, h * 65:(h + 1) * 65].rearrange("t p f -> p t f"))]

    def diff_k(self, kT, row):
        return [(kT[:, 0:T], self.D["diff_kT"][row:row + 64, 0:T]), (kT[:, T:2 * T], self.D["diff_kT"][row:row + 64, T:2 * T])]

    def diff_v(self, V, h):
        return [(V[:], self.D["diff_v"][:, :, h * 129:(h + 1) * 129].rearrange("t p f -> p t f"))]

    def nat_k(self, kT, h):
        return [(kT[:], self.D["nat_kT"][h * 64:(h + 1) * 64, :])]

    def nat_v(self, V, h):
        return [(V[:], self.D["nat_v"][:, :, h * 65:(h + 1) * 65].rearrange("t p f -> p t f"))]


class GatherSrc:
    def __init__(self, O, G):
        self.O = O; self.G = G

    def swa_k(self, kT, kv):
        g = self.G["swa_kT"][0]
        r = slice(kv * 64, (kv + 1) * 64); r1 = slice(128 + kv * 64, 128 + (kv + 1) * 64)
        return [(kT[:, kv, 0:128], g[r, T - 128:T]), (kT[:, kv, 128:128 + T], self.O["swa_kT"][r, :]),
                (kT[:, kv, 128 + T:256 + T], g[r1, 0:128])]

    def swa_v(self, V):
        g = self.G["swa_v"][0]
        return [(V[:, 0, :], g[T - 128:T, :]), (V[:, 1:33, :], self.O["swa_v"].rearrange("t p f -> p t f")),
                (V[:, 33, :], g[T:T + 128, :])]

    def mla_k(self, kT, h):
        g = self.G["mla_kT"][h // 2]
        r0 = (h % 2) * 96
        return [(kT[:, 0:T], g[r0:r0 + 96, :]), (kT[:, T:2 * T], g[192 + r0:192 + r0 + 96, :])]

    def mla_v(self, V, h):
        out = []
        for k in range(4):
            g = self.G["mla_v"][k]
            for r in range(2):
                out.append((V[:, r * 32 + k * 8:r * 32 + k * 8 + 8, :],
                            g[r * 1024:(r + 1) * 1024, h * 65:(h + 1) * 65].rearrange("(t p) f -> p t f", p=128)))
        return out

    def diff_k(self, kT, row):
        g = self.G["diff_kT"][row // 128]
        r0 = row % 128
        return [(kT[:, 0:T], g[r0:r0 + 64, :]), (kT[:, T:2 * T], g[128 + r0:128 + r0 + 64, :])]

    def diff_v(self, V, h):
        out = []
        for k in range(4):
            g = self.G["diff_v"][k]
            for r in range(2):
                out.append((V[:, r * 32 + k * 8:r * 32 + k * 8 + 8, :],
                            g[r * 1024:(r + 1) * 1024, h * 129:(h + 1) * 129].rearrange("(t p) f -> p t f", p=128)))
        return out

    def nat_k(self, kT, h):
        g = self.G["nat_kT"][h // 2]
        r0 = (h % 2) * 64
        return [(kT[:, 0:256], g[r0:r0 + 64, T - 256:T]), (kT[:, 256:256 + T], self.O["nat_kT"][h * 64:(h + 1) * 64, :]),
                (kT[:, 256 + T:512 + T], g[128 + r0:128 + r0 + 64, 0:256])]

    def nat_v(self, V, h):
        gl = self.G["nat_v"][1]
        gf = self.G["nat_v"][0]
        c = slice(h * 65, (h + 1) * 65)
        return [(V[:, 0:2, :], gl[768:1024, c].rearrange("(t p) f -> p t f", p=128)),
                (V[:, 2:34, :], self.O["nat_v"][:, :, c].rearrange("t p f -> p t f")),
                (V[:, 34:36, :], gf[1024:1280, c].rearrange("(t p) f -> p t f", p=128))]


def emit_attention(P, D, oT_d, which=("swa", "mla", "diff", "nat"), SRC=None, bg=None):
    if SRC is None:
        SRC = HostSrc(D)

    def bg_step():
        if bg is not None:
            next(bg, None)
    C = AttnCtx()
    identf = P.sb("a_identf", [128, 128], F32); bidf = Buf()
    C.ident = P.sb("a_ident", [128, 128], BF16); C.bid = Buf()
    P.dma("pool", identf[:], D["ident"], writes=[bidf])
    P.op("dve", lambda e: e.tensor_copy(out=C.ident[:], in_=identf[:]), reads=[bidf], writes=[C.bid])
    epsb = P.sb("a_eps", [128, 1], F32); beps = Buf()
    P.op("pool", lambda e: e.memset(epsb[:], 1e-5), writes=[beps])
    C.ST = Rot(P, "a_ST", [128, 512], F32, 3, "ps")
    C.PT = Rot(P, "a_PT", [128, 512], BF16, 3)
    C.acc = Rot(P, "a_acc", [128, 4, 512], F32, 1, "ps")
    C.ptr = Rot(P, "a_ptr", [128, 4, 128], BF16, 1, "ps")
    C.oTs = Rot(P, "a_oTs", [128, 4, 512], BF16, 2)
    o_tok = P.sb("a_otok", [128, NTT, 512], BF16); bo = Buf("otok")
    rz = Rot(P, "a_rz", [128, 4, 1], F32, 4)

    if "swa" in which:
        P.push()
        qT = P.sb("swa_q", [64, 8, T], BF16); bq = Buf()
        for h in range(8):
            P.dma(("sp", "act", "pool")[h % 3], qT[:, h, :], D["swa_qT"][h * 64:(h + 1) * 64, :], writes=[bq], append=(h > 0))
        kT = P.sb("swa_k", [64, 2, 34 * 128], BF16); bk = Buf()
        n_ = 0
        for kv in range(2):
            for (d_, s_) in SRC.swa_k(kT, kv):
                P.dma("sp", d_, s_, writes=[bk], append=(n_ > 0)); n_ += 1
        V = P.sb("swa_vv", [128, 34, 130], BF16); bv = Buf()
        for n_, (d_, s_) in enumerate(SRC.swa_v(V)):
            P.dma("sp", d_, s_, writes=[bv], append=(n_ > 0))
        mf = P.sb("swa_mf", [128, 4, 512], F32); bmf = Buf()
        mk = P.sb("swa_mk", [128, 4, 512], BF16); bmk = Buf()
        P.dma("pool", mf[:], D["swa_masks"].rearrange("m p n -> p m n"), writes=[bmf])
        P.op("dve", lambda e: e.tensor_copy(out=mk[:], in_=mf[:]), reads=[bmf], writes=[bmk])
        es = P.sb("swa_es", [128, 8], F32); bes = Buf()
        P.dma("pool", es[:], D["sinkb"], writes=[bes])
        P.op("act", lambda e: e.activation(out=es[:], in_=es[:], func=AF.Exp), reads=[bes], writes=[bes])
        for qt in range(NTT):
            for kv in range(2):
                acc, bacc = C.acc.next()
                rhs_q = qT[:, kv * 4:(kv + 1) * 4, qt * 128:(qt + 1) * 128]
                kts = []
                for d_ in range(3):
                    ki = qt + d_
                    if d_ == 1:
                        bias = None
                    elif d_ == 0:
                        bias = mk[:, 2, :] if qt == 0 else mk[:, 0, :]
                    else:
                        bias = mk[:, 3, :] if qt == NTT - 1 else mk[:, 1, :]
                    kts.append((kT[:, kv, ki * 128:(ki + 1) * 128], [bk], ki, [bv], bias, [bmk]))
                attn_block(P, C, rhs_q, [bq], 512, kts, 0.125, lambda j, acc=acc: acc[:, j, 0:65], bacc, 4,
                           lambda ki, j, kv=kv: V[:, ki, kv * 65:(kv + 1) * 65])
                r_, br = rz.next()
                P.op("dve", lambda e, r_=r_, acc=acc, kv=kv: e.tensor_tensor(
                    out=r_[:], in0=acc[:, :, 64:65], in1=es[:, kv * 4:(kv + 1) * 4].rearrange("p (g o) -> p g o", o=1), op=ALU.add),
                    reads=[bacc, bes], writes=[br])
                P.op("dve", lambda e, r_=r_: e.reciprocal(out=r_[:], in_=r_[:]), reads=[br], writes=[br])
                P.op("dve", lambda e, r_=r_, acc=acc, kv=kv, qt=qt: e.tensor_tensor(
                    out=o_tok[:, qt, kv * 256:(kv + 1) * 256].rearrange("p (g d) -> p g d", g=4),
                    in0=acc[:, :, 0:64], in1=r_[:].to_broadcast([128, 4, 64]), op=ALU.mult),
                    reads=[bacc, br], writes=[bo])
        emit_oT(P, C, o_tok, bo, oT_d[0])
        P.pop()

    if "mla" in which:
        P.push()
        qTr = Rot(P, "mla_q", [96, T], BF16, 2)
        kTr = Rot(P, "mla_k", [96, 2 * T], BF16, 2)
        Vr = Rot(P, "mla_vv", [128, 64, 65], BF16, 2)
        sc = 96.0 ** -0.5
        for h in range(8):
            qT, bq = qTr.next(); kT, bk = kTr.next(); V, bv = Vr.next()
            P.dma("sp", qT[:], D["mla_qT"][h], writes=[bq])
            for n_, (d_, s_) in enumerate(SRC.mla_k(kT, h)):
                P.dma("sp", d_, s_, writes=[bk], append=(n_ > 0))
            for n_, (d_, s_) in enumerate(SRC.mla_v(V, h)):
                P.dma("sp", d_, s_, writes=[bv], append=(n_ > 0))
            for qb in range(NTB):
                if qb % 2 == 0:
                    bg_step()
                acc, bacc = C.acc.next()
                kts = [(kT[:, ki * 128:(ki + 1) * 128], [bk], ki, [bv], None, []) for ki in range(64)]
                attn_block(P, C, qT[:, qb * 512:(qb + 1) * 512], [bq], 512, kts, sc, lambda j, acc=acc: acc[:, j, 0:65], bacc, 4,
                           lambda ki, j, V=V: V[:, ki, :])
                r_, br = rz.next()
                P.op("dve", lambda e, r_=r_, acc=acc: e.reciprocal(out=r_[:], in_=acc[:, :, 64:65]), reads=[bacc], writes=[br])
                P.op("dve", lambda e, r_=r_, acc=acc, qb=qb, h=h: e.tensor_tensor(
                    out=o_tok[:, qb * 4:(qb + 1) * 4, h * 64:(h + 1) * 64],
                    in0=acc[:, :, 0:64], in1=r_[:].to_broadcast([128, 4, 64]), op=ALU.mult),
                    reads=[bacc, br], writes=[bo])
        emit_oT(P, C, o_tok, bo, oT_d[1])
        P.pop()

    if "diff" in which:
        P.push()
        qTr = Rot(P, "df_q", [64, T], BF16, 2)
        kTr = Rot(P, "df_k", [64, 2 * T], BF16, 2)
        Vr = Rot(P, "df_vv", [128, 64, 129], BF16, 1)
        o1 = P.sb("df_o1", [128, NTT, 128], F32); bo1 = Buf()
        lamb = P.sb("df_lamb", [128, 4, 64], F32); blamb = Buf()
        lami = P.sb("df_lami", [128, 2], F32)
        sg = P.sb("df_sg", [128, 128], F32); bsg = Buf()
        P.dma("pool", lamb[:], D["lamb"], writes=[blamb])
        P.dma("pool", lami[:], D["lami"], writes=[blamb], append=True)
        P.dma("pool", sg[:], D["subln"], writes=[bsg])
        P.op("dve", lambda e: e.tensor_scalar(out=sg[:], in0=sg[:], scalar1=lami[:, 1:2], scalar2=None, op0=ALU.mult), reads=[bsg, blamb], writes=[bsg])
        lp = P.sb("df_lp", [128, 2, 64], F32); blp = Buf()
        ls = P.sb("df_ls", [128, 2], F32); bls = Buf()
        nlam = P.sb("df_nlam", [128, 1], F32); bnl = Buf()
        P.op("dve", lambda e: e.tensor_tensor(out=lp[:, 0, :], in0=lamb[:, 0, :], in1=lamb[:, 1, :], op=ALU.mult), reads=[blamb], writes=[blp])
        P.op("dve", lambda e: e.tensor_tensor(out=lp[:, 1, :], in0=lamb[:, 2, :], in1=lamb[:, 3, :], op=ALU.mult), reads=[blamb, blp], writes=[blp])
        P.op("dve", lambda e: e.reduce_sum(out=ls[:], in_=lp[:], axis=AX.X), reads=[blp], writes=[bls])
        P.op("act", lambda e: e.activation(out=ls[:], in_=ls[:], func=AF.Exp), reads=[bls], writes=[bls])
        P.op("dve", lambda e: e.tensor_tensor(out=nlam[:], in0=ls[:, 1:2], in1=ls[:, 0:1], op=ALU.subtract), reads=[bls], writes=[bnl])
        P.op("dve", lambda e: e.tensor_tensor(out=nlam[:], in0=nlam[:], in1=lami[:, 0:1], op=ALU.subtract), reads=[bnl, blamb], writes=[bnl])
        ot = Rot(P, "df_ot", [128, 4, 128], F32, 2)
        sq = Rot(P, "df_sq", [128, 4, 128], F32, 1)
        ss = Rot(P, "df_ss", [128, 4], F32, 2)
        for h in range(4):
            V, bv = Vr.next()
            for n_, (d_, s_) in enumerate(SRC.diff_v(V, h)):
                P.dma("sp", d_, s_, writes=[bv], append=(n_ > 0))
            for m in range(2):
                row = (h * 2 + m) * 64
                qT, bq = qTr.next(); kT, bk = kTr.next()
                P.dma("sp", qT[:], D["diff_qT"][row:row + 64, :], writes=[bq])
                for n_, (d_, s_) in enumerate(SRC.diff_k(kT, row)):
                    P.dma("sp", d_, s_, writes=[bk], append=(n_ > 0))
                for qb in range(NTB):
                    acc, bacc = C.acc.next()
                    kts = [(kT[:, ki * 128:(ki + 1) * 128], [bk], ki, [bv], None, []) for ki in range(64)]
                    attn_block(P, C, qT[:, qb * 512:(qb + 1) * 512], [bq], 512, kts, 0.125, lambda j, acc=acc: acc[:, j, 0:129], bacc, 4,
                               lambda ki, j, V=V: V[:, ki, :])
                    r_, br = rz.next()
                    P.op("dve", lambda e, r_=r_, acc=acc: e.reciprocal(out=r_[:], in_=acc[:, :, 128:129]), reads=[bacc], writes=[br])
                    tl = slice(qb * 4, (qb + 1) * 4)
                    if m == 0:
                        P.op("dve", lambda e, r_=r_, acc=acc, tl=tl: e.tensor_tensor(
                            out=o1[:, tl, :], in0=acc[:, :, 0:128], in1=r_[:].to_broadcast([128, 4, 128]), op=ALU.mult),
                            reads=[bacc, br], writes=[bo1])
                    else:
                        o_, bo_ = ot.next()
                        P.op("dve", lambda e, r_=r_, acc=acc, o_=o_: e.tensor_tensor(
                            out=o_[:], in0=acc[:, :, 0:128], in1=r_[:].to_broadcast([128, 4, 128]), op=ALU.mult),
                            reads=[bacc, br], writes=[bo_])
                        P.op("dve", lambda e, o_=o_, tl=tl: e.scalar_tensor_tensor(
                            out=o_[:], in0=o_[:], scalar=nlam[:, 0:1], in1=o1[:, tl, :], op0=ALU.mult, op1=ALU.add),
                            reads=[bo_, bnl, bo1], writes=[bo_])
                        sq_, bsq = sq.next(); ss_, bss = ss.next()
                        P.op("pool", lambda e, o_=o_, sq_=sq_: e.tensor_tensor(out=sq_[:], in0=o_[:], in1=o_[:], op=ALU.mult), reads=[bo_], writes=[bsq])
                        P.op("dve", lambda e, sq_=sq_, ss_=ss_: e.reduce_sum(out=ss_[:], in_=sq_[:], axis=AX.X), reads=[bsq], writes=[bss])
                        P.op("act", lambda e, ss_=ss_: e.activation(out=ss_[:], in_=ss_[:], func=AF.Ln, bias=epsb[:, 0:1], scale=1.0 / 128.0), reads=[bss, beps], writes=[bss])
                        P.op("act", lambda e, ss_=ss_: e.activation(out=ss_[:], in_=ss_[:], func=AF.Exp, scale=-0.5), reads=[bss], writes=[bss])
                        P.op("pool", lambda e, o_=o_, ss_=ss_: e.tensor_tensor(
                            out=o_[:], in0=o_[:], in1=ss_[:].rearrange("p (g o) -> p g o", o=1).to_broadcast([128, 4, 128]), op=ALU.mult),
                            reads=[bo_, bss], writes=[bo_])
                        P.op("pool", lambda e, o_=o_, tl=tl, h=h: e.tensor_tensor(
                            out=o_tok[:, tl, h * 128:(h + 1) * 128], in0=o_[:],
                            in1=sg[:].rearrange("p (o d) -> p o d", o=1).to_broadcast([128, 4, 128]), op=ALU.mult),
                            reads=[bo_, bsg], writes=[bo])
        emit_oT(P, C, o_tok, bo, oT_d[2])
        P.pop()

    if "nat" in which:
        P.push()
        qTr = Rot(P, "nat_q", [64, T], BF16, 2)
        kTr = Rot(P, "nat_k", [64, 36 * 128], BF16, 2)
        Vr = Rot(P, "nat_vv", [128, 36, 65], BF16, 2)
        bfr = Rot(P, "nat_bf", [128, 29 * 128], F32, 1)
        bbr = Rot(P, "nat_bb", [128, 29, 128], BF16, 2)
        nat_accb = [Buf(f"nat_acc{j}", excl=True) for j in range(4)]
        for h in range(8):
            qT, bq = qTr.next(); kT, bk = kTr.next(); V, bv = Vr.next()
            bf_, bbf = bfr.next(); bb_, bbb = bbr.next()
            P.dma("sp", qT[:], D["nat_qT"][h * 64:(h + 1) * 64, :], writes=[bq])
            for n_, (d_, s_) in enumerate(SRC.nat_k(kT, h)):
                P.dma("sp", d_, s_, writes=[bk], append=(n_ > 0))
            for n_, (d_, s_) in enumerate(SRC.nat_v(V, h)):
                P.dma("sp", d_, s_, writes=[bv], append=(n_ > 0))
            P.dma("sp", bf_[:], D["nat_bias"][h], writes=[bbf])
            P.op("dve", lambda e, bb_=bb_, bf_=bf_: e.tensor_copy(out=bb_[:].rearrange("p a b -> p (a b)"), in_=bf_[:]), reads=[bbf], writes=[bbb])
            for qt in range(NTT):
                if qt == 0:
                    kis = list(range(0, 6)); tbl = list(range(5, 11))
                elif qt == 1:
                    kis = list(range(1, 7)); tbl = list(range(11, 17))
                elif qt == NTT - 2:
                    kis = list(range(29, 35)); tbl = list(range(17, 23))
                elif qt == NTT - 1:
                    kis = list(range(30, 36)); tbl = list(range(23, 29))
                else:
                    kis = list(range(qt, qt + 5)); tbl = list(range(0, 5))
                acc = C.acc.tiles[0]
                jb = qt % 4
                bacc = nat_accb[jb]
                kts = [(kT[:, ki * 128:(ki + 1) * 128], [bk], ki, [bv], bb_[:, ti, :], [bbb]) for ki, ti in zip(kis, tbl)]
                attn_block(P, C, qT[:, qt * 128:(qt + 1) * 128], [bq], 128, kts, 1.0, lambda j, acc=acc, jb=jb: acc[:, jb, 0:65], bacc, 1,
                           lambda ki, j, V=V: V[:, ki, :])
                r_, br = rz.next()
                P.op("dve", lambda e, r_=r_, acc=acc, jb=jb: e.reciprocal(out=r_[:, 0, :], in_=acc[:, jb, 64:65]), reads=[bacc], writes=[br])
                P.op("dve", lambda e, r_=r_, acc=acc, qt=qt, h=h, jb=jb: e.tensor_scalar(
                    out=o_tok[:, qt, h * 64:(h + 1) * 64], in0=acc[:, jb, 0:64], scalar1=r_[:, 0, 0:1], scalar2=None, op0=ALU.mult),
                    reads=[bacc, br], writes=[bo])
        emit_oT(P, C, o_tok, bo, oT_d[3])
        P.pop()


def nat_bias_tables(rpb, half):
    a = np.arange(128)
    out = np.full((8, 29, 128, 128), NEGM, np.float32)

    def table(tq, tk):
        if tk < 0 or tk >= 64:
            return None
        rk = 2 * tk + a // 64; ck = a % 64
        rq = 2 * tq + a // 64; cq = a % 64
        r0 = np.clip(rq - 4, 0, 120); cs = np.clip(cq - 8, 0, 48)
        valid = ((rk[:, None] >= r0[None, :]) & (rk[:, None] < r0[None, :] + 8) &
                 (ck[:, None] >= cs[None, :]) & (ck[:, None] < cs[None, :] + 16))
        ri = np.clip(rk[:, None] - rq[None, :] + 7, 0, 14)
        ci = np.clip(ck[:, None] - cq[None, :] + 15, 0, 30)
        t = rpb[:, ri, ci]
        return np.where(valid[None], t, np.float32(NEGM)).astype(np.float32)

    g0 = half * 32
    specs = [(10, off) for off in range(-2, 3)]
    specs += [(g0 + 0, off) for off in range(-2, 4)]
    specs += [(g0 + 1, off) for off in range(-2, 4)]
    specs += [(g0 + 30, off) for off in range(-3, 3)]
    specs += [(g0 + 31, off) for off in range(-3, 3)]
    for i, (tq, off) in enumerate(specs):
        t = table(tq, tq + off)
        if t is not None:
            out[:, i] = t
    return np.ascontiguousarray(out.transpose(0, 2, 1, 3).reshape(8, 128, 29 * 128))


def swa_masks(half):
    a = np.arange(128)
    L = np.where(a[None, :] <= a[:, None], 0.0, NEGM).astype(np.float32)
    R = np.where(a[:, None] <= a[None, :], 0.0, NEGM).astype(np.float32)
    allm = np.full((128, 128), NEGM, np.float32)
    first = allm if half == 0 else L
    last = allm if half == 1 else R
    return np.ascontiguousarray(np.stack([np.tile(m, (1, 4)) for m in (L, R, first, last)]))


def k2a_host_inputs(inp, l, core, k1o):
    import ml_dtypes
    b, half = core // 2, core % 2
    me, pa = k1o[core], k1o[core ^ 1]
    lo, hi = (me, pa) if half == 0 else (pa, me)
    bf = lambda a: np.ascontiguousarray(a)
    z = lambda shape: np.zeros(shape, ml_dtypes.bfloat16)
    m = {}
    m["swa_qT"] = bf(me["swa_qT"])
    left = pa["swa_kT"][:, -128:] if half == 1 else z((128, 128))
    right = pa["swa_kT"][:, :128] if half == 0 else z((128, 128))
    m["swa_kT"] = bf(np.concatenate([left, me["swa_kT"], right], 1))
    left = pa["swa_v"][-1:] if half == 1 else z((1, 128, 130))
    right = pa["swa_v"][:1] if half == 0 else z((1, 128, 130))
    m["swa_v"] = bf(np.concatenate([left, me["swa_v"], right], 0))
    m["swa_masks"] = swa_masks(half)
    m["sinkb"] = np.ascontiguousarray(np.broadcast_to(inp["swa_sink"][l][None, :], (128, 8))).astype(np.float32)
    m["mla_qT"] = bf(me["mla_qT"])
    m["mla_kT"] = bf(np.concatenate([lo["mla_kT"], hi["mla_kT"]], 2))
    m["mla_v"] = bf(np.concatenate([lo["mla_v"], hi["mla_v"]], 0))
    m["diff_qT"] = bf(me["diff_qT"])
    m["diff_kT"] = bf(np.concatenate([lo["diff_kT"], hi["diff_kT"]], 1))
    m["diff_v"] = bf(np.concatenate([lo["diff_v"], hi["diff_v"]], 0))
    lam = np.stack([inp["diff_lambda_q1"][l], inp["diff_lambda_k1"][l], inp["diff_lambda_q2"][l], inp["diff_lambda_k2"][l]])
    m["lamb"] = np.ascontiguousarray(np.broadcast_to(lam[None], (128, 4, 64))).astype(np.float32)
    m["subln"] = np.ascontiguousarray(np.broadcast_to(inp["diff_subln"][l][None], (128, 128))).astype(np.float32)
    import math
    li = 0.8 - 0.6 * math.exp(-0.3 * l)
    m["lami"] = np.ascontiguousarray(np.broadcast_to(np.array([li, 1.0 - li], np.float32)[None], (128, 2)))
    m["nat_qT"] = bf(me["nat_qT"])
    left = pa["nat_kT"][:, -256:] if half == 1 else z((512, 256))
    right = pa["nat_kT"][:, :256] if half == 0 else z((512, 256))
    m["nat_kT"] = bf(np.concatenate([left, me["nat_kT"], right], 1))
    left = pa["nat_v"][-2:] if half == 1 else z((2, 128, 520))
    right = pa["nat_v"][:2] if half == 0 else z((2, 128, 520))
    m["nat_v"] = bf(np.concatenate([left, me["nat_v"], right], 0))
    m["nat_bias"] = nat_bias_tables(inp["nat_rpb"][l], half)
    m["ident"] = np.eye(128, dtype=np.float32)
    return m


DN_ALPHA = 4.0 ** 0.25


def emit_gvec_bcast(P, cT_d, adaw_d, adabb_d, gb, bgb, pA, wst_rot, tag):
    cs = P.sb(f"{tag}_cs", [128, 8], F32); bcs = Buf()
    csr = P.sb(f"{tag}_csr", [128, 8, 128], F32); bcsr = Buf()
    ab = P.sb(f"{tag}_ab", [128, 1024], F32); bab = Buf()
    P.dma("sp", cs[:], cT_d, writes=[bcs])
    P.dma("act", ab[:], adabb_d, writes=[bab])
    P.op("act", lambda e: e.activation(out=cs[:], in_=cs[:], func=AF.Silu), reads=[bcs], writes=[bcs])
    P.op("dve", lambda e: e.tensor_copy(out=csr[:], in_=cs[:].rearrange("p (k o) -> p k o", o=1).to_broadcast([128, 8, 128])),
         reads=[bcs], writes=[bcsr])
    for hf in range(2):
        ps, bps = pA.next()
        for q4 in range(2):
            ws_, bws = wst_rot.next()
            ws = ws_[:, 0:2048].rearrange("p (c n) -> p c n", c=8)
            c0 = hf * 512 + q4 * 256
            P.dma("sp" if q4 == 0 else "act", ws, adaw_d[:, c0:c0 + 256].rearrange("(c p) n -> p c n", p=128), writes=[bws])
            for kc in range(8):
                P.op("pe", lambda e, kc=kc, ps=ps, ws=ws, q4=q4: e.matmul(ps[:, q4 * 256:(q4 + 1) * 256], lhsT=csr[:, kc, :], rhs=ws[:, kc, :],
                                                                 start=(kc == 0), stop=(kc == 7)), reads=[bcsr, bws], writes=[bps])
        P.op("dve", lambda e, ps=ps, hf=hf: e.tensor_tensor(out=gb[:, hf * 512:(hf + 1) * 512], in0=ps[:], in1=ab[:, hf * 512:(hf + 1) * 512], op=ALU.add),
             reads=[bps, bab], writes=[bgb])


def emit_resid_ln(P, C, y_ps, by, xt, bx, gb, bgb, lng, lnb, bln, epsb, beps, out_tile, bout, tag):
    t1, bt1 = C["t1"].next()
    for hf in range(2):
        P.op("dve", lambda e, hf=hf, t1=t1: e.tensor_tensor(out=t1[:, hf * 512:(hf + 1) * 512], in0=y_ps[hf][:], in1=gb[:, hf * 512:(hf + 1) * 512], op=ALU.mult),
             reads=[by[hf], bgb], writes=[bt1])
    P.op("dve", lambda e, t1=t1: e.scalar_tensor_tensor(out=t1[:], in0=xt[:], scalar=DN_ALPHA, in1=t1[:], op0=ALU.mult, op1=ALU.add),
         reads=[bx, bt1], writes=[bt1])
    s_, bs = C["st"].next(); m_, bm = C["mv"].next(); r_, br = C["rs"].next()
    for c in range(2):
        P.op("dve", lambda e, c=c, s_=s_, t1=t1: e.bn_stats(out=s_[:, c, :], in_=t1[:, c * 512:(c + 1) * 512]), reads=[bt1], writes=[bs])
    P.op("dve", lambda e, m_=m_, s_=s_: e.bn_aggr(out=m_[:], in_=s_[:]), reads=[bs], writes=[bm])
    P.op("act", lambda e, r_=r_, m_=m_: e.activation(out=r_[:], in_=m_[:, 1:2], func=AF.Ln, bias=epsb[:, 0:1], scale=1.0), reads=[bm, beps], writes=[br])
    P.op("act", lambda e, r_=r_: e.activation(out=r_[:], in_=r_[:], func=AF.Exp, scale=-0.5), reads=[br], writes=[br])
    P.op("dve", lambda e, t1=t1, m_=m_, r_=r_: e.tensor_scalar(out=t1[:], in0=t1[:], scalar1=m_[:, 0:1], scalar2=r_[:, 0:1], op0=ALU.subtract, op1=ALU.mult),
         reads=[bt1, bm, br], writes=[bt1])
    P.op("pool", lambda e, t1=t1: e.tensor_tensor(out=t1[:], in0=t1[:], in1=lng[:], op=ALU.mult), reads=[bt1, bln], writes=[bt1])
    P.op("pool", lambda e, t1=t1: e.tensor_tensor(out=out_tile[:], in0=t1[:], in1=lnb[:], op=ALU.add), reads=[bt1, bln], writes=[bout])


def build_k2b(nc):
    P = Prog(nc)
    din = lambda n, s, d=F32: nc.dram_tensor(n, list(s), d, kind="ExternalInput").ap()
    D = {}
    D["oT"] = din("oT", [4, 512, T], BF16); D["uT"] = din("uT", [1024, T], BF16); D["x"] = din("x", [T, 1024])
    D["wg"] = din("wg", [4, 1024, 1024]); D["wb"] = din("wb", [4, 512, 1024]); D["wo"] = din("wo", [1024, 1024])
    D["cT"] = din("cT", [128, 8]); D["adaw_g"] = din("adaw_g", [1024, 1024]); D["adab_g"] = din("adab_g", [128, 1024])
    D["lng"] = din("lng", [128, 1024]); D["lnb"] = din("lnb", [128, 1024])
    xo = nc.dram_tensor("xmid", [T, 1024], F32, kind="ExternalOutput").ap()
    emit_merge(P, D, xo)
    P.finish(); P.emit()
    return P


def emit_merge(P, D, xo):
    P.push()
    pA = Rot(P, "m_pA", [128, 512], F32, 3, "ps")
    pB = Rot(P, "m_pB", [128, 512], F32, 3, "ps")
    wst = Rot(P, "m_wst", [128, 2048], F32, 2)
    epsb = P.sb("m_eps", [128, 1], F32); beps = Buf()
    P.op("pool", lambda e: e.memset(epsb[:], 1e-5), writes=[beps])
    gb = P.sb("m_gb", [128, 1024], F32); bgb = Buf()
    emit_gvec_bcast(P, D["cT"], D["adaw_g"], D["adab_g"], gb, bgb, pA, wst, "m")
    mT = P.sb("m_mT", [128, 8, T], BF16)
    bmT = [[Buf() for _ in range(NTB)] for _ in range(8)]
    P.push()
    wgf = Rot(P, "m_wgf", [128, 8, 4, 128], F32, 1)
    wgb = Rot(P, "m_wgb", [128, 8, 4, 128], BF16, 2)
    wbf = Rot(P, "m_wbf", [128, 4, 4, 128], F32, 1)
    wbb = Rot(P, "m_wbb", [128, 4, 4, 128], BF16, 2)
    ub = Rot(P, "m_ub", [128, 8, 512], BF16, 2)
    ob = Rot(P, "m_ob", [128, 4, 4, 512], BF16, 2)
    gs = Rot(P, "m_gs", [128, 512], BF16, 2)
    ac = Rot(P, "m_ac", [128, 512], F32, 2)
    tm = Rot(P, "m_tm", [128, 512], F32, 2)
    for oc in range(8):
        wgf_, bwgf = wgf.next(); wgb_, bwgb = wgb.next(); wbf_, bwbf = wbf.next(); wbb_, bwbb = wbb.next()
        for n in range(4):
            P.dma("sp", wgf_[:, :, n, :], D["wg"][n, :, oc * 128:(oc + 1) * 128].rearrange("(c p) n -> p c n", p=128),
                  writes=[bwgf], append=(n > 0))
            P.dma("sp", wbf_[:, :, n, :], D["wb"][n, :, oc * 128:(oc + 1) * 128].rearrange("(c p) n -> p c n", p=128),
                  writes=[bwbf], append=(n > 0))
        P.op("pool", lambda e, a=wgb_, b=wgf_: e.tensor_copy(out=a[:], in_=b[:]), reads=[bwgf], writes=[bwgb])
        P.op("pool", lambda e, a=wbb_, b=wbf_: e.tensor_copy(out=a[:], in_=b[:]), reads=[bwbf], writes=[bwbb])
        for tb in range(NTB):
            sl = slice(tb * 512, (tb + 1) * 512)
            u_, bu = ub.next(); o_, bo_ = ob.next()
            P.dma("sp", u_[:], D["uT"].rearrange("(c p) t -> p c t", p=128)[:, :, sl], writes=[bu])
            for n in range(4):
                P.dma("sp", o_[:, n, :, :], D["oT"][n].rearrange("(c p) t -> p c t", p=128)[:, :, sl], writes=[bo_], append=(n > 0))
            a_, ba = ac.next()
            for n in range(4):
                pg, bpg = pA.next(); pb, bpb = pB.next()
                for kc in range(8):
                    P.op("pe", lambda e, kc=kc, n=n, pg=pg, wgb_=wgb_, u_=u_: e.matmul(pg[:], lhsT=wgb_[:, kc, n, :], rhs=u_[:, kc, :], start=(kc == 0), stop=(kc == 7)),
                         reads=[bwgb, bu], writes=[bpg])
                for kc in range(4):
                    P.op("pe", lambda e, kc=kc, n=n, pb=pb, wbb_=wbb_, o_=o_: e.matmul(pb[:], lhsT=wbb_[:, kc, n, :], rhs=o_[:, n, kc, :], start=(kc == 0), stop=(kc == 3)),
                         reads=[bwbb, bo_], writes=[bpb])
                g_, bg = gs.next()
                P.op("act", lambda e, g_=g_, pg=pg: e.activation(out=g_[:], in_=pg[:], func=AF.Sigmoid), reads=[bpg], writes=[bg])
                if n == 0:
                    P.op("dve", lambda e, a_=a_, g_=g_, pb=pb: e.tensor_tensor(out=a_[:], in0=pb[:], in1=g_[:], op=ALU.mult), reads=[bpb, bg], writes=[ba])
                else:
                    t_, bt = tm.next()
                    P.op("dve", lambda e, t_=t_, g_=g_, pb=pb: e.tensor_tensor(out=t_[:], in0=pb[:], in1=g_[:], op=ALU.mult), reads=[bpb, bg], writes=[bt])
                    if n < 3:
                        P.op("pool", lambda e, a_=a_, t_=t_: e.tensor_tensor(out=a_[:], in0=a_[:], in1=t_[:], op=ALU.add), reads=[ba, bt], writes=[ba])
                    else:
                        P.op("pool", lambda e, a_=a_, t_=t_, oc=oc, sl=sl: e.tensor_tensor(out=mT[:, oc, sl], in0=a_[:], in1=t_[:], op=ALU.add),
                             reads=[ba, bt], writes=[bmT[oc][tb]])
    P.pop()
    P.push()
    wo = P.sb("m_wo", [128, 8, 1024], BF16); bwo = Buf()
    for q8 in range(4):
        ws_, bws = wst.next()
        ws = ws_[:, 0:2048].rearrange("p (c n) -> p c n", c=8)
        P.dma(("sp", "act")[q8 % 2], ws, D["wo"][:, q8 * 256:(q8 + 1) * 256].rearrange("(c p) n -> p c n", p=128), writes=[bws])
        P.op("pool", lambda e, ws=ws, q8=q8: e.tensor_copy(out=wo[:, :, q8 * 256:(q8 + 1) * 256], in_=ws), reads=[bws], writes=[bwo], )
    lng = P.sb("m_lng", [128, 1024], F32); lnb = P.sb("m_lnb", [128, 1024], F32); bln = Buf()
    P.dma("sp", lng[:], D["lng"], writes=[bln]); P.dma("act", lnb[:], D["lnb"], writes=[bln], append=True)
    C = {"t1": Rot(P, "m_t1", [128, 1024], F32, 2), "st": Rot(P, "m_st", [128, 2, 6], F32, 2),
         "mv": Rot(P, "m_mv", [128, 2], F32, 2), "rs": Rot(P, "m_rs", [128, 1], F32, 2)}
    xin = Rot(P, "m_xin", [128, 1024], F32, 2)
    xout = Rot(P, "m_xout", [128, 1024], F32, 2)
    for t in range(NTT):
        xt, bx = xin.next()
        P.dma("sp", xt[:], D["x"][t * 128:(t + 1) * 128, :], writes=[bx])
        ys = []; bys = []
        for hf in range(2):
            py, bpy = pA.next()
            for kc in range(8):
                P.op("pe", lambda e, kc=kc, py=py, hf=hf, t=t: e.matmul(py[:], lhsT=mT[:, kc, t * 128:(t + 1) * 128], rhs=wo[:, kc, hf * 512:(hf + 1) * 512],
                                                                 start=(kc == 0), stop=(kc == 7)), reads=[bwo, bmT[kc][t // 4]], writes=[bpy])
            ys.append(py); bys.append(bpy)
        xo_, bxo = xout.next()
        emit_resid_ln(P, C, ys, bys, xt, bx, gb, bgb, lng, lnb, bln, epsb, beps, xo_, bxo, "m")
        P.dma("pool", xo[t * 128:(t + 1) * 128, :], xo_[:], reads=[bxo])
    P.pop()
    P.pop()


def k2b_host_inputs(inp, l, core, xcur, oT, uT):

    b, half = core // 2, core % 2
    rep = lambda v: np.ascontiguousarray(np.broadcast_to(np.asarray(v, np.float32)[None, :], (128, len(v))))
    return {
        "oT": np.ascontiguousarray(oT), "uT": np.ascontiguousarray(uT),
        "x": np.ascontiguousarray(xcur[b, half * T:(half + 1) * T]),
        "wg": np.ascontiguousarray(inp["w_gate"][l]), "wb": np.ascontiguousarray(inp["w_branch"][l]),
        "wo": np.ascontiguousarray(inp["w_out"][l]),
        "cT": to_pc(inp["c"][b], 8), "adaw_g": np.ascontiguousarray(inp["ada_w"][l][:, 2048:3072]),
        "adab_g": rep(inp["ada_b"][l][2048:3072]),
        "lng": rep(inp["ln1_g"][l]), "lnb": rep(inp["ln1_b"][l]),
    }


TBLK = 256
NBLK = T // TBLK
NCH = 128
NEG = -1.0e30
U32 = mybir.dt.uint32


def build_k3(nc, nblk=NBLK, dbg=False):
    P = Prog(nc)
    din = lambda n, s, d=F32: nc.dram_tensor(n, list(s), d, kind="ExternalInput").ap()
    D = {}
    D["xmid"] = din("xmid", [T, 1024]); D["cT"] = din("cT", [128, 8])
    D["adaw_f"] = din("adaw_f", [1024, 3072]); D["adab_f"] = din("adab_f", [128, 16]); D["adab_g"] = din("adab_g", [128, 1024])
    D["wq"] = din("wq", [1024, 2048]); D["keysT"] = din("keysT", [128, 16, 128])
    D["UT"] = din("UT", [1024, 16384]); D["V"] = din("V", [16384, 1024])
    D["lng"] = din("lng", [128, 1024]); D["lnb"] = din("lnb", [128, 1024])
    D["ident"] = din("ident", [128, 128]); D["iota"] = din("iota", [128, 128])
    xo = nc.dram_tensor("xout", [T, 1024], F32, kind="ExternalOutput").ap()
    dbg_d = nc.dram_tensor("dbg", [128, 16, 128], F32, kind="ExternalOutput").ap() if dbg else None
    emit_peer(P, nc, D, xo, nblk, dbg_d)
    P.finish(); P.emit()
    return P


def peer_scratch(nc, tag):
    Ub_d = nc.dram_tensor(f"peer_Ub{tag}", [NCH, 128, 8 * 128], BF16, kind="Internal").ap()
    Vb_d = nc.dram_tensor(f"peer_Vb{tag}", [NCH, 128, 1024], BF16, kind="Internal").ap()
    return Ub_d, Vb_d, Buf("Ub_d"), Buf("Vb_d")


def peer_cast_gen(P, UT, V, Ub_d, Vb_d, bUb, bVb, cf, cb):
    for c4 in range(NCH // 4):
        f_, bf_ = cf.next(); b_, bb_ = cb.next()
        P.dma("sp", f_[:].rearrange("p (k n) -> p k n", k=8),
              UT[:, c4 * 512:(c4 + 1) * 512].rearrange("(k p) n -> p k n", p=128), writes=[bf_])
        P.op(("dve", "pool")[c4 % 2], lambda e, f_=f_, b_=b_: e.tensor_copy(
            out=b_[:].rearrange("p (c k n) -> p c k n", c=4, k=8), in_=f_[:].rearrange("p (k c n) -> p c k n", k=8, c=4)),
            reads=[bf_], writes=[bb_])
        P.dma("pool", Ub_d[c4 * 4:(c4 + 1) * 4].rearrange("c p n -> p c n"), b_[:].rearrange("p (c n) -> p c n", c=4),
              reads=[bb_], writes=[bUb], append=True)
        f_, bf_ = cf.next(); b_, bb_ = cb.next()
        P.dma("sp", f_[:].rearrange("p (c n) -> p c n", c=4),
              V[c4 * 512:(c4 + 1) * 512, :].rearrange("(c p) n -> p c n", p=128), writes=[bf_])
        P.op(("pool", "dve")[c4 % 2], lambda e, f_=f_, b_=b_: e.tensor_copy(out=b_[:], in_=f_[:]), reads=[bf_], writes=[bb_])
        P.dma("pool", Vb_d[c4 * 4:(c4 + 1) * 4].rearrange("c p n -> p c n"), b_[:].rearrange("p (c n) -> p c n", c=4),
              reads=[bb_], writes=[bVb], append=True)
        yield


def emit_peer(P, nc, D, xo, nblk=NBLK, dbg_d=None, tag="", pre=None):
    u2_d = nc.dram_tensor(f"peer_u2{tag}", [1024, T], BF16, kind="Internal").ap()
    P.push()
    bu2d = Buf("u2_d")
    if pre is None:
        Ub_d, Vb_d, bUb, bVb = peer_scratch(nc, tag)
        P.push()
        cf = Rot(P, "pa_cf", [128, 4096], F32, 2)
        cb = Rot(P, "pa_cb", [128, 4096], BF16, 2)
        for _ in peer_cast_gen(P, D["UT"], D["V"], Ub_d, Vb_d, bUb, bVb, cf, cb):
            pass
        P.pop()
    else:
        Ub_d, Vb_d, bUb, bVb = pre

    pA = Rot(P, "p_pA", [128, 512], F32, 2, "ps")
    ident_f = P.sb("p_identf", [128, 128], F32); bidf = Buf()
    ident = P.sb("p_ident", [128, 128], BF16); bid = Buf()
    epsb = P.sb("p_eps", [128, 1], F32); beps = Buf()
    P.dma("pool", ident_f[:], D["ident"], writes=[bidf])
    P.op("dve", lambda e: e.tensor_copy(out=ident[:], in_=ident_f[:]), reads=[bidf], writes=[bid])
    P.op("pool", lambda e: e.memset(epsb[:], 1e-5), writes=[beps])
    gb = P.sb("p_gb", [128, 1024], F32); bgb = Buf()
    P.push()
    wst = Rot(P, "p_wst", [128, 2048], F32, 2)
    emit_gvec_bcast(P, D["cT"], D["adaw_f"][:, 2048:3072], D["adab_g"], gb, bgb, pA, wst, "p")
    P.push()
    modc = P.sb("p_modc", [128, 16], F32); bmodc = Buf()
    sc1 = P.sb("p_sc1", [128, 8], F32); bsc1 = Buf()
    pm = Rot(P, "p_pm", [128, 512], F32, 1, "ps")
    emit_mod_cols(P, nc, D["cT"], D["adaw_f"][:, 0:2048], D["adab_f"], 2, modc, bmodc, pm, wst)
    P.op("dve", lambda e: e.tensor_scalar(out=sc1[:], in0=modc[:, 8:16], scalar1=1.0, scalar2=None, op0=ALU.add), reads=[bmodc], writes=[bsc1])
    bmod = Buf()
    P.op("dve", lambda e: e.tensor_copy(out=modc[:, 0:8], in_=modc[:, 0:8]), reads=[bmodc, bsc1], writes=[bmod])
    u2T = P.sb("p_u2T", [128, 8, T], BF16)
    bu2 = [Buf() for _ in range(NTT)]
    ptr = Rot(P, "p_ptr", [128, 8, 128], BF16, 2, "ps")
    emit_ln_mod(P, D["xmid"], u2T, bu2, modc, sc1, bmod, ident, bid, epsb, beps, ptr, tag="p")
    for c in range(8):
        P.dma(("sp", "act")[c % 2], u2_d[c * 128:(c + 1) * 128, :], u2T[:, c, :], reads=bu2, writes=[bu2d], append=(c > 0))
    P.pop()

    wqb_d = nc.dram_tensor(f"peer_wqb{tag}", [128, 8 * 2048], BF16, kind="Internal").ap()
    bwqd = Buf("wqb_d")
    P.push()
    wtmp = P.sb("p_wqtmp", [128, 8, 2048], BF16); bwt = Buf()
    for g in range(8):
        ws_, bws = wst.next()
        ws = ws_[:, 0:2048].rearrange("p (c n) -> p c n", c=8)
        P.dma(("sp", "act")[g % 2], ws, D["wq"][:, g * 256:(g + 1) * 256].rearrange("(c p) n -> p c n", p=128), writes=[bws])
        P.op("pool", lambda e, ws=ws, g=g: e.tensor_copy(out=wtmp[:, :, g * 256:(g + 1) * 256], in_=ws), reads=[bws], writes=[bwt])
    P.dma("sp", wqb_d, wtmp[:].rearrange("p k n -> p (k n)"), reads=[bwt], writes=[bwqd])
    P.pop()
    P.pop()

    keysT = P.sb("p_keysT", [128, 16, 128], F32); bkeys = Buf()
    P.dma("sp", keysT[:], D["keysT"], writes=[bkeys])
    iota = P.sb("p_iota", [128, 128], F32); biota = Buf()
    P.dma("act", iota[:], D["iota"], writes=[biota])
    lng = P.sb("p_lng", [128, 1024], F32); lnb = P.sb("p_lnb", [128, 1024], F32); bln = Buf()
    P.dma("sp", lng[:], D["lng"], writes=[bln]); P.dma("act", lnb[:], D["lnb"], writes=[bln], append=True)

    pO = [P.ps(f"p_pO{i}", [128, 512], F32) for i in range(4)]
    bpO = [Buf(f"pO{i}", excl=True) for i in range(4)]
    pH = Rot(P, "p_pH", [128, 512], F32, 2, "ps")
    pM = pA
    NTL = TBLK // 128

    Wd = nc.dram_tensor(f"peer_W{tag}", [nblk, 8, 128, TBLK * 16], BF16, kind="Internal").ap()
    bWd = [Buf(f"Wd{b}") for b in range(nblk)]

    wq = P.sb("p_wq", [128, 8, 2048], BF16); bwq = Buf()
    P.dma("act", wq[:].rearrange("p k n -> p (k n)"), wqb_d, reads=[bwqd], writes=[bwq])
    ublk = Rot(P, "p_ublk", [128, 8, TBLK], BF16, 2)
    qT = Rot(P, "p_qT", [128, 16, TBLK], F32, 1)
    C = {"t1": Rot(P, "p_t1", [128, 1024], F32, 2), "st": Rot(P, "p_st", [128, 2, 6], F32, 2),
         "mv": Rot(P, "p_mv", [128, 2], F32, 2), "rs": Rot(P, "p_rs", [128, 1], F32, 2)}
    xin = Rot(P, "p_xin", [128, 1024], F32, 1)
    xout = Rot(P, "p_xout", [128, 1024], F32, 1)
    S = Rot(P, "p_S", [128, 16, 128], F32, 1)
    S2 = Rot(P, "p_S2", [128, 128], F32, 2)
    top = Rot(P, "p_top", [128, 16, 16], F32, 1)
    jix = Rot(P, "p_jix", [128, 8, 16], U32, 1)
    jif = Rot(P, "p_jif", [128, 128], F32, 1)
    jT = Rot(P, "p_jT", [128, 128], F32, 1)
    cand = Rot(P, "p_cand", [128, 256], F32, 2)
    ctop = Rot(P, "p_ctop", [128, 24], F32, 2)
    sm = Rot(P, "p_sm", [128, 8, 8], F32, 1)
    junk = Rot(P, "p_junk", [128, 16], F32, 2)
    e0 = Rot(P, "p_e0", [128, 8, 128], BF16, 1)
    thr2 = Rot(P, "p_thr2", [128, 8, 16], F32, 1)
    sc2 = Rot(P, "p_sc2", [128, 8, 16], F32, 1)
    sc2b = Rot(P, "p_sc2b", [128, 8, 16], BF16, 1)
    Ytm = Rot(P, "p_Ytm", [128, 128, 64], BF16, 1)
    Ysm = Rot(P, "p_Ysm", [128, 64, 128], BF16, 1)
    Xsm = Rot(P, "p_Xsm", [128, 64, 128], BF16, 1)
    Wst = Rot(P, "p_Wst", [128, 4, 128, 16], BF16, 1)
    wsl = Rot(P, "p_wsl", [128, TBLK, 16], BF16, 2)
    uch = Rot(P, "p_uch", [128, 8, 128], BF16, 3)
    vch = Rot(P, "p_vch", [128, 1024], BF16, 4)
    gl = Rot(P, "p_gl", [128, TBLK], BF16, 2)
    zt = Rot(P, "p_zt", [128, TBLK], BF16, 3)
    ublocks = {}

    def sel_block(blk):
        tsl = slice(blk * TBLK, (blk + 1) * TBLK)
        u_, bu = ublk.next()
        ublocks[blk] = (u_, bu)
        P.dma("sp", u_[:], u2_d.rearrange("(c p) t -> p c t", p=128)[:, :, tsl], reads=[bu2d], writes=[bu])
        q_, bq = qT.next()
        for hp in range(16):
            ph, bph = pH.next()
            for kc in range(8):
                P.op("pe", lambda e, kc=kc, hp=hp, ph=ph, u_=u_: e.matmul(ph[:, 0:TBLK], lhsT=wq[:, kc, hp * 128:(hp + 1) * 128], rhs=u_[:, kc, :],
                                                                  start=(kc == 0), stop=(kc == 7)), reads=[bwq, bu], writes=[bph])
            P.op("act", lambda e, hp=hp, ph=ph, q_=q_: e.activation(out=q_[:, hp, :], in_=ph[:, 0:TBLK], func=AF.Copy), reads=[bph], writes=[bq])
            if hp % 4 == 3:
                yield
        for tt in range(NTL):
            S_, bS = S.next()
            for g4 in range(4):
                pm_, bpm = pM.next()
                for i4 in range(4):
                    hp = g4 * 4 + i4
                    P.op("pe", lambda e, hp=hp, i4=i4, pm_=pm_, q_=q_, tt=tt: e.matmul(
                        pm_[:, i4 * 128:(i4 + 1) * 128], lhsT=q_[:, hp, tt * 128:(tt + 1) * 128], rhs=keysT[:, hp, :], start=True, stop=True),
                        reads=[bq, bkeys], writes=[bpm])
                P.op("act", lambda e, g4=g4, pm_=pm_, S_=S_: e.activation(out=S_[:, g4 * 4:(g4 + 1) * 4, :].rearrange("p a b -> p (a b)"), in_=pm_[:], func=AF.Copy),
                     reads=[bpm], writes=[bS])
            if dbg_d is not None and blk == 0 and tt == 0:
                P.dma("sp", dbg_d, S_[:], reads=[bS])
            yield
            top_, btop = top.next(); jix_, bjix = jix.next()
            for hp in range(16):
                s2, bs2 = S2.next()
                P.op("dve", lambda e, hp=hp, top_=top_, S_=S_: e.max(out=top_[:, hp, 0:8], in_=S_[:, hp, :]), reads=[bS], writes=[btop])
                P.op("dve", lambda e, hp=hp, top_=top_, S_=S_, s2=s2: e.match_replace(out=s2[:], in_to_replace=top_[:, hp, 0:8], in_values=S_[:, hp, :], imm_value=NEG),
                     reads=[bS, btop], writes=[bs2])
                P.op("dve", lambda e, hp=hp, top_=top_, s2=s2: e.max(out=top_[:, hp, 8:16], in_=s2[:]), reads=[bs2, btop], writes=[btop])
                if hp % 2 == 1:
                    h = hp // 2
                    P.op("dve", lambda e, hp=hp, h=h, top_=top_, S_=S_, jix_=jix_: e.max_index(out=jix_[:, h, 0:8], in_max=top_[:, hp, 0:8], in_values=S_[:, hp, :]),
                         reads=[bS, btop], writes=[bjix])
                    P.op("dve", lambda e, hp=hp, h=h, top_=top_, S_=S_, jix_=jix_: e.max_index(out=jix_[:, h, 8:16], in_max=top_[:, hp, 8:16], in_values=S_[:, hp, :]),
                         reads=[bS, btop, bjix], writes=[bjix])
                if hp % 4 == 3:
                    yield
            jif_, bjif = jif.next()
            P.op("dve", lambda e, jif_=jif_, jix_=jix_: e.tensor_copy(out=jif_[:], in_=jix_[:].rearrange("p a b -> p (a b)")), reads=[bjix], writes=[bjif])
            pm_, bpm = pM.next()
            P.op("pe", lambda e, pm_=pm_, jif_=jif_: e.transpose(out=pm_[:, 0:128], in_=jif_[:], identity=ident_f[:]), reads=[bjif, bidf], writes=[bpm])
            jT_, bjT = jT.next()
            P.op("act", lambda e, pm_=pm_, jT_=jT_: e.activation(out=jT_[:], in_=pm_[:, 0:128], func=AF.Copy), reads=[bpm], writes=[bjT])
            sm_, bsm = sm.next(); thr_, bthr = thr2.next(); sc_, bsc = sc2.next(); e0_, be0 = e0.next()
            P.op("pool", lambda e, sm_=sm_: e.memset(sm_[:], 0.0), writes=[bsm])
            for h in range(8):
                cd, bcd = cand.next(); ct, bct = ctop.next()
                P.op("dve", lambda e, h=h, cd=cd, top_=top_: e.tensor_tensor(
                    out=cd[:].rearrange("p (a b) -> p a b", a=16),
                    in0=top_[:, 2 * h, :].rearrange("p (a o) -> p a o", o=1).to_broadcast([128, 16, 16]),
                    in1=top_[:, 2 * h + 1, :].rearrange("p (o b) -> p o b", o=1).to_broadcast([128, 16, 16]), op=ALU.add),
                    reads=[btop], writes=[bcd])
                for r in range(3):
                    P.op("dve", lambda e, r=r, cd=cd, ct=ct: e.max(out=ct[:, r * 8:(r + 1) * 8], in_=cd[:]), reads=[bcd], writes=[bct])
                    if r < 2:
                        P.op("dve", lambda e, r=r, cd=cd, ct=ct: e.match_replace(out=cd[:], in_to_replace=ct[:, r * 8:(r + 1) * 8], in_values=cd[:], imm_value=NEG),
                             reads=[bct, bcd], writes=[bcd])
                P.op("dve", lambda e, h=h, ct=ct, sm_=sm_: e.tensor_tensor(out=sm_[:, h, 0:1], in0=ct[:, 15:16], in1=ct[:, 16:17], op=ALU.add), reads=[bct, bsm], writes=[bsm])
                P.op("dve", lambda e, h=h, sm_=sm_: e.tensor_scalar(out=sm_[:, h, 0:1], in0=sm_[:, h, 0:1], scalar1=0.5, scalar2=None, op0=ALU.mult), reads=[bsm], writes=[bsm])
                P.op("dve", lambda e, h=h, top_=top_, sm_=sm_: e.tensor_scalar(out=sm_[:, h, 1:2], in0=top_[:, 2 * h, 0:1], scalar1=-1.0, scalar2=None, op0=ALU.mult), reads=[btop, bsm], writes=[bsm])
                P.op("dve", lambda e, h=h, top_=top_, sm_=sm_: e.tensor_scalar(out=sm_[:, h, 2:3], in0=top_[:, 2 * h + 1, 0:1], scalar1=-1.0, scalar2=None, op0=ALU.mult), reads=[btop, bsm], writes=[bsm])
                P.op("dve", lambda e, h=h, ct=ct, sm_=sm_: e.tensor_scalar(out=sm_[:, h, 5:6], in0=ct[:, 0:1], scalar1=-1.0, scalar2=None, op0=ALU.mult), reads=[bct, bsm], writes=[bsm])
                jk, bjk = junk.next()
                P.op("act", lambda e, h=h, ct=ct, sm_=sm_, jk=jk: e.activation(out=jk[:], in_=ct[:, 0:16], func=AF.Exp, bias=sm_[:, h, 5:6], scale=1.0, accum_out=sm_[:, h, 3:4]),
                     reads=[bct, bsm], writes=[bjk, bsm])
                P.op("dve", lambda e, h=h, sm_=sm_: e.reciprocal(out=sm_[:, h, 4:5], in_=sm_[:, h, 3:4]), reads=[bsm], writes=[bsm])
                P.op("act", lambda e, h=h, S_=S_, sm_=sm_, e0_=e0_: e.activation(out=e0_[:, h, :], in_=S_[:, 2 * h, :], func=AF.Exp, bias=sm_[:, h, 1:2], scale=1.0),
                     reads=[bS, bsm], writes=[be0])
                P.op("dve", lambda e, h=h, top_=top_, sm_=sm_, thr_=thr_: e.tensor_scalar(out=thr_[:, h, :], in0=top_[:, 2 * h + 1, :], scalar1=-1.0, scalar2=sm_[:, h, 0:1], op0=ALU.mult, op1=ALU.add),
                     reads=[btop, bsm], writes=[bthr])
                P.op("act", lambda e, h=h, top_=top_, sm_=sm_, sc_=sc_: e.activation(out=sc_[:, h, :], in_=top_[:, 2 * h + 1, :], func=AF.Exp, bias=sm_[:, h, 2:3], scale=1.0),
                     reads=[btop, bsm], writes=[bsc])
                P.op("dve", lambda e, h=h, sm_=sm_, sc_=sc_: e.tensor_scalar(out=sc_[:, h, :], in0=sc_[:, h, :], scalar1=sm_[:, h, 4:5], scalar2=None, op0=ALU.mult),
                     reads=[bsc, bsm], writes=[bsc])
                if h % 2 == 1:
                    yield
            scb_, bscb = sc2b.next()
            P.op("dve", lambda e, scb_=scb_, sc_=sc_: e.tensor_copy(out=scb_[:], in_=sc_[:]), reads=[bsc], writes=[bscb])
            for ch in range(2):
                csl = slice(ch * 64, (ch + 1) * 64)
                Y_, bY = Ytm.next()
                for h in range(8):
                    yv = Y_[:, h * 16:(h + 1) * 16, :]
                    P.op("dve", lambda e, h=h, yv=yv, S_=S_, thr_=thr_, csl=csl: e.tensor_tensor(
                        out=yv, in0=S_[:, 2 * h, csl].rearrange("p (o n) -> p o n", o=1).to_broadcast([128, 16, 64]),
                        in1=thr_[:, h, :].rearrange("p (s o) -> p s o", o=1).to_broadcast([128, 16, 64]), op=ALU.is_ge),
                        reads=[bS, bthr], writes=[bY])
                    P.op("dve", lambda e, h=h, yv=yv, e0_=e0_, csl=csl: e.tensor_tensor(
                        out=yv, in0=yv, in1=e0_[:, h, csl].rearrange("p (o n) -> p o n", o=1).to_broadcast([128, 16, 64]), op=ALU.mult),
                        reads=[bY, be0], writes=[bY])
                    P.op("dve", lambda e, h=h, yv=yv, scb_=scb_: e.tensor_tensor(
                        out=yv, in0=yv, in1=scb_[:, h, :].rearrange("p (s o) -> p s o", o=1).to_broadcast([128, 16, 64]), op=ALU.mult),
                        reads=[bY, bscb], writes=[bY])
                    if h % 4 == 3:
                        yield
                Ys_, bYs = Ysm.next()
                for c4 in range(16):
                    pm_, bpm = pM.next()
                    pmb = pm_[:].bitcast(BF16)
                    for i in range(4):
                        cc = c4 * 4 + i
                        P.op("pe", lambda e, cc=cc, i=i, pmb=pmb, Y_=Y_: e.transpose(out=pmb[:, i * 128:(i + 1) * 128], in_=Y_[:, :, cc], identity=ident[:]),
                             reads=[bY, bid], writes=[bpm])
                    if c4 % 2 == 0:
                        P.op("act", lambda e, c4=c4, pmb=pmb, Ys_=Ys_: e.activation(out=Ys_[:, c4 * 4:(c4 + 1) * 4, :].rearrange("p a b -> p (a b)"), in_=pmb[:, 0:512], func=AF.Copy),
                             reads=[bpm], writes=[bYs])
                    else:
                        P.op("dve", lambda e, c4=c4, pmb=pmb, Ys_=Ys_: e.tensor_copy(out=Ys_[:, c4 * 4:(c4 + 1) * 4, :].rearrange("p a b -> p (a b)"), in_=pmb[:, 0:512]),
                             reads=[bpm], writes=[bYs])
                    if c4 % 4 == 3:
                        yield
                Ws_, bWs = Wst.next()
                for th in range(2):
                    X_, bX = Xsm.next()
                    for q2 in range(2):
                        P.op("dve", lambda e, q2=q2, th=th, X_=X_, jT_=jT_: e.tensor_tensor(
                            out=X_[:, q2 * 32:(q2 + 1) * 32, :],
                            in0=iota[:].rearrange("p (o n) -> p o n", o=1).to_broadcast([128, 32, 128]),
                            in1=jT_[:, th * 64 + q2 * 32:th * 64 + (q2 + 1) * 32].rearrange("p (s o) -> p s o", o=1).to_broadcast([128, 32, 128]), op=ALU.is_equal),
                            reads=[biota, bjT], writes=[bX])
                    for t8 in range(8):
                        pm_, bpm = pM.next()
                        for i in range(8):
                            tl = t8 * 8 + i
                            tk = th * 64 + tl
                            P.op("pe", lambda e, tk=tk, tl=tl, i=i, pm_=pm_, X_=X_, Ys_=Ys_: e.matmul(pm_[:, i * 64:(i + 1) * 64], lhsT=X_[:, tl, :], rhs=Ys_[:, :, tk], start=True, stop=True),
                                 reads=[bX, bYs], writes=[bpm])
                        tok0 = th * 64 + t8 * 8
                        src = pm_[:].rearrange("p (t g c) -> p g t c", t=8, g=4)
                        dst = Ws_[:, :, tok0:tok0 + 8, :]
                        if t8 % 2 == 1:
                            P.op("act", lambda e, src=src, dst=dst: e.activation(out=dst, in_=src, func=AF.Copy), reads=[bpm], writes=[bWs])
                        else:
                            P.op("dve", lambda e, src=src, dst=dst: e.tensor_copy(out=dst, in_=src), reads=[bpm], writes=[bWs])
                        if t8 % 4 == 3:
                            yield
                for g in range(4):
                    cg = ch * 4 + g
                    P.dma("act", Wd[blk, cg, :, tt * 128 * 16:(tt + 1) * 128 * 16], Ws_[:, g, :, :].rearrange("p t c -> p (t c)"),
                          reads=[bWs], writes=[bWd[blk]], append=True)
                yield

    def drain(gen, n):
        if gen is None:
            return None
        for _ in range(n):
            try:
                next(gen)
            except StopIteration:
                return None
        return gen

    gen = sel_block(0)
    gen = drain(gen, 10 ** 9)
    for blk in range(nblk):
        u_, bu = ublocks[blk]
        nxt = sel_block(blk + 1) if blk + 1 < nblk else None
        LAG = 2
        stage = {}
        for c in range(NCH + LAG):
            if c < NCH:
                if c % 16 == 0:
                    w_, bw = wsl.next()
                    P.dma("sp", w_[:].rearrange("p t c -> p (t c)"), Wd[blk, c // 16], reads=[bWd[blk]], writes=[bw])
                uc, buc = uch.next(); vc, bvc = vch.next()
                P.dma("sp", uc[:].rearrange("p k n -> p (k n)"), Ub_d[c], reads=[bUb], writes=[buc])
                P.dma("sp", vc[:], Vb_d[c], reads=[bVb], writes=[bvc])
                ph, bph = pH.next()
                for kc in range(8):
                    P.op("pe", lambda e, kc=kc, ph=ph, uc=uc, u_=u_: e.matmul(ph[:, 0:TBLK], lhsT=uc[:, kc, :], rhs=u_[:, kc, :], start=(kc == 0), stop=(kc == 7)),
                         reads=[buc, bu], writes=[bph])
                g_, bg = gl.next()
                P.op("act", lambda e, ph=ph, g_=g_: e.activation(out=g_[:], in_=ph[:, 0:TBLK], func=AF.Gelu), reads=[bph], writes=[bg])
                z_, bz = zt.next()
                P.op("pool", lambda e, c=c, z_=z_, g_=g_, w_=w_: e.tensor_tensor(out=z_[:], in0=w_[:, :, c % 16], in1=g_[:], op=ALU.mult),
                     reads=[bw, bg], writes=[bz])
                stage[c] = (z_, bz, vc, bvc)
            if c >= LAG:
                cc = c - LAG
                z_, bz, vc, bvc = stage.pop(cc)
                for tt in range(NTL):
                    for hf in range(2):
                        k = tt * 2 + hf
                        P.op("pe", lambda e, cc=cc, tt=tt, hf=hf, k=k, z_=z_, vc=vc: e.matmul(pO[k][:], lhsT=z_[:, tt * 128:(tt + 1) * 128], rhs=vc[:, hf * 512:(hf + 1) * 512],
                                                                                  start=(cc == 0), stop=(cc == NCH - 1)), reads=[bz, bvc], writes=[bpO[k]])
            if c % 2 == 1:
                nxt = drain(nxt, 1)
        nxt = drain(nxt, 10 ** 9)
        for tt in range(NTL):
            tg = blk * NTL + tt
            xt, bx = xin.next()
            P.dma("sp", xt[:], D["xmid"][tg * 128:(tg + 1) * 128, :], writes=[bx])
            xo_, bxo = xout.next()
            emit_resid_ln(P, C, [pO[tt * 2], pO[tt * 2 + 1]], [bpO[tt * 2], bpO[tt * 2 + 1]], xt, bx, gb, bgb, lng, lnb, bln, epsb, beps, xo_, bxo, "p")
            P.dma("pool", xo[tg * 128:(tg + 1) * 128, :], xo_[:], reads=[bxo])
    P.pop()


def k3_host_inputs(inp, l, core, xmid_core):
    b = core // 2
    rep = lambda v: np.ascontiguousarray(np.broadcast_to(np.asarray(v, np.float32)[None, :], (128, len(v))))
    keys = inp["peer_keys"][l].reshape(16, 128, 128)
    return {
        "xmid": np.ascontiguousarray(xmid_core),
        "cT": to_pc(inp["c"][b], 8),
        "adaw_f": np.ascontiguousarray(inp["ada_w"][l][:, 3072:6144]),
        "adab_f": to_pc(inp["ada_b"][l][3072:5120], 16),
        "adab_g": rep(inp["ada_b"][l][5120:6144]),
        "wq": np.ascontiguousarray(inp["peer_wq"][l]),
        "keysT": np.ascontiguousarray(keys.transpose(2, 0, 1)),
        "UT": np.ascontiguousarray(inp["peer_u"][l].T),
        "V": np.ascontiguousarray(inp["peer_v"][l]),
        "lng": rep(inp["ln2_g"][l]), "lnb": rep(inp["ln2_b"][l]),
        "ident": np.eye(128, dtype=np.float32),
        "iota": np.ascontiguousarray(np.broadcast_to(np.arange(128, dtype=np.float32)[None, :], (128, 128))),
    }


import math

PAIRS = [[0, 1], [2, 3], [4, 5], [6, 7]]

GATHER = [
    ("mla_kT", [768, T], 192, [0, 1, 2, 3]),
    ("mla_v", [T, 520], 1024, [0, 1, 2, 3]),
    ("diff_kT", [512, T], 128, [0, 1, 2, 3]),
    ("diff_v", [T, 516], 1024, [0, 1, 2, 3]),
    ("swa_kT", [128, T], 128, [0]),
    ("swa_v", [T, 130], 2048, [1, 0]),
    ("nat_kT", [512, T], 128, [0, 1, 2, 3]),
    ("nat_v", [T, 520], 1024, [0, 3]),
]

LAYER_IN = [("adaw", [1024, 6144]), ("adab1", [128, 16]), ("adabg", [128, 1024]), ("adabf", [128, 16]), ("adabg2", [128, 1024]),
            ("w1", None), ("qupA", [384, 768]), ("qupB", [384, 768]), ("kvn", [256, 512]), ("kvv", [256, 512]),
            ("gq", [128, 3]), ("gkv", [128, 2]), ("sinkb", [128, 8]), ("lamb", [128, 4, 64]), ("subln", [128, 128]), ("lami", [128, 2]),
            ("nat_bias", [8, 128, 29 * 128]), ("wg", [4, 1024, 1024]), ("wb", [4, 512, 1024]), ("wo", [1024, 1024]),
            ("lng1", [128, 1024]), ("lnb1", [128, 1024]), ("wq", [1024, 2048]), ("keysT", [128, 16, 128]),
            ("UT", [1024, 16384]), ("V", [16384, 1024]), ("lng2", [128, 1024]), ("lnb2", [128, 1024])]
COMMON_IN = [("x", [T, 1024]), ("cT", [128, 8]), ("C64", [128, T]), ("S64", [128, T]), ("CM", [128, T]), ("SM", [128, T]),
             ("ident", [128, 128]), ("iota", [128, 128]), ("swa_masks", [4, 128, 512])]


def build_fused(nc, nlayers=2, peer_blocks=NBLK, dbg=False):
    P = Prog(nc)
    units, w1cols = k1_weight_layout()
    NC1 = len(w1cols)
    I = {}
    for n, s in COMMON_IN:
        I[n] = nc.dram_tensor(n, list(s), F32, kind="ExternalInput").ap()
    for l in range(nlayers):
        for n, s in LAYER_IN:
            if n == "w1":
                s = [1024, NC1]
            I[f"{n}_{l}"] = nc.dram_tensor(f"{n}_{l}", list(s), F32, kind="ExternalInput").ap()
    out_d = nc.dram_tensor("out", [T, 1024], F32, kind="ExternalOutput").ap()
    x_cur = I["x"]
    for l in range(nlayers):
        L = lambda n: I[f"{n}_{l}"]
        it = lambda n, s, d=BF16: nc.dram_tensor(f"{n}_L{l}", list(s), d, kind="Internal").ap()
        O2 = {"uT": it("uT", [1024, T]), "swa_qT": it("swa_qT", [512, T]), "swa_kT": it("swa_kT", [128, T]),
              "diff_qT": it("diff_qT", [512, T]), "diff_kT": it("diff_kT", [512, T]), "nat_qT": it("nat_qT", [512, T]),
              "nat_kT": it("nat_kT", [512, T]), "mla_qT": it("mla_qT", [768, T]), "mla_kT": it("mla_kT", [768, T]),
              "swa_v": it("swa_v", [T, 130]), "diff_v": it("diff_v", [T, 516]), "nat_v": it("nat_v", [T, 520]), "mla_v": it("mla_v", [T, 520])}
        O = dict(O2)
        O["mla_qT"] = O2["mla_qT"].rearrange("(h d) t -> h d t", d=96)
        O["mla_kT"] = O2["mla_kT"].rearrange("(h d) t -> h d t", d=96)
        for n in ("swa_v", "diff_v", "nat_v", "mla_v"):
            O[n] = O2[n].rearrange("(t p) f -> t p f", p=128)
        D1 = {"x": x_cur, "cT": I["cT"], "adaw": L("adaw")[:, 0:2048], "adab": L("adab1"), "w1": L("w1"),
              "qupA": L("qupA"), "qupB": L("qupB"), "kvn": L("kvn"), "kvv": L("kvv"), "gq": L("gq"), "gkv": L("gkv"),
              "C64": I["C64"], "S64": I["S64"], "CM": I["CM"], "SM": I["SM"], "ident": I["ident"]}
        P.push()
        emit_k1(P, nc, D1, O)
        P.pop()
        G = {}
        for (name, shp, rc, chunks) in GATHER:
            G[name] = []
            if name == "swa_v":
                dst = it(f"g_{name}", [2 * T, 130])
                P.collective(O2[name], dst, PAIRS)
                G[name].append(dst)
                continue
            for k in chunks:
                dst = it(f"g_{name}{k}", [2 * rc, shp[1]])
                P.collective(O2[name][k * rc:(k + 1) * rc, :], dst, PAIRS)
                G[name].append(dst)
        P.barrier()
        oT_d = it("oT", [4, 512, T])
        D2 = {"swa_qT": O["swa_qT"], "swa_masks": I["swa_masks"], "sinkb": L("sinkb"), "mla_qT": O["mla_qT"],
              "diff_qT": O["diff_qT"], "lamb": L("lamb"), "subln": L("subln"), "lami": L("lami"),
              "nat_qT": O["nat_qT"], "nat_bias": L("nat_bias"), "ident": I["ident"]}
        P.push()
        pre = peer_scratch(nc, f"_L{l}")
        cf = Rot(P, "pa_cf", [128, 4096], F32, 2)
        cb = Rot(P, "pa_cb", [128, 4096], BF16, 2)
        bgen = peer_cast_gen(P, L("UT"), L("V"), pre[0], pre[1], pre[2], pre[3], cf, cb)
        emit_attention(P, D2, oT_d, SRC=GatherSrc(O, G), bg=bgen)
        for _ in bgen:
            pass
        P.pop()
        xmid_d = it("xmid", [T, 1024], F32)
        D3 = {"oT": oT_d, "uT": O["uT"], "x": x_cur, "wg": L("wg"), "wb": L("wb"), "wo": L("wo"), "cT": I["cT"],
              "adaw_g": L("adaw")[:, 2048:3072], "adab_g": L("adabg"), "lng": L("lng1"), "lnb": L("lnb1")}
        emit_merge(P, D3, xmid_d)
        xo = out_d if l == nlayers - 1 else it("xout", [T, 1024], F32)
        D4 = {"xmid": xmid_d, "cT": I["cT"], "adaw_f": L("adaw")[:, 3072:6144], "adab_f": L("adabf"), "adab_g": L("adabg2"),
              "wq": L("wq"), "keysT": L("keysT"), "UT": L("UT"), "V": L("V"), "lng": L("lng2"), "lnb": L("lnb2"),
              "ident": I["ident"], "iota": I["iota"]}
        emit_peer(P, nc, D4, xo, nblk=peer_blocks, tag=f"_L{l}", pre=pre)
        P.barrier()
        if dbg and l == 0:
            d1 = nc.dram_tensor("dbg_oT", [4, 512, T], BF16, kind="ExternalOutput").ap()
            d2 = nc.dram_tensor("dbg_xmid", [T, 1024], F32, kind="ExternalOutput").ap()
            d3 = nc.dram_tensor("dbg_uT", [1024, T], BF16, kind="ExternalOutput").ap()
            for n in range(4):
                P.dma("sp", d1[n], oT_d[n])
            P.dma("act", d2, xmid_d)
            P.dma("act", d3, O["uT"])
            P.barrier()
        x_cur = xo
    P.finish()
    P.emit()
    return P


def fused_host_inputs(inp, core, nlayers=2):
    b, half = core // 2, core % 2
    units, w1cols = k1_weight_layout()
    qa, qb, knope, vcols = mla_up_layout()
    C64, S64, CM, SM = rope_tables(half * T, T)
    rep = lambda v: np.ascontiguousarray(np.broadcast_to(np.asarray(v, np.float32)[None, :], (128, len(v))))
    m = {
        "x": np.ascontiguousarray(inp["x"][b, half * T:(half + 1) * T], dtype=np.float32),
        "cT": to_pc(inp["c"][b], 8), "C64": C64, "S64": S64, "CM": CM, "SM": SM,
        "ident": np.eye(128, dtype=np.float32),
        "iota": np.ascontiguousarray(np.broadcast_to(np.arange(128, dtype=np.float32)[None, :], (128, 128))),
        "swa_masks": swa_masks(half),
    }
    for l in range(nlayers):
        ab = inp["ada_b"][l]
        lam = np.stack([inp["diff_lambda_q1"][l], inp["diff_lambda_k1"][l], inp["diff_lambda_q2"][l], inp["diff_lambda_k2"][l]])
        li = 0.8 - 0.6 * math.exp(-0.3 * l)
        keys = inp["peer_keys"][l].reshape(16, 128, 128)
        d = {
            "adaw": np.ascontiguousarray(inp["ada_w"][l]), "adab1": to_pc(ab[0:2048], 16), "adabg": rep(ab[2048:3072]),
            "adabf": to_pc(ab[3072:5120], 16), "adabg2": rep(ab[5120:6144]),
            "w1": np.ascontiguousarray(inp["w_in"][l][:, w1cols]),
            "qupA": np.ascontiguousarray(inp["mla_q_up"][l][:, qa]), "qupB": np.ascontiguousarray(inp["mla_q_up"][l][:, qb]),
            "kvn": np.ascontiguousarray(inp["mla_kv_up"][l][:, knope]), "kvv": np.ascontiguousarray(inp["mla_kv_up"][l][:, vcols]),
            "gq": to_pc(inp["mla_q_norm"][l], 3), "gkv": to_pc(inp["mla_kv_norm"][l], 2),
            "sinkb": rep(inp["swa_sink"][l]),
            "lamb": np.ascontiguousarray(np.broadcast_to(lam[None], (128, 4, 64))).astype(np.float32),
            "subln": rep(inp["diff_subln"][l]),
            "lami": np.ascontiguousarray(np.broadcast_to(np.array([li, 1.0 - li], np.float32)[None], (128, 2))),
            "nat_bias": nat_bias_tables(inp["nat_rpb"][l], half),
            "wg": np.ascontiguousarray(inp["w_gate"][l]), "wb": np.ascontiguousarray(inp["w_branch"][l]), "wo": np.ascontiguousarray(inp["w_out"][l]),
            "lng1": rep(inp["ln1_g"][l]), "lnb1": rep(inp["ln1_b"][l]),
            "wq": np.ascontiguousarray(inp["peer_wq"][l]), "keysT": np.ascontiguousarray(keys.transpose(2, 0, 1)),
            "UT": np.ascontiguousarray(inp["peer_u"][l].T), "V": np.ascontiguousarray(inp["peer_v"][l]),
            "lng2": rep(inp["ln2_g"][l]), "lnb2": rep(inp["ln2_b"][l]),
        }
        for k, v in d.items():
            m[f"{k}_{l}"] = np.ascontiguousarray(v, dtype=np.float32)
    return m


NCORES = 8
_PROGS = {}


def kernel(**inputs):
    inp = {k: np.asarray(v) for k, v in inputs.items()}
    if "fused" not in _PROGS:
        nc = bass.Bass("TRN2", target_bir_lowering=False)
        build_fused(nc)
        _PROGS["fused"] = nc
    nc = _PROGS["fused"]
    maps = [fused_host_inputs(inp, c) for c in range(NCORES)]
    res = run_bass_kernel_spmd(nc, maps, core_ids=list(range(NCORES)))
    out = np.empty((4, 2 * T, 1024), np.float32)
    for c in range(NCORES):
        b, half = c // 2, c % 2
        out[b, half * T:(half + 1) * T] = np.asarray(res.results[c]["out"])
    return out
```

```python
import numpy as np
import concourse.bass as bass
import concourse.mybir as mybir
from concourse.bass_utils import run_bass_kernel_spmd
from contextlib import ExitStack

F32 = mybir.dt.float32
BF16 = mybir.dt.bfloat16
I32 = mybir.dt.int32
U32 = mybir.dt.uint32
AF = mybir.ActivationFunctionType
ALU = mybir.AluOpType
AX = mybir.AxisListType

ENGS = ["pe", "act", "dve", "pool", "sp"]
DMA_RING = 12


class Buf:
    __slots__ = ("name", "lastw", "readers", "excl", "pre")

    def __init__(self, name="", excl=False):
        self.name = name
        self.excl = excl
        self.pre = []
        self.lastw = []
        self.readers = []


class Prog:
    def __init__(self, nc, same_engine_sync=True):
        self.nc = nc
        self.es = ExitStack()
        self.stack = [self.es]
        self.q = {e: [] for e in ENGS}
        self.cnt = {e: 0 for e in ENGS}
        self.sems = {}
        self.EPOCH = 50000
        self.ring_n = {e: 0 for e in ENGS}
        self.ring_val = {}
        self.seen = {e: {} for e in ENGS}
        self.same_engine_sync = same_engine_sync
        self.n_waits = 0
        self.n_ops = 0
        self.uid = 0
        self.E = {"pe": nc.tensor, "act": nc.scalar, "dve": nc.vector, "pool": nc.gpsimd, "sp": nc.sync}

    def sb(self, name, shape, dtype):
        self.uid += 1
        return self.stack[-1].enter_context(self.nc.sbuf_tensor(f"s{self.uid}_{name}", list(shape), dtype))

    def ps(self, name, shape, dtype=F32):
        self.uid += 1
        return self.stack[-1].enter_context(self.nc.psum_tensor(f"p{self.uid}_{name}", list(shape), dtype))

    def push(self):
        self.stack.append(ExitStack())

    def pop(self):
        self.barrier()
        self.stack.pop().close()

    def barrier(self):
        evs = [self._last_ev(e) for e in ENGS if self.cnt[e] > 0]
        evs += [(k, v) for k, v in self.ring_val.items() if v > 0]
        for e in ENGS:
            for ev in evs:
                self._wait(e, ev)

    def dram(self, name, shape, dtype, kind="Internal"):
        return self.nc.dram_tensor(name, list(shape), dtype, kind=kind).ap()

    def _wait(self, eng, ev):
        key, val = ev
        if key[0] == "eng" and key[1] == eng and (eng == "pe" or not self.same_engine_sync):
            return
        if self.seen[eng].get(key, 0) >= val:
            return
        self.seen[eng][key] = val
        self.E[eng].wait_ge(self.sems[key], val)
        self.n_waits += 1

    def _deps(self, eng, reads, writes):
        for b in reads:
            for ev in b.lastw:
                self._wait(eng, ev)
            if b.excl:
                for ev in b.readers:
                    self._wait(eng, ev)
        for b in writes:
            for ev in b.lastw:
                self._wait(eng, ev)
            for ev in b.readers:
                self._wait(eng, ev)

    def _commit(self, ev, reads, writes):
        for b in writes:
            b.pre = list(b.lastw) + list(b.readers)
            b.lastw = [ev]
            b.readers = []
        for b in reads:
            b.readers = [r for r in b.readers if r[0] != ev[0]] + [ev]

    def _next_ev(self, eng):
        ep, k = divmod(self.cnt[eng], self.EPOCH)
        self.cnt[eng] += 1
        key = ("eng", eng, ep)
        if key not in self.sems:
            self.sems[key] = self.nc.alloc_semaphore(name=f"pg_{eng}_{ep}")
        return (key, k + 1)

    def _last_ev(self, eng):
        if self.cnt[eng] == 0:
            return None
        ep, k = divmod(self.cnt[eng] - 1, self.EPOCH)
        return (("eng", eng, ep), k + 1)

    def collective(self, ins_ap, outs_ap, groups, reads=(), writes=()):
        eng = "pool"
        self._deps(eng, reads, writes)
        k = self.ring_n.get("cc", 0) % 4
        self.ring_n["cc"] = self.ring_n.get("cc", 0) + 1
        key = ("ring", "cc", k)
        if key not in self.sems:
            self.sems[key] = self.nc.alloc_semaphore(name=f"pg_cc_{k}")
            self.ring_val[key] = 0
        prev = self.ring_val[key]
        if prev > 0:
            self._wait(eng, (key, prev))
        val = prev + 1
        self.ring_val[key] = val
        ev = (key, val)
        self.E[eng].collective_compute("AllGather", ALU.bypass, replica_groups=groups, ins=[ins_ap], outs=[outs_ap]).then_inc(self.sems[key])
        self.n_ops += 1
        self._commit(ev, reads, writes)
        return ev

    def op(self, eng, fn, reads=(), writes=()):
        self._deps(eng, reads, writes)
        ev = self._next_ev(eng)
        ins = fn(self.E[eng])
        ins.then_inc(self.sems[ev[0]], 1)
        self.n_ops += 1
        self._commit(ev, reads, writes)
        return ev

    def dma(self, eng, out, in_, reads=(), writes=(), append=False, **kw):
        if append:
            self._deps(eng, reads, ())
            for b in writes:
                for ev in b.pre:
                    self._wait(eng, ev)
                for ev in b.readers:
                    self._wait(eng, ev)
        else:
            self._deps(eng, reads, writes)
        k = self.ring_n[eng] % DMA_RING
        self.ring_n[eng] += 1
        key = ("ring", eng, k)
        if key not in self.sems:
            self.sems[key] = self.nc.alloc_semaphore(name=f"pg_r_{eng}_{k}")
            self.ring_val[key] = 0
        prev = self.ring_val[key]
        if prev > 0:
            self._wait(eng, (key, prev))
        val = prev + 16
        self.ring_val[key] = val
        ev = (key, val)
        self.E[eng].dma_start(out=out, in_=in_, **kw).then_inc(self.sems[key], 16)
        self.n_ops += 1
        if append:
            for b in writes:
                b.lastw = b.lastw + [ev]
            for b in reads:
                b.readers = [r for r in b.readers if r[0] != ev[0]] + [ev]
        else:
            self._commit(ev, reads, writes)
        return ev

    def finish(self):
        for key, val in list(self.ring_val.items()):
            if val > 0:
                self._wait("sp", (key, val))
        for e in ENGS:
            if e != "sp" and self.cnt[e] > 0:
                self._wait("sp", self._last_ev(e))

    def emit(self):
        self.es.close()


T = 4096
NTB = 8
NTT = 32

O_QA, O_KA, O_VA, O_CQ, O_CKV, O_KR, O_QC, O_KC, O_VC, O_QD, O_KD, O_VD = (
    0, 512, 640, 768, 1152, 1408, 1440, 1952, 2464, 2976, 3488, 4000)


class Rot:
    def __init__(self, P, name, shape, dtype, n, space="sb"):
        mk = P.sb if space == "sb" else P.ps
        self.tiles = [mk(f"{name}{i}", shape, dtype) for i in range(n)]
        self.bufs = [Buf(f"{name}{i}", excl=(space == "ps")) for i in range(n)]
        self.i = 0
        self.n = n

    def next(self):
        t, b = self.tiles[self.i % self.n], self.bufs[self.i % self.n]
        self.i += 1
        return t, b


def k1_weight_layout():
    def rot64(cols):
        cols = np.asarray(cols).reshape(-1, 64)
        return np.concatenate([cols[:, 32:], cols[:, :32]], axis=1).reshape(-1)

    units = []
    cols = []

    def add(name, kind, c):
        units.append((name, kind, sum(len(x) for x in cols), len(c)))
        cols.append(np.asarray(c))

    for name, off, n in (("swa_q", O_QA, 512), ("swa_k", O_KA, 128), ("diff_q", O_QC, 512), ("diff_k", O_KC, 512)):
        for ch in range(n // 128):
            a = np.arange(off + ch * 128, off + (ch + 1) * 128)
            add(f"{name}{ch}", "rope", np.concatenate([a, rot64(a)]))
    for name, off in (("nat_q", O_QD), ("nat_k", O_KD)):
        for ch in range(4):
            add(f"{name}{ch}", "plain", np.arange(off + ch * 128, off + (ch + 1) * 128))
    kr = np.arange(O_KR, O_KR + 32)
    krB = np.concatenate([kr[16:], kr[:16]])
    pad = np.arange(O_CQ, O_CQ + 64)
    add("mla", "mla", np.concatenate([np.arange(O_CQ, O_CQ + 384), np.arange(O_CKV, O_CKV + 256),
                                      pad, kr, pad, krB]))
    add("swa_v", "v", np.arange(O_VA, O_VA + 128))
    add("diff_v", "v", np.arange(O_VC, O_VC + 512))
    add("nat_v", "v", np.arange(O_VD, O_VD + 512))
    return units, np.concatenate(cols)


def mla_up_layout():
    qa = np.arange(768)
    qb = qa.copy().reshape(8, 96)
    qb = np.concatenate([qb[:, :64], qb[:, 80:96], qb[:, 64:80]], axis=1).reshape(-1)
    kv = np.arange(1024).reshape(8, 128)
    knope = kv[:, :64].reshape(-1)
    vcols = kv[:, 64:].reshape(-1)
    return qa, qb, knope, vcols


def rope_tables(pos0, n):
    pos = np.arange(pos0, pos0 + n, dtype=np.float32)
    inv64 = (10000.0 ** (-np.arange(0, 64, 2, dtype=np.float32) / 64)).astype(np.float32)
    ang = (pos[None, :] * inv64[:, None]).astype(np.float32)
    c, s = np.cos(ang).astype(np.float32), np.sin(ang).astype(np.float32)
    C64 = np.concatenate([c, c, c, c], 0)
    S64 = np.concatenate([-s, s, -s, s], 0)
    inv32 = (10000.0 ** (-np.arange(0, 32, 2, dtype=np.float32) / 32)).astype(np.float32)
    ang = (pos[None, :] * inv32[:, None]).astype(np.float32)
    c, s = np.cos(ang).astype(np.float32), np.sin(ang).astype(np.float32)
    CM = np.zeros((128, n), np.float32)
    SM = np.zeros((128, n), np.float32)
    CM[64:96] = np.concatenate([c, c], 0)
    SM[64:96] = np.concatenate([-s, s], 0)
    return C64, S64, CM, SM


def emit_mod_cols(P, nc, cT_d, adaw_d, adab_d, ngrp, modc, bmodc, pm_rot, wst_rot):
    cs = P.sb("mod_cs", [128, 8], F32)
    bcs = Buf("cs")
    adab = P.sb("mod_adab", [128, ngrp * 8], F32)
    badab = Buf("adab")
    P.dma("sp", cs[:], cT_d, writes=[bcs])
    P.dma("sp", adab[:], adab_d, writes=[badab])
    P.op("act", lambda e: e.activation(out=cs[:], in_=cs[:], func=AF.Silu), reads=[bcs], writes=[bcs])
    pm, bpm = pm_rot.next()
    noc = ngrp * 8
    for g in range(noc // 2):
        wst_, bw = wst_rot.next()
        wst = wst_[:, 0:2048].rearrange("p (c n) -> p c n", c=8)
        P.dma("sp" if g % 2 == 0 else "act", wst,
              adaw_d[:, g * 256:(g + 1) * 256].rearrange("(c p) n -> p c n", p=128), writes=[bw])
        for o4 in range(2):
            oc = g * 2 + o4
            for kc in range(8):
                P.op("pe", lambda e, oc=oc, kc=kc, o4=o4, wst=wst: e.matmul(
                    pm[:, oc:oc + 1], lhsT=wst[:, kc, o4 * 128:(o4 + 1) * 128], rhs=cs[:, kc:kc + 1],
                    start=(kc == 0), stop=(kc == 7)), reads=[bw, bcs], writes=[bpm])
    P.op("dve", lambda e: e.tensor_tensor(out=modc[:, 0:noc], in0=pm[:, 0:noc], in1=adab[:], op=ALU.add),
         reads=[bpm, badab], writes=[bmodc])


def emit_ln_mod(P, x_d, uT, buT_tiles, shcol, sc1col, bmod, ident, bid, epsb, beps, ptr_rot, tag=""):
    xin = Rot(P, f"ln_x{tag}", [128, 1024], F32, 2)
    xn = Rot(P, f"ln_xn{tag}", [128, 1024], BF16, 2)
    st = Rot(P, f"ln_st{tag}", [128, 2, 6], F32, 2)
    mv = Rot(P, f"ln_mv{tag}", [128, 2], F32, 2)
    rs = Rot(P, f"ln_rs{tag}", [128, 1], F32, 2)
    for t in range(NTT):
        xt, bx = xin.next()
        P.dma("sp", xt[:], x_d[t * 128:(t + 1) * 128, :], writes=[bx])
        s_, bs = st.next()
        for c in range(2):
            P.op("dve", lambda e, c=c, s_=s_, xt=xt: e.bn_stats(out=s_[:, c, :], in_=xt[:, c * 512:(c + 1) * 512]),
                 reads=[bx], writes=[bs])
        m_, bm = mv.next()
        P.op("dve", lambda e, m_=m_, s_=s_: e.bn_aggr(out=m_[:], in_=s_[:]), reads=[bs], writes=[bm])
        r_, br = rs.next()
        P.op("act", lambda e, r_=r_, m_=m_: e.activation(out=r_[:], in_=m_[:, 1:2], func=AF.Ln, bias=epsb[:, 0:1], scale=1.0),
             reads=[bm, beps], writes=[br])
        P.op("act", lambda e, r_=r_: e.activation(out=r_[:], in_=r_[:], func=AF.Exp, scale=-0.5), reads=[br], writes=[br])
        xn_, bxn = xn.next()
        P.op("dve", lambda e, xn_=xn_, xt=xt, m_=m_, r_=r_: e.tensor_scalar(
            out=xn_[:], in0=xt[:], scalar1=m_[:, 0:1], scalar2=r_[:, 0:1], op0=ALU.subtract, op1=ALU.mult),
            reads=[bx, bm, br], writes=[bxn])
        pt, bpt = ptr_rot.next()
        for c in range(8):
            P.op("pe", lambda e, c=c, pt=pt, xn_=xn_: e.transpose(out=pt[:, c, :], in_=xn_[:, c * 128:(c + 1) * 128], identity=ident[:]),
                 reads=[bxn, bid], writes=[bpt])
        for c in range(8):
            P.op("act", lambda e, c=c, pt=pt, t=t: e.activation(
                out=uT[:, c, t * 128:(t + 1) * 128], in_=pt[:, c, :], func=AF.Identity,
                scale=sc1col[:, c:c + 1], bias=shcol[:, c:c + 1]), reads=[bpt, bmod], writes=[buT_tiles[t]])


def k1_io():
    units, w1cols = k1_weight_layout()
    NC1 = len(w1cols)
    ins = [("x", [T, 1024]), ("cT", [128, 8]), ("adaw", [1024, 2048]), ("adab", [128, 16]), ("w1", [1024, NC1]),
           ("qupA", [384, 768]), ("qupB", [384, 768]), ("kvn", [256, 512]), ("kvv", [256, 512]), ("gq", [128, 3]), ("gkv", [128, 2]),
           ("C64", [128, T]), ("S64", [128, T]), ("CM", [128, T]), ("SM", [128, T]), ("ident", [128, 128])]
    outs = [("uT", [1024, T]), ("swa_qT", [512, T]), ("swa_kT", [128, T]), ("diff_qT", [512, T]), ("diff_kT", [512, T]),
            ("nat_qT", [512, T]), ("nat_kT", [512, T]), ("mla_qT", [8, 96, T]), ("mla_kT", [8, 96, T]),
            ("swa_v", [NTT, 128, 2 * 65]), ("diff_v", [NTT, 128, 4 * 129]), ("nat_v", [NTT, 128, 8 * 65]), ("mla_v", [NTT, 128, 8 * 65])]
    return ins, outs


def build_k1(nc, stop=None):
    P = Prog(nc)
    ins, outs = k1_io()
    D = {n: nc.dram_tensor(n, list(s), F32, kind="ExternalInput").ap() for n, s in ins}
    O = {n: nc.dram_tensor(n, list(s), BF16, kind="ExternalOutput").ap() for n, s in outs}
    emit_k1(P, nc, D, O, stop)
    P.finish()
    P.emit()
    return P


def emit_k1(P, nc, D, O, stop=None):
    units, w1cols = k1_weight_layout()
    x_d, cT_d, adaw_d, adab_d, w1_d = D["x"], D["cT"], D["adaw"], D["adab"], D["w1"]
    qupA_d, qupB_d, kvn_d, kvv_d, gq_d, gkv_d = D["qupA"], D["qupB"], D["kvn"], D["kvv"], D["gq"], D["gkv"]
    C64_d, S64_d, CM_d, SM_d, ident_d = D["C64"], D["S64"], D["CM"], D["SM"], D["ident"]
    o_uT = O["uT"]
    o = {"swa_q": O["swa_qT"], "swa_k": O["swa_kT"], "diff_q": O["diff_qT"], "diff_k": O["diff_kT"],
         "nat_q": O["nat_qT"], "nat_k": O["nat_kT"], "mla_q": O["mla_qT"], "mla_k": O["mla_kT"],
         "swa_v": O["swa_v"], "diff_v": O["diff_v"], "nat_v": O["nat_v"], "mla_v": O["mla_v"]}

    identf = P.sb("identf", [128, 128], F32); bidf = Buf()
    ident = P.sb("ident", [128, 128], BF16); bid = Buf()
    onesb = P.sb("onesb", [128, 128], BF16); bones = Buf()
    epsb = P.sb("epsb", [128, 1], F32); beps = Buf()
    P.dma("pool", identf[:], ident_d, writes=[bidf])
    P.op("dve", lambda e: e.tensor_copy(out=ident[:], in_=identf[:]), reads=[bidf], writes=[bid])
    P.op("pool", lambda e: e.memset(onesb[:], 1.0), writes=[bones])
    P.op("pool", lambda e: e.memset(epsb[:], 1e-5), writes=[beps])

    uT = P.sb("uT", [128, 8, T], BF16)
    buT = [Buf(f"uT{t}") for t in range(NTT)]
    modc = P.sb("modc", [128, 16], F32); bmodc = Buf()
    sc1 = P.sb("sc1", [128, 8], F32); bsc1 = Buf()

    pA = Rot(P, "pA", [128, 512], F32, 2, "ps")
    pB = Rot(P, "pB", [128, 512], F32, 2, "ps")
    ptr = Rot(P, "ptr", [128, 8, 128], BF16, 2, "ps")
    pmisc = Rot(P, "pmisc", [128, 512], F32, 1, "ps")

    wst = Rot(P, "wst", [128, 2048], F32, 2)
    wbf = Rot(P, "wbf", [128, 8, 256], BF16, 2)

    emit_mod_cols(P, nc, cT_d, adaw_d, adab_d, 2, modc, bmodc, pmisc, wst)
    P.op("dve", lambda e: e.tensor_scalar(out=sc1[:], in0=modc[:, 8:16], scalar1=1.0, scalar2=None, op0=ALU.add),
         reads=[bmodc], writes=[bsc1])
    bmod = Buf("mod")
    P.op("dve", lambda e: e.tensor_copy(out=modc[:, 0:8], in_=modc[:, 0:8]), reads=[bmodc, bsc1], writes=[bmod])

    if stop == "mod":
        return
    emit_ln_mod(P, x_d, uT, buT, modc, sc1, bmod, ident, bid, epsb, beps, ptr)
    if stop == "ln":
        return

    for c in range(8):
        P.dma("pool", o_uT[c * 128:(c + 1) * 128, :], uT[:, c, :], reads=buT)

    c64r = Rot(P, "c64r", [128, 512], F32, 2)
    s64r = Rot(P, "s64r", [128, 512], F32, 2)

    t1r = Rot(P, "t1r", [128, 512], F32, 2)
    t2r = Rot(P, "t2r", [128, 512], F32, 2)
    ostg = Rot(P, "ostg", [128, 512], BF16, 3)

    def load_w(col0, ncols):
        ws_, bws = wst.next()
        wb, bwb = wbf.next()
        ws = ws_[:, 0:8 * ncols].rearrange("p (c n) -> p c n", c=8)
        h = ncols // 2
        P.dma("sp", ws[:, :, 0:h], w1_d[:, col0:col0 + h].rearrange("(c p) n -> p c n", p=128), writes=[bws])
        P.dma("sp", ws[:, :, h:ncols], w1_d[:, col0 + h:col0 + ncols].rearrange("(c p) n -> p c n", p=128),
              writes=[bws], append=True)
        P.op("pool", lambda e: e.tensor_copy(out=wb[:, :, 0:ncols], in_=ws), reads=[bws], writes=[bwb])
        return wb, bwb

    def mm_fm(ps, bps, wb, bwb, c0, M, tb):
        for kc in range(8):
            P.op("pe", lambda e, kc=kc: e.matmul(ps[0:M, :], lhsT=wb[:, kc, c0:c0 + M], rhs=uT[:, kc, tb * 512:(tb + 1) * 512],
                                               start=(kc == 0), stop=(kc == 7)),
                 reads=[bwb] + buT[tb * 4:(tb + 1) * 4], writes=[bps])

    dq = ["act", "pool"]
    dqi = [0]

    def nextq():
        dqi[0] += 1
        return dq[dqi[0] % 2]

    for (name, kind, col0, ncols) in units:
        if stop is not None and stop == name:
            break
        if kind == "rope":
            base = name[:-1]; ch = int(name[-1])
            wb, bwb = load_w(col0, 256)
            for tb in range(NTB):
                a, ba = pA.next(); b, bb = pB.next()
                mm_fm(a, ba, wb, bwb, 0, 128, tb)
                mm_fm(b, bb, wb, bwb, 128, 128, tb)
                t1, bt1 = t1r.next(); t2, bt2 = t2r.next()
                sl = slice(tb * 512, (tb + 1) * 512)
                C64, bc64 = c64r.next(); S64, bs64 = s64r.next()
                P.dma("sp", C64[:], C64_d[:, sl], writes=[bc64])
                P.dma("sp", S64[:], S64_d[:, sl], writes=[bs64])
                P.op("dve", lambda e, t1=t1, a=a, C64=C64: e.tensor_tensor(out=t1[:], in0=a[:], in1=C64[:], op=ALU.mult),
                     reads=[ba, bc64], writes=[bt1])
                P.op("dve", lambda e, t2=t2, b=b, S64=S64: e.tensor_tensor(out=t2[:], in0=b[:], in1=S64[:], op=ALU.mult),
                     reads=[bb, bs64], writes=[bt2])
                og, bog = ostg.next()
                P.op("pool", lambda e, og=og, t1=t1, t2=t2: e.tensor_tensor(out=og[:], in0=t1[:], in1=t2[:], op=ALU.add),
                     reads=[bt1, bt2], writes=[bog])
                P.dma("pool", o[base][ch * 128:(ch + 1) * 128, sl], og[:], reads=[bog])
        elif kind == "plain":
            base = name[:-1]; ch = int(name[-1])
            wb, bwb = load_w(col0, 128)
            scale = 0.125 if base == "nat_q" else 1.0
            for tb in range(NTB):
                a, ba = pA.next()
                mm_fm(a, ba, wb, bwb, 0, 128, tb)
                og, bog = ostg.next()
                sl = slice(tb * 512, (tb + 1) * 512)
                P.op("act", lambda e, og=og, a=a, scale=scale: e.activation(out=og[:], in_=a[:], func=AF.Copy, scale=scale),
                     reads=[ba], writes=[bog])
                P.dma("act", o[base][ch * 128:(ch + 1) * 128, sl], og[:], reads=[bog])
        elif kind == "v":
            H, dv = {"swa_v": (2, 64), "diff_v": (4, 128), "nat_v": (8, 64)}[name]
            vst = Rot(P, f"vst_{name}", [128, H, dv + 1], BF16, 2)
            for vt, vb in zip(vst.tiles, vst.bufs):
                P.op("pool", lambda e, vt=vt: e.memset(vt[:], 1.0), writes=[vb])
            nsub = max(1, ncols // 256)
            sc = ncols // nsub
            hs = H // nsub
            wbs = [load_w(col0 + i * sc, sc) for i in range(nsub)] if nsub <= 2 else None
            assert wbs is not None
            for t in range(NTT):
                vt, vb = vst.next()
                for i in range(nsub):
                    wb, bwb = wbs[i]
                    a, ba = pA.next()
                    for kc in range(8):
                        P.op("pe", lambda e, kc=kc, a=a, t=t, wb=wb: e.matmul(a[:, 0:sc], lhsT=uT[:, kc, t * 128:(t + 1) * 128],
                                                                     rhs=wb[:, kc, 0:sc], start=(kc == 0), stop=(kc == 7)),
                             reads=[bwb, buT[t]], writes=[ba])
                    P.op("act", lambda e, vt=vt, a=a, i=i: e.activation(
                        out=vt[:, i * hs:(i + 1) * hs, 0:dv], in_=a[:, 0:sc].rearrange("p (h d) -> p h d", h=hs), func=AF.Copy),
                        reads=[ba], writes=[vb])
                P.dma("act", o[name][t], vt[:].rearrange("p h d -> p (h d)"), reads=[vb])
        elif kind == "mla":
            emit_mla(P, locals())


def emit_mla(P, L):
    nc = P.nc
    (w1_d, uT, buT, pA, pB, pmisc, o, onesb, bones, epsb, beps, qupA_d, qupB_d, kvn_d, kvv_d, gq_d, gkv_d,
     CM_d, SM_d, col0, nextq, ostg) = (L[k] for k in (
         "w1_d", "uT", "buT", "pA", "pB", "pmisc", "o", "onesb", "bones", "epsb", "beps", "qupA_d", "qupB_d",
         "kvn_d", "kvv_d", "gq_d", "gkv_d", "CM_d", "SM_d", "col0", "nextq", "ostg"))
    L = dict(L)
    NCM = 384 + 256 + 96 + 96
    wst = L["wst"]
    wm = P.sb("mla_w", [128, 8, NCM], BF16); bwm = Buf()
    first = True
    for i in range(4):
        ws_, bws = wst.next()
        ws = ws_[:, 0:8 * 208].rearrange("p (c n) -> p c n", c=8)
        P.dma("sp" if i % 2 == 0 else "act", ws, w1_d[:, col0 + i * 208:col0 + (i + 1) * 208].rearrange("(c p) n -> p c n", p=128), writes=[bws])
        P.op("pool", lambda e, ws=ws, i=i: e.tensor_copy(out=wm[:, :, i * 208:(i + 1) * 208], in_=ws), reads=[bws], writes=[bwm])
    qA = P.sb("mla_qA", [128, 3, 768], BF16); bqA = Buf()
    qB = P.sb("mla_qB", [128, 3, 768], BF16); bqB = Buf()
    kvn = P.sb("mla_kvn", [128, 2, 512], BF16); bkvn = Buf()
    kvv = P.sb("mla_kvv", [128, 2, 512], BF16); bkvv = Buf()
    for (src, dst, bdst, nch, ncol) in ((qupA_d, qA, bqA, 3, 768), (qupB_d, qB, bqB, 3, 768)):
        for hh in range(2):
            ws_, bws = wst.next()
            ws = ws_[:, 0:nch * 384].rearrange("p (c n) -> p c n", c=nch)
            P.dma("sp", ws, src[:, hh * 384:(hh + 1) * 384].rearrange("(c p) n -> p c n", p=128), writes=[bws])
            P.op("pool", lambda e, ws=ws, dst=dst, hh=hh: e.tensor_copy(out=dst[:, :, hh * 384:(hh + 1) * 384], in_=ws), reads=[bws], writes=[bdst])
    for (src, dst, bdst) in ((kvn_d, kvn, bkvn), (kvv_d, kvv, bkvv)):
        ws_, bws = wst.next()
        ws = ws_[:, 0:1024].rearrange("p (c n) -> p c n", c=2)
        P.dma("act", ws, src.rearrange("(c p) n -> p c n", p=128), writes=[bws])
        P.op("pool", lambda e, ws=ws, dst=dst: e.tensor_copy(out=dst[:], in_=ws), reads=[bws], writes=[bdst])
    gq = P.sb("mla_gq", [128, 3], F32); gkv = P.sb("mla_gkv", [128, 2], F32); bg = Buf()
    P.dma("pool", gq[:], gq_d, writes=[bg])
    P.dma("pool", gkv[:], gkv_d, writes=[bg], append=True)
    epsq = P.sb("mla_epsq", [128, 1], F32)
    cg = Rot(P, "mla_cg", [128, 5, 512], BF16, 1)
    sq = Rot(P, "mla_sq", [128, 5, 512], BF16, 1)
    rq = Rot(P, "mla_rq", [128, 512], F32, 1)
    rkv = Rot(P, "mla_rkv", [128, 512], F32, 1)
    rkvt = Rot(P, "mla_rkvt", [128, 4], F32, 2)
    cmt = Rot(P, "mla_cm", [128, 512], F32, 1); smt = Rot(P, "mla_sm", [128, 512], F32, 1)
    cr = Rot(P, "mla_cr", [128, 512], F32, 1); sr = Rot(P, "mla_sr", [128, 512], F32, 1)
    tA = Rot(P, "mla_tA", [128, 512], F32, 1); tB = Rot(P, "mla_tB", [128, 512], F32, 1)
    krs = Rot(P, "mla_krs", [128, 512], BF16, 2)
    vst = Rot(P, "mla_vst", [128, 8, 65], BF16, 2)
    for vt, vb in zip(vst.tiles, vst.bufs):
        P.op("pool", lambda e, vt=vt: e.memset(vt[:], 1.0), writes=[vb])

    import os
    MS = int(os.environ.get("MLASTOP", "99"))
    for tb in range(NTB):
        sl = slice(tb * 512, (tb + 1) * 512)
        ubufs = buT[tb * 4:(tb + 1) * 4]
        cg_, bcg = cg.next(); sq_, bsq = sq.next()
        if MS <= 0: continue
        for j in range(5):
            a, ba = pA.next()
            for kc in range(8):
                P.op("pe", lambda e, kc=kc, a=a, j=j: e.matmul(a[:], lhsT=wm[:, kc, j * 128:(j + 1) * 128], rhs=uT[:, kc, sl],
                                                            start=(kc == 0), stop=(kc == 7)), reads=[bwm] + ubufs, writes=[ba])
            gcol = gq[:, j:j + 1] if j < 3 else gkv[:, j - 3:j - 2]
            VAR = os.environ.get("MLAVAR", "AB")
            if "A" in VAR:
                P.op("act", lambda e, a=a, j=j, sq_=sq_: e.activation(out=sq_[:, j, :], in_=a[:], func=AF.Square), reads=[ba], writes=[bsq])
            if "B" in VAR:
              P.op("dve", lambda e, a=a, j=j, cg_=cg_, gcol=gcol: e.tensor_scalar(out=cg_[:, j, :], in0=a[:], scalar1=gcol, scalar2=None, op0=ALU.mult),
                 reads=[ba, bg], writes=[bcg])
        if MS <= 1: continue
        rq_, brq = rq.next(); rkv_, brkv = rkv.next(); rkt, brkt = rkvt.next()
        for (r_, br_, js, dim) in ((rq_, brq, (0, 1, 2), 384.0), (rkv_, brkv, (3, 4), 256.0)):
            pm, bpm = pmisc.next()
            for i, j in enumerate(js):
                P.op("pe", lambda e, pm=pm, j=j, i=i, n=len(js): e.matmul(pm[:], lhsT=onesb[:], rhs=sq_[:, j, :], start=(i == 0), stop=(i == n - 1)),
                     reads=[bones, bsq], writes=[bpm])
            P.op("act", lambda e, r_=r_, pm=pm, dim=dim: e.activation(out=r_[:], in_=pm[:], func=AF.Ln, bias=epsb[:, 0:1], scale=1.0 / dim),
                 reads=[bpm, beps], writes=[br_])
            P.op("act", lambda e, r_=r_: e.activation(out=r_[:], in_=r_[:], func=AF.Exp, scale=-0.5), reads=[br_], writes=[br_])
        if MS <= 2: continue
        pm, bpm = pmisc.next()
        for tt in range(4):
            for i, j in enumerate((3, 4)):
                P.op("pe", lambda e, pm=pm, tt=tt, j=j, i=i: e.matmul(pm[:, tt:tt + 1], lhsT=sq_[:, j, tt * 128:(tt + 1) * 128], rhs=onesb[:, 0:1],
                                                                   start=(i == 0), stop=(i == 1)), reads=[bones, bsq], writes=[bpm])
        P.op("act", lambda e, rkt=rkt, pm=pm: e.activation(out=rkt[:], in_=pm[:, 0:4], func=AF.Ln, bias=epsb[:, 0:1], scale=1.0 / 256.0),
             reads=[bpm, beps], writes=[brkt])
        P.op("act", lambda e, rkt=rkt: e.activation(out=rkt[:], in_=rkt[:], func=AF.Exp, scale=-0.5), reads=[brkt], writes=[brkt])
        if MS <= 3: continue
        cm_, bcm = cmt.next(); sm_, bsm = smt.next()
        P.dma("sp", cm_[:], CM_d[:, sl], writes=[bcm])
        P.dma("sp", sm_[:], SM_d[:, sl], writes=[bsm])
        cr_, bcr = cr.next(); sr_, bsr = sr.next()
        P.op("pool", lambda e, cr_=cr_, cm_=cm_, rq_=rq_: e.tensor_tensor(out=cr_[64:96, :], in0=cm_[64:96, :], in1=rq_[64:96, :], op=ALU.mult),
             reads=[bcm, brq], writes=[bcr])
        P.op("pool", lambda e, sr_=sr_, sm_=sm_, rq_=rq_: e.tensor_tensor(out=sr_[64:96, :], in0=sm_[64:96, :], in1=rq_[64:96, :], op=ALU.mult),
             reads=[bsm, brq], writes=[bsr])
        if MS <= 4: continue
        for h in range(8):
            a, ba = pA.next(); b, bb = pB.next()
            for j in range(3):
                P.op("pe", lambda e, a=a, j=j, h=h: e.matmul(a[0:96, :], lhsT=qA[:, j, h * 96:(h + 1) * 96], rhs=cg_[:, j, :], start=(j == 0), stop=(j == 2)),
                     reads=[bqA, bcg], writes=[ba])
            for j in range(3):
                P.op("pe", lambda e, b=b, j=j, h=h: e.matmul(b[0:96, :], lhsT=qB[:, j, h * 96:(h + 1) * 96], rhs=cg_[:, j, :], start=(j == 0), stop=(j == 2)),
                     reads=[bqB, bcg], writes=[bb])
            og, bog = ostg.next()
            tA_, btA = tA.next(); tB_, btB = tB.next()
            P.op("dve", lambda e, og=og, a=a, rq_=rq_: e.tensor_tensor(out=og[0:64, :], in0=a[0:64, :], in1=rq_[0:64, :], op=ALU.mult),
                 reads=[ba, brq], writes=[bog])
            P.op("dve", lambda e, tA_=tA_, a=a, cr_=cr_: e.tensor_tensor(out=tA_[64:96, :], in0=a[64:96, :], in1=cr_[64:96, :], op=ALU.mult),
                 reads=[ba, bcr], writes=[btA])
            P.op("dve", lambda e, tB_=tB_, b=b, sr_=sr_: e.tensor_tensor(out=tB_[64:96, :], in0=b[64:96, :], in1=sr_[64:96, :], op=ALU.mult),
                 reads=[bb, bsr], writes=[btB])
            P.op("pool", lambda e, og=og, tA_=tA_, tB_=tB_: e.tensor_tensor(out=og[64:96, :], in0=tA_[64:96, :], in1=tB_[64:96, :], op=ALU.add),
                 reads=[btA, btB, bog], writes=[bog])
            P.dma("pool", o["mla_q"][h, :, sl], og[0:96, :], reads=[bog])
        if MS <= 5: continue
        a, ba = pA.next(); b, bb = pB.next()
        for kc in range(8):
            P.op("pe", lambda e, kc=kc, a=a: e.matmul(a[0:96, :], lhsT=wm[:, kc, 640:736], rhs=uT[:, kc, sl], start=(kc == 0), stop=(kc == 7)),
                 reads=[bwm] + ubufs, writes=[ba])
        for kc in range(8):
            P.op("pe", lambda e, kc=kc, b=b: e.matmul(b[0:96, :], lhsT=wm[:, kc, 736:832], rhs=uT[:, kc, sl], start=(kc == 0), stop=(kc == 7)),
                 reads=[bwm] + ubufs, writes=[bb])
        tA_, btA = tA.next(); tB_, btB = tB.next()
        P.op("dve", lambda e, tA_=tA_, a=a, cm_=cm_: e.tensor_tensor(out=tA_[64:96, :], in0=a[64:96, :], in1=cm_[64:96, :], op=ALU.mult),
             reads=[ba, bcm], writes=[btA])
        P.op("dve", lambda e, tB_=tB_, b=b, sm_=sm_: e.tensor_tensor(out=tB_[64:96, :], in0=b[64:96, :], in1=sm_[64:96, :], op=ALU.mult),
             reads=[bb, bsm], writes=[btB])
        kr_, bkr = krs.next()
        P.op("pool", lambda e, kr_=kr_, tA_=tA_, tB_=tB_: e.tensor_tensor(out=kr_[64:96, :], in0=tA_[64:96, :], in1=tB_[64:96, :], op=ALU.add),
             reads=[btA, btB], writes=[bkr])
        for h in range(8):
            P.dma("pool", o["mla_k"][h, 64:96, sl], kr_[64:96, :], reads=[bkr])
        if MS <= 6: continue
        for h in range(8):
            a, ba = pA.next()
            for j in range(2):
                P.op("pe", lambda e, a=a, j=j, h=h: e.matmul(a[0:64, :], lhsT=kvn[:, j, h * 64:(h + 1) * 64], rhs=cg_[:, 3 + j, :], start=(j == 0), stop=(j == 1)),
                     reads=[bkvn, bcg], writes=[ba])
            og, bog = ostg.next()
            P.op("dve", lambda e, og=og, a=a, rkv_=rkv_: e.tensor_tensor(out=og[0:64, :], in0=a[0:64, :], in1=rkv_[0:64, :], op=ALU.mult),
                 reads=[ba, brkv], writes=[bog])
            P.dma(nextq(), o["mla_k"][h, 0:64, sl], og[0:64, :], reads=[bog])
        if MS <= 7: continue
        for tt in range(4):
            a, ba = pA.next()
            for j in range(2):
                P.op("pe", lambda e, a=a, j=j, tt=tt: e.matmul(a[:], lhsT=cg_[:, 3 + j, tt * 128:(tt + 1) * 128], rhs=kvv[:, j, :], start=(j == 0), stop=(j == 1)),
                     reads=[bkvv, bcg], writes=[ba])
            vt, vb = vst.next()
            P.op("act", lambda e, vt=vt, a=a, rkt=rkt, tt=tt: e.activation(
                out=vt[:, :, 0:64], in_=a[:].rearrange("p (h d) -> p h d", h=8), func=AF.Copy, scale=rkt[:, tt:tt + 1]),
                reads=[ba, brkt], writes=[vb])
            P.dma("act", o["mla_v"][tb * 4 + tt], vt[:].rearrange("p h d -> p (h d)"), reads=[vb])


def to_pc(v, ncol):
    return np.ascontiguousarray(np.asarray(v).reshape(ncol, 128).T)


def k1_host_inputs(inp, l, core, xcur):
    b, half = core // 2, core % 2
    units, w1cols = k1_weight_layout()
    qa, qb, knope, vcols = mla_up_layout()
    C64, S64, CM, SM = rope_tables(half * T, T)
    m = {
        "x": np.ascontiguousarray(xcur[b, half * T:(half + 1) * T]),
        "cT": to_pc(inp["c"][b], 8),
        "adaw": np.ascontiguousarray(inp["ada_w"][l][:, 0:2048]),
        "adab": to_pc(inp["ada_b"][l][0:2048], 16),
        "w1": np.ascontiguousarray(inp["w_in"][l][:, w1cols]),
        "qupA": np.ascontiguousarray(inp["mla_q_up"][l][:, qa]),
        "qupB": np.ascontiguousarray(inp["mla_q_up"][l][:, qb]),
        "kvn": np.ascontiguousarray(inp["mla_kv_up"][l][:, knope]),
        "kvv": np.ascontiguousarray(inp["mla_kv_up"][l][:, vcols]),
        "gq": to_pc(inp["mla_q_norm"][l], 3),
        "gkv": to_pc(inp["mla_kv_norm"][l], 2),
        "C64": C64, "S64": S64, "CM": CM, "SM": SM,
        "ident": np.eye(128, dtype=np.float32),
    }
    return m


NEGM = -30000.0


class AttnCtx:
    pass


def attn_block(P, C, rhs_q, bq, N, ktiles, scale, acc, bacc, nsub, v_of, pt_cols=None):
    n = len(ktiles)
    LA = 3
    pend = {}
    for i in range(n + LA):
        if i < n:
            kT_ap, kb, vkey, vb, bias_ap, bb = ktiles[i]
            st, bst = C.ST.next()
            P.op("pe", lambda e, st=st, kT_ap=kT_ap, bias_ap=bias_ap: e.matmul(
                st[:, 0:N], lhsT=kT_ap, rhs=rhs_q, start=True, stop=(bias_ap is None)),
                reads=list(kb) + list(bq), writes=[bst])
            if bias_ap is not None:
                P.op("pe", lambda e, st=st, bias_ap=bias_ap: e.matmul(
                    st[:, 0:N], lhsT=C.ident[:], rhs=bias_ap, start=False, stop=True),
                    reads=list(bb) + [C.bid], writes=[bst])
            pt, bpt = C.PT.next()
            P.op("act", lambda e, st=st, pt=pt: e.activation(out=pt[:, 0:N], in_=st[:, 0:N], func=AF.Exp, scale=scale),
                 reads=[bst], writes=[bpt])
            pend[i] = (pt, bpt, vkey, vb)
        if i >= LA:
            k = i - LA
            pt, bpt, vkey, vb = pend.pop(k)
            for j in range(nsub):
                P.op("pe", lambda e, pt=pt, j=j, vkey=vkey, k=k: e.matmul(
                    acc(j), lhsT=pt[:, j * 128:(j + 1) * 128], rhs=v_of(vkey, j), start=(k == 0), stop=(k == n - 1)),
                    reads=[bpt] + list(vb), writes=[bacc])


def emit_oT(P, C, o_tok, bo, oT_d):
    for tb in range(NTB):
        stg, bs = C.oTs.next()
        for tt in range(4):
            t = tb * 4 + tt
            st_, bp = C.ST.next()
            ptr = st_[:].bitcast(BF16)
            for c in range(4):
                P.op("pe", lambda e, c=c, t=t, ptr=ptr: e.transpose(out=ptr[:, c * 128:(c + 1) * 128], in_=o_tok[:, t, c * 128:(c + 1) * 128], identity=C.ident[:]),
                     reads=[bo, C.bid], writes=[bp])
            P.op("dve", lambda e, ptr=ptr, stg=stg, tt=tt: e.tensor_copy(out=stg[:, :, tt * 128:(tt + 1) * 128], in_=ptr[:, 0:512].rearrange("p (c n) -> p c n", c=4)),
                 reads=[bp], writes=[bs])
        P.dma("sp" if tb % 2 == 0 else "act", oT_d.rearrange("(c p) t -> p c t", p=128)[:, :, tb * 512:(tb + 1) * 512], stg[:], reads=[bs])


def build_k2a(nc, which=("swa", "mla", "diff", "nat")):
    P = Prog(nc)
    din = lambda n, s, d=BF16: nc.dram_tensor(n, list(s), d, kind="ExternalInput").ap()
    dout = lambda n, s, d=BF16: nc.dram_tensor(n, list(s), d, kind="ExternalOutput").ap()
    D = {}
    D["swa_qT"] = din("swa_qT", [512, T]); D["swa_kT"] = din("swa_kT", [128, 34 * 128]); D["swa_v"] = din("swa_v", [34, 128, 130])
    D["swa_masks"] = din("swa_masks", [4, 128, 512], F32); D["sinkb"] = din("sinkb", [128, 8], F32)
    D["mla_qT"] = din("mla_qT", [8, 96, T]); D["mla_kT"] = din("mla_kT", [8, 96, 2 * T]); D["mla_v"] = din("mla_v", [64, 128, 520])
    D["diff_qT"] = din("diff_qT", [512, T]); D["diff_kT"] = din("diff_kT", [512, 2 * T]); D["diff_v"] = din("diff_v", [64, 128, 516])
    D["lamb"] = din("lamb", [128, 4, 64], F32); D["subln"] = din("subln", [128, 128], F32); D["lami"] = din("lami", [128, 2], F32)
    D["nat_qT"] = din("nat_qT", [512, T]); D["nat_kT"] = din("nat_kT", [512, 36 * 128]); D["nat_v"] = din("nat_v", [36, 128, 520])
    D["nat_bias"] = din("nat_bias", [8, 128, 29 * 128], F32)
    D["ident"] = din("ident", [128, 128], F32)
    oT_d = dout("oT", [4, 512, T])
    emit_attention(P, D, oT_d, which)
    P.finish()
    P.emit()
    return P


class HostSrc:
    def __init__(self, D):
        self.D = D

    def swa_k(self, kT, kv):
        return [(kT[:, kv, :], self.D["swa_kT"][kv * 64:(kv + 1) * 64, :])]

    def swa_v(self, V):
        return [(V[:], self.D["swa_v"].rearrange("t p f -> p t f"))]

    def mla_k(self, kT, h):
        return [(kT[:, 0:T], self.D["mla_kT"][h, :, 0:T]), (kT[:, T:2 * T], self.D["mla_kT"][h, :, T:2 * T])]

    def mla_v(self, V, h):
        return [(V[:], self.D["mla_v"][:, :, h * 65:(h + 1) * 65].rearrange("t p f -> p t f"))]

    def diff_k(self, kT, row):
        return [(kT[:, 0:T], self.D["diff_kT"][row:row + 64, 0:T]), (kT[:, T:2 * T], self.D["diff_kT"][row:row + 64, T:2 * T])]

    def diff_v(self, V, h):
        return [(V[:], self.D["diff_v"][:, :, h * 129:(h + 1) * 129].rearrange("t p f -> p t f"))]

    def nat_k(self, kT, h):
        return [(kT[:], self.D["nat_kT"][h * 64:(h + 1) * 64, :])]

    def nat_v(self, V, h):
        return [(V[:], self.D["nat_v"][:, :, h * 65:(h + 1) * 65].rearrange("t p f -> p t f"))]


class GatherSrc:
    def __init__(self, O, G):
        self.O = O; self.G = G

    def swa_k(self, kT, kv):
        g = self.G["swa_kT"][0]
        r = slice(kv * 64, (kv + 1) * 64); r1 = slice(128 + kv * 64, 128 + (kv + 1) * 64)
        return [(kT[:, kv, 0:128], g[r, T - 128:T]), (kT[:, kv, 128:128 + T], self.O["swa_kT"][r, :]),
                (kT[:, kv, 128 + T:256 + T], g[r1, 0:128])]

    def swa_v(self, V):
        g = self.G["swa_v"][0]
        return [(V[:, 0, :], g[T - 128:T, :]), (V[:, 1:33, :], self.O["swa_v"].rearrange("t p f -> p t f")),
                (V[:, 33, :], g[T:T + 128, :])]

    def mla_k(self, kT, h):
        g = self.G["mla_kT"][h // 2]
        r0 = (h % 2) * 96
        return [(kT[:, 0:T], g[r0:r0 + 96, :]), (kT[:, T:2 * T], g[192 + r0:192 + r0 + 96, :])]

    def mla_v(self, V, h):
        out = []
        for k in range(4):
            g = self.G["mla_v"][k]
            for r in range(2):
                out.append((V[:, r * 32 + k * 8:r * 32 + k * 8 + 8, :],
                            g[r * 1024:(r + 1) * 1024, h * 65:(h + 1) * 65].rearrange("(t p) f -> p t f", p=128)))
        return out

    def diff_k(self, kT, row):
        g = self.G["diff_kT"][row // 128]
        r0 = row % 128
        return [(kT[:, 0:T], g[r0:r0 + 64, :]), (kT[:, T:2 * T], g[128 + r0:128 + r0 + 64, :])]

    def diff_v(self, V, h):
        out = []
        for k in range(4):
            g = self.G["diff_v"][k]
            for r in range(2):
                out.append((V[:, r * 32 + k * 8:r * 32 + k * 8 + 8, :],
                            g[r * 1024:(r + 1) * 1024, h * 129:(h + 1) * 129].rearrange("(t p) f -> p t f", p=128)))
        return out

    def nat_k(self, kT, h):
        g = self.G["nat_kT"][h // 2]
        r0 = (h % 2) * 64
        return [(kT[:, 0:256], g[r0:r0 + 64, T - 256:T]), (kT[:, 256:256 + T], self.O["nat_kT"][h * 64:(h + 1) * 64, :]),
                (kT[:, 256 + T:512 + T], g[128 + r0:128 + r0 + 64, 0:256])]

    def nat_v(self, V, h):
        gl = self.G["nat_v"][1]
        gf = self.G["nat_v"][0]
        c = slice(h * 65, (h + 1) * 65)
        return [(V[:, 0:2, :], gl[768:1024, c].rearrange("(t p) f -> p t f", p=128)),
                (V[:, 2:34, :], self.O["nat_v"][:, :, c].rearrange("t p f -> p t f")),
                (V[:, 34:36, :], gf[1024:1280, c].rearrange("(t p) f -> p t f", p=128))]


def emit_attention(P, D, oT_d, which=("swa", "mla", "diff", "nat"), SRC=None, bg=None):
    if SRC is None:
        SRC = HostSrc(D)

    def bg_step():
        if bg is not None:
            next(bg, None)
    C = AttnCtx()
    identf = P.sb("a_identf", [128, 128], F32); bidf = Buf()
    C.ident = P.sb("a_ident", [128, 128], BF16); C.bid = Buf()
    P.dma("pool", identf[:], D["ident"], writes=[bidf])
    P.op("dve", lambda e: e.tensor_copy(out=C.ident[:], in_=identf[:]), reads=[bidf], writes=[C.bid])
    epsb = P.sb("a_eps", [128, 1], F32); beps = Buf()
    P.op("pool", lambda e: e.memset(epsb[:], 1e-5), writes=[beps])
    C.ST = Rot(P, "a_ST", [128, 512], F32, 4, "ps")
    C.PT = Rot(P, "a_PT", [128, 512], BF16, 4)
    C.acc = Rot(P, "a_acc", [128, 4, 512], F32, 1, "ps")
    C.oTs = Rot(P, "a_oTs", [128, 4, 512], BF16, 2)
    o_tok = P.sb("a_otok", [128, NTT, 512], BF16); bo = Buf("otok")
    rz = Rot(P, "a_rz", [128, 4, 1], F32, 4)

    if "swa" in which:
        P.push()
        qT = P.sb("swa_q", [64, 8, T], BF16); bq = Buf()
        for h in range(8):
            P.dma(("sp", "act", "pool")[h % 3], qT[:, h, :], D["swa_qT"][h * 64:(h + 1) * 64, :], writes=[bq], append=(h > 0))
        kT = P.sb("swa_k", [64, 2, 34 * 128], BF16); bk = Buf()
        n_ = 0
        for kv in range(2):
            for (d_, s_) in SRC.swa_k(kT, kv):
                P.dma("sp", d_, s_, writes=[bk], append=(n_ > 0)); n_ += 1
        V = P.sb("swa_vv", [128, 34, 130], BF16); bv = Buf()
        for n_, (d_, s_) in enumerate(SRC.swa_v(V)):
            P.dma("sp", d_, s_, writes=[bv], append=(n_ > 0))
        mf = P.sb("swa_mf", [128, 4, 512], F32); bmf = Buf()
        mk = P.sb("swa_mk", [128, 4, 512], BF16); bmk = Buf()
        P.dma("pool", mf[:], D["swa_masks"].rearrange("m p n -> p m n"), writes=[bmf])
        P.op("dve", lambda e: e.tensor_copy(out=mk[:], in_=mf[:]), reads=[bmf], writes=[bmk])
        es = P.sb("swa_es", [128, 8], F32); bes = Buf()
        P.dma("pool", es[:], D["sinkb"], writes=[bes])
        P.op("act", lambda e: e.activation(out=es[:], in_=es[:], func=AF.Exp), reads=[bes], writes=[bes])
        for qt in range(NTT):
            for kv in range(2):
                acc, bacc = C.acc.next()
                rhs_q = qT[:, kv * 4:(kv + 1) * 4, qt * 128:(qt + 1) * 128]
                kts = []
                for d_ in range(3):
                    ki = qt + d_
                    if d_ == 1:
                        bias = None
                    elif d_ == 0:
                        bias = mk[:, 2, :] if qt == 0 else mk[:, 0, :]
                    else:
                        bias = mk[:, 3, :] if qt == NTT - 1 else mk[:, 1, :]
                    kts.append((kT[:, kv, ki * 128:(ki + 1) * 128], [bk], ki, [bv], bias, [bmk]))
                attn_block(P, C, rhs_q, [bq], 512, kts, 0.125, lambda j, acc=acc: acc[:, j, 0:65], bacc, 4,
                           lambda ki, j, kv=kv: V[:, ki, kv * 65:(kv + 1) * 65])
                r_, br = rz.next()
                P.op("dve", lambda e, r_=r_, acc=acc, kv=kv: e.tensor_tensor(
                    out=r_[:], in0=acc[:, :, 64:65], in1=es[:, kv * 4:(kv + 1) * 4].rearrange("p (g o) -> p g o", o=1), op=ALU.add),
                    reads=[bacc, bes], writes=[br])
                P.op("dve", lambda e, r_=r_: e.reciprocal(out=r_[:], in_=r_[:]), reads=[br], writes=[br])
                P.op("dve", lambda e, r_=r_, acc=acc, kv=kv, qt=qt: e.tensor_tensor(
                    out=o_tok[:, qt, kv * 256:(kv + 1) * 256].rearrange("p (g d) -> p g d", g=4),
                    in0=acc[:, :, 0:64], in1=r_[:].to_broadcast([128, 4, 64]), op=ALU.mult),
                    reads=[bacc, br], writes=[bo])
        emit_oT(P, C, o_tok, bo, oT_d[0])
        P.pop()

    if "mla" in which:
        P.push()
        qTr = Rot(P, "mla_q", [96, T], BF16, 2)
        kTr = Rot(P, "mla_k", [96, 2 * T], BF16, 2)
        Vr = Rot(P, "mla_vv", [128, 64, 65], BF16, 2)
        sc = 96.0 ** -0.5
        for h in range(8):
            qT, bq = qTr.next(); kT, bk = kTr.next(); V, bv = Vr.next()
            P.dma("sp", qT[:], D["mla_qT"][h], writes=[bq])
            for n_, (d_, s_) in enumerate(SRC.mla_k(kT, h)):
                P.dma("sp", d_, s_, writes=[bk], append=(n_ > 0))
            for n_, (d_, s_) in enumerate(SRC.mla_v(V, h)):
                P.dma("sp", d_, s_, writes=[bv], append=(n_ > 0))
            for qb in range(NTB):
                if qb % 2 == 0:
                    bg_step()
                acc, bacc = C.acc.next()
                kts = [(kT[:, ki * 128:(ki + 1) * 128], [bk], ki, [bv], None, []) for ki in range(64)]
                attn_block(P, C, qT[:, qb * 512:(qb + 1) * 512], [bq], 512, kts, sc, lambda j, acc=acc: acc[:, j, 0:65], bacc, 4,
                           lambda ki, j, V=V: V[:, ki, :])
                r_, br = rz.next()
                P.op("dve", lambda e, r_=r_, acc=acc: e.reciprocal(out=r_[:], in_=acc[:, :, 64:65]), reads=[bacc], writes=[br])
                P.op("dve", lambda e, r_=r_, acc=acc, qb=qb, h=h: e.tensor_tensor(
                    out=o_tok[:, qb * 4:(qb + 1) * 4, h * 64:(h + 1) * 64],
                    in0=acc[:, :, 0:64], in1=r_[:].to_broadcast([128, 4, 64]), op=ALU.mult),
                    reads=[bacc, br], writes=[bo])
        emit_oT(P, C, o_tok, bo, oT_d[1])
        P.pop()

    if "diff" in which:
        P.push()
        qTr = Rot(P, "df_q", [64, T], BF16, 2)
        kTr = Rot(P, "df_k", [64, 2 * T], BF16, 2)
        Vr = Rot(P, "df_vv", [128, 64, 129], BF16, 1)
        o1 = P.sb("df_o1", [128, NTT, 128], F32); bo1 = Buf()
        lamb = P.sb("df_lamb", [128, 4, 64], F32); blamb = Buf()
        lami = P.sb("df_lami", [128, 2], F32)
        sg = P.sb("df_sg", [128, 128], F32); bsg = Buf()
        P.dma("pool", lamb[:], D["lamb"], writes=[blamb])
        P.dma("pool", lami[:], D["lami"], writes=[blamb], append=True)
        P.dma("pool", sg[:], D["subln"], writes=[bsg])
        P.op("dve", lambda e: e.tensor_scalar(out=sg[:], in0=sg[:], scalar1=lami[:, 1:2], scalar2=None, op0=ALU.mult), reads=[bsg, blamb], writes=[bsg])
        lp = P.sb("df_lp", [128, 2, 64], F32); blp = Buf()
        ls = P.sb("df_ls", [128, 2], F32); bls = Buf()
        nlam = P.sb("df_nlam", [128, 1], F32); bnl = Buf()
        P.op("dve", lambda e: e.tensor_tensor(out=lp[:, 0, :], in0=lamb[:, 0, :], in1=lamb[:, 1, :], op=ALU.mult), reads=[blamb], writes=[blp])
        P.op("dve", lambda e: e.tensor_tensor(out=lp[:, 1, :], in0=lamb[:, 2, :], in1=lamb[:, 3, :], op=ALU.mult), reads=[blamb, blp], writes=[blp])
        P.op("dve", lambda e: e.reduce_sum(out=ls[:], in_=lp[:], axis=AX.X), reads=[blp], writes=[bls])
        P.op("act", lambda e: e.activation(out=ls[:], in_=ls[:], func=AF.Exp), reads=[bls], writes=[bls])
        P.op("dve", lambda e: e.tensor_tensor(out=nlam[:], in0=ls[:, 1:2], in1=ls[:, 0:1], op=ALU.subtract), reads=[bls], writes=[bnl])
        P.op("dve", lambda e: e.tensor_tensor(out=nlam[:], in0=nlam[:], in1=lami[:, 0:1], op=ALU.subtract), reads=[bnl, blamb], writes=[bnl])
        ot = Rot(P, "df_ot", [128, 4, 128], F32, 2)
        sq = Rot(P, "df_sq", [128, 4, 128], F32, 1)
        ss = Rot(P, "df_ss", [128, 4], F32, 2)
        for h in range(4):
            V, bv = Vr.next()
            for n_, (d_, s_) in enumerate(SRC.diff_v(V, h)):
                P.dma("sp", d_, s_, writes=[bv], append=(n_ > 0))
            for m in range(2):
                row = (h * 2 + m) * 64
                qT, bq = qTr.next(); kT, bk = kTr.next()
                P.dma("sp", qT[:], D["diff_qT"][row:row + 64, :], writes=[bq])
                for n_, (d_, s_) in enumerate(SRC.diff_k(kT, row)):
                    P.dma("sp", d_, s_, writes=[bk], append=(n_ > 0))
                for qb in range(NTB):
                    acc, bacc = C.acc.next()
                    kts = [(kT[:, ki * 128:(ki + 1) * 128], [bk], ki, [bv], None, []) for ki in range(64)]
                    attn_block(P, C, qT[:, qb * 512:(qb + 1) * 512], [bq], 512, kts, 0.125, lambda j, acc=acc: acc[:, j, 0:129], bacc, 4,
                               lambda ki, j, V=V: V[:, ki, :])
                    r_, br = rz.next()
                    P.op("dve", lambda e, r_=r_, acc=acc: e.reciprocal(out=r_[:], in_=acc[:, :, 128:129]), reads=[bacc], writes=[br])
                    tl = slice(qb * 4, (qb + 1) * 4)
                    if m == 0:
                        P.op("dve", lambda e, r_=r_, acc=acc, tl=tl: e.tensor_tensor(
                            out=o1[:, tl, :], in0=acc[:, :, 0:128], in1=r_[:].to_broadcast([128, 4, 128]), op=ALU.mult),
                            reads=[bacc, br], writes=[bo1])
                    else:
                        o_, bo_ = ot.next()
                        P.op("dve", lambda e, r_=r_, acc=acc, o_=o_: e.tensor_tensor(
                            out=o_[:], in0=acc[:, :, 0:128], in1=r_[:].to_broadcast([128, 4, 128]), op=ALU.mult),
                            reads=[bacc, br], writes=[bo_])
                        P.op("dve", lambda e, o_=o_, tl=tl: e.scalar_tensor_tensor(
                            out=o_[:], in0=o_[:], scalar=nlam[:, 0:1], in1=o1[:, tl, :], op0=ALU.mult, op1=ALU.add),
                            reads=[bo_, bnl, bo1], writes=[bo_])
                        sq_, bsq = sq.next(); ss_, bss = ss.next()
                        P.op("pool", lambda e, o_=o_, sq_=sq_: e.tensor_tensor(out=sq_[:], in0=o_[:], in1=o_[:], op=ALU.mult), reads=[bo_], writes=[bsq])
                        P.op("dve", lambda e, sq_=sq_, ss_=ss_: e.reduce_sum(out=ss_[:], in_=sq_[:], axis=AX.X), reads=[bsq], writes=[bss])
                        P.op("act", lambda e, ss_=ss_: e.activation(out=ss_[:], in_=ss_[:], func=AF.Ln, bias=epsb[:, 0:1], scale=1.0 / 128.0), reads=[bss, beps], writes=[bss])
                        P.op("act", lambda e, ss_=ss_: e.activation(out=ss_[:], in_=ss_[:], func=AF.Exp, scale=-0.5), reads=[bss], writes=[bss])
                        P.op("pool", lambda e, o_=o_, ss_=ss_: e.tensor_tensor(
                            out=o_[:], in0=o_[:], in1=ss_[:].rearrange("p (g o) -> p g o", o=1).to_broadcast([128, 4, 128]), op=ALU.mult),
                            reads=[bo_, bss], writes=[bo_])
                        P.op("pool", lambda e, o_=o_, tl=tl, h=h: e.tensor_tensor(
                            out=o_tok[:, tl, h * 128:(h + 1) * 128], in0=o_[:],
                            in1=sg[:].rearrange("p (o d) -> p o d", o=1).to_broadcast([128, 4, 128]), op=ALU.mult),
                            reads=[bo_, bsg], writes=[bo])
        emit_oT(P, C, o_tok, bo, oT_d[2])
        P.pop()

    if "nat" in which:
        P.push()
        qTr = Rot(P, "nat_q", [64, T], BF16, 2)
        kTr = Rot(P, "nat_k", [64, 36 * 128], BF16, 2)
        Vr = Rot(P, "nat_vv", [128, 36, 65], BF16, 2)
        bfr = Rot(P, "nat_bf", [128, 29 * 128], F32, 1)
        bbr = Rot(P, "nat_bb", [128, 29, 128], BF16, 2)
        nat_accb = [Buf(f"nat_acc{j}", excl=True) for j in range(4)]
        for h in range(8):
            qT, bq = qTr.next(); kT, bk = kTr.next(); V, bv = Vr.next()
            bf_, bbf = bfr.next(); bb_, bbb = bbr.next()
            P.dma("sp", qT[:], D["nat_qT"][h * 64:(h + 1) * 64, :], writes=[bq])
            for n_, (d_, s_) in enumerate(SRC.nat_k(kT, h)):
                P.dma("sp", d_, s_, writes=[bk], append=(n_ > 0))
            for n_, (d_, s_) in enumerate(SRC.nat_v(V, h)):
                P.dma("sp", d_, s_, writes=[bv], append=(n_ > 0))
            P.dma("sp", bf_[:], D["nat_bias"][h], writes=[bbf])
            P.op("dve", lambda e, bb_=bb_, bf_=bf_: e.tensor_copy(out=bb_[:].rearrange("p a b -> p (a b)"), in_=bf_[:]), reads=[bbf], writes=[bbb])
            for qt in range(NTT):
                if qt == 0:
                    kis = list(range(0, 6)); tbl = list(range(5, 11))
                elif qt == 1:
                    kis = list(range(1, 7)); tbl = list(range(11, 17))
                elif qt == NTT - 2:
                    kis = list(range(29, 35)); tbl = list(range(17, 23))
                elif qt == NTT - 1:
                    kis = list(range(30, 36)); tbl = list(range(23, 29))
                else:
                    kis = list(range(qt, qt + 5)); tbl = list(range(0, 5))
                acc = C.acc.tiles[0]
                jb = qt % 4
                bacc = nat_accb[jb]
                kts = [(kT[:, ki * 128:(ki + 1) * 128], [bk], ki, [bv], bb_[:, ti, :], [bbb]) for ki, ti in zip(kis, tbl)]
                attn_block(P, C, qT[:, qt * 128:(qt + 1) * 128], [bq], 128, kts, 1.0, lambda j, acc=acc, jb=jb: acc[:, jb, 0:65], bacc, 1,
                           lambda ki, j, V=V: V[:, ki, :])
                r_, br = rz.next()
                P.op("dve", lambda e, r_=r_, acc=acc, jb=jb: e.reciprocal(out=r_[:, 0, :], in_=acc[:, jb, 64:65]), reads=[bacc], writes=[br])
                P.op("dve", lambda e, r_=r_, acc=acc, qt=qt, h=h, jb=jb: e.tensor_scalar(
                    out=o_tok[:, qt, h * 64:(h + 1) * 64], in0=acc[:, jb, 0:64], scalar1=r_[:, 0, 0:1], scalar2=None, op0=ALU.mult),
                    reads=[bacc, br], writes=[bo])
        emit_oT(P, C, o_tok, bo, oT_d[3])
        P.pop()


def nat_bias_tables(rpb, half):
    a = np.arange(128)
    out = np.full((8, 29, 128, 128), NEGM, np.float32)

    def table(tq, tk):
        if tk < 0 or tk >= 64:
            return None
        rk = 2 * tk + a // 64; ck = a % 64
        rq = 2 * tq + a // 64; cq = a % 64
        r0 = np.clip(rq - 4, 0, 120); cs = np.clip(cq - 8, 0, 48)
        valid = ((rk[:, None] >= r0[None, :]) & (rk[:, None] < r0[None, :] + 8) &
                 (ck[:, None] >= cs[None, :]) & (ck[:, None] < cs[None, :] + 16))
        ri = np.clip(rk[:, None] - rq[None, :] + 7, 0, 14)
        ci = np.clip(ck[:, None] - cq[None, :] + 15, 0, 30)
        t = rpb[:, ri, ci]
        return np.where(valid[None], t, np.float32(NEGM)).astype(np.float32)

    g0 = half * 32
    specs = [(10, off) for off in range(-2, 3)]
    specs += [(g0 + 0, off) for off in range(-2, 4)]
    specs += [(g0 + 1, off) for off in range(-2, 4)]
    specs += [(g0 + 30, off) for off in range(-3, 3)]
    specs += [(g0 + 31, off) for off in range(-3, 3)]
    for i, (tq, off) in enumerate(specs):
        t = table(tq, tq + off)
        if t is not None:
            out[:, i] = t
    return np.ascontiguousarray(out.transpose(0, 2, 1, 3).reshape(8, 128, 29 * 128))


def swa_masks(half):
    a = np.arange(128)
    L = np.where(a[None, :] <= a[:, None], 0.0, NEGM).astype(np.float32)
    R = np.where(a[:, None] <= a[None, :], 0.0, NEGM).astype(np.float32)
    allm = np.full((128, 128), NEGM, np.float32)
    first = allm if half == 0 else L
    last = allm if half == 1 else R
    return np.ascontiguousarray(np.stack([np.tile(m, (1, 4)) for m in (L, R, first, last)]))


def k2a_host_inputs(inp, l, core, k1o):
    import ml_dtypes
    b, half = core // 2, core % 2
    me, pa = k1o[core], k1o[core ^ 1]
    lo, hi = (me, pa) if half == 0 else (pa, me)
    bf = lambda a: np.ascontiguousarray(a)
    z = lambda shape: np.zeros(shape, ml_dtypes.bfloat16)
    m = {}
    m["swa_qT"] = bf(me["swa_qT"])
    left = pa["swa_kT"][:, -128:] if half == 1 else z((128, 128))
    right = pa["swa_kT"][:, :128] if half == 0 else z((128, 128))
    m["swa_kT"] = bf(np.concatenate([left, me["swa_kT"], right], 1))
    left = pa["swa_v"][-1:] if half == 1 else z((1, 128, 130))
    right = pa["swa_v"][:1] if half == 0 else z((1, 128, 130))
    m["swa_v"] = bf(np.concatenate([left, me["swa_v"], right], 0))
    m["swa_masks"] = swa_masks(half)
    m["sinkb"] = np.ascontiguousarray(np.broadcast_to(inp["swa_sink"][l][None, :], (128, 8))).astype(np.float32)
    m["mla_qT"] = bf(me["mla_qT"])
    m["mla_kT"] = bf(np.concatenate([lo["mla_kT"], hi["mla_kT"]], 2))
    m["mla_v"] = bf(np.concatenate([lo["mla_v"], hi["mla_v"]], 0))
    m["diff_qT"] = bf(me["diff_qT"])
    m["diff_kT"] = bf(np.concatenate([lo["diff_kT"], hi["diff_kT"]], 1))
    m["diff_v"] = bf(np.concatenate([lo["diff_v"], hi["diff_v"]], 0))
    lam = np.stack([inp["diff_lambda_q1"][l], inp["diff_lambda_k1"][l], inp["diff_lambda_q2"][l], inp["diff_lambda_k2"][l]])
    m["lamb"] = np.ascontiguousarray(np.broadcast_to(lam[None], (128, 4, 64))).astype(np.float32)
    m["subln"] = np.ascontiguousarray(np.broadcast_to(inp["diff_subln"][l][None], (128, 128))).astype(np.float32)
    import math
    li = 0.8 - 0.6 * math.exp(-0.3 * l)
    m["lami"] = np.ascontiguousarray(np.broadcast_to(np.array([li, 1.0 - li], np.float32)[None], (128, 2)))
    m["nat_qT"] = bf(me["nat_qT"])
    left = pa["nat_kT"][:, -256:] if half == 1 else z((512, 256))
    right = pa["nat_kT"][:, :256] if half == 0 else z((512, 256))
    m["nat_kT"] = bf(np.concatenate([left, me["nat_kT"], right], 1))
    left = pa["nat_v"][-2:] if half == 1 else z((2, 128, 520))
    right = pa["nat_v"][:2] if half == 0 else z((2, 128, 520))
    m["nat_v"] = bf(np.concatenate([left, me["nat_v"], right], 0))
    m["nat_bias"] = nat_bias_tables(inp["nat_rpb"][l], half)
    m["ident"] = np.eye(128, dtype=np.float32)
    return m


DN_ALPHA = 4.0 ** 0.25


def emit_gvec_bcast(P, cT_d, adaw_d, adabb_d, gb, bgb, pA, wst_rot, tag):
    cs = P.sb(f"{tag}_cs", [128, 8], F32); bcs = Buf()
    csr = P.sb(f"{tag}_csr", [128, 8, 128], F32); bcsr = Buf()
    ab = P.sb(f"{tag}_ab", [128, 1024], F32); bab = Buf()
    P.dma("sp", cs[:], cT_d, writes=[bcs])
    P.dma("act", ab[:], adabb_d, writes=[bab])
    P.op("act", lambda e: e.activation(out=cs[:], in_=cs[:], func=AF.Silu), reads=[bcs], writes=[bcs])
    P.op("dve", lambda e: e.tensor_copy(out=csr[:], in_=cs[:].rearrange("p (k o) -> p k o", o=1).to_broadcast([128, 8, 128])),
         reads=[bcs], writes=[bcsr])
    for hf in range(2):
        ps, bps = pA.next()
        for q4 in range(2):
            ws_, bws = wst_rot.next()
            ws = ws_[:, 0:2048].rearrange("p (c n) -> p c n", c=8)
            c0 = hf * 512 + q4 * 256
            P.dma("sp" if q4 == 0 else "act", ws, adaw_d[:, c0:c0 + 256].rearrange("(c p) n -> p c n", p=128), writes=[bws])
            for kc in range(8):
                P.op("pe", lambda e, kc=kc, ps=ps, ws=ws, q4=q4: e.matmul(ps[:, q4 * 256:(q4 + 1) * 256], lhsT=csr[:, kc, :], rhs=ws[:, kc, :],
                                                                 start=(kc == 0), stop=(kc == 7)), reads=[bcsr, bws], writes=[bps])
        P.op("dve", lambda e, ps=ps, hf=hf: e.tensor_tensor(out=gb[:, hf * 512:(hf + 1) * 512], in0=ps[:], in1=ab[:, hf * 512:(hf + 1) * 512], op=ALU.add),
             reads=[bps, bab], writes=[bgb])


def emit_resid_ln(P, C, y_ps, by, xt, bx, gb, bgb, lng, lnb, bln, epsb, beps, out_tile, bout, tag):
    t1, bt1 = C["t1"].next()
    for hf in range(2):
        P.op("dve", lambda e, hf=hf, t1=t1: e.tensor_tensor(out=t1[:, hf * 512:(hf + 1) * 512], in0=y_ps[hf][:], in1=gb[:, hf * 512:(hf + 1) * 512], op=ALU.mult),
             reads=[by[hf], bgb], writes=[bt1])
    P.op("dve", lambda e, t1=t1: e.scalar_tensor_tensor(out=t1[:], in0=xt[:], scalar=DN_ALPHA, in1=t1[:], op0=ALU.mult, op1=ALU.add),
         reads=[bx, bt1], writes=[bt1])
    s_, bs = C["st"].next(); m_, bm = C["mv"].next(); r_, br = C["rs"].next()
    for c in range(2):
        P.op("dve", lambda e, c=c, s_=s_, t1=t1: e.bn_stats(out=s_[:, c, :], in_=t1[:, c * 512:(c + 1) * 512]), reads=[bt1], writes=[bs])
    P.op("dve", lambda e, m_=m_, s_=s_: e.bn_aggr(out=m_[:], in_=s_[:]), reads=[bs], writes=[bm])
    P.op("act", lambda e, r_=r_, m_=m_: e.activation(out=r_[:], in_=m_[:, 1:2], func=AF.Ln, bias=epsb[:, 0:1], scale=1.0), reads=[bm, beps], writes=[br])
    P.op("act", lambda e, r_=r_: e.activation(out=r_[:], in_=r_[:], func=AF.Exp, scale=-0.5), reads=[br], writes=[br])
    P.op("dve", lambda e, t1=t1, m_=m_, r_=r_: e.tensor_scalar(out=t1[:], in0=t1[:], scalar1=m_[:, 0:1], scalar2=r_[:, 0:1], op0=ALU.subtract, op1=ALU.mult),
         reads=[bt1, bm, br], writes=[bt1])
    P.op("pool", lambda e, t1=t1: e.tensor_tensor(out=t1[:], in0=t1[:], in1=lng[:], op=ALU.mult), reads=[bt1, bln], writes=[bt1])
    P.op("pool", lambda e, t1=t1: e.tensor_tensor(out=out_tile[:], in0=t1[:], in1=lnb[:], op=ALU.add), reads=[bt1, bln], writes=[bout])


def build_k2b(nc):
    P = Prog(nc)
    din = lambda n, s, d=F32: nc.dram_tensor(n, list(s), d, kind="ExternalInput").ap()
    D = {}
    D["oT"] = din("oT", [4, 512, T], BF16); D["uT"] = din("uT", [1024, T], BF16); D["x"] = din("x", [T, 1024])
    D["wg"] = din("wg", [4, 1024, 1024]); D["wb"] = din("wb", [4, 512, 1024]); D["wo"] = din("wo", [1024, 1024])
    D["cT"] = din("cT", [128, 8]); D["adaw_g"] = din("adaw_g", [1024, 1024]); D["adab_g"] = din("adab_g", [128, 1024])
    D["lng"] = din("lng", [128, 1024]); D["lnb"] = din("lnb", [128, 1024])
    xo = nc.dram_tensor("xmid", [T, 1024], F32, kind="ExternalOutput").ap()
    emit_merge(P, D, xo)
    P.finish(); P.emit()
    return P


def emit_merge(P, D, xo):
    P.push()
    pA = Rot(P, "m_pA", [128, 512], F32, 3, "ps")
    pB = Rot(P, "m_pB", [128, 512], F32, 3, "ps")
    wst = Rot(P, "m_wst", [128, 2048], F32, 2)
    epsb = P.sb("m_eps", [128, 1], F32); beps = Buf()
    P.op("pool", lambda e: e.memset(epsb[:], 1e-5), writes=[beps])
    gb = P.sb("m_gb", [128, 1024], F32); bgb = Buf()
    emit_gvec_bcast(P, D["cT"], D["adaw_g"], D["adab_g"], gb, bgb, pA, wst, "m")
    mT = P.sb("m_mT", [128, 8, T], BF16)
    bmT = [[Buf() for _ in range(NTB)] for _ in range(8)]
    P.push()
    wgf = Rot(P, "m_wgf", [128, 8, 4, 128], F32, 1)
    wgb = Rot(P, "m_wgb", [128, 8, 4, 128], BF16, 2)
    wbf = Rot(P, "m_wbf", [128, 4, 4, 128], F32, 1)
    wbb = Rot(P, "m_wbb", [128, 4, 4, 128], BF16, 2)
    ub = Rot(P, "m_ub", [128, 8, 512], BF16, 2)
    ob = Rot(P, "m_ob", [128, 4, 4, 512], BF16, 2)
    gs = Rot(P, "m_gs", [128, 512], BF16, 2)
    ac = Rot(P, "m_ac", [128, 512], F32, 2)
    tm = Rot(P, "m_tm", [128, 512], F32, 2)
    for oc in range(8):
        wgf_, bwgf = wgf.next(); wgb_, bwgb = wgb.next(); wbf_, bwbf = wbf.next(); wbb_, bwbb = wbb.next()
        for n in range(4):
            P.dma("sp", wgf_[:, :, n, :], D["wg"][n, :, oc * 128:(oc + 1) * 128].rearrange("(c p) n -> p c n", p=128),
                  writes=[bwgf], append=(n > 0))
            P.dma("sp", wbf_[:, :, n, :], D["wb"][n, :, oc * 128:(oc + 1) * 128].rearrange("(c p) n -> p c n", p=128),
                  writes=[bwbf], append=(n > 0))
        P.op("pool", lambda e, a=wgb_, b=wgf_: e.tensor_copy(out=a[:], in_=b[:]), reads=[bwgf], writes=[bwgb])
        P.op("pool", lambda e, a=wbb_, b=wbf_: e.tensor_copy(out=a[:], in_=b[:]), reads=[bwbf], writes=[bwbb])
        for tb in range(NTB):
            sl = slice(tb * 512, (tb + 1) * 512)
            u_, bu = ub.next(); o_, bo_ = ob.next()
            P.dma("sp", u_[:], D["uT"].rearrange("(c p) t -> p c t", p=128)[:, :, sl], writes=[bu])
            for n in range(4):
                P.dma("sp", o_[:, n, :, :], D["oT"][n].rearrange("(c p) t -> p c t", p=128)[:, :, sl], writes=[bo_], append=(n > 0))
            a_, ba = ac.next()
            for n in range(4):
                pg, bpg = pA.next(); pb, bpb = pB.next()
                for kc in range(8):
                    P.op("pe", lambda e, kc=kc, n=n, pg=pg, wgb_=wgb_, u_=u_: e.matmul(pg[:], lhsT=wgb_[:, kc, n, :], rhs=u_[:, kc, :], start=(kc == 0), stop=(kc == 7)),
                         reads=[bwgb, bu], writes=[bpg])
                for kc in range(4):
                    P.op("pe", lambda e, kc=kc, n=n, pb=pb, wbb_=wbb_, o_=o_: e.matmul(pb[:], lhsT=wbb_[:, kc, n, :], rhs=o_[:, n, kc, :], start=(kc == 0), stop=(kc == 3)),
                         reads=[bwbb, bo_], writes=[bpb])
                g_, bg = gs.next()
                P.op("act", lambda e, g_=g_, pg=pg: e.activation(out=g_[:], in_=pg[:], func=AF.Sigmoid), reads=[bpg], writes=[bg])
                if n == 0:
                    P.op("dve", lambda e, a_=a_, g_=g_, pb=pb: e.tensor_tensor(out=a_[:], in0=pb[:], in1=g_[:], op=ALU.mult), reads=[bpb, bg], writes=[ba])
                else:
                    t_, bt = tm.next()
                    P.op("dve", lambda e, t_=t_, g_=g_, pb=pb: e.tensor_tensor(out=t_[:], in0=pb[:], in1=g_[:], op=ALU.mult), reads=[bpb, bg], writes=[bt])
                    if n < 3:
                        P.op("pool", lambda e, a_=a_, t_=t_: e.tensor_tensor(out=a_[:], in0=a_[:], in1=t_[:], op=ALU.add), reads=[ba, bt], writes=[ba])
                    else:
                        P.op("pool", lambda e, a_=a_, t_=t_, oc=oc, sl=sl: e.tensor_tensor(out=mT[:, oc, sl], in0=a_[:], in1=t_[:], op=ALU.add),
                             reads=[ba, bt], writes=[bmT[oc][tb]])
    P.pop()
    P.push()
    wo = P.sb("m_wo", [128, 8, 1024], BF16); bwo = Buf()
    for q8 in range(4):
        ws_, bws = wst.next()
        ws = ws_[:, 0:2048].rearrange("p (c n) -> p c n", c=8)
        P.dma(("sp", "act")[q8 % 2], ws, D["wo"][:, q8 * 256:(q8 + 1) * 256].rearrange("(c p) n -> p c n", p=128), writes=[bws])
        P.op("pool", lambda e, ws=ws, q8=q8: e.tensor_copy(out=wo[:, :, q8 * 256:(q8 + 1) * 256], in_=ws), reads=[bws], writes=[bwo], )
    lng = P.sb("m_lng", [128, 1024], F32); lnb = P.sb("m_lnb", [128, 1024], F32); bln = Buf()
    P.dma("sp", lng[:], D["lng"], writes=[bln]); P.dma("act", lnb[:], D["lnb"], writes=[bln], append=True)
    C = {"t1": Rot(P, "m_t1", [128, 1024], F32, 2), "st": Rot(P, "m_st", [128, 2, 6], F32, 2),
         "mv": Rot(P, "m_mv", [128, 2], F32, 2), "rs": Rot(P, "m_rs", [128, 1], F32, 2)}
    xin = Rot(P, "m_xin", [128, 1024], F32, 2)
    xout = Rot(P, "m_xout", [128, 1024], F32, 2)
    for t in range(NTT):
        xt, bx = xin.next()
        P.dma("sp", xt[:], D["x"][t * 128:(t + 1) * 128, :], writes=[bx])
        ys = []; bys = []
        for hf in range(2):
            py, bpy = pA.next()
            for kc in range(8):
                P.op("pe", lambda e, kc=kc, py=py, hf=hf, t=t: e.matmul(py[:], lhsT=mT[:, kc, t * 128:(t + 1) * 128], rhs=wo[:, kc, hf * 512:(hf + 1) * 512],
                                                                 start=(kc == 0), stop=(kc == 7)), reads=[bwo, bmT[kc][t // 4]], writes=[bpy])
            ys.append(py); bys.append(bpy)
        xo_, bxo = xout.next()
        emit_resid_ln(P, C, ys, bys, xt, bx, gb, bgb, lng, lnb, bln, epsb, beps, xo_, bxo, "m")
        P.dma("pool", xo[t * 128:(t + 1) * 128, :], xo_[:], reads=[bxo])
    P.pop()
    P.pop()


def k2b_host_inputs(inp, l, core, xcur, oT, uT):

    b, half = core // 2, core % 2
    rep = lambda v: np.ascontiguousarray(np.broadcast_to(np.asarray(v, np.float32)[None, :], (128, len(v))))
    return {
        "oT": np.ascontiguousarray(oT), "uT": np.ascontiguousarray(uT),
        "x": np.ascontiguousarray(xcur[b, half * T:(half + 1) * T]),
        "wg": np.ascontiguousarray(inp["w_gate"][l]), "wb": np.ascontiguousarray(inp["w_branch"][l]),
        "wo": np.ascontiguousarray(inp["w_out"][l]),
        "cT": to_pc(inp["c"][b], 8), "adaw_g": np.ascontiguousarray(inp["ada_w"][l][:, 2048:3072]),
        "adab_g": rep(inp["ada_b"][l][2048:3072]),
        "lng": rep(inp["ln1_g"][l]), "lnb": rep(inp["ln1_b"][l]),
    }


TBLK = 256
NBLK = T // TBLK
NCH = 128
NEG = -1.0e30
U32 = mybir.dt.uint32


def build_k3(nc, nblk=NBLK, dbg=False):
    P = Prog(nc)
    din = lambda n, s, d=F32: nc.dram_tensor(n, list(s), d, kind="ExternalInput").ap()
    D = {}
    D["xmid"] = din("xmid", [T, 1024]); D["cT"] = din("cT", [128, 8])
    D["adaw_f"] = din("adaw_f", [1024, 3072]); D["adab_f"] = din("adab_f", [128, 16]); D["adab_g"] = din("adab_g", [128, 1024])
    D["wq"] = din("wq", [1024, 2048]); D["keysT"] = din("keysT", [128, 16, 128])
    D["UT"] = din("UT", [1024, 16384]); D["V"] = din("V", [16384, 1024])
    D["lng"] = din("lng", [128, 1024]); D["lnb"] = din("lnb", [128, 1024])
    D["ident"] = din("ident", [128, 128]); D["iota"] = din("iota", [128, 128])
    xo = nc.dram_tensor("xout", [T, 1024], F32, kind="ExternalOutput").ap()
    dbg_d = nc.dram_tensor("dbg", [128, 16, 128], F32, kind="ExternalOutput").ap() if dbg else None
    emit_peer(P, nc, D, xo, nblk, dbg_d)
    P.finish(); P.emit()
    return P


def peer_scratch(nc, tag):
    Ub_d = nc.dram_tensor(f"peer_Ub{tag}", [NCH, 128, 8 * 128], BF16, kind="Internal").ap()
    Vb_d = nc.dram_tensor(f"peer_Vb{tag}", [NCH, 128, 1024], BF16, kind="Internal").ap()
    return Ub_d, Vb_d, Buf("Ub_d"), Buf("Vb_d")


def peer_cast_gen(P, UT, V, Ub_d, Vb_d, bUb, bVb, cf, cb):
    for c4 in range(NCH // 4):
        f_, bf_ = cf.next(); b_, bb_ = cb.next()
        P.dma("sp", f_[:].rearrange("p (k n) -> p k n", k=8),
              UT[:, c4 * 512:(c4 + 1) * 512].rearrange("(k p) n -> p k n", p=128), writes=[bf_])
        P.op(("dve", "pool")[c4 % 2], lambda e, f_=f_, b_=b_: e.tensor_copy(
            out=b_[:].rearrange("p (c k n) -> p c k n", c=4, k=8), in_=f_[:].rearrange("p (k c n) -> p c k n", k=8, c=4)),
            reads=[bf_], writes=[bb_])
        P.dma("pool", Ub_d[c4 * 4:(c4 + 1) * 4].rearrange("c p n -> p c n"), b_[:].rearrange("p (c n) -> p c n", c=4),
              reads=[bb_], writes=[bUb], append=True)
        f_, bf_ = cf.next(); b_, bb_ = cb.next()
        P.dma("sp", f_[:].rearrange("p (c n) -> p c n", c=4),
              V[c4 * 512:(c4 + 1) * 512, :].rearrange("(c p) n -> p c n", p=128), writes=[bf_])
        P.op(("pool", "dve")[c4 % 2], lambda e, f_=f_, b_=b_: e.tensor_copy(out=b_[:], in_=f_[:]), reads=[bf_], writes=[bb_])
        P.dma("pool", Vb_d[c4 * 4:(c4 + 1) * 4].rearrange("c p n -> p c n"), b_[:].rearrange("p (c n) -> p c n", c=4),
              reads=[bb_], writes=[bVb], append=True)
        yield


def emit_peer(P, nc, D, xo, nblk=NBLK, dbg_d=None, tag="", pre=None):
    u2_d = nc.dram_tensor(f"peer_u2{tag}", [1024, T], BF16, kind="Internal").ap()
    P.push()
    bu2d = Buf("u2_d")
    if pre is None:
        Ub_d, Vb_d, bUb, bVb = peer_scratch(nc, tag)
        P.push()
        cf = Rot(P, "pa_cf", [128, 4096], F32, 2)
        cb = Rot(P, "pa_cb", [128, 4096], BF16, 2)
        for _ in peer_cast_gen(P, D["UT"], D["V"], Ub_d, Vb_d, bUb, bVb, cf, cb):
            pass
        P.pop()
    else:
        Ub_d, Vb_d, bUb, bVb = pre

    pA = Rot(P, "p_pA", [128, 512], F32, 2, "ps")
    ident_f = P.sb("p_identf", [128, 128], F32); bidf = Buf()
    ident = P.sb("p_ident", [128, 128], BF16); bid = Buf()
    epsb = P.sb("p_eps", [128, 1], F32); beps = Buf()
    P.dma("pool", ident_f[:], D["ident"], writes=[bidf])
    P.op("dve", lambda e: e.tensor_copy(out=ident[:], in_=ident_f[:]), reads=[bidf], writes=[bid])
    P.op("pool", lambda e: e.memset(epsb[:], 1e-5), writes=[beps])
    gb = P.sb("p_gb", [128, 1024], F32); bgb = Buf()
    P.push()
    wst = Rot(P, "p_wst", [128, 2048], F32, 2)
    emit_gvec_bcast(P, D["cT"], D["adaw_f"][:, 2048:3072], D["adab_g"], gb, bgb, pA, wst, "p")
    P.push()
    modc = P.sb("p_modc", [128, 16], F32); bmodc = Buf()
    sc1 = P.sb("p_sc1", [128, 8], F32); bsc1 = Buf()
    pm = Rot(P, "p_pm", [128, 512], F32, 1, "ps")
    emit_mod_cols(P, nc, D["cT"], D["adaw_f"][:, 0:2048], D["adab_f"], 2, modc, bmodc, pm, wst)
    P.op("dve", lambda e: e.tensor_scalar(out=sc1[:], in0=modc[:, 8:16], scalar1=1.0, scalar2=None, op0=ALU.add), reads=[bmodc], writes=[bsc1])
    bmod = Buf()
    P.op("dve", lambda e: e.tensor_copy(out=modc[:, 0:8], in_=modc[:, 0:8]), reads=[bmodc, bsc1], writes=[bmod])
    u2T = P.sb("p_u2T", [128, 8, T], BF16)
    bu2 = [Buf() for _ in range(NTT)]
    ptr = Rot(P, "p_ptr", [128, 8, 128], BF16, 2, "ps")
    emit_ln_mod(P, D["xmid"], u2T, bu2, modc, sc1, bmod, ident, bid, epsb, beps, ptr, tag="p")
    for c in range(8):
        P.dma(("sp", "act")[c % 2], u2_d[c * 128:(c + 1) * 128, :], u2T[:, c, :], reads=bu2, writes=[bu2d], append=(c > 0))
    P.pop()

    wqb_d = nc.dram_tensor(f"peer_wqb{tag}", [128, 8 * 2048], BF16, kind="Internal").ap()
    bwqd = Buf("wqb_d")
    P.push()
    wtmp = P.sb("p_wqtmp", [128, 8, 2048], BF16); bwt = Buf()
    for g in range(8):
        ws_, bws = wst.next()
        ws = ws_[:, 0:2048].rearrange("p (c n) -> p c n", c=8)
        P.dma(("sp", "act")[g % 2], ws, D["wq"][:, g * 256:(g + 1) * 256].rearrange("(c p) n -> p c n", p=128), writes=[bws])
        P.op("pool", lambda e, ws=ws, g=g: e.tensor_copy(out=wtmp[:, :, g * 256:(g + 1) * 256], in_=ws), reads=[bws], writes=[bwt])
    P.dma("sp", wqb_d, wtmp[:].rearrange("p k n -> p (k n)"), reads=[bwt], writes=[bwqd])
    P.pop()
    P.pop()

    keysT = P.sb("p_keysT", [128, 16, 128], F32); bkeys = Buf()
    P.dma("sp", keysT[:], D["keysT"], writes=[bkeys])
    iota = P.sb("p_iota", [128, 128], F32); biota = Buf()
    P.dma("act", iota[:], D["iota"], writes=[biota])
    lng = P.sb("p_lng", [128, 1024], F32); lnb = P.sb("p_lnb", [128, 1024], F32); bln = Buf()
    P.dma("sp", lng[:], D["lng"], writes=[bln]); P.dma("act", lnb[:], D["lnb"], writes=[bln], append=True)

    pO = [P.ps(f"p_pO{i}", [128, 512], F32) for i in range(4)]
    bpO = [Buf(f"pO{i}", excl=True) for i in range(4)]
    pH = Rot(P, "p_pH", [128, 512], F32, 2, "ps")
    pM = pA
    NTL = TBLK // 128

    Wd = nc.dram_tensor(f"peer_W{tag}", [nblk, 8, 128, TBLK * 16], BF16, kind="Internal").ap()
    bWd = [Buf(f"Wd{b}") for b in range(nblk)]

    wq = P.sb("p_wq", [128, 8, 2048], BF16); bwq = Buf()
    P.dma("act", wq[:].rearrange("p k n -> p (k n)"), wqb_d, reads=[bwqd], writes=[bwq])
    ublk = Rot(P, "p_ublk", [128, 8, TBLK], BF16, 2)
    qT = Rot(P, "p_qT", [128, 16, TBLK], F32, 1)
    C = {"t1": Rot(P, "p_t1", [128, 1024], F32, 2), "st": Rot(P, "p_st", [128, 2, 6], F32, 2),
         "mv": Rot(P, "p_mv", [128, 2], F32, 2), "rs": Rot(P, "p_rs", [128, 1], F32, 2)}
    xin = Rot(P, "p_xin", [128, 1024], F32, 1)
    xout = Rot(P, "p_xout", [128, 1024], F32, 1)
    S = Rot(P, "p_S", [128, 16, 128], F32, 1)
    S2 = Rot(P, "p_S2", [128, 128], F32, 2)
    top = Rot(P, "p_top", [128, 16, 16], F32, 1)
    jix = Rot(P, "p_jix", [128, 8, 16], U32, 1)
    jif = Rot(P, "p_jif", [128, 128], F32, 1)
    jT = Rot(P, "p_jT", [128, 128], F32, 1)
    cand = Rot(P, "p_cand", [128, 256], F32, 2)
    ctop = Rot(P, "p_ctop", [128, 24], F32, 2)
    sm = Rot(P, "p_sm", [128, 8, 8], F32, 1)
    junk = Rot(P, "p_junk", [128, 16], F32, 2)
    e0 = Rot(P, "p_e0", [128, 8, 128], BF16, 1)
    thr2 = Rot(P, "p_thr2", [128, 8, 16], F32, 1)
    sc2 = Rot(P, "p_sc2", [128, 8, 16], F32, 1)
    sc2b = Rot(P, "p_sc2b", [128, 8, 16], BF16, 1)
    Ytm = Rot(P, "p_Ytm", [128, 128, 64], BF16, 1)
    Ysm = Rot(P, "p_Ysm", [128, 64, 128], BF16, 1)
    Xsm = Rot(P, "p_Xsm", [128, 64, 128], BF16, 1)
    Wst = Rot(P, "p_Wst", [128, 4, 128, 16], BF16, 1)
    wsl = Rot(P, "p_wsl", [128, TBLK, 16], BF16, 2)
    uch = Rot(P, "p_uch", [128, 8, 128], BF16, 3)
    vch = Rot(P, "p_vch", [128, 1024], BF16, 4)
    gl = Rot(P, "p_gl", [128, TBLK], BF16, 2)
    zt = Rot(P, "p_zt", [128, TBLK], BF16, 3)
    ublocks = {}

    def sel_block(blk):
        tsl = slice(blk * TBLK, (blk + 1) * TBLK)
        u_, bu = ublk.next()
        ublocks[blk] = (u_, bu)
        P.dma("sp", u_[:], u2_d.rearrange("(c p) t -> p c t", p=128)[:, :, tsl], reads=[bu2d], writes=[bu])
        q_, bq = qT.next()
        for hp in range(16):
            ph, bph = pH.next()
            for kc in range(8):
                P.op("pe", lambda e, kc=kc, hp=hp, ph=ph, u_=u_: e.matmul(ph[:, 0:TBLK], lhsT=wq[:, kc, hp * 128:(hp + 1) * 128], rhs=u_[:, kc, :],
                                                                  start=(kc == 0), stop=(kc == 7)), reads=[bwq, bu], writes=[bph])
            P.op("act", lambda e, hp=hp, ph=ph, q_=q_: e.activation(out=q_[:, hp, :], in_=ph[:, 0:TBLK], func=AF.Copy), reads=[bph], writes=[bq])
            if hp % 4 == 3:
                yield
        for tt in range(NTL):
            S_, bS = S.next()
            for g4 in range(4):
                pm_, bpm = pM.next()
                for i4 in range(4):
                    hp = g4 * 4 + i4
                    P.op("pe", lambda e, hp=hp, i4=i4, pm_=pm_, q_=q_, tt=tt: e.matmul(
                        pm_[:, i4 * 128:(i4 + 1) * 128], lhsT=q_[:, hp, tt * 128:(tt + 1) * 128], rhs=keysT[:, hp, :], start=True, stop=True),
                        reads=[bq, bkeys], writes=[bpm])
                P.op("act", lambda e, g4=g4, pm_=pm_, S_=S_: e.activation(out=S_[:, g4 * 4:(g4 + 1) * 4, :].rearrange("p a b -> p (a b)"), in_=pm_[:], func=AF.Copy),
                     reads=[bpm], writes=[bS])
            if dbg_d is not None and blk == 0 and tt == 0:
                P.dma("sp", dbg_d, S_[:], reads=[bS])
            yield
            top_, btop = top.next(); jix_, bjix = jix.next()
            for hp in range(16):
                s2, bs2 = S2.next()
                P.op("dve", lambda e, hp=hp, top_=top_, S_=S_: e.max(out=top_[:, hp, 0:8], in_=S_[:, hp, :]), reads=[bS], writes=[btop])
                P.op("dve", lambda e, hp=hp, top_=top_, S_=S_, s2=s2: e.match_replace(out=s2[:], in_to_replace=top_[:, hp, 0:8], in_values=S_[:, hp, :], imm_value=NEG),
                     reads=[bS, btop], writes=[bs2])
                P.op("dve", lambda e, hp=hp, top_=top_, s2=s2: e.max(out=top_[:, hp, 8:16], in_=s2[:]), reads=[bs2, btop], writes=[btop])
                if hp % 2 == 1:
                    h = hp // 2
                    P.op("dve", lambda e, hp=hp, h=h, top_=top_, S_=S_, jix_=jix_: e.max_index(out=jix_[:, h, 0:8], in_max=top_[:, hp, 0:8], in_values=S_[:, hp, :]),
                         reads=[bS, btop], writes=[bjix])
                    P.op("dve", lambda e, hp=hp, h=h, top_=top_, S_=S_, jix_=jix_: e.max_index(out=jix_[:, h, 8:16], in_max=top_[:, hp, 8:16], in_values=S_[:, hp, :]),
                         reads=[bS, btop, bjix], writes=[bjix])
                if hp % 4 == 3:
                    yield
            jif_, bjif = jif.next()
            P.op("dve", lambda e, jif_=jif_, jix_=jix_: e.tensor_copy(out=jif_[:], in_=jix_[:].rearrange("p a b -> p (a b)")), reads=[bjix], writes=[bjif])
            pm_, bpm = pM.next()
            P.op("pe", lambda e, pm_=pm_, jif_=jif_: e.transpose(out=pm_[:, 0:128], in_=jif_[:], identity=ident_f[:]), reads=[bjif, bidf], writes=[bpm])
            jT_, bjT = jT.next()
            P.op("act", lambda e, pm_=pm_, jT_=jT_: e.activation(out=jT_[:], in_=pm_[:, 0:128], func=AF.Copy), reads=[bpm], writes=[bjT])
            sm_, bsm = sm.next(); thr_, bthr = thr2.next(); sc_, bsc = sc2.next(); e0_, be0 = e0.next()
            P.op("pool", lambda e, sm_=sm_: e.memset(sm_[:], 0.0), writes=[bsm])
            for h in range(8):
                cd, bcd = cand.next(); ct, bct = ctop.next()
                P.op("dve", lambda e, h=h, cd=cd, top_=top_: e.tensor_tensor(
                    out=cd[:].rearrange("p (a b) -> p a b", a=16),
                    in0=top_[:, 2 * h, :].rearrange("p (a o) -> p a o", o=1).to_broadcast([128, 16, 16]),
                    in1=top_[:, 2 * h + 1, :].rearrange("p (o b) -> p o b", o=1).to_broadcast([128, 16, 16]), op=ALU.add),
                    reads=[btop], writes=[bcd])
                for r in range(3):
                    P.op("dve", lambda e, r=r, cd=cd, ct=ct: e.max(out=ct[:, r * 8:(r + 1) * 8], in_=cd[:]), reads=[bcd], writes=[bct])
                    if r < 2:
                        P.op("dve", lambda e, r=r, cd=cd, ct=ct: e.match_replace(out=cd[:], in_to_replace=ct[:, r * 8:(r + 1) * 8], in_values=cd[:], imm_value=NEG),
                             reads=[bct, bcd], writes=[bcd])
                P.op("dve", lambda e, h=h, ct=ct, sm_=sm_: e.tensor_tensor(out=sm_[:, h, 0:1], in0=ct[:, 15:16], in1=ct[:, 16:17], op=ALU.add), reads=[bct, bsm], writes=[bsm])
                P.op("dve", lambda e, h=h, sm_=sm_: e.tensor_scalar(out=sm_[:, h, 0:1], in0=sm_[:, h, 0:1], scalar1=0.5, scalar2=None, op0=ALU.mult), reads=[bsm], writes=[bsm])
                P.op("dve", lambda e, h=h, top_=top_, sm_=sm_: e.tensor_scalar(out=sm_[:, h, 1:2], in0=top_[:, 2 * h, 0:1], scalar1=-1.0, scalar2=None, op0=ALU.mult), reads=[btop, bsm], writes=[bsm])
                P.op("dve", lambda e, h=h, top_=top_, sm_=sm_: e.tensor_scalar(out=sm_[:, h, 2:3], in0=top_[:, 2 * h + 1, 0:1], scalar1=-1.0, scalar2=None, op0=ALU.mult), reads=[btop, bsm], writes=[bsm])
                P.op("dve", lambda e, h=h, ct=ct, sm_=sm_: e.tensor_scalar(out=sm_[:, h, 5:6], in0=ct[:, 0:1], scalar1=-1.0, scalar2=None, op0=ALU.mult), reads=[bct, bsm], writes=[bsm])
                jk, bjk = junk.next()
                P.op("act", lambda e, h=h, ct=ct, sm_=sm_, jk=jk: e.activation(out=jk[:], in_=ct[:, 0:16], func=AF.Exp, bias=sm_[:, h, 5:6], scale=1.0, accum_out=sm_[:, h, 3:4]),
                     reads=[bct, bsm], writes=[bjk, bsm])
                P.op("dve", lambda e, h=h, sm_=sm_: e.reciprocal(out=sm_[:, h, 4:5], in_=sm_[:, h, 3:4]), reads=[bsm], writes=[bsm])
                P.op("act", lambda e, h=h, S_=S_, sm_=sm_, e0_=e0_: e.activation(out=e0_[:, h, :], in_=S_[:, 2 * h, :], func=AF.Exp, bias=sm_[:, h, 1:2], scale=1.0),
                     reads=[bS, bsm], writes=[be0])
                P.op("dve", lambda e, h=h, top_=top_, sm_=sm_, thr_=thr_: e.tensor_scalar(out=thr_[:, h, :], in0=top_[:, 2 * h + 1, :], scalar1=-1.0, scalar2=sm_[:, h, 0:1], op0=ALU.mult, op1=ALU.add),
                     reads=[btop, bsm], writes=[bthr])
                P.op("act", lambda e, h=h, top_=top_, sm_=sm_, sc_=sc_: e.activation(out=sc_[:, h, :], in_=top_[:, 2 * h + 1, :], func=AF.Exp, bias=sm_[:, h, 2:3], scale=1.0),
                     reads=[btop, bsm], writes=[bsc])
                P.op("dve", lambda e, h=h, sm_=sm_, sc_=sc_: e.tensor_scalar(out=sc_[:, h, :], in0=sc_[:, h, :], scalar1=sm_[:, h, 4:5], scalar2=None, op0=ALU.mult),
                     reads=[bsc, bsm], writes=[bsc])
                if h % 2 == 1:
                    yield
            scb_, bscb = sc2b.next()
            P.op("dve", lambda e, scb_=scb_, sc_=sc_: e.tensor_copy(out=scb_[:], in_=sc_[:]), reads=[bsc], writes=[bscb])
            for ch in range(2):
                csl = slice(ch * 64, (ch + 1) * 64)
                Y_, bY = Ytm.next()
                for h in range(8):
                    yv = Y_[:, h * 16:(h + 1) * 16, :]
                    P.op("dve", lambda e, h=h, yv=yv, S_=S_, thr_=thr_, csl=csl: e.tensor_tensor(
                        out=yv, in0=S_[:, 2 * h, csl].rearrange("p (o n) -> p o n", o=1).to_broadcast([128, 16, 64]),
                        in1=thr_[:, h, :].rearrange("p (s o) -> p s o", o=1).to_broadcast([128, 16, 64]), op=ALU.is_ge),
                        reads=[bS, bthr], writes=[bY])
                    P.op("dve", lambda e, h=h, yv=yv, e0_=e0_, csl=csl: e.tensor_tensor(
                        out=yv, in0=yv, in1=e0_[:, h, csl].rearrange("p (o n) -> p o n", o=1).to_broadcast([128, 16, 64]), op=ALU.mult),
                        reads=[bY, be0], writes=[bY])
                    P.op("dve", lambda e, h=h, yv=yv, scb_=scb_: e.tensor_tensor(
                        out=yv, in0=yv, in1=scb_[:, h, :].rearrange("p (s o) -> p s o", o=1).to_broadcast([128, 16, 64]), op=ALU.mult),
                        reads=[bY, bscb], writes=[bY])
                    if h % 4 == 3:
                        yield
                Ys_, bYs = Ysm.next()
                for c4 in range(16):
                    pm_, bpm = pM.next()
                    pmb = pm_[:].bitcast(BF16)
                    for i in range(4):
                        cc = c4 * 4 + i
                        P.op("pe", lambda e, cc=cc, i=i, pmb=pmb, Y_=Y_: e.transpose(out=pmb[:, i * 128:(i + 1) * 128], in_=Y_[:, :, cc], identity=ident[:]),
                             reads=[bY, bid], writes=[bpm])
                    if c4 % 2 == 0:
                        P.op("act", lambda e, c4=c4, pmb=pmb, Ys_=Ys_: e.activation(out=Ys_[:, c4 * 4:(c4 + 1) * 4, :].rearrange("p a b -> p (a b)"), in_=pmb[:, 0:512], func=AF.Copy),
                             reads=[bpm], writes=[bYs])
                    else:
                        P.op("dve", lambda e, c4=c4, pmb=pmb, Ys_=Ys_: e.tensor_copy(out=Ys_[:, c4 * 4:(c4 + 1) * 4, :].rearrange("p a b -> p (a b)"), in_=pmb[:, 0:512]),
                             reads=[bpm], writes=[bYs])
                    if c4 % 4 == 3:
                        yield
                Ws_, bWs = Wst.next()
                for th in range(2):
                    X_, bX = Xsm.next()
                    for q2 in range(2):
                        P.op("dve", lambda e, q2=q2, th=th, X_=X_, jT_=jT_: e.tensor_tensor(
                            out=X_[:, q2 * 32:(q2 + 1) * 32, :],
                            in0=iota[:].rearrange("p (o n) -> p o n", o=1).to_broadcast([128, 32, 128]),
                            in1=jT_[:, th * 64 + q2 * 32:th * 64 + (q2 + 1) * 32].rearrange("p (s o) -> p s o", o=1).to_broadcast([128, 32, 128]), op=ALU.is_equal),
                            reads=[biota, bjT], writes=[bX])
                    for t8 in range(8):
                        pm_, bpm = pM.next()
                        for i in range(8):
                            tl = t8 * 8 + i
                            tk = th * 64 + tl
                            P.op("pe", lambda e, tk=tk, tl=tl, i=i, pm_=pm_, X_=X_, Ys_=Ys_: e.matmul(pm_[:, i * 64:(i + 1) * 64], lhsT=X_[:, tl, :], rhs=Ys_[:, :, tk], start=True, stop=True),
                                 reads=[bX, bYs], writes=[bpm])
                        tok0 = th * 64 + t8 * 8
                        src = pm_[:].rearrange("p (t g c) -> p g t c", t=8, g=4)
                        dst = Ws_[:, :, tok0:tok0 + 8, :]
                        if t8 % 2 == 1:
                            P.op("act", lambda e, src=src, dst=dst: e.activation(out=dst, in_=src, func=AF.Copy), reads=[bpm], writes=[bWs])
                        else:
                            P.op("dve", lambda e, src=src, dst=dst: e.tensor_copy(out=dst, in_=src), reads=[bpm], writes=[bWs])
                        if t8 % 4 == 3:
                            yield
                for g in range(4):
                    cg = ch * 4 + g
                    P.dma("act", Wd[blk, cg, :, tt * 128 * 16:(tt + 1) * 128 * 16], Ws_[:, g, :, :].rearrange("p t c -> p (t c)"),
                          reads=[bWs], writes=[bWd[blk]], append=True)
                yield

    def drain(gen, n):
        if gen is None:
            return None
        for _ in range(n):
            try:
                next(gen)
            except StopIteration:
                return None
        return gen

    gen = sel_block(0)
    gen = drain(gen, 10 ** 9)
    for blk in range(nblk):
        u_, bu = ublocks[blk]
        nxt = sel_block(blk + 1) if blk + 1 < nblk else None
        LAG = 2
        stage = {}
        for c in range(NCH + LAG):
            if c < NCH:
                if c % 16 == 0:
                    w_, bw = wsl.next()
                    P.dma("sp", w_[:].rearrange("p t c -> p (t c)"), Wd[blk, c // 16], reads=[bWd[blk]], writes=[bw])
                uc, buc = uch.next(); vc, bvc = vch.next()
                P.dma("sp", uc[:].rearrange("p k n -> p (k n)"), Ub_d[c], reads=[bUb], writes=[buc])
                P.dma("sp", vc[:], Vb_d[c], reads=[bVb], writes=[bvc])
                ph, bph = pH.next()
                for kc in range(8):
                    P.op("pe", lambda e, kc=kc, ph=ph, uc=uc, u_=u_: e.matmul(ph[:, 0:TBLK], lhsT=uc[:, kc, :], rhs=u_[:, kc, :], start=(kc == 0), stop=(kc == 7)),
                         reads=[buc, bu], writes=[bph])
                g_, bg = gl.next()
                P.op("act", lambda e, ph=ph, g_=g_: e.activation(out=g_[:], in_=ph[:, 0:TBLK], func=AF.Gelu), reads=[bph], writes=[bg])
                z_, bz = zt.next()
                P.op("pool", lambda e, c=c, z_=z_, g_=g_, w_=w_: e.tensor_tensor(out=z_[:], in0=w_[:, :, c % 16], in1=g_[:], op=ALU.mult),
                     reads=[bw, bg], writes=[bz])
                stage[c] = (z_, bz, vc, bvc)
            if c >= LAG:
                cc = c - LAG
                z_, bz, vc, bvc = stage.pop(cc)
                for tt in range(NTL):
                    for hf in range(2):
                        k = tt * 2 + hf
                        P.op("pe", lambda e, cc=cc, tt=tt, hf=hf, k=k, z_=z_, vc=vc: e.matmul(pO[k][:], lhsT=z_[:, tt * 128:(tt + 1) * 128], rhs=vc[:, hf * 512:(hf + 1) * 512],
                                                                                  start=(cc == 0), stop=(cc == NCH - 1)), reads=[bz, bvc], writes=[bpO[k]])
            if c % 2 == 1:
                nxt = drain(nxt, 1)
        nxt = drain(nxt, 10 ** 9)
        for tt in range(NTL):
            tg = blk * NTL + tt
            xt, bx = xin.next()
            P.dma("sp", xt[:], D["xmid"][tg * 128:(tg + 1) * 128, :], writes=[bx])
            xo_, bxo = xout.next()
            emit_resid_ln(P, C, [pO[tt * 2], pO[tt * 2 + 1]], [bpO[tt * 2], bpO[tt * 2 + 1]], xt, bx, gb, bgb, lng, lnb, bln, epsb, beps, xo_, bxo, "p")
            P.dma("pool", xo[tg * 128:(tg + 1) * 128, :], xo_[:], reads=[bxo])
    P.pop()


def k3_host_inputs(inp, l, core, xmid_core):
    b = core // 2
    rep = lambda v: np.ascontiguousarray(np.broadcast_to(np.asarray(v, np.float32)[None, :], (128, len(v))))
    keys = inp["peer_keys"][l].reshape(16, 128, 128)
    return {
        "xmid": np.ascontiguousarray(xmid_core),
        "cT": to_pc(inp["c"][b], 8),
        "adaw_f": np.ascontiguousarray(inp["ada_w"][l][:, 3072:6144]),
        "adab_f": to_pc(inp["ada_b"][l][3072:5120], 16),
        "adab_g": rep(inp["ada_b"][l][5120:6144]),
        "wq": np.ascontiguousarray(inp["peer_wq"][l]),
        "keysT": np.ascontiguousarray(keys.transpose(2, 0, 1)),
        "UT": np.ascontiguousarray(inp["peer_u"][l].T),
        "V": np.ascontiguousarray(inp["peer_v"][l]),
        "lng": rep(inp["ln2_g"][l]), "lnb": rep(inp["ln2_b"][l]),
        "ident": np.eye(128, dtype=np.float32),
        "iota": np.ascontiguousarray(np.broadcast_to(np.arange(128, dtype=np.float32)[None, :], (128, 128))),
    }


import math

PAIRS = [[0, 1], [2, 3], [4, 5], [6, 7]]

GATHER = [
    ("mla_kT", [768, T], 192, [0, 1, 2, 3]),
    ("mla_v", [T, 520], 1024, [0, 1, 2, 3]),
    ("diff_kT", [512, T], 128, [0, 1, 2, 3]),
    ("diff_v", [T, 516], 1024, [0, 1, 2, 3]),
    ("swa_kT", [128, T], 128, [0]),
    ("swa_v", [T, 130], 2048, [1, 0]),
    ("nat_kT", [512, T], 128, [0, 1, 2, 3]),
    ("nat_v", [T, 520], 1024, [0, 3]),
]

LAYER_IN = [("adaw", [1024, 6144]), ("adab1", [128, 16]), ("adabg", [128, 1024]), ("adabf", [128, 16]), ("adabg2", [128, 1024]),
            ("w1", None), ("qupA", [384, 768]), ("qupB", [384, 768]), ("kvn", [256, 512]), ("kvv", [256, 512]),
            ("gq", [128, 3]), ("gkv", [128, 2]), ("sinkb", [128, 8]), ("lamb", [128, 4, 64]), ("subln", [128, 128]), ("lami", [128, 2]),
            ("nat_bias", [8, 128, 29 * 128]), ("wg", [4, 1024, 1024]), ("wb", [4, 512, 1024]), ("wo", [1024, 1024]),
            ("lng1", [128, 1024]), ("lnb1", [128, 1024]), ("wq", [1024, 2048]), ("keysT", [128, 16, 128]),
            ("UT", [1024, 16384]), ("V", [16384, 1024]), ("lng2", [128, 1024]), ("lnb2", [128, 1024])]
COMMON_IN = [("x", [T, 1024]), ("cT", [128, 8]), ("C64", [128, T]), ("S64", [128, T]), ("CM", [128, T]), ("SM", [128, T]),
             ("ident", [128, 128]), ("iota", [128, 128]), ("swa_masks", [4, 128, 512])]


def build_fused(nc, nlayers=2, peer_blocks=NBLK, dbg=False):
    P = Prog(nc)
    units, w1cols = k1_weight_layout()
    NC1 = len(w1cols)
    I = {}
    for n, s in COMMON_IN:
        I[n] = nc.dram_tensor(n, list(s), F32, kind="ExternalInput").ap()
    for l in range(nlayers):
        for n, s in LAYER_IN:
            if n == "w1":
                s = [1024, NC1]
            I[f"{n}_{l}"] = nc.dram_tensor(f"{n}_{l}", list(s), F32, kind="ExternalInput").ap()
    out_d = nc.dram_tensor("out", [T, 1024], F32, kind="ExternalOutput").ap()
    x_cur = I["x"]
    for l in range(nlayers):
        L = lambda n: I[f"{n}_{l}"]
        it = lambda n, s, d=BF16: nc.dram_tensor(f"{n}_L{l}", list(s), d, kind="Internal").ap()
        O2 = {"uT": it("uT", [1024, T]), "swa_qT": it("swa_qT", [512, T]), "swa_kT": it("swa_kT", [128, T]),
              "diff_qT": it("diff_qT", [512, T]), "diff_kT": it("diff_kT", [512, T]), "nat_qT": it("nat_qT", [512, T]),
              "nat_kT": it("nat_kT", [512, T]), "mla_qT": it("mla_qT", [768, T]), "mla_kT": it("mla_kT", [768, T]),
              "swa_v": it("swa_v", [T, 130]), "diff_v": it("diff_v", [T, 516]), "nat_v": it("nat_v", [T, 520]), "mla_v": it("mla_v", [T, 520])}
        O = dict(O2)
        O["mla_qT"] = O2["mla_qT"].rearrange("(h d) t -> h d t", d=96)
        O["mla_kT"] = O2["mla_kT"].rearrange("(h d) t -> h d t", d=96)
        for n in ("swa_v", "diff_v", "nat_v", "mla_v"):
            O[n] = O2[n].rearrange("(t p) f -> t p f", p=128)
        D1 = {"x": x_cur, "cT": I["cT"], "adaw": L("adaw")[:, 0:2048], "adab": L("adab1"), "w1": L("w1"),
              "qupA": L("qupA"), "qupB": L("qupB"), "kvn": L("kvn"), "kvv": L("kvv"), "gq": L("gq"), "gkv": L("gkv"),
              "C64": I["C64"], "S64": I["S64"], "CM": I["CM"], "SM": I["SM"], "ident": I["ident"]}
        P.push()
        emit_k1(P, nc, D1, O)
        P.pop()
        G = {}
        for (name, shp, rc, chunks) in GATHER:
            G[name] = []
            if name == "swa_v":
                dst = it(f"g_{name}", [2 * T, 130])
                P.collective(O2[name], dst, PAIRS)
                G[name].append(dst)
                continue
            for k in chunks:
                dst = it(f"g_{name}{k}", [2 * rc, shp[1]])
                P.collective(O2[name][k * rc:(k + 1) * rc, :], dst, PAIRS)
                G[name].append(dst)
        P.barrier()
        oT_d = it("oT", [4, 512, T])
        D2 = {"swa_qT": O["swa_qT"], "swa_masks": I["swa_masks"], "sinkb": L("sinkb"), "mla_qT": O["mla_qT"],
              "diff_qT": O["diff_qT"], "lamb": L("lamb"), "subln": L("subln"), "lami": L("lami"),
              "nat_qT": O["nat_qT"], "nat_bias": L("nat_bias"), "ident": I["ident"]}
        P.push()
        pre = peer_scratch(nc, f"_L{l}")
        cf = Rot(P, "pa_cf", [128, 4096], F32, 2)
        cb = Rot(P, "pa_cb", [128, 4096], BF16, 2)
        bgen = peer_cast_gen(P, L("UT"), L("V"), pre[0], pre[1], pre[2], pre[3], cf, cb)
        emit_attention(P, D2, oT_d, SRC=GatherSrc(O, G), bg=bgen)
        for _ in bgen:
            pass
        P.pop()
        xmid_d = it("xmid", [T, 1024], F32)
        D3 = {"oT": oT_d, "uT": O["uT"], "x": x_cur, "wg": L("wg"), "wb": L("wb"), "wo": L("wo"), "cT": I["cT"],
              "adaw_g": L("adaw")[:, 2048:3072], "adab_g": L("adabg"), "lng": L("lng1"), "lnb": L("lnb1")}
        emit_merge(P, D3, xmid_d)
        xo = out_d if l == nlayers - 1 else it("xout", [T, 1024], F32)
        D4 = {"xmid": xmid_d, "cT": I["cT"], "adaw_f": L("adaw")[:, 3072:6144], "adab_f": L("adabf"), "adab_g": L("adabg2"),
              "wq": L("wq"), "keysT": L("keysT"), "UT": L("UT"), "V": L("V"), "lng": L("lng2"), "lnb": L("lnb2"),
              "ident": I["ident"], "iota": I["iota"]}
        emit_peer(P, nc, D4, xo, nblk=peer_blocks, tag=f"_L{l}", pre=pre)
        P.barrier()
        if dbg and l == 0:
            d1 = nc.dram_tensor("dbg_oT", [4, 512, T], BF16, kind="ExternalOutput").ap()
            d2 = nc.dram_tensor("dbg_xmid", [T, 1024], F32, kind="ExternalOutput").ap()
            d3 = nc.dram_tensor("dbg_uT", [1024, T], BF16, kind="ExternalOutput").ap()
            for n in range(4):
                P.dma("sp", d1[n], oT_d[n])
            P.dma("act", d2, xmid_d)
            P.dma("act", d3, O["uT"])
            P.barrier()
        x_cur = xo
    P.finish()
    P.emit()
    return P


def fused_host_inputs(inp, core, nlayers=2):
    b, half = core // 2, core % 2
    units, w1cols = k1_weight_layout()
    qa, qb, knope, vcols = mla_up_layout()
    C64, S64, CM, SM = rope_tables(half * T, T)
    rep = lambda v: np.ascontiguousarray(np.broadcast_to(np.asarray(v, np.float32)[None, :], (128, len(v))))
    m = {
        "x": np.ascontiguousarray(inp["x"][b, half * T:(half + 1) * T], dtype=np.float32),
        "cT": to_pc(inp["c"][b], 8), "C64": C64, "S64": S64, "CM": CM, "SM": SM,
        "ident": np.eye(128, dtype=np.float32),
        "iota": np.ascontiguousarray(np.broadcast_to(np.arange(128, dtype=np.float32)[None, :], (128, 128))),
        "swa_masks": swa_masks(half),
    }
    for l in range(nlayers):
        ab = inp["ada_b"][l]
        lam = np.stack([inp["diff_lambda_q1"][l], inp["diff_lambda_k1"][l], inp["diff_lambda_q2"][l], inp["diff_lambda_k2"][l]])
        li = 0.8 - 0.6 * math.exp(-0.3 * l)
        keys = inp["peer_keys"][l].reshape(16, 128, 128)
        d = {
            "adaw": np.ascontiguousarray(inp["ada_w"][l]), "adab1": to_pc(ab[0:2048], 16), "adabg": rep(ab[2048:3072]),
            "adabf": to_pc(ab[3072:5120], 16), "adabg2": rep(ab[5120:6144]),
            "w1": np.ascontiguousarray(inp["w_in"][l][:, w1cols]),
            "qupA": np.ascontiguousarray(inp["mla_q_up"][l][:, qa]), "qupB": np.ascontiguousarray(inp["mla_q_up"][l][:, qb]),
            "kvn": np.ascontiguousarray(inp["mla_kv_up"][l][:, knope]), "kvv": np.ascontiguousarray(inp["mla_kv_up"][l][:, vcols]),
            "gq": to_pc(inp["mla_q_norm"][l], 3), "gkv": to_pc(inp["mla_kv_norm"][l], 2),
            "sinkb": rep(inp["swa_sink"][l]),
            "lamb": np.ascontiguousarray(np.broadcast_to(lam[None], (128, 4, 64))).astype(np.float32),
            "subln": rep(inp["diff_subln"][l]),
            "lami": np.ascontiguousarray(np.broadcast_to(np.array([li, 1.0 - li], np.float32)[None], (128, 2))),
            "nat_bias": nat_bias_tables(inp["nat_rpb"][l], half),
            "wg": np.ascontiguousarray(inp["w_gate"][l]), "wb": np.ascontiguousarray(inp["w_branch"][l]), "wo": np.ascontiguousarray(inp["w_out"][l]),
            "lng1": rep(inp["ln1_g"][l]), "lnb1": rep(inp["ln1_b"][l]),
            "wq": np.ascontiguousarray(inp["peer_wq"][l]), "keysT": np.ascontiguousarray(keys.transpose(2, 0, 1)),
            "UT": np.ascontiguousarray(inp["peer_u"][l].T), "V": np.ascontiguousarray(inp["peer_v"][l]),
            "lng2": rep(inp["ln2_g"][l]), "lnb2": rep(inp["ln2_b"][l]),
        }
        for k, v in d.items():
            m[f"{k}_{l}"] = np.ascontiguousarray(v, dtype=np.float32)
    return m


NCORES = 8
_PROGS = {}


def kernel(**inputs):
    inp = {k: np.asarray(v) for k, v in inputs.items()}
    if "fused" not in _PROGS:
        nc = bass.Bass("TRN2", target_bir_lowering=False)
        build_fused(nc)
        _PROGS["fused"] = nc
    nc = _PROGS["fused"]
    maps = [fused_host_inputs(inp, c) for c in range(NCORES)]
    res = run_bass_kernel_spmd(nc, maps, core_ids=list(range(NCORES)))
    out = np.empty((4, 2 * T, 1024), np.float32)
    for c in range(NCORES):
        b, half = c // 2, c % 2
        out[b, half * T:(half + 1) * T] = np.asarray(res.results[c]["out"])
    return out
```
